# Optimizing a Trainium2 kernel written in Bass

```python
import math
import jax
import jax.numpy as jnp
from jax import lax
import numpy as np


D_MODEL = 1024
BATCH = 2
SEQ = 8192
DEPTH = 2

GRID_W = 64
CTX_LEN = 256
HEAD_DIM = 64
MIX_W = D_MODEL
Q_BLOCK = 128
ROPE_THETA = 10000.0
DIFF_HEADS = MIX_W // 2 // (2 * HEAD_DIM)
DIFF_QK = DIFF_HEADS * 2 * HEAD_DIM
DIFF_V = DIFF_HEADS * 2 * HEAD_DIM
GQA_HEADS = MIX_W // 2 // HEAD_DIM
GQA_KV_HEADS = max(1, GQA_HEADS // 4)
GQA_GROUP = GQA_HEADS // GQA_KV_HEADS
GQA_Q = GQA_HEADS * HEAD_DIM
GQA_KV_W = GQA_KV_HEADS * HEAD_DIM
AB_Q = DIFF_QK + GQA_Q
AB_KV = DIFF_QK + DIFF_V + 2 * GQA_KV_W
AB_OUT = DIFF_V + GQA_Q
WINDOW = 128
WIN_HEADS = MIX_W // 2 // HEAD_DIM
WIN_KV_HEADS = max(1, WIN_HEADS // 4)
WIN_GROUP = WIN_HEADS // WIN_KV_HEADS
WIN_Q = WIN_HEADS * HEAD_DIM
WIN_KV_W = WIN_KV_HEADS * HEAD_DIM
NA_HEADS = MIX_W // 2 // HEAD_DIM
NA_W = NA_HEADS * HEAD_DIM
NA_ROWS_MAX = 8
NA_COLS = 16
CD_Q = WIN_Q + NA_W
CD_KV = 2 * WIN_KV_W + 2 * NA_W
CD_OUT = WIN_Q + NA_W
N_GROUPS = 4
EXPERTS_PER_GROUP = 8
N_EXPERTS = N_GROUPS * EXPERTS_PER_GROUP
TOP_K_IN_GROUP = 2
D_EXPERT = D_MODEL // 2
MOE_BLOCK = 128
N_EVEN = (DEPTH + 1) // 2
N_ODD = DEPTH // 2
ALPHA = (2.0 * DEPTH) ** 0.25
BETA = (8.0 * DEPTH) ** -0.25
LN_EPS = 1e-5
RMS_EPS = 1e-6
ATTN_SCALE = HEAD_DIM ** -0.5
NEG_INF = -1e30

kernel_name = 'hybrid_flow_backbone'


def layer_norm(x, g, b):
    xf = x.astype(jnp.float32)
    mu = jnp.mean(xf, -1, keepdims=True)
    var = jnp.mean(jnp.square(xf - mu), -1, keepdims=True)
    y = (xf - mu) * lax.rsqrt(var + LN_EPS) * g.astype(jnp.float32) + b.astype(jnp.float32)
    return y.astype(x.dtype)


def rms_norm(x, g):
    xf = x.astype(jnp.float32)
    y = xf * lax.rsqrt(jnp.mean(jnp.square(xf), -1, keepdims=True) + RMS_EPS) * g.astype(jnp.float32)
    return y.astype(x.dtype)


def modulate(x, shift, scale):
    return x * (1.0 + scale) + shift


def lambda_init(layer_idx):
    return 0.8 - 0.6 * math.exp(-0.3 * layer_idx)


def axial_rope_tables(n_tokens):
    t = jnp.arange(n_tokens, dtype=jnp.int32)
    row = (t // GRID_W).astype(jnp.float32)
    col = (t % GRID_W).astype(jnp.float32)
    n_freq = HEAD_DIM // 4
    inv = ROPE_THETA ** (-jnp.arange(n_freq, dtype=jnp.float32) / n_freq)
    ang = jnp.concatenate([row[:, None] * inv, col[:, None] * inv], -1)
    return jnp.cos(ang), jnp.sin(ang)


def apply_rope(x, cos, sin):
    shp = (1, cos.shape[0]) + (1,) * (x.ndim - 3) + (cos.shape[1],)
    cos = cos.reshape(shp)
    sin = sin.reshape(shp)
    xf = x.astype(jnp.float32)
    x1, x2 = jnp.split(xf, 2, -1)
    return jnp.concatenate([x1 * cos - x2 * sin, x2 * cos + x1 * sin], -1).astype(x.dtype)


def sweep_query_blocks(fn, q):
    b, s = q.shape[:2]
    nb = s // Q_BLOCK
    qb = jnp.moveaxis(q.reshape((b, nb, Q_BLOCK) + q.shape[2:]), 1, 0)
    out = lax.map(lambda a: fn(a[0], a[1]), (jnp.arange(nb, dtype=jnp.int32), qb))
    out = jnp.moveaxis(out, 0, 1)
    return out.reshape((b, s) + out.shape[3:])


def diff_attend(qb, k, v, lam):
    s = jnp.einsum('bqhcd,bkhcd->bhcqk', qb, k).astype(jnp.float32) * ATTN_SCALE
    p = jax.nn.softmax(s, -1)
    a = p[:, :, 0] - lam * p[:, :, 1]
    return jnp.einsum('bhqk,bkhe->bqhe', a.astype(v.dtype), v)


def gqa_attend(qb, k, v):
    s = jnp.einsum('bqhgd,bkhd->bhgqk', qb, k).astype(jnp.float32) * ATTN_SCALE
    p = jax.nn.softmax(s, -1)
    return jnp.einsum('bhgqk,bkhd->bqhgd', p.astype(v.dtype), v)


def sink_attend(qb, k, v, sink, valid=None):
    s = jnp.einsum('bqhgd,bkhd->bhgqk', qb, k).astype(jnp.float32) * ATTN_SCALE
    if valid is not None:
        s = jnp.where(valid, s, NEG_INF)
    sk = jnp.broadcast_to(sink.astype(jnp.float32)[None, :, :, None, None], s.shape[:-1] + (1,))
    p = jax.nn.softmax(jnp.concatenate([s, sk], -1), -1)[..., :-1]
    return jnp.einsum('bhgqk,bkhd->bqhgd', p.astype(v.dtype), v)


def window_attention(q, k, v, kc, vc, sink):
    s = q.shape[1]
    n_ctx = kc.shape[1]
    span = Q_BLOCK + 2 * WINDOW
    pad = ((0, 0), (WINDOW, WINDOW), (0, 0), (0, 0))
    kp = jnp.pad(k, pad)
    vp = jnp.pad(v, pad)
    ctx_valid = jnp.ones((Q_BLOCK, n_ctx), dtype=bool)

    def block(bi, qb):
        start = bi * Q_BLOCK
        kb = lax.dynamic_slice_in_dim(kp, start, span, axis=1)
        vb = lax.dynamic_slice_in_dim(vp, start, span, axis=1)
        qpos = start + jnp.arange(Q_BLOCK)
        kpos = start - WINDOW + jnp.arange(span)
        local = (jnp.abs(qpos[:, None] - kpos[None, :]) <= WINDOW) & (kpos >= 0)[None, :] & (kpos < s)[None, :]
        valid = jnp.concatenate([local, ctx_valid], 1)
        return sink_attend(qb, jnp.concatenate([kb, kc], 1), jnp.concatenate([vb, vc], 1), sink, valid)

    return sweep_query_blocks(block, q)


def neighbourhood_attention(q, k, v, kc, vc, rpb):
    b, s, h, d = q.shape
    rows = s // GRID_W
    kr = min(NA_ROWS_MAX, rows)
    n_nb = kr * NA_COLS
    qg = jnp.moveaxis(q.reshape(b, rows, GRID_W, h, d), 1, 0)
    kg = k.reshape(b, rows, GRID_W, h, d)
    vg = v.reshape(b, rows, GRID_W, h, d)
    col = jnp.arange(GRID_W)
    col_start = jnp.clip(col - NA_COLS // 2, 0, GRID_W - NA_COLS)
    cols = col_start[:, None] + jnp.arange(NA_COLS)
    col_off = cols - col[:, None] + (NA_COLS - 1)

    def row_block(a):
        r, qr = a
        r0 = jnp.clip(r - kr // 2, 0, rows - kr)
        slab_k = lax.dynamic_slice_in_dim(kg, r0, kr, axis=1)
        slab_v = lax.dynamic_slice_in_dim(vg, r0, kr, axis=1)
        k_nb = jnp.moveaxis(slab_k[:, :, cols], 1, 2).reshape(b, GRID_W, n_nb, h, d)
        v_nb = jnp.moveaxis(slab_v[:, :, cols], 1, 2).reshape(b, GRID_W, n_nb, h, d)
        row_off = r0 + jnp.arange(kr) - r + (NA_ROWS_MAX - 1)
        bias = rpb[:, row_off[None, :, None], col_off[:, None, :]].reshape(h, GRID_W, n_nb)
        s_nb = jnp.einsum('bqhd,bqkhd->bhqk', qr, k_nb).astype(jnp.float32) * ATTN_SCALE + bias.astype(jnp.float32)[None]
        s_cx = jnp.einsum('bqhd,bkhd->bhqk', qr, kc).astype(jnp.float32) * ATTN_SCALE
        p = jax.nn.softmax(jnp.concatenate([s_nb, s_cx], -1), -1).astype(v.dtype)
        return (jnp.einsum('bhqk,bqkhd->bqhd', p[..., :n_nb], v_nb)
                + jnp.einsum('bhqk,bkhd->bqhd', p[..., n_nb:], vc))

    out = lax.map(row_block, (jnp.arange(rows, dtype=jnp.int32), qg))
    return jnp.moveaxis(out, 0, 1).reshape(b, s, h, d)


def mixer_ab(a_lat, a_ctx, ctx_kv_only, w_in, w_out, lam_vecs, subln_g, qk_g, lam_init, cos, sin):
    b = a_lat.shape[0]

    def split_q(p):
        n = p.shape[1]
        q_d = p[..., :DIFF_QK].reshape(b, n, DIFF_HEADS, 2, HEAD_DIM)
        q_g = rms_norm(p[..., DIFF_QK:AB_Q].reshape(b, n, GQA_KV_HEADS, GQA_GROUP, HEAD_DIM), qk_g[0])
        return q_d, q_g

    def split_kv(p):
        n = p.shape[1]
        o1 = DIFF_QK
        o2 = o1 + DIFF_V
        o3 = o2 + GQA_KV_W
        k_d = p[..., :o1].reshape(b, n, DIFF_HEADS, 2, HEAD_DIM)
        v_d = p[..., o1:o2].reshape(b, n, DIFF_HEADS, 2 * HEAD_DIM)
        k_g = rms_norm(p[..., o2:o3].reshape(b, n, GQA_KV_HEADS, HEAD_DIM), qk_g[1])
        v_g = p[..., o3:].reshape(b, n, GQA_KV_HEADS, HEAD_DIM)
        return k_d, v_d, k_g, v_g

    lv = lam_vecs.astype(jnp.float32)
    lam = jnp.exp(jnp.sum(lv[0] * lv[1])) - jnp.exp(jnp.sum(lv[2] * lv[3])) + lam_init

    p_lat = a_lat @ w_in
    q_d, q_g = split_q(p_lat[..., :AB_Q])
    k_d, v_d, k_g, v_g = split_kv(p_lat[..., AB_Q:])
    q_d, k_d, q_g, k_g = (apply_rope(t, cos, sin) for t in (q_d, k_d, q_g, k_g))
    if ctx_kv_only:
        ck_d, cv_d, ck_g, cv_g = split_kv(a_ctx @ w_in[:, AB_Q:])
    else:
        p_ctx = a_ctx @ w_in
        cq_d, cq_g = split_q(p_ctx[..., :AB_Q])
        ck_d, cv_d, ck_g, cv_g = split_kv(p_ctx[..., AB_Q:])

    kd_all = jnp.concatenate([k_d, ck_d], 1)
    vd_all = jnp.concatenate([v_d, cv_d], 1)
    kg_all = jnp.concatenate([k_g, ck_g], 1)
    vg_all = jnp.concatenate([v_g, cv_g], 1)
    o_d = sweep_query_blocks(lambda _, qb: diff_attend(qb, kd_all, vd_all, lam), q_d)
    o_g = sweep_query_blocks(lambda _, qb: gqa_attend(qb, kg_all, vg_all), q_g)

    def merge(od, og):
        n = od.shape[1]
        od = rms_norm(od, subln_g) * (1.0 - lam_init)
        return jnp.concatenate([od.reshape(b, n, DIFF_V), og.reshape(b, n, GQA_Q)], -1) @ w_out

    out_lat = merge(o_d, o_g)
    if ctx_kv_only:
        return out_lat, None
    out_ctx = merge(diff_attend(cq_d, ck_d, cv_d, lam), gqa_attend(cq_g, ck_g, cv_g))
    return out_lat, out_ctx


def mixer_cd(a_lat, a_ctx, ctx_kv_only, w_in, w_out, sink, rpb, cos, sin):
    b = a_lat.shape[0]
    sink_hg = sink.reshape(WIN_KV_HEADS, WIN_GROUP)

    def split_q(p):
        n = p.shape[1]
        q_w = p[..., :WIN_Q].reshape(b, n, WIN_KV_HEADS, WIN_GROUP, HEAD_DIM)
        q_n = p[..., WIN_Q:CD_Q].reshape(b, n, NA_HEADS, HEAD_DIM)
        return q_w, q_n

    def split_kv(p):
        n = p.shape[1]
        o1 = WIN_KV_W
        o2 = o1 + WIN_KV_W
        o3 = o2 + NA_W
        k_w = p[..., :o1].reshape(b, n, WIN_KV_HEADS, HEAD_DIM)
        v_w = p[..., o1:o2].reshape(b, n, WIN_KV_HEADS, HEAD_DIM)
        k_n = p[..., o2:o3].reshape(b, n, NA_HEADS, HEAD_DIM)
        v_n = p[..., o3:].reshape(b, n, NA_HEADS, HEAD_DIM)
        return k_w, v_w, k_n, v_n

    p_lat = a_lat @ w_in
    q_w, q_n = split_q(p_lat[..., :CD_Q])
    k_w, v_w, k_n, v_n = split_kv(p_lat[..., CD_Q:])
    q_w = apply_rope(q_w, cos, sin)
    k_w = apply_rope(k_w, cos, sin)
    if ctx_kv_only:
        ck_w, cv_w, ck_n, cv_n = split_kv(a_ctx @ w_in[:, CD_Q:])
    else:
        p_ctx = a_ctx @ w_in
        cq_w, cq_n = split_q(p_ctx[..., :CD_Q])
        ck_w, cv_w, ck_n, cv_n = split_kv(p_ctx[..., CD_Q:])

    o_w = window_attention(q_w, k_w, v_w, ck_w, cv_w, sink_hg)
    o_n = neighbourhood_attention(q_n, k_n, v_n, ck_n, cv_n, rpb)

    def merge(ow, on):
        n = ow.shape[1]
        return jnp.concatenate([ow.reshape(b, n, WIN_Q), on.reshape(b, n, NA_W)], -1) @ w_out

    out_lat = merge(o_w, o_n)
    if ctx_kv_only:
        return out_lat, None
    n_ctx = a_ctx.shape[1]
    co_w = sink_attend(cq_w, ck_w, cv_w, sink_hg)
    co_n = gqa_attend(cq_n.reshape(b, n_ctx, NA_HEADS, 1, HEAD_DIM), ck_n, cv_n)
    return out_lat, merge(co_w, co_n)


def hier_moe(h, w_group, b_group, w_router, b_router, w1, w3, w2):
    t, d = h.shape
    g_logits = (h @ w_group).astype(jnp.float32) + b_group.astype(jnp.float32)
    _, g_idx = lax.top_k(g_logits, 1)
    g_w = jnp.take_along_axis(jax.nn.softmax(g_logits, -1), g_idx, -1)
    e_logits = ((h @ w_router).astype(jnp.float32) + b_router.astype(jnp.float32)).reshape(t, N_GROUPS, EXPERTS_PER_GROUP)
    e_in = jnp.take_along_axis(e_logits, g_idx[:, :, None], 1)[:, 0]
    top_v, top_i = lax.top_k(e_in, TOP_K_IN_GROUP)
    weights = g_w * jax.nn.softmax(top_v, -1)
    experts = g_idx * EXPERTS_PER_GROUP + top_i

    n_assign = t * TOP_K_IN_GROUP
    e_flat = experts.reshape(n_assign)
    tok = jnp.repeat(jnp.arange(t, dtype=jnp.int32), TOP_K_IN_GROUP)
    w_flat = weights.reshape(n_assign)
    order = jnp.argsort(e_flat)
    e_s = e_flat[order]
    tok_s = tok[order]
    w_s = w_flat[order]
    counts = jnp.bincount(e_flat, length=N_EXPERTS)
    starts = jnp.cumsum(counts) - counts
    padded = (counts + MOE_BLOCK - 1) // MOE_BLOCK * MOE_BLOCK
    p_ends = jnp.cumsum(padded)
    p_starts = p_ends - padded
    dest = p_starts[e_s] + (jnp.arange(n_assign, dtype=jnp.int32) - starts[e_s])
    n_blocks = (n_assign + MOE_BLOCK - 1) // MOE_BLOCK + N_EXPERTS
    buf = jnp.zeros((n_blocks * MOE_BLOCK, d), h.dtype).at[dest].set(h[tok_s])
    blk_e = jnp.minimum(jnp.searchsorted(p_ends, jnp.arange(n_blocks) * MOE_BLOCK, side='right'), N_EXPERTS - 1)

    def expert_block(a):
        xb, e = a
        return (jax.nn.silu(xb @ w1[e]) * (xb @ w3[e])) @ w2[e]

    yb = lax.map(expert_block, (buf.reshape(n_blocks, MOE_BLOCK, d), blk_e)).reshape(n_blocks * MOE_BLOCK, d)
    return jnp.zeros((t, d), h.dtype).at[tok_s].add(w_s[:, None].astype(h.dtype) * yb[dest])


def setup_inputs(seed: int = 0) -> dict:
    key = jax.random.key(seed)
    ks = jax.random.split(key, 32)
    D = D_MODEL

    def nrm(k, shape, s):
        return jax.random.normal(k, shape, jnp.float32) * s

    ab_kv_scale = jnp.concatenate([jnp.ones((DIFF_QK,)), jnp.full((DIFF_V,), BETA), jnp.ones((GQA_KV_W,)), jnp.full((GQA_KV_W,), BETA)]).astype(jnp.float32)
    cd_kv_scale = jnp.concatenate([jnp.ones((WIN_KV_W,)), jnp.full((WIN_KV_W,), BETA), jnp.ones((NA_W,)), jnp.full((NA_W,), BETA)]).astype(jnp.float32)
    return {
        'x': nrm(ks[0], (BATCH, SEQ, D), 1.0),
        'c': nrm(ks[1], (BATCH, D), 1.0),
        'ctx': nrm(ks[2], (BATCH, CTX_LEN, D), 1.0),
        'c_ctx': nrm(ks[3], (D,), 1.0),
        'mod_w': nrm(ks[4], (DEPTH, D, 6 * D), 0.5 * D ** -0.5),
        'mod_b': nrm(ks[5], (DEPTH, 6 * D), 0.02),
        'ln_g': 1.0 + nrm(ks[6], (DEPTH, 2, D), 0.02),
        'ln_b': nrm(ks[7], (DEPTH, 2, D), 0.02),
        'ab_w_in': jnp.concatenate([nrm(ks[8], (N_EVEN, D, AB_Q), D ** -0.5), nrm(ks[9], (N_EVEN, D, AB_KV), D ** -0.5) * ab_kv_scale], -1),
        'ab_w_out': nrm(ks[10], (N_EVEN, AB_OUT, D), AB_OUT ** -0.5 * BETA),
        'diff_lambda': nrm(ks[11], (N_EVEN, 4, HEAD_DIM), 0.1),
        'diff_subln_g': 1.0 + nrm(ks[12], (N_EVEN, 2 * HEAD_DIM), 0.02),
        'gqa_qk_g': 1.0 + nrm(ks[13], (N_EVEN, 2, HEAD_DIM), 0.02),
        'cd_w_in': jnp.concatenate([nrm(ks[14], (N_ODD, D, CD_Q), D ** -0.5), nrm(ks[15], (N_ODD, D, CD_KV), D ** -0.5) * cd_kv_scale], -1),
        'cd_w_out': nrm(ks[16], (N_ODD, CD_OUT, D), CD_OUT ** -0.5 * BETA),
        'win_sink': nrm(ks[17], (N_ODD, WIN_HEADS), 0.5),
        'na_rpb': nrm(ks[18], (N_ODD, NA_HEADS, 2 * NA_ROWS_MAX - 1, 2 * NA_COLS - 1), 0.1),
        'moe_w_group': nrm(ks[19], (DEPTH, D, N_GROUPS), D ** -0.5),
        'moe_b_group': nrm(ks[20], (DEPTH, N_GROUPS), 0.01),
        'moe_w_router': nrm(ks[21], (DEPTH, D, N_EXPERTS), D ** -0.5),
        'moe_b_router': nrm(ks[22], (DEPTH, N_EXPERTS), 0.01),
        'moe_w1': nrm(ks[23], (DEPTH, N_EXPERTS, D, D_EXPERT), D ** -0.5),
        'moe_w3': nrm(ks[24], (DEPTH, N_EXPERTS, D, D_EXPERT), D ** -0.5),
        'moe_w2': nrm(ks[25], (DEPTH, N_EXPERTS, D_EXPERT, D), D_EXPERT ** -0.5 * BETA),
    }


def reference(x, c, ctx, c_ctx, mod_w, mod_b, ln_g, ln_b, ab_w_in, ab_w_out, diff_lambda, diff_subln_g, gqa_qk_g, cd_w_in, cd_w_out, win_sink, na_rpb, moe_w_group, moe_b_group, moe_w_router, moe_b_router, moe_w1, moe_w3, moe_w2):
    b, s, d = x.shape
    n_ctx = ctx.shape[1]
    cos, sin = axial_rope_tables(s)
    silu_c = jax.nn.silu(c)
    silu_cc = jax.nn.silu(c_ctx)
    h_lat, h_ctx = x, ctx
    for i in range(DEPTH):
        last = i == DEPTH - 1
        j = i // 2
        mods = (silu_c @ mod_w[i] + mod_b[i]).reshape(b, 1, 6, d)
        n_cm = 2 if last else 6
        cmods = (silu_cc @ mod_w[i][:, :n_cm * d] + mod_b[i][:n_cm * d]).reshape(1, 1, n_cm, d)

        a_lat = modulate(h_lat, mods[:, :, 0], mods[:, :, 1])
        a_ctx = modulate(h_ctx, cmods[:, :, 0], cmods[:, :, 1])
        if i % 2 == 0:
            o_lat, o_ctx = mixer_ab(a_lat, a_ctx, last, ab_w_in[j], ab_w_out[j], diff_lambda[j], diff_subln_g[j], gqa_qk_g[j], lambda_init(i), cos, sin)
        else:
            o_lat, o_ctx = mixer_cd(a_lat, a_ctx, last, cd_w_in[j], cd_w_out[j], win_sink[j], na_rpb[j], cos, sin)
        h_lat = layer_norm(ALPHA * h_lat + mods[:, :, 2] * o_lat, ln_g[i, 0], ln_b[i, 0])

        def moe(tokens):
            return hier_moe(tokens, moe_w_group[i], moe_b_group[i], moe_w_router[i], moe_b_router[i], moe_w1[i], moe_w3[i], moe_w2[i])

        f_lat = modulate(h_lat, mods[:, :, 3], mods[:, :, 4]).reshape(b * s, d)
        if last:
            y_lat = moe(f_lat)
        else:
            h_ctx = layer_norm(ALPHA * h_ctx + cmods[:, :, 2] * o_ctx, ln_g[i, 0], ln_b[i, 0])
            f_ctx = modulate(h_ctx, cmods[:, :, 3], cmods[:, :, 4]).reshape(b * n_ctx, d)
            y = moe(jnp.concatenate([f_lat, f_ctx], 0))
            y_lat = y[:b * s]
            h_ctx = layer_norm(ALPHA * h_ctx + cmods[:, :, 5] * y[b * s:].reshape(b, n_ctx, d), ln_g[i, 1], ln_b[i, 1])
        h_lat = layer_norm(ALPHA * h_lat + mods[:, :, 5] * y_lat.reshape(b, s, d), ln_g[i, 1], ln_b[i, 1])
    return h_lat
```

```python
import math
from contextlib import ExitStack

import numpy as np
import concourse.bass as bass
import concourse.mybir as mybir
from concourse.bass_utils import run_bass_kernel_spmd

F32 = mybir.dt.float32
BF16 = mybir.dt.bfloat16
AF = mybir.ActivationFunctionType
ALU = mybir.AluOpType
AX = mybir.AxisListType

DEBUG = False

D = 1024
SEQ = 8192
NCTX = 256
NEXT = 2560
NOWN = 2048
NQ = NEXT + NCTX
NK0 = SEQ + NCTX
NK1 = NEXT + NCTX
VW = 650
DEPTH = 2
ALPHA = (2.0 * DEPTH) ** 0.25
LN_EPS = 1e-5
RMS_EPS = 1e-6
LAM_INIT0 = 0.8 - 0.6 * math.exp(0.0)
NEG = -30000.0
NEXP = 32

SEM_LIM = 8000
NDMA = 12
ARENA_WORDS = 53000


class V:
    def __init__(self, ap, tid, p0, p1, f0, f1, wpe):
        self.ap, self.tid, self.p0, self.p1, self.f0, self.f1, self.wpe = ap, tid, p0, p1, f0, f1, wpe

    def reg(self):
        return (self.tid, self.p0, self.p1, self.f0, self.f1)

    def s(self, c0, c1, p0=None, p1=None):
        q0 = 0 if p0 is None else p0
        q1 = (self.p1 - self.p0) if p1 is None else p1
        return V(self.ap[q0:q1, c0:c1], self.tid, self.p0 + q0, self.p0 + q1,
                 self.f0 + c0 * self.wpe, self.f0 + c1 * self.wpe, self.wpe)


class Buf:
    def __init__(self, t, tid, P, F, wpe):
        self.t, self.tid, self.P, self.F, self.wpe = t, tid, P, F, wpe

    def v(self, p0=0, p1=None, f0=0, f1=None):
        p1 = self.P if p1 is None else p1
        f1 = self.F if f1 is None else f1
        return V(self.t[p0:p1, f0:f1], self.tid, p0, p1, f0 * self.wpe, f1 * self.wpe, self.wpe)


class Prog:
    ENGS = ["pe", "act", "dve", "pool", "sp"]

    def __init__(self, nc, stack):
        self.nc, self.stack = nc, stack
        self.recs = {e: [] for e in self.ENGS}
        self.sig = {e: 0 for e in self.ENGS}
        self.known = {e: {} for e in self.ENGS}
        self.csems = {e: [] for e in self.ENGS}
        self.dsems, self.dcnt, self.drr = {}, {}, {}
        for q in ["sp", "act", "pool"]:
            self.dsems[q] = [stack.enter_context(nc.semaphore(f"d_{q}_{k}")) for k in range(NDMA)]
            self.dcnt[q] = [0] * NDMA
            self.drr[q] = 0
        self.ent = {}
        self.ntid = 0

    def new_tid(self):
        self.ntid += 1
        return self.ntid

    def dram(self, name, P, F, dtype, kind):
        t = self.nc.dram_tensor(name, [P, F], dtype, kind=kind)
        return Buf(t, self.new_tid(), P, F, 1.0 if dtype == F32 else 0.5)

    def _csem(self, e, idx):
        while len(self.csems[e]) <= idx:
            self.csems[e].append(self.stack.enter_context(self.nc.semaphore(f"c_{e}_{len(self.csems[e])}")))
        return self.csems[e][idx]

    @staticmethod
    def _ov(a, b):
        return a[1] < b[2] and b[1] < a[2] and a[3] < b[4] and b[3] < a[4]

    @staticmethod
    def _cov(a, b):
        return a[1] <= b[1] and a[2] >= b[2] and a[3] <= b[3] and a[4] >= b[4]

    BUCK = 256

    def _cands(self, r):
        d = self.ent.get(r[0])
        if not d:
            return []
        seen, out = set(), []
        for b in range(int(r[3]) // self.BUCK, int(math.ceil(r[4])) // self.BUCK + 1):
            for en in d.get(b, ()):
                if en[3] and id(en) not in seen:
                    seen.add(id(en))
                    out.append(en)
        return out

    def _add(self, en):
        r = en[0]
        d = self.ent.setdefault(r[0], {})
        for b in range(int(r[3]) // self.BUCK, int(math.ceil(r[4])) // self.BUCK + 1):
            lst = d.setdefault(b, [])
            if len(lst) > 64:
                lst[:] = [x for x in lst if x[3]]
            lst.append(en)

    def _deps(self, reads, writes):
        raw, other = [], []
        for r in reads:
            for en in self._cands(r):
                if en[1] is not None and self._ov(en[0], r):
                    raw.append(en[1])
        for w in writes:
            for en in self._cands(w):
                if self._ov(en[0], w):
                    if en[1] is not None:
                        other.append(en[1])
                    other.extend(en[2])
        return raw, other

    def _update(self, tok, reads, writes):
        for r in reads:
            hit = False
            for en in self._cands(r):
                if self._ov(en[0], r):
                    if tok[0] == "c":
                        en[2][:] = [t for t in en[2] if not (t[0] == "c" and t[1] == tok[1])]
                    en[2].append(tok)
                    if self._cov(en[0], r):
                        hit = True
            if not hit:
                self._add([r, None, [tok], True])
        for w in writes:
            for en in self._cands(w):
                if self._cov(w, en[0]):
                    en[3] = False
            self._add([w, tok, [], True])

    def _waits(self, e, raw, other):
        waits = []
        kn = self.known[e]
        for kind, toks in (("raw", raw), ("oth", other)):
            for t in toks:
                if t[0] == "c":
                    _, f, v = t
                    if f == e and (kind == "oth" or e == "pe"):
                        continue
                    if kn.get(f, 0) >= v:
                        continue
                    kn[f] = v
                    waits.append(t)
                else:
                    _, q, k, c = t
                    if kn.get((q, k), 0) >= c:
                        continue
                    kn[(q, k)] = c
                    waits.append(t)
        return waits

    def op(self, e, fn, reads=(), writes=(), sig=True):
        reads = [r.reg() for r in reads]
        writes = [w.reg() for w in writes]
        raw, other = self._deps(reads, writes)
        waits = self._waits(e, raw, other)
        if sig:
            self.sig[e] += 1
            v = self.sig[e]
        else:
            v = self.sig[e] + 1
        tok = ("c", e, v)
        self._update(tok, reads, writes)
        self.recs[e].append((waits, fn, tok if sig else None))
        return tok

    def dma(self, q, out, in_, **kw):
        reads, writes = [in_.reg()], [out.reg()]
        raw, other = self._deps(reads, writes)
        k = self.drr[q]
        self.drr[q] = (k + 1) % NDMA
        prev = self.dcnt[q][k]
        if prev > 0:
            other = other + [("d", q, k, prev)]
        waits = self._waits(q, raw, other)
        self.dcnt[q][k] = prev + 1
        tok = ("d", q, k, prev + 1)
        self._update(tok, reads, writes)
        oa, ia = out.ap, in_.ap

        def fn(eng, oa=oa, ia=ia, kw=kw):
            return eng.dma_start(out=oa, in_=ia, **kw)

        self.recs[q].append((waits, fn, tok))
        return tok

    def finish(self):
        waits = []
        for q in self.dsems:
            for k in range(NDMA):
                if self.dcnt[q][k] > 0:
                    waits.append(("d", q, k, self.dcnt[q][k]))
        for e in ["pe", "act", "dve", "pool"]:
            if self.sig[e] > 0:
                waits.append(("c", e, self.sig[e]))
        self.recs["sp"].append((waits, None, None))

    def _emit_wait(self, eng, w):
        if w[0] == "c":
            _, f, v = w
            eng.wait_ge(self._csem(f, (v - 1) // SEM_LIM), (v - 1) % SEM_LIM + 1)
        else:
            _, q, k, c = w
            eng.wait_ge(self.dsems[q][k], 16 * c)

    def emit(self):
        nc = self.nc
        for e in self.ENGS:
            for idx in range((self.sig[e] + SEM_LIM - 1) // SEM_LIM + 1):
                self._csem(e, idx)
        recs, me = self.recs, self

        def play(e, eng):
            for waits, fn, tok in recs[e]:
                for w in waits:
                    me._emit_wait(eng, w)
                if fn is None:
                    continue
                ins = fn(eng)
                if tok is not None:
                    if tok[0] == "c":
                        ins.then_inc(me._csem(e, (tok[2] - 1) // SEM_LIM), 1)
                    else:
                        ins.then_inc(me.dsems[tok[1]][tok[2]], 16)

        with nc.Block() as block:
            @block.tensor
            def _(eng):
                play("pe", eng)

            @block.scalar
            def _(eng):
                play("act", eng)

            @block.vector
            def _(eng):
                play("dve", eng)

            @block.gpsimd
            def _(eng):
                play("pool", eng)

            @block.sync
            def _(eng):
                play("sp", eng)


class Arena:
    def __init__(self, buf):
        self.buf, self.top, self.hi = buf, 0, 0

    def _alloc(self, words):
        off = self.top
        self.top += (words + 7) // 8 * 8
        self.hi = max(self.hi, self.top)
        assert self.top <= self.buf.F, f"arena overflow {self.top}"
        return off

    def f32(self, n, P=128):
        off = self._alloc(n)
        return V(self.buf.t[0:P, off:off + n], self.buf.tid, 0, P, off, off + n, 1.0)

    def bf(self, n, P=128):
        w = (n + 1) // 2
        off = self._alloc(w)
        return V(self.buf.t[0:P, off:off + w].bitcast(BF16), self.buf.tid, 0, P, off, off + w, 0.5)


class Rot:
    def __init__(self, items):
        self.items, self.i = items, 0

    def get(self):
        it = self.items[self.i % len(self.items)]
        self.i += 1
        return it


def r3(ap, k):
    return ap.rearrange("p (k n) -> p k n", k=k)


def OP(name, *a, **k):
    return lambda e: getattr(e, name)(*a, **k)


def build_program():
    nc = bass.Bass("TRN2", target_bir_lowering=False)
    st = ExitStack()
    with st:
        P = Prog(nc, st)
        I = lambda name, p, f, dt=F32: P.dram(name, p, f, dt, "ExternalInput")
        xT = I("xT", D, SEQ)
        xeT = I("xeT", D, NEXT)
        cxT = I("cxT", D, NCTX)
        ccT = I("ccT", D, 2)
        ident_d = I("ident", 128, 128)
        bones_d = I("bones", 128, 128)
        modw = [I(f"modw{i}", D, 6 * D) for i in range(2)]
        modb = [I(f"modb{i}", 128, 48) for i in range(2)]
        lnT_d = I("lnT", 128, 64)
        wq_d = [I(f"wq{i}", D, 1024) for i in range(2)]
        wqs_d = [I(f"wqs{i}", D, 1024) for i in range(2)]
        wk_d = [I(f"wk{i}", D, 768) for i in range(2)]
        wks_d = [I(f"wks{i}", D, 768) for i in range(2)]
        wv_d = [I(f"wv{i}", D, 640) for i in range(2)]
        wo_d = [I(f"wo{i}", D, 1024) for i in range(2)]
        gt_d = I("gt", 128, 4)
        dlam_d = I("dlam", 1, 256)
        subg_d = I("subg", 1, 128)
        sink_d = I("sink", 1, 8)
        cK_d = I("cK", 128, NK0)
        sK_d = I("sK", 128, NK0)
        cQ_d = I("cQ", 128, NQ)
        sQ_d = I("sQ", 128, NQ)
        wr_d = [I(f"wr{i}", D, 36) for i in range(2)]
        br_d = [I(f"br{i}", 1, 36) for i in range(2)]
        w1_d = [I(f"w1_{i}", NEXP * D, 512) for i in range(2)]
        w3_d = [I(f"w3_{i}", NEXP * D, 512) for i in range(2)]
        w2_d = [I(f"w2_{i}", NEXP * 512, D) for i in range(2)]
        wmask_d = I("wmask", 128, 16 * 384)
        nab_d = I("nab", 128, 16 * 8 * 640)
        outT = P.dram("outT", D, NOWN, F32, "ExternalOutput")
        skind = "ExternalOutput" if DEBUG else "Internal"
        QT = P.dram("QT", 1024, NQ, BF16, skind)
        KT = P.dram("KT", 768, NK0, BF16, skind)
        VA = P.dram("VA", 128, 66 * VW, BF16, skind)
        h1T = P.dram("h1T", D, NEXT, F32, skind)
        hc1T = P.dram("hc1T", D, NCTX, F32, skind)

        arena_t = st.enter_context(nc.sbuf_tensor("arena", [128, ARENA_WORDS], F32))
        AR = Arena(Buf(arena_t, P.new_tid(), 128, ARENA_WORDS, 1.0))
        psum_t = st.enter_context(nc.psum_tensor("psum", [128, 4096], F32))
        PS = Buf(psum_t, P.new_tid(), 128, 4096, 1.0)

        def bank(b, n=512, p=128):
            return PS.v(0, p, b * 512, b * 512 + n)

        def bank_bf(b, n):
            return V(PS.t[:, b * 512:b * 512 + n // 2].bitcast(BF16), PS.tid, 0, 128, b * 512, b * 512 + n // 2, 0.5)

        def dchunks(buf, c0, c1, r0=0, nchunk=8):
            ap = buf.t[r0:r0 + nchunk * 128, c0:c1].rearrange("(c p) n -> p c n", p=128)
            return V(ap, buf.tid, r0, r0 + nchunk * 128, c0 * buf.wpe, c1 * buf.wpe, buf.wpe)

        def v3(v, k):
            return V(r3(v.ap, k), v.tid, v.p0, v.p1, v.f0, v.f1, v.wpe)

        def bcast(buf, n):
            return V(buf.t[0:1, 0:n].partition_broadcast(128), buf.tid, 0, 1, 0, n * buf.wpe, buf.wpe)

        ident32 = AR.f32(128)
        identb = AR.bf(128)
        ones = AR.f32(128)
        bones = AR.f32(128)
        lnT = AR.f32(64)
        gt = AR.f32(4)
        dl = AR.f32(256)
        sg1 = AR.f32(128)
        esink = AR.f32(8)
        eps_ln = AR.f32(1)
        eps_rms = AR.f32(1)
        nlam = AR.f32(1)
        ML = [AR.f32(48) for _ in range(2)]
        MC = [AR.f32(48) for _ in range(2)]
        sc = AR.f32(16)
        wr = [AR.f32(8 * 36) for _ in range(2)]
        brb = [AR.f32(36) for _ in range(2)]
        P.dma("sp", ident32, ident_d.v())
        P.dma("sp", bones, bones_d.v())
        P.dma("sp", lnT, lnT_d.v())
        P.dma("sp", gt, gt_d.v())
        P.dma("sp", dl, bcast(dlam_d, 256))
        P.dma("sp", sg1, bcast(subg_d, 128))
        P.dma("sp", esink, bcast(sink_d, 8))
        for i in range(2):
            P.dma("sp", v3(wr[i], 8), dchunks(wr_d[i], 0, 36))
            P.dma("sp", brb[i], bcast(br_d[i], 36))
        P.op("dve", OP("tensor_copy", out=identb.ap, in_=ident32.ap), [ident32], [identb])
        P.op("dve", OP("memset", ones.ap, 1.0), [], [ones])
        P.op("dve", OP("memset", eps_ln.ap, LN_EPS), [], [eps_ln])
        P.op("dve", OP("memset", eps_rms.ap, RMS_EPS), [], [eps_rms])
        P.op("act", OP("activation", out=esink.ap, in_=esink.ap, func=AF.Exp), [esink], [esink])
        P.op("dve", OP("tensor_scalar", out=sg1.ap, in0=sg1.ap, scalar1=1.0 - LAM_INIT0, scalar2=None, op0=ALU.mult), [sg1], [sg1])
        lt = AR.f32(128)
        ls = AR.f32(2)
        dl4 = dl.ap.rearrange("p (a b n) -> p a b n", a=2, b=2)
        P.op("dve", OP("tensor_tensor", out=lt.ap.rearrange("p (a n) -> p a n", a=2), in0=dl4[:, :, 0, :], in1=dl4[:, :, 1, :], op=ALU.mult), [dl], [lt])
        P.op("dve", OP("tensor_reduce", out=ls.ap, in_=lt.ap.rearrange("p (a n) -> p a n", a=2), axis=AX.X, op=ALU.add), [lt], [ls])
        P.op("act", OP("activation", out=ls.ap, in_=ls.ap, func=AF.Exp), [ls], [ls])
        P.op("dve", OP("tensor_tensor", out=nlam.ap, in0=ls.ap[:, 1:2], in1=ls.ap[:, 0:1], op=ALU.subtract), [ls], [nlam])
        P.op("dve", OP("tensor_scalar", out=nlam.ap, in0=nlam.ap, scalar1=-LAM_INIT0, scalar2=None, op0=ALU.add), [nlam], [nlam])

        base_top = AR.top
        P.dma("sp", v3(sc, 8), dchunks(ccT, 0, 2))
        P.op("act", OP("activation", out=sc.ap, in_=sc.ap, func=AF.Silu), [sc], [sc])
        mwb = [AR.f32(8 * 512) for _ in range(2)]
        mbt = AR.f32(48)
        for i in range(2):
            P.dma("sp", mbt, modb[i].v())
            pm = bank(6, 96)
            for pc in range(12):
                w = mwb[pc % 2]
                P.dma("sp", v3(w, 8), dchunks(modw[i], pc * 512, pc * 512 + 512))
                for j in range(4):
                    cc = pc * 4 + j
                    for dc in range(8):
                        P.op("pe", OP("matmul",
                            pm.ap[:, cc * 2:cc * 2 + 2], lhsT=w.ap[:, dc * 512 + j * 128: dc * 512 + j * 128 + 128],
                            rhs=sc.ap[:, dc * 2:dc * 2 + 2], start=(dc == 0), stop=(dc == 7)),
                            [w, sc], [pm], sig=(dc == 7))
            pm3 = pm.ap.rearrange("p (c n) -> p c n", n=2)
            P.op("dve", OP("tensor_tensor", out=ML[i].ap, in0=pm3[:, :, 0], in1=mbt.ap, op=ALU.add), [pm, mbt], [ML[i]])
            P.op("dve", OP("tensor_tensor", out=MC[i].ap, in0=pm3[:, :, 1], in1=mbt.ap, op=ALU.add), [pm, mbt], [MC[i]])
            for M in (ML[i], MC[i]):
                for k in (1, 4):
                    P.op("dve", OP("tensor_scalar", out=M.ap[:, k * 8:k * 8 + 8], in0=M.ap[:, k * 8:k * 8 + 8],
                                                                    scalar1=1.0, scalar2=None, op0=ALU.add), [M], [M])
        AR.top = base_top

        FT = AR.bf(8 * NQ)
        H = AR.f32(8 * NQ)
        free_top = AR.top

        def ln_block(ucf, nt, li, which, out_fn, tmp):
            s1, s2 = bank(6, nt), bank(7, nt)
            for c in range(8):
                uc = ucf(c)
                P.op("pe", OP("matmul", s1.ap, lhsT=ones.ap, rhs=uc.ap, start=(c == 0), stop=(c == 7)),
                     [ones, uc], [s1], sig=(c == 7))
            for c in range(8):
                uc = ucf(c)
                q = tmp["sq"].get().s(0, nt)
                P.op("act", OP("activation", out=q.ap, in_=uc.ap, func=AF.Square), [uc], [q])
                P.op("pe", OP("matmul", s2.ap, lhsT=ones.ap, rhs=q.ap, start=(c == 0), stop=(c == 7)),
                     [ones, q], [s2], sig=True)
            mean, msq, rstd = tmp["mean"].s(0, nt), tmp["msq"].s(0, nt), tmp["rstd"].s(0, nt)
            P.op("act", OP("activation", out=mean.ap, in_=s1.ap, func=AF.Copy, scale=1.0 / D), [s1], [mean])
            P.op("act", OP("activation", out=msq.ap, in_=s1.ap, func=AF.Square, scale=1.0 / D), [s1], [msq])
            P.op("dve", OP("scalar_tensor_tensor", out=rstd.ap, in0=s2.ap, scalar=1.0 / D, in1=msq.ap, op0=ALU.mult, op1=ALU.subtract),
                 [s2, msq], [rstd])
            P.op("act", OP("activation", out=rstd.ap, in_=rstd.ap, func=AF.Sqrt, bias=eps_ln.ap, scale=1.0), [rstd, eps_ln], [rstd])
            P.op("dve", OP("reciprocal", out=rstd.ap, in_=rstd.ap), [rstd], [rstd])
            gi = ((li * 2 + which) * 2 + 0) * 8
            bi = ((li * 2 + which) * 2 + 1) * 8
            for c in range(8):
                uc = ucf(c)
                o = out_fn(c)
                P.op("pool", OP("tensor_tensor", out=uc.ap, in0=uc.ap, in1=mean.ap, op=ALU.subtract), [uc, mean], [uc])
                P.op("dve", OP("tensor_tensor", out=uc.ap, in0=uc.ap, in1=rstd.ap, op=ALU.mult), [uc, rstd], [uc])
                P.op("dve", OP("tensor_scalar", out=o.ap, in0=uc.ap, scalar1=lnT.ap[:, gi + c:gi + c + 1],
                                                                      scalar2=lnT.ap[:, bi + c:bi + c + 1], op0=ALU.mult, op1=ALU.add),
                     [uc, lnT], [o])

        def layer(li):
            ab = (li == 0)
            NT = NQ if ab else NOWN

            def Hc(c, t0, t1):
                return H.s(c * NT + t0, c * NT + t1)

            AR.top = FT.f0 if False else int(FT.f0)
            wq, wqs, wk, wks, wv = AR.bf(8 * 1024), AR.bf(8 * 1024), AR.bf(8 * 768), AR.bf(8 * 768), AR.bf(8 * 640)
            for dst, src, n in ((wq, wq_d[li], 1024), (wqs, wqs_d[li], 1024), (wk, wk_d[li], 768), (wks, wks_d[li], 768), (wv, wv_d[li], 640)):
                P.dma("pool", v3(dst, 8), dchunks(src, 0, n))
            xb = Rot([AR.f32(8 * 512) for _ in range(2)])
            abuf = Rot([AR.bf(8 * 512) for _ in range(2)])
            ctab = Rot([AR.f32(512) for _ in range(2)])
            stab = Rot([AR.f32(512) for _ in range(2)])
            t1r = Rot([AR.f32(512) for _ in range(2)])
            t2r = Rot([AR.f32(512) for _ in range(2)])
            obr = Rot([AR.bf(512) for _ in range(3)])
            sqr = Rot([AR.f32(512) for _ in range(2)])
            rvr = Rot([AR.f32(512) for _ in range(2)])
            vst = Rot([AR.bf(VW) for _ in range(2)])
            for vs in vst.items:
                P.op("pool", OP("memset", vs.ap, 1.0), [], [vs])
            pbank = Rot([0, 1, 2, 3, 4, 5])
            if ab:
                q_rope, q_norm = [True] * 8, [False] * 4 + [True] * 4
                k_rope, k_norm = [True] * 6, [False] * 4 + [True] * 2
            else:
                q_rope, q_norm = [False] * 4 + [True] * 4, [False] * 8
                k_rope, k_norm = [False] * 4 + [True] * 2, [False] * 6

            def fm_proj(aT, nt, W, Ws, wn, ch, rope, norm, gcol, ct, stb, dst):
                pa = bank(pbank.get(), nt)
                for c in range(8):
                    P.op("pe", OP("matmul", pa.ap, lhsT=W.ap[:, c * wn + ch * 128: c * wn + ch * 128 + 128],
                                                      rhs=aT.ap[:, c * nt:c * nt + nt], start=(c == 0), stop=(c == 7)),
                         [W, aT], [pa], sig=(c == 7))
                ob = obr.get().s(0, nt)
                if not rope:
                    P.op("act", OP("activation", out=ob.ap, in_=pa.ap, func=AF.Copy), [pa], [ob])
                else:
                    pb = bank(pbank.get(), nt)
                    for c in range(8):
                        P.op("pe", OP("matmul", pb.ap, lhsT=Ws.ap[:, c * wn + ch * 128: c * wn + ch * 128 + 128],
                                                          rhs=aT.ap[:, c * nt:c * nt + nt], start=(c == 0), stop=(c == 7)),
                             [Ws, aT], [pb], sig=(c == 7))
                    t1, t2 = t1r.get().s(0, nt), t2r.get().s(0, nt)
                    if norm:
                        sq = sqr.get().s(0, nt)
                        P.op("act", OP("activation", out=sq.ap, in_=pa.ap, func=AF.Square), [pa], [sq])
                        pss = bank(pbank.get(), nt)
                        P.op("pe", OP("matmul", pss.ap, lhsT=bones.ap, rhs=sq.ap, start=True, stop=True), [bones, sq], [pss])
                        rv = rvr.get().s(0, nt)
                        P.op("act", OP("activation", out=rv.ap, in_=pss.ap, func=AF.Sqrt, bias=eps_rms.ap, scale=1.0 / 64), [pss, eps_rms], [rv])
                        P.op("dve", OP("reciprocal", out=rv.ap, in_=rv.ap), [rv], [rv])
                        P.op("dve", OP("scalar_tensor_tensor", out=t1.ap, in0=pa.ap, scalar=gt.ap[:, gcol:gcol + 1], in1=ct.ap, op0=ALU.mult, op1=ALU.mult),
                             [pa, gt, ct], [t1])
                        P.op("dve", OP("scalar_tensor_tensor", out=t2.ap, in0=pb.ap, scalar=gt.ap[:, gcol + 1:gcol + 2], in1=stb.ap, op0=ALU.mult, op1=ALU.mult),
                             [pb, gt, stb], [t2])
                        P.op("pool", OP("tensor_tensor", out=t1.ap, in0=t1.ap, in1=t2.ap, op=ALU.add), [t1, t2], [t1])
                        P.op("dve", OP("tensor_tensor", out=ob.ap, in0=t1.ap, in1=rv.ap, op=ALU.mult), [t1, rv], [ob])
                    else:
                        P.op("dve", OP("tensor_tensor", out=t1.ap, in0=pa.ap, in1=ct.ap, op=ALU.mult), [pa, ct], [t1])
                        P.op("dve", OP("tensor_tensor", out=t2.ap, in0=pb.ap, in1=stb.ap, op=ALU.mult), [pb, stb], [t2])
                        P.op("pool", OP("tensor_tensor", out=ob.ap, in0=t1.ap, in1=t2.ap, op=ALU.add), [t1, t2], [ob])
                P.dma("sp", dst, ob)

            def source(src, ntok, M, ctd, std, toff, want_q, q_off, want_kv, k_off):
                for t0 in range(0, ntok, 512):
                    nt = min(512, ntok - t0)
                    x = xb.get().s(0, 8 * nt)
                    P.dma("sp", v3(x, 8), dchunks(src, t0, t0 + nt))
                    aT = abuf.get().s(0, 8 * nt)
                    for c in range(8):
                        P.op("act", OP("activation",
                            out=aT.ap[:, c * nt:c * nt + nt], in_=x.ap[:, c * nt:c * nt + nt], func=AF.Identity,
                            scale=M.ap[:, 8 + c:9 + c], bias=M.ap[:, c:c + 1]), [x, M], [aT])
                    ct, stb = ctab.get().s(0, nt), stab.get().s(0, nt)
                    P.dma("sp", ct, ctd.v(0, 128, toff + t0, toff + t0 + nt))
                    P.dma("sp", stb, std.v(0, 128, toff + t0, toff + t0 + nt))
                    if want_kv:
                        for ch in range(6):
                            fm_proj(aT, nt, wk, wks, 768, ch, k_rope[ch], k_norm[ch], 2, ct, stb,
                                    KT.v(ch * 128, ch * 128 + 128, k_off + t0, k_off + t0 + nt))
                        for tt in range(nt // 128):
                            pv1, pv2 = bank(pbank.get(), 512), bank(pbank.get(), 128)
                            for c in range(8):
                                lh = aT.ap[:, c * nt + tt * 128:c * nt + tt * 128 + 128]
                                P.op("pe", OP("matmul", pv1.ap, lhsT=lh, rhs=wv.ap[:, c * 640:c * 640 + 512], start=(c == 0), stop=(c == 7)),
                                     [aT, wv], [pv1], sig=(c == 7))
                            for c in range(8):
                                lh = aT.ap[:, c * nt + tt * 128:c * nt + tt * 128 + 128]
                                P.op("pe", OP("matmul", pv2.ap, lhsT=lh, rhs=wv.ap[:, c * 640 + 512:c * 640 + 640], start=(c == 0), stop=(c == 7)),
                                     [aT, wv], [pv2], sig=(c == 7))
                            vs = vst.get()
                            if ab:
                                o1 = vs.ap[:, 0:516].rearrange("p (h n) -> p h n", n=129)[:, :, 0:128]
                                i1 = pv1.ap.rearrange("p (h n) -> p h n", n=128)
                                o2 = vs.ap[:, 516:646].rearrange("p (h n) -> p h n", n=65)[:, :, 0:64]
                            else:
                                o1 = vs.ap[:, 0:520].rearrange("p (h n) -> p h n", n=65)[:, :, 0:64]
                                i1 = pv1.ap.rearrange("p (h n) -> p h n", n=64)
                                o2 = vs.ap[:, 520:650].rearrange("p (h n) -> p h n", n=65)[:, :, 0:64]
                            i2 = pv2.ap.rearrange("p (h n) -> p h n", n=64)
                            P.op("act", OP("activation", out=o1, in_=i1, func=AF.Copy), [pv1], [vs])
                            P.op("dve", OP("tensor_copy", out=o2, in_=i2), [pv2], [vs])
                            kt = (k_off + t0) // 128 + tt
                            P.dma("sp", VA.v(0, 128, kt * VW, kt * VW + VW), vs)
                    if want_q:
                        for ch in range(8):
                            fm_proj(aT, nt, wq, wqs, 1024, ch, q_rope[ch], q_norm[ch], 0, ct, stb,
                                    QT.v(ch * 128, ch * 128 + 128, q_off + t0, q_off + t0 + nt))

            if ab:
                source(xT, SEQ, ML[0], cK_d, sK_d, 0, False, 0, True, 0)
                source(cxT, NCTX, MC[0], cK_d, sK_d, SEQ, True, NEXT, True, SEQ)
                source(xeT, NEXT, ML[0], cQ_d, sQ_d, 0, True, 0, False, 0)
                nkt = SEQ // 128 + 2
            else:
                source(h1T, NEXT, ML[1], cQ_d, sQ_d, 0, True, 0, True, 0)
                source(hc1T, NCTX, MC[1], cQ_d, sQ_d, NEXT, False, 0, True, NEXT)
                nkt = NEXT // 128 + 2

            AR.top = int(H.f0)
            O = FT
            qbuf = Rot([AR.bf(NQ) for _ in range(2)])
            kbuf = Rot([AR.bf(nkt * 128) for _ in range(2)])
            vbuf = Rot([AR.bf(nkt * 130) for _ in range(2)])
            ptile = Rot([AR.bf(1024) for _ in range(3)])
            rz = Rot([AR.f32(1) for _ in range(6)])
            od1 = [AR.f32(128) for _ in range(4)]
            t_o2 = Rot([AR.f32(128) for _ in range(2)])
            t_od = Rot([AR.f32(128) for _ in range(2)])
            t_sq = Rot([AR.f32(128) for _ in range(2)])
            ssq = Rot([AR.f32(1) for _ in range(4)])
            stmp = Rot([AR.f32(640) for _ in range(2)])
            if not ab:
                wm = AR.f32(16 * 384)
                P.dma("sp", wm, wmask_d.v())
                nbb = Rot([AR.f32(640) for _ in range(2)])

            def attend512(kt_, qt_, vv, vcw, voff, dv, r0, q0, nq, kts, fin):
                nsub = nq // 128
                accs = [bank(2 + s, dv + 1) for s in range(nsub)]
                nk = len(kts)
                for ki, kt in enumerate(kts):
                    sb = bank(ki % 2, nq)
                    P.op("pe", OP("matmul",
                        sb.ap, lhsT=kt_.ap[r0:r0 + 64, kt * 128:kt * 128 + 128], rhs=qt_.ap[r0:r0 + 64, q0:q0 + nq], start=True, stop=True),
                        [kt_, qt_], [sb])
                    pt = ptile.get().s(0, nq)
                    P.op("act", OP("activation", out=pt.ap, in_=sb.ap, func=AF.Exp, scale=0.125), [sb], [pt])
                    for s in range(nsub):
                        P.op("pe", OP("matmul",
                            accs[s].ap, lhsT=pt.ap[:, s * 128:s * 128 + 128], rhs=vv.ap[:, kt * vcw + voff:kt * vcw + voff + dv + 1],
                            start=(ki == 0), stop=(ki == nk - 1)), [pt, vv], [accs[s]], sig=(s == nsub - 1))
                for s in range(nsub):
                    fin(s, accs[s], q0 // 128 + s)

            for u in range(8):
                kc = u if u < 4 else 4 + (u - 4) // 2
                qt_, kt_, vt_ = qbuf.get(), kbuf.get(), vbuf.get()
                P.dma("sp", qt_, QT.v(u * 128, u * 128 + 128, 0, NQ))
                P.dma("sp", kt_, KT.v(kc * 128, kc * 128 + 128, 0, nkt * 128))
                if ab:
                    vc0, vcw = (u * 129, 129) if u < 4 else (516 + ((u - 4) // 2) * 65, 65)
                else:
                    vc0, vcw = (u * 130, 130) if u < 4 else (520 + ((u - 4) // 2) * 65, 65)
                vv = vt_.s(0, nkt * vcw)
                for g0 in range(0, nkt, 11):
                    src = V(VA.t[:, g0 * VW:(g0 + 11) * VW].rearrange("p (t c) -> p t c", c=VW)[:, :, vc0:vc0 + vcw],
                            VA.tid, 0, 128, g0 * VW * 0.5, (g0 + 11) * VW * 0.5, 0.5)
                    dstv = V(vv.ap[:, g0 * vcw:(g0 + 11) * vcw].rearrange("p (t c) -> p t c", c=vcw), vv.tid, 0, 128,
                             vv.f0 + g0 * vcw * 0.5, vv.f0 + (g0 + 11) * vcw * 0.5, 0.5)
                    P.dma("sp", dstv, src)

                if ab:
                    qblocks = [(q0, 512, list(range(nkt))) for q0 in range(0, NEXT, 512)] + [(NEXT, 256, [nkt - 2, nkt - 1])]
                    for (q0, nq, kts) in qblocks:
                        for c in range(2):
                            if u < 4:
                                def fin(s, a, ot, c=c, u=u):
                                    r = rz.get()
                                    P.op("dve", OP("reciprocal", out=r.ap, in_=a.ap[:, 128:129]), [a], [r])
                                    if c == 0:
                                        d1 = od1[s]
                                        P.op("dve", OP("tensor_scalar", out=d1.ap, in0=a.ap[:, 0:128], scalar1=r.ap[:, 0:1], scalar2=None, op0=ALU.mult), [a, r], [d1])
                                    else:
                                        d1 = od1[s]
                                        o2, od, sq, ss = t_o2.get(), t_od.get(), t_sq.get(), ssq.get()
                                        P.op("dve", OP("tensor_scalar", out=o2.ap, in0=a.ap[:, 0:128], scalar1=r.ap[:, 0:1], scalar2=None, op0=ALU.mult), [a, r], [o2])
                                        P.op("dve", OP("scalar_tensor_tensor", out=od.ap, in0=o2.ap, scalar=nlam.ap[:, 0:1], in1=d1.ap, op0=ALU.mult, op1=ALU.add), [o2, nlam, d1], [od])
                                        P.op("pool", OP("tensor_tensor", out=sq.ap, in0=od.ap, in1=od.ap, op=ALU.mult), [od], [sq])
                                        P.op("dve", OP("tensor_reduce", out=ss.ap, in_=sq.ap, axis=AX.X, op=ALU.add), [sq], [ss])
                                        P.op("act", OP("activation", out=ss.ap, in_=ss.ap, func=AF.Sqrt, bias=eps_rms.ap, scale=1.0 / 128), [ss, eps_rms], [ss])
                                        P.op("dve", OP("reciprocal", out=ss.ap, in_=ss.ap), [ss], [ss])
                                        oo = O.s(ot * 1024 + u * 128, ot * 1024 + u * 128 + 128)
                                        P.op("dve", OP("scalar_tensor_tensor", out=oo.ap, in0=od.ap, scalar=ss.ap[:, 0:1], in1=sg1.ap, op0=ALU.mult, op1=ALU.mult), [od, ss, sg1], [oo])
                                attend512(kt_, qt_, vv, 129, 0, 128, 64 * c, q0, nq, kts, fin)
                            else:
                                def fin(s, a, ot, c=c, u=u):
                                    r = rz.get()
                                    P.op("dve", OP("reciprocal", out=r.ap, in_=a.ap[:, 64:65]), [a], [r])
                                    col = 512 + ((u - 4) * 2 + c) * 64
                                    oo = O.s(ot * 1024 + col, ot * 1024 + col + 64)
                                    P.op("dve", OP("tensor_scalar", out=oo.ap, in0=a.ap[:, 0:64], scalar1=r.ap[:, 0:1], scalar2=None, op0=ALU.mult), [a, r], [oo])
                                attend512(kt_, qt_, vv, 65, 0, 64, 64 * c, q0, nq, kts, fin)
                else:
                    for c in range(2):
                        r0 = 64 * c
                        for t in range(16):
                            if u < 4:
                                h = 2 * u + c
                                kts = list(range(t, t + 5)) + [20, 21]
                                nb = 5
                                bt = nbb.get()
                                P.dma("sp", bt, nab_d.v(0, 128, (t * 8 + h) * 640, (t * 8 + h) * 640 + 640))
                                voff = c * 65
                                col = 512 + h * 64
                            else:
                                h = (u - 4) * 2 + c
                                kts = list(range(t + 1, t + 4)) + [20, 21]
                                nb = 3
                                bt = wm.s(t * 384, t * 384 + 384)
                                voff = 0
                                col = h * 64
                            nk = len(kts)
                            sreg = PS.v(0, 128, (t % 2) * 1024, (t % 2) * 1024 + nk * 128)
                            qs = qt_.ap[r0:r0 + 64, 256 + t * 128:256 + t * 128 + 128]
                            for i, kt in enumerate(kts):
                                P.op("pe", OP("matmul",
                                    sreg.ap[:, i * 128:i * 128 + 128], lhsT=kt_.ap[r0:r0 + 64, kt * 128:kt * 128 + 128], rhs=qs, start=True, stop=True),
                                    [kt_, qt_], [sreg], sig=(i == nk - 1))
                            pt = ptile.get().s(0, nk * 128)
                            tm = stmp.get().s(0, nb * 128)
                            P.op("dve", OP("scalar_tensor_tensor",
                                out=tm.ap, in0=sreg.ap[:, 0:nb * 128], scalar=0.125, in1=bt.ap[:, 0:nb * 128], op0=ALU.mult, op1=ALU.add), [sreg, bt], [tm])
                            P.op("act", OP("activation", out=pt.ap[:, 0:nb * 128], in_=tm.ap, func=AF.Exp), [tm], [pt])
                            P.op("act", OP("activation", out=pt.ap[:, nb * 128:nk * 128], in_=sreg.ap[:, nb * 128:nk * 128], func=AF.Exp, scale=0.125),
                                 [sreg], [pt])
                            a = bank(4 + (t % 2), 65)
                            for i, kt in enumerate(kts):
                                P.op("pe", OP("matmul",
                                    a.ap, lhsT=pt.ap[:, i * 128:i * 128 + 128], rhs=vv.ap[:, kt * vcw + voff:kt * vcw + voff + 65], start=(i == 0), stop=(i == nk - 1)),
                                    [pt, vv], [a], sig=(i == nk - 1))
                            r = rz.get()
                            if u >= 4:
                                P.op("dve", OP("tensor_scalar", out=r.ap, in0=a.ap[:, 64:65], scalar1=esink.ap[:, h:h + 1], scalar2=None, op0=ALU.add), [a, esink], [r])
                                P.op("dve", OP("reciprocal", out=r.ap, in_=r.ap), [r], [r])
                            else:
                                P.op("dve", OP("reciprocal", out=r.ap, in_=a.ap[:, 64:65]), [a], [r])
                            oo = O.s(t * 1024 + col, t * 1024 + col + 64)
                            P.op("dve", OP("tensor_scalar", out=oo.ap, in0=a.ap[:, 0:64], scalar1=r.ap[:, 0:1], scalar2=None, op0=ALU.mult), [a, r], [oo])

            AR.top = free_top
            wo = AR.bf(8 * 1024)
            P.dma("pool", v3(wo, 8), dchunks(wo_d[li], 0, 1024))
            oTr = Rot([AR.bf(8 * 512) for _ in range(2)])
            lntmp = {"sq": Rot([AR.f32(512) for _ in range(2)]), "mean": AR.f32(512), "msq": AR.f32(512), "rstd": AR.f32(512)}
            if ab:
                blocks = [(t0, 512, xeT, t0, ML[0]) for t0 in range(0, NEXT, 512)] + [(NEXT, 256, cxT, 0, MC[0])]
            else:
                blocks = [(t0, 512, h1T, 256 + t0, ML[1]) for t0 in range(0, NOWN, 512)]
            pb3 = Rot([0, 1, 2, 3, 4, 5])
            for (t0, nt, src, c0, M) in blocks:
                for c in range(8):
                    P.dma("sp", Hc(c, t0, t0 + nt), src.v(c * 128, c * 128 + 128, c0, c0 + nt))
                oT = oTr.get().s(0, 8 * nt)
                for ch in range(8):
                    b = pb3.get()
                    pst = bank_bf(b, nt)
                    for tt in range(nt // 128):
                        tile = t0 // 128 + tt
                        oin = O.s(tile * 1024 + ch * 128, tile * 1024 + ch * 128 + 128)
                        P.op("pe", OP("transpose", out=pst.ap[:, tt * 128:tt * 128 + 128], in_=oin.ap, identity=identb.ap),
                             [oin, identb], [pst], sig=(tt == nt // 128 - 1))
                    od_ = oT.s(ch * nt, ch * nt + nt)
                    if ch % 2 == 0:
                        P.op("act", OP("activation", out=od_.ap, in_=pst.ap, func=AF.Copy), [pst], [od_])
                    else:
                        P.op("dve", OP("tensor_copy", out=od_.ap, in_=pst.ap), [pst], [od_])
                for dc in range(8):
                    pp = bank(pb3.get(), nt)
                    for ch in range(8):
                        P.op("pe", OP("matmul", pp.ap, lhsT=wo.ap[:, ch * 1024 + dc * 128:ch * 1024 + dc * 128 + 128],
                                                                                    rhs=oT.ap[:, ch * nt:ch * nt + nt], start=(ch == 0), stop=(ch == 7)),
                             [wo, oT], [pp], sig=(ch == 7))
                    hc = Hc(dc, t0, t0 + nt)
                    P.op("act", OP("activation", out=hc.ap, in_=hc.ap, func=AF.Copy, scale=ALPHA), [hc], [hc])
                    P.op("dve", OP("scalar_tensor_tensor", out=hc.ap, in0=pp.ap, scalar=M.ap[:, 16 + dc:17 + dc], in1=hc.ap, op0=ALU.mult, op1=ALU.add),
                         [pp, M, hc], [hc])
                ln_block(lambda c: Hc(c, t0, t0 + nt), nt, li, 0, lambda c: Hc(c, t0, t0 + nt), lntmp)

            AR.top = free_top
            WT = AR.f32(NT, P=32)
            moe_top = AR.top
            ftmp = AR.f32(1024)
            Lg = Rot([AR.f32(36) for _ in range(2)])
            sm = Rot([AR.f32(8) for _ in range(24)])
            em_r = Rot([AR.f32(32) for _ in range(2)])
            wt_r = Rot([AR.f32(32) for _ in range(2)])
            w2t_r = Rot([AR.f32(32) for _ in range(2)])
            pb4 = Rot([0, 1, 2, 3, 4, 5, 6, 7])
            mblocks = [(t0, min(512, NT - t0)) for t0 in range(0, NT, 512)]

            def Mof(t0):
                return (MC[li] if (ab and t0 >= NEXT) else ML[li])

            for (t0, nt) in mblocks:
                M = Mof(t0)
                for tt in range(nt // 128):
                    tk = t0 + tt * 128
                    for c in range(8):
                        hc = Hc(c, tk, tk + 128)
                        fo = ftmp.s(c * 128, c * 128 + 128)
                        P.op("dve", OP("tensor_scalar", out=fo.ap, in0=hc.ap, scalar1=M.ap[:, 32 + c:33 + c], scalar2=M.ap[:, 24 + c:25 + c],
                                                                                 op0=ALU.mult, op1=ALU.add), [hc, M], [fo])
                    pl = bank(pb4.get(), 36)
                    for c in range(8):
                        fo = ftmp.s(c * 128, c * 128 + 128)
                        P.op("pe", OP("matmul", pl.ap, lhsT=fo.ap, rhs=wr[li].ap[:, c * 36:c * 36 + 36], start=(c == 0), stop=(c == 7)),
                             [fo, wr[li]], [pl], sig=(c == 7))
                    L = Lg.get()
                    P.op("dve", OP("tensor_tensor", out=L.ap, in0=pl.ap, in1=brb[li].ap, op=ALU.add), [pl, brb[li]], [L])
                    gmax, ngmax, gexp, gsum, pen, m8, dd, w1, w2 = [sm.get() for _ in range(9)]
                    em, Wt, W2 = em_r.get(), wt_r.get(), w2t_r.get()
                    P.op("dve", OP("tensor_reduce", out=gmax.ap[:, 0:1], in_=L.ap[:, 0:4], axis=AX.X, op=ALU.max), [L], [gmax])
                    P.op("dve", OP("tensor_scalar", out=ngmax.ap[:, 0:1], in0=gmax.ap[:, 0:1], scalar1=-1.0, scalar2=None, op0=ALU.mult), [gmax], [ngmax])
                    P.op("act", OP("activation", out=gexp.ap[:, 0:4], in_=L.ap[:, 0:4], func=AF.Exp, bias=ngmax.ap[:, 0:1], scale=1.0), [L, ngmax], [gexp])
                    P.op("dve", OP("tensor_reduce", out=gsum.ap[:, 0:1], in_=gexp.ap[:, 0:4], axis=AX.X, op=ALU.add), [gexp], [gsum])
                    P.op("dve", OP("reciprocal", out=gsum.ap[:, 0:1], in_=gsum.ap[:, 0:1]), [gsum], [gsum])
                    P.op("dve", OP("tensor_scalar", out=pen.ap[:, 0:4], in0=L.ap[:, 0:4], scalar1=gmax.ap[:, 0:1], scalar2=1e30, op0=ALU.is_equal, op1=ALU.mult), [L, gmax], [pen])
                    P.op("dve", OP("tensor_scalar", out=pen.ap[:, 0:4], in0=pen.ap[:, 0:4], scalar1=-1e30, scalar2=None, op0=ALU.add), [pen], [pen])
                    for g in range(4):
                        P.op("dve", OP("tensor_scalar", out=em.ap[:, 8 * g:8 * g + 8], in0=L.ap[:, 4 + 8 * g:12 + 8 * g], scalar1=pen.ap[:, g:g + 1], scalar2=None, op0=ALU.add),
                             [L, pen], [em])
                    P.op("dve", OP("max", out=m8.ap, in_=em.ap), [em], [m8])
                    P.op("dve", OP("tensor_tensor", out=dd.ap[:, 0:1], in0=m8.ap[:, 1:2], in1=m8.ap[:, 0:1], op=ALU.subtract), [m8], [dd])
                    P.op("act", OP("activation", out=dd.ap[:, 0:1], in_=dd.ap[:, 0:1], func=AF.Exp), [dd], [dd])
                    P.op("dve", OP("tensor_scalar", out=w1.ap[:, 0:1], in0=dd.ap[:, 0:1], scalar1=1.0, scalar2=None, op0=ALU.add), [dd], [w1])
                    P.op("dve", OP("reciprocal", out=w1.ap[:, 0:1], in_=w1.ap[:, 0:1]), [w1], [w1])
                    P.op("dve", OP("tensor_tensor", out=w1.ap[:, 0:1], in0=w1.ap[:, 0:1], in1=gsum.ap[:, 0:1], op=ALU.mult), [w1, gsum], [w1])
                    P.op("dve", OP("tensor_tensor", out=w2.ap[:, 0:1], in0=w1.ap[:, 0:1], in1=dd.ap[:, 0:1], op=ALU.mult), [w1, dd], [w2])
                    P.op("dve", OP("tensor_scalar", out=Wt.ap, in0=em.ap, scalar1=m8.ap[:, 0:1], scalar2=w1.ap[:, 0:1], op0=ALU.is_equal, op1=ALU.mult), [em, m8, w1], [Wt])
                    P.op("dve", OP("tensor_scalar", out=W2.ap, in0=em.ap, scalar1=m8.ap[:, 1:2], scalar2=w2.ap[:, 0:1], op0=ALU.is_equal, op1=ALU.mult), [em, m8, w2], [W2])
                    P.op("dve", OP("tensor_tensor", out=Wt.ap, in0=Wt.ap, in1=W2.ap, op=ALU.add), [Wt, W2], [Wt])
                    ptr = bank(pb4.get(), 128, p=32)
                    P.op("pe", OP("transpose", out=ptr.ap, in_=Wt.ap, identity=ident32.ap), [Wt, ident32], [ptr])
                    wts = WT.s(tk, tk + 128)
                    P.op("act", OP("activation", out=wts.ap, in_=ptr.ap, func=AF.Copy), [ptr], [wts])
                for c in range(8):
                    hc = Hc(c, t0, t0 + nt)
                    fo = FT.s(c * NT + t0, c * NT + t0 + nt)
                    P.op("act", OP("activation", out=fo.ap, in_=hc.ap, func=AF.Identity, scale=M.ap[:, 32 + c:33 + c], bias=M.ap[:, 24 + c:25 + c]), [hc, M], [fo])
                    P.op("pool", OP("tensor_scalar", out=hc.ap, in0=hc.ap, scalar1=ALPHA, scalar2=None, op0=ALU.mult), [hc], [hc])

            AR.top = moe_top
            w1b = Rot([AR.bf(8 * 512) for _ in range(2)])
            w3b = Rot([AR.bf(8 * 512) for _ in range(2)])
            w2b = Rot([AR.bf(4 * 1024) for _ in range(1)])
            gb = Rot([AR.bf(4 * 512) for _ in range(1)])
            wbc = Rot([AR.f32(512) for _ in range(2)])
            sb_ = Rot([AR.f32(512) for _ in range(2)])
            Eb = Rot([AR.f32(128, P=32) for _ in range(2)])
            for ex in range(NEXP):
                w1e, w3e, w2e = w1b.get(), w3b.get(), w2b.get()
                P.dma("pool", v3(w1e, 8), dchunks(w1_d[li], 0, 512, r0=ex * 1024))
                P.dma("pool", v3(w3e, 8), dchunks(w3_d[li], 0, 512, r0=ex * 1024))
                P.dma("pool", v3(w2e, 4), dchunks(w2_d[li], 0, 1024, r0=ex * 512, nchunk=4))
                E = Eb.get()
                P.op("dve", OP("tensor_copy", out=E.ap, in_=ident32.ap[0:32, ex:ex + 1].to_broadcast([32, 128])), [ident32], [E])
                for (t0, nt) in mblocks:
                    M = Mof(t0)
                    pw = bank(pb4.get(), nt)
                    wts = WT.s(t0, t0 + nt)
                    P.op("pe", OP("matmul", pw.ap, lhsT=E.ap, rhs=wts.ap, start=True, stop=True), [E, wts], [pw])
                    wb = wbc.get().s(0, nt)
                    P.op("act", OP("activation", out=wb.ap, in_=pw.ap, func=AF.Copy), [pw], [wb])
                    g = gb.get().s(0, 4 * nt)
                    for ec in range(4):
                        p1, p3 = bank(pb4.get(), nt), bank(pb4.get(), nt)
                        for c in range(8):
                            fr = FT.s(c * NT + t0, c * NT + t0 + nt)
                            P.op("pe", OP("matmul", p1.ap, lhsT=w1e.ap[:, c * 512 + ec * 128:c * 512 + ec * 128 + 128], rhs=fr.ap, start=(c == 0), stop=(c == 7)),
                                 [w1e, fr], [p1], sig=(c == 7))
                        for c in range(8):
                            fr = FT.s(c * NT + t0, c * NT + t0 + nt)
                            P.op("pe", OP("matmul", p3.ap, lhsT=w3e.ap[:, c * 512 + ec * 128:c * 512 + ec * 128 + 128], rhs=fr.ap, start=(c == 0), stop=(c == 7)),
                                 [w3e, fr], [p3], sig=(c == 7))
                        sv = sb_.get().s(0, nt)
                        P.op("act", OP("activation", out=sv.ap, in_=p1.ap, func=AF.Silu), [p1], [sv])
                        P.op("dve", OP("tensor_tensor", out=sv.ap, in0=p3.ap, in1=sv.ap, op=ALU.mult), [p3, sv], [sv])
                        gv = g.s(ec * nt, ec * nt + nt)
                        P.op("pool", OP("tensor_tensor", out=gv.ap, in0=sv.ap, in1=wb.ap, op=ALU.mult), [sv, wb], [gv])
                    for dc in range(8):
                        py = bank(pb4.get(), nt)
                        for ec in range(4):
                            P.op("pe", OP("matmul", py.ap, lhsT=w2e.ap[:, ec * 1024 + dc * 128:ec * 1024 + dc * 128 + 128],
                                                                                          rhs=g.ap[:, ec * nt:ec * nt + nt], start=(ec == 0), stop=(ec == 3)),
                                 [w2e, g], [py], sig=(ec == 3))
                        hc = Hc(dc, t0, t0 + nt)
                        P.op("dve", OP("scalar_tensor_tensor", out=hc.ap, in0=py.ap, scalar=M.ap[:, 40 + dc:41 + dc], in1=hc.ap, op0=ALU.mult, op1=ALU.add),
                             [py, M, hc], [hc])

            AR.top = moe_top
            lntmp = {"sq": Rot([AR.f32(512) for _ in range(2)]), "mean": AR.f32(512), "msq": AR.f32(512), "rstd": AR.f32(512)}
            for (t0, nt) in mblocks:
                ln_block(lambda c: Hc(c, t0, t0 + nt), nt, li, 1, lambda c: Hc(c, t0, t0 + nt), lntmp)
                for c in range(8):
                    if ab:
                        if t0 < NEXT:
                            dst = h1T.v(c * 128, c * 128 + 128, t0, t0 + nt)
                        else:
                            dst = hc1T.v(c * 128, c * 128 + 128, 0, nt)
                    else:
                        dst = outT.v(c * 128, c * 128 + 128, t0, t0 + nt)
                    P.dma("sp", dst, Hc(c, t0, t0 + nt))

        layer(0)
        layer(1)
        P.finish()
        P.emit()
    return nc


def _swap_cols(w):
    n = w.shape[1]
    idx = np.arange(n).reshape(n // 64, 2, 32)[:, ::-1, :].reshape(n)
    return w[:, idx]


def _rope_tables(pos):
    pos = np.asarray(pos)
    valid = pos >= 0
    p = np.where(valid, pos, 0)
    row = (p // 64).astype(np.float32)
    col = (p % 64).astype(np.float32)
    inv = (np.float32(10000.0) ** (-np.arange(16, dtype=np.float32) / np.float32(16))).astype(np.float32)
    ang = np.concatenate([row[:, None] * inv, col[:, None] * inv], -1).astype(np.float32)
    cos = np.cos(ang).astype(np.float32)
    sin = np.sin(ang).astype(np.float32)
    cos = np.where(valid[:, None], cos, np.float32(1.0))
    sin = np.where(valid[:, None], sin, np.float32(0.0))
    c64 = np.concatenate([cos, cos], 1)
    s64 = np.concatenate([-sin, sin], 1)
    cT = np.concatenate([c64, c64], 1).T
    sT = np.concatenate([s64, s64], 1).T
    return np.ascontiguousarray(cT, dtype=np.float32), np.ascontiguousarray(sT, dtype=np.float32)


def _ext_positions(j):
    pos = 2048 * j - 256 + np.arange(NEXT)
    if j == 0:
        pos[0:256] = 256 + np.arange(256)
    if j == 3:
        pos[2304:2560] = 7680 + np.arange(256)
    return pos


def _first_occurrence(kp):
    seen, out = set(), np.zeros(len(kp), dtype=bool)
    for i, p in enumerate(kp):
        if p not in seen:
            seen.add(p)
            out[i] = True
    return out


def _layer1_tables(j, rpb):
    pos = _ext_positions(j)
    wmask = np.full((128, 16, 3, 128), NEG, dtype=np.float32)
    nab = np.full((128, 16, 8, 5, 128), NEG, dtype=np.float32)
    for t in range(16):
        qp = 2048 * j + 128 * t + np.arange(128)
        kp = pos[128 * (t + 1):128 * (t + 4)]
        first = _first_occurrence(kp)
        ok = (np.abs(qp[None, :] - kp[:, None]) <= 128) & first[:, None]
        m = np.where(ok, np.float32(0.0), np.float32(NEG)).reshape(3, 128, 128)
        wmask[:, t] = m.transpose(1, 0, 2)
        kp = pos[128 * t:128 * (t + 5)]
        first = _first_occurrence(kp)
        qr, qc = qp // 64, qp % 64
        kr, kcl = kp // 64, kp % 64
        r0 = np.clip(qr - 4, 0, 120)
        c0 = np.clip(qc - 8, 0, 48)
        ok = ((kr[:, None] >= r0[None, :]) & (kr[:, None] < r0[None, :] + 8) &
              (kcl[:, None] >= c0[None, :]) & (kcl[:, None] < c0[None, :] + 16) & first[:, None])
        ro = np.clip(kr[:, None] - qr[None, :] + 7, 0, 14)
        co = np.clip(kcl[:, None] - qc[None, :] + 15, 0, 30)
        for h in range(8):
            b = np.where(ok, rpb[h][ro, co], np.float32(NEG)).astype(np.float32).reshape(5, 128, 128)
            nab[:, t, h] = b.transpose(1, 0, 2)
    return wmask.reshape(128, 16 * 384), nab.reshape(128, 16 * 8 * 640)


_NC_CACHE = {}


def kernel(x, c, ctx, c_ctx, mod_w, mod_b, ln_g, ln_b, ab_w_in, ab_w_out, diff_lambda, diff_subln_g, gqa_qk_g,
           cd_w_in, cd_w_out, win_sink, na_rpb, moe_w_group, moe_b_group, moe_w_router, moe_b_router,
           moe_w1, moe_w3, moe_w2):
    f = lambda a: np.ascontiguousarray(np.asarray(a), dtype=np.float32)
    x, c, ctx, c_ctx = f(x), f(c), f(ctx), f(c_ctx)
    mod_w, mod_b, ln_g, ln_b = f(mod_w), f(mod_b), f(ln_g), f(ln_b)
    ab_w_in, ab_w_out, cd_w_in, cd_w_out = f(ab_w_in)[0], f(ab_w_out)[0], f(cd_w_in)[0], f(cd_w_out)[0]
    diff_lambda, diff_subln_g, gqa_qk_g = f(diff_lambda)[0], f(diff_subln_g)[0], f(gqa_qk_g)[0]
    win_sink, na_rpb = f(win_sink)[0], f(na_rpb)[0]
    moe_w1, moe_w3, moe_w2 = f(moe_w1), f(moe_w3), f(moe_w2)

    shared = {}
    shared["ident"] = np.eye(128, dtype=np.float32)
    bo = np.zeros((128, 128), dtype=np.float32)
    bo[:64, :64] = 1.0
    bo[64:, 64:] = 1.0
    shared["bones"] = bo
    for i in range(2):
        shared[f"modw{i}"] = mod_w[i]
        shared[f"modb{i}"] = np.ascontiguousarray(mod_b[i].reshape(48, 128).T)
        shared[f"wr{i}"] = np.ascontiguousarray(np.concatenate([f(moe_w_group)[i], f(moe_w_router)[i]], 1))
        shared[f"br{i}"] = np.concatenate([f(moe_b_group)[i], f(moe_b_router)[i]])[None, :].copy()
        shared[f"w1_{i}"] = moe_w1[i].reshape(NEXP * D, 512)
        shared[f"w3_{i}"] = moe_w3[i].reshape(NEXP * D, 512)
        shared[f"w2_{i}"] = moe_w2[i].reshape(NEXP * 512, D)
    lnT = np.zeros((128, 64), dtype=np.float32)
    for i in range(2):
        for wch in range(2):
            lnT[:, ((i * 2 + wch) * 2 + 0) * 8:((i * 2 + wch) * 2 + 0) * 8 + 8] = ln_g[i, wch].reshape(8, 128).T
            lnT[:, ((i * 2 + wch) * 2 + 1) * 8:((i * 2 + wch) * 2 + 1) * 8 + 8] = ln_b[i, wch].reshape(8, 128).T
    shared["lnT"] = lnT
    q0 = ab_w_in[:, 0:1024]
    kd, vd = ab_w_in[:, 1024:1536], ab_w_in[:, 1536:2048]
    kg, vg = ab_w_in[:, 2048:2176], ab_w_in[:, 2176:2304]
    k0 = np.concatenate([kd, kg[:, 0:64], kg[:, 0:64], kg[:, 64:128], kg[:, 64:128]], 1)
    shared["wq0"], shared["wqs0"] = np.ascontiguousarray(q0), np.ascontiguousarray(_swap_cols(q0))
    shared["wk0"], shared["wks0"] = np.ascontiguousarray(k0), np.ascontiguousarray(_swap_cols(k0))
    shared["wv0"] = np.ascontiguousarray(np.concatenate([vd, vg], 1))
    shared["wo0"] = ab_w_out
    qw, qn = cd_w_in[:, 0:512], cd_w_in[:, 512:1024]
    kw, vw = cd_w_in[:, 1024:1152], cd_w_in[:, 1152:1280]
    kn, vn = cd_w_in[:, 1280:1792], cd_w_in[:, 1792:2304]
    q1 = np.concatenate([qn, qw], 1)
    k1 = np.concatenate([kn, kw[:, 0:64], kw[:, 0:64], kw[:, 64:128], kw[:, 64:128]], 1)
    shared["wq1"], shared["wqs1"] = np.ascontiguousarray(q1), np.ascontiguousarray(_swap_cols(q1))
    shared["wk1"], shared["wks1"] = np.ascontiguousarray(k1), np.ascontiguousarray(_swap_cols(k1))
    shared["wv1"] = np.ascontiguousarray(np.concatenate([vn, vw], 1))
    shared["wo1"] = cd_w_out
    sw = lambda g: np.concatenate([g[32:64], g[0:32]])
    gq, gk = gqa_qk_g[0], gqa_qk_g[1]
    shared["gt"] = np.ascontiguousarray(np.stack([np.tile(gq, 2), np.tile(sw(gq), 2), np.tile(gk, 2), np.tile(sw(gk), 2)], 1))
    shared["dlam"] = diff_lambda.reshape(1, 256).copy()
    shared["subg"] = diff_subln_g.reshape(1, 128).copy()
    shared["sink"] = win_sink.reshape(1, 8).copy()
    ck, sk = _rope_tables(np.concatenate([np.arange(SEQ), -np.ones(NCTX, dtype=np.int64)]))
    shared["cK"], shared["sK"] = ck, sk

    in_maps = []
    tabs = {}
    for core in range(8):
        b, j = core // 4, core % 4
        pos = _ext_positions(j)
        m = dict(shared)
        m["xT"] = np.ascontiguousarray(x[b].T)
        m["xeT"] = np.ascontiguousarray(x[b][pos].T)
        m["cxT"] = np.ascontiguousarray(ctx[b].T)
        m["ccT"] = np.ascontiguousarray(np.stack([c[b], c_ctx], 1))
        if j not in tabs:
            cq, sq = _rope_tables(np.concatenate([pos, -np.ones(NCTX, dtype=np.int64)]))
            wmask, nab = _layer1_tables(j, na_rpb)
            tabs[j] = (cq, sq, wmask, nab)
        m["cQ"], m["sQ"], m["wmask"], m["nab"] = tabs[j]
        in_maps.append(m)

    if "nc" not in _NC_CACHE:
        _NC_CACHE["nc"] = build_program()
    res = run_bass_kernel_spmd(_NC_CACHE["nc"], in_maps, core_ids=list(range(8)))
    out = np.empty((2, SEQ, D), dtype=np.float32)
    for core in range(8):
        b, j = core // 4, core % 4
        out[b, 2048 * j:2048 * (j + 1), :] = np.asarray(res.results[core]["outT"]).T
    if DEBUG:
        kernel.last = res
    return out
```

```python
import math
from contextlib import ExitStack

import numpy as np
import concourse.bass as bass
import concourse.mybir as mybir
from concourse.bass_utils import run_bass_kernel_spmd

F32 = mybir.dt.float32
BF16 = mybir.dt.bfloat16
AF = mybir.ActivationFunctionType
ALU = mybir.AluOpType
AX = mybir.AxisListType

DEBUG = False

D = 1024
SEQ = 8192
NCTX = 256
NEXT = 2560
NOWN = 2048
NQ = NEXT + NCTX
NK0 = SEQ + NCTX
NK1 = NEXT + NCTX
VW = 650
DEPTH = 2
ALPHA = (2.0 * DEPTH) ** 0.25
LN_EPS = 1e-5
RMS_EPS = 1e-6
LAM_INIT0 = 0.8 - 0.6 * math.exp(0.0)
NEG = -30000.0
NEXP = 32

SEM_LIM = 8000
NDMA = 12
ARENA_WORDS = 53000


class V:
    def __init__(self, ap, tid, p0, p1, f0, f1, wpe):
        self.ap, self.tid, self.p0, self.p1, self.f0, self.f1, self.wpe = ap, tid, p0, p1, f0, f1, wpe

    def reg(self):
        return (self.tid, self.p0, self.p1, self.f0, self.f1)

    def s(self, c0, c1, p0=None, p1=None):
        q0 = 0 if p0 is None else p0
        q1 = (self.p1 - self.p0) if p1 is None else p1
        return V(self.ap[q0:q1, c0:c1], self.tid, self.p0 + q0, self.p0 + q1,
                 self.f0 + c0 * self.wpe, self.f0 + c1 * self.wpe, self.wpe)


class Buf:
    def __init__(self, t, tid, P, F, wpe):
        self.t, self.tid, self.P, self.F, self.wpe = t, tid, P, F, wpe

    def v(self, p0=0, p1=None, f0=0, f1=None):
        p1 = self.P if p1 is None else p1
        f1 = self.F if f1 is None else f1
        return V(self.t[p0:p1, f0:f1], self.tid, p0, p1, f0 * self.wpe, f1 * self.wpe, self.wpe)


class Prog:
    ENGS = ["pe", "act", "dve", "pool", "sp"]

    def __init__(self, nc, stack):
        self.nc, self.stack = nc, stack
        self.recs = {e: [] for e in self.ENGS}
        self.sig = {e: 0 for e in self.ENGS}
        self.known = {e: {} for e in self.ENGS}
        self.csems = {e: [] for e in self.ENGS}
        self.dsems, self.dcnt, self.drr = {}, {}, {}
        for q in ["sp", "act", "pool"]:
            self.dsems[q] = [stack.enter_context(nc.semaphore(f"d_{q}_{k}")) for k in range(NDMA)]
            self.dcnt[q] = [0] * NDMA
            self.drr[q] = 0
        self.ent = {}
        self.ntid = 0

    def new_tid(self):
        self.ntid += 1
        return self.ntid

    def dram(self, name, P, F, dtype, kind):
        t = self.nc.dram_tensor(name, [P, F], dtype, kind=kind)
        return Buf(t, self.new_tid(), P, F, 1.0 if dtype == F32 else 0.5)

    def _csem(self, e, idx):
        while len(self.csems[e]) <= idx:
            self.csems[e].append(self.stack.enter_context(self.nc.semaphore(f"c_{e}_{len(self.csems[e])}")))
        return self.csems[e][idx]

    @staticmethod
    def _ov(a, b):
        return a[1] < b[2] and b[1] < a[2] and a[3] < b[4] and b[3] < a[4]

    @staticmethod
    def _cov(a, b):
        return a[1] <= b[1] and a[2] >= b[2] and a[3] <= b[3] and a[4] >= b[4]

    BUCK = 256

    def _cands(self, r):
        d = self.ent.get(r[0])
        if not d:
            return []
        seen, out = set(), []
        for b in range(int(r[3]) // self.BUCK, int(math.ceil(r[4])) // self.BUCK + 1):
            for en in d.get(b, ()):
                if en[3] and id(en) not in seen:
                    seen.add(id(en))
                    out.append(en)
        return out

    def _add(self, en):
        r = en[0]
        d = self.ent.setdefault(r[0], {})
        for b in range(int(r[3]) // self.BUCK, int(math.ceil(r[4])) // self.BUCK + 1):
            lst = d.setdefault(b, [])
            if len(lst) > 64:
                lst[:] = [x for x in lst if x[3]]
            lst.append(en)

    def _deps(self, reads, writes):
        raw, other = [], []
        for r in reads:
            for en in self._cands(r):
                if en[1] is not None and self._ov(en[0], r):
                    raw.append(en[1])
        for w in writes:
            for en in self._cands(w):
                if self._ov(en[0], w):
                    if en[1] is not None:
                        other.append(en[1])
                    other.extend(en[2])
        return raw, other

    def _update(self, tok, reads, writes):
        for r in reads:
            hit = False
            for en in self._cands(r):
                if self._ov(en[0], r):
                    if tok[0] == "c":
                        en[2][:] = [t for t in en[2] if not (t[0] == "c" and t[1] == tok[1])]
                    en[2].append(tok)
                    if self._cov(en[0], r):
                        hit = True
            if not hit:
                self._add([r, None, [tok], True])
        for w in writes:
            for en in self._cands(w):
                if self._cov(w, en[0]):
                    en[3] = False
            self._add([w, tok, [], True])

    def _waits(self, e, raw, other):
        waits = []
        kn = self.known[e]
        for kind, toks in (("raw", raw), ("oth", other)):
            for t in toks:
                if t[0] == "c":
                    _, f, v = t
                    if f == e and (kind == "oth" or e == "pe"):
                        continue
                    if kn.get(f, 0) >= v:
                        continue
                    kn[f] = v
                    waits.append(t)
                else:
                    _, q, k, c = t
                    if kn.get((q, k), 0) >= c:
                        continue
                    kn[(q, k)] = c
                    waits.append(t)
        return waits

    def op(self, e, fn, reads=(), writes=(), sig=True):
        reads = [r.reg() for r in reads]
        writes = [w.reg() for w in writes]
        raw, other = self._deps(reads, writes)
        waits = self._waits(e, raw, other)
        if sig:
            self.sig[e] += 1
            v = self.sig[e]
        else:
            v = self.sig[e] + 1
        tok = ("c", e, v)
        self._update(tok, reads, writes)
        self.recs[e].append((waits, fn, tok if sig else None))
        return tok

    def dma(self, q, out, in_, **kw):
        reads, writes = [in_.reg()], [out.reg()]
        raw, other = self._deps(reads, writes)
        k = self.drr[q]
        self.drr[q] = (k + 1) % NDMA
        prev = self.dcnt[q][k]
        if prev > 0:
            other = other + [("d", q, k, prev)]
        waits = self._waits(q, raw, other)
        self.dcnt[q][k] = prev + 1
        tok = ("d", q, k, prev + 1)
        self._update(tok, reads, writes)
        oa, ia = out.ap, in_.ap

        def fn(eng, oa=oa, ia=ia, kw=kw):
            return eng.dma_start(out=oa, in_=ia, **kw)

        self.recs[q].append((waits, fn, tok))
        return tok

    def finish(self):
        waits = []
        for q in self.dsems:
            for k in range(NDMA):
                if self.dcnt[q][k] > 0:
                    waits.append(("d", q, k, self.dcnt[q][k]))
        for e in ["pe", "act", "dve", "pool"]:
            if self.sig[e] > 0:
                waits.append(("c", e, self.sig[e]))
        self.recs["sp"].append((waits, None, None))

    def _emit_wait(self, eng, w):
        if w[0] == "c":
            _, f, v = w
            eng.wait_ge(self._csem(f, (v - 1) // SEM_LIM), (v - 1) % SEM_LIM + 1)
        else:
            _, q, k, c = w
            eng.wait_ge(self.dsems[q][k], 16 * c)

    def emit(self):
        nc = self.nc
        for e in self.ENGS:
            for idx in range((self.sig[e] + SEM_LIM - 1) // SEM_LIM + 1):
                self._csem(e, idx)
        recs, me = self.recs, self

        def play(e, eng):
            for waits, fn, tok in recs[e]:
                for w in waits:
                    me._emit_wait(eng, w)
                if fn is None:
                    continue
                ins = fn(eng)
                if tok is not None:
                    if tok[0] == "c":
                        ins.then_inc(me._csem(e, (tok[2] - 1) // SEM_LIM), 1)
                    else:
                        ins.then_inc(me.dsems[tok[1]][tok[2]], 16)

        with nc.Block() as block:
            @block.tensor
            def _(eng):
                play("pe", eng)

            @block.scalar
            def _(eng):
                play("act", eng)

            @block.vector
            def _(eng):
                play("dve", eng)

            @block.gpsimd
            def _(eng):
                play("pool", eng)

            @block.sync
            def _(eng):
                play("sp", eng)


class Arena:
    def __init__(self, buf):
        self.buf, self.top, self.hi = buf, 0, 0

    def _alloc(self, words):
        off = self.top
        self.top += (words + 7) // 8 * 8
        self.hi = max(self.hi, self.top)
        assert self.top <= self.buf.F, f"arena overflow {self.top}"
        return off

    def f32(self, n, P=128):
        off = self._alloc(n)
        return V(self.buf.t[0:P, off:off + n], self.buf.tid, 0, P, off, off + n, 1.0)

    def bf(self, n, P=128):
        w = (n + 1) // 2
        off = self._alloc(w)
        return V(self.buf.t[0:P, off:off + w].bitcast(BF16), self.buf.tid, 0, P, off, off + w, 0.5)


class Rot:
    def __init__(self, items):
        self.items, self.i = items, 0

    def get(self):
        it = self.items[self.i % len(self.items)]
        self.i += 1
        return it


def r3(ap, k):
    return ap.rearrange("p (k n) -> p k n", k=k)


def OP(name, *a, **k):
    return lambda e: getattr(e, name)(*a, **k)


def build_program():
    nc = bass.Bass("TRN2", target_bir_lowering=False)
    st = ExitStack()
    with st:
        P = Prog(nc, st)
        I = lambda name, p, f, dt=F32: P.dram(name, p, f, dt, "ExternalInput")
        xT = I("xT", D, SEQ)
        xeT = I("xeT", D, NEXT)
        cxT = I("cxT", D, NCTX)
        ccT = I("ccT", D, 2)
        ident_d = I("ident", 128, 128)
        bones_d = I("bones", 128, 128)
        modw = [I(f"modw{i}", D, 6 * D) for i in range(2)]
        modb = [I(f"modb{i}", 128, 48) for i in range(2)]
        lnT_d = I("lnT", 128, 64)
        wq_d = [I(f"wq{i}", D, 1024) for i in range(2)]
        wqs_d = [I(f"wqs{i}", D, 1024) for i in range(2)]
        wk_d = [I(f"wk{i}", D, 768) for i in range(2)]
        wks_d = [I(f"wks{i}", D, 768) for i in range(2)]
        wv_d = [I(f"wv{i}", D, 640) for i in range(2)]
        wo_d = [I(f"wo{i}", D, 1024) for i in range(2)]
        gt_d = I("gt", 128, 4)
        dlam_d = I("dlam", 1, 256)
        subg_d = I("subg", 1, 128)
        sink_d = I("sink", 1, 8)
        cK_d = I("cK", 128, NK0)
        sK_d = I("sK", 128, NK0)
        cQ_d = I("cQ", 128, NQ)
        sQ_d = I("sQ", 128, NQ)
        wr_d = [I(f"wr{i}", D, 36) for i in range(2)]
        br_d = [I(f"br{i}", 1, 36) for i in range(2)]
        w1_d = [I(f"w1_{i}", NEXP * D, 512) for i in range(2)]
        w3_d = [I(f"w3_{i}", NEXP * D, 512) for i in range(2)]
        w2_d = [I(f"w2_{i}", NEXP * 512, D) for i in range(2)]
        wmask_d = I("wmask", 128, 16 * 384)
        nab_d = I("nab", 128, 16 * 8 * 640)
        outT = P.dram("outT", D, NOWN, F32, "ExternalOutput")
        skind = "ExternalOutput" if DEBUG else "Internal"
        QT = P.dram("QT", 1024, NQ, BF16, skind)
        KT = P.dram("KT", 768, NK0, BF16, skind)
        VA = P.dram("VA", 128, 66 * VW, BF16, skind)
        h1T = P.dram("h1T", D, NEXT, F32, skind)
        hc1T = P.dram("hc1T", D, NCTX, F32, skind)

        arena_t = st.enter_context(nc.sbuf_tensor("arena", [128, ARENA_WORDS], F32))
        AR = Arena(Buf(arena_t, P.new_tid(), 128, ARENA_WORDS, 1.0))
        psum_t = st.enter_context(nc.psum_tensor("psum", [128, 4096], F32))
        PS = Buf(psum_t, P.new_tid(), 128, 4096, 1.0)

        def bank(b, n=512, p=128):
            return PS.v(0, p, b * 512, b * 512 + n)

        def bank_bf(b, n):
            return V(PS.t[:, b * 512:b * 512 + n // 2].bitcast(BF16), PS.tid, 0, 128, b * 512, b * 512 + n // 2, 0.5)

        def dchunks(buf, c0, c1, r0=0, nchunk=8):
            ap = buf.t[r0:r0 + nchunk * 128, c0:c1].rearrange("(c p) n -> p c n", p=128)
            return V(ap, buf.tid, r0, r0 + nchunk * 128, c0 * buf.wpe, c1 * buf.wpe, buf.wpe)

        def v3(v, k):
            return V(r3(v.ap, k), v.tid, v.p0, v.p1, v.f0, v.f1, v.wpe)

        def bcast(buf, n):
            return V(buf.t[0:1, 0:n].partition_broadcast(128), buf.tid, 0, 1, 0, n * buf.wpe, buf.wpe)

        ident32 = AR.f32(128)
        identb = AR.bf(128)
        ones = AR.f32(128)
        bones = AR.f32(128)
        lnT = AR.f32(64)
        gt = AR.f32(4)
        dl = AR.f32(256)
        sg1 = AR.f32(128)
        esink = AR.f32(8)
        eps_ln = AR.f32(1)
        eps_rms = AR.f32(1)
        nlam = AR.f32(1)
        ML = [AR.f32(48) for _ in range(2)]
        MC = [AR.f32(48) for _ in range(2)]
        sc = AR.f32(16)
        wr = [AR.f32(8 * 36) for _ in range(2)]
        brb = [AR.f32(36) for _ in range(2)]
        P.dma("sp", ident32, ident_d.v())
        P.dma("sp", bones, bones_d.v())
        P.dma("sp", lnT, lnT_d.v())
        P.dma("sp", gt, gt_d.v())
        P.dma("sp", dl, bcast(dlam_d, 256))
        P.dma("sp", sg1, bcast(subg_d, 128))
        P.dma("sp", esink, bcast(sink_d, 8))
        for i in range(2):
            P.dma("sp", v3(wr[i], 8), dchunks(wr_d[i], 0, 36))
            P.dma("sp", brb[i], bcast(br_d[i], 36))
        P.op("dve", OP("tensor_copy", out=identb.ap, in_=ident32.ap), [ident32], [identb])
        P.op("dve", OP("memset", ones.ap, 1.0), [], [ones])
        P.op("dve", OP("memset", eps_ln.ap, LN_EPS), [], [eps_ln])
        P.op("dve", OP("memset", eps_rms.ap, RMS_EPS), [], [eps_rms])
        P.op("act", OP("activation", out=esink.ap, in_=esink.ap, func=AF.Exp), [esink], [esink])
        P.op("dve", OP("tensor_scalar", out=sg1.ap, in0=sg1.ap, scalar1=1.0 - LAM_INIT0, scalar2=None, op0=ALU.mult), [sg1], [sg1])
        lt = AR.f32(128)
        ls = AR.f32(2)
        dl4 = dl.ap.rearrange("p (a b n) -> p a b n", a=2, b=2)
        P.op("dve", OP("tensor_tensor", out=lt.ap.rearrange("p (a n) -> p a n", a=2), in0=dl4[:, :, 0, :], in1=dl4[:, :, 1, :], op=ALU.mult), [dl], [lt])
        P.op("dve", OP("tensor_reduce", out=ls.ap, in_=lt.ap.rearrange("p (a n) -> p a n", a=2), axis=AX.X, op=ALU.add), [lt], [ls])
        P.op("act", OP("activation", out=ls.ap, in_=ls.ap, func=AF.Exp), [ls], [ls])
        P.op("dve", OP("tensor_tensor", out=nlam.ap, in0=ls.ap[:, 1:2], in1=ls.ap[:, 0:1], op=ALU.subtract), [ls], [nlam])
        P.op("dve", OP("tensor_scalar", out=nlam.ap, in0=nlam.ap, scalar1=-LAM_INIT0, scalar2=None, op0=ALU.add), [nlam], [nlam])

        base_top = AR.top
        P.dma("sp", v3(sc, 8), dchunks(ccT, 0, 2))
        P.op("act", OP("activation", out=sc.ap, in_=sc.ap, func=AF.Silu), [sc], [sc])
        mwb = [AR.f32(8 * 512) for _ in range(2)]
        mbt = AR.f32(48)
        for i in range(2):
            P.dma("sp", mbt, modb[i].v())
            pm = bank(6, 96)
            for pc in range(12):
                w = mwb[pc % 2]
                P.dma("sp", v3(w, 8), dchunks(modw[i], pc * 512, pc * 512 + 512))
                for j in range(4):
                    cc = pc * 4 + j
                    for dc in range(8):
                        P.op("pe", OP("matmul",
                            pm.ap[:, cc * 2:cc * 2 + 2], lhsT=w.ap[:, dc * 512 + j * 128: dc * 512 + j * 128 + 128],
                            rhs=sc.ap[:, dc * 2:dc * 2 + 2], start=(dc == 0), stop=(dc == 7)),
                            [w, sc], [pm], sig=(dc == 7))
            pm3 = pm.ap.rearrange("p (c n) -> p c n", n=2)
            P.op("dve", OP("tensor_tensor", out=ML[i].ap, in0=pm3[:, :, 0], in1=mbt.ap, op=ALU.add), [pm, mbt], [ML[i]])
            P.op("dve", OP("tensor_tensor", out=MC[i].ap, in0=pm3[:, :, 1], in1=mbt.ap, op=ALU.add), [pm, mbt], [MC[i]])
            for M in (ML[i], MC[i]):
                for k in (1, 4):
                    P.op("dve", OP("tensor_scalar", out=M.ap[:, k * 8:k * 8 + 8], in0=M.ap[:, k * 8:k * 8 + 8],
                                                                    scalar1=1.0, scalar2=None, op0=ALU.add), [M], [M])
        AR.top = base_top

        FT = AR.bf(8 * NQ)
        H = AR.f32(8 * NQ)
        free_top = AR.top

        def ln_block(ucf, nt, li, which, out_fn, tmp):
            s1, s2 = bank(6, nt), bank(7, nt)
            for c in range(8):
                uc = ucf(c)
                P.op("pe", OP("matmul", s1.ap, lhsT=ones.ap, rhs=uc.ap, start=(c == 0), stop=(c == 7)),
                     [ones, uc], [s1], sig=(c == 7))
            for c in range(8):
                uc = ucf(c)
                q = tmp["sq"].get().s(0, nt)
                P.op("act", OP("activation", out=q.ap, in_=uc.ap, func=AF.Square), [uc], [q])
                P.op("pe", OP("matmul", s2.ap, lhsT=ones.ap, rhs=q.ap, start=(c == 0), stop=(c == 7)),
                     [ones, q], [s2], sig=True)
            mean, msq, rstd = tmp["mean"].s(0, nt), tmp["msq"].s(0, nt), tmp["rstd"].s(0, nt)
            P.op("act", OP("activation", out=mean.ap, in_=s1.ap, func=AF.Copy, scale=1.0 / D), [s1], [mean])
            P.op("act", OP("activation", out=msq.ap, in_=s1.ap, func=AF.Square, scale=1.0 / D), [s1], [msq])
            P.op("dve", OP("scalar_tensor_tensor", out=rstd.ap, in0=s2.ap, scalar=1.0 / D, in1=msq.ap, op0=ALU.mult, op1=ALU.subtract),
                 [s2, msq], [rstd])
            P.op("act", OP("activation", out=rstd.ap, in_=rstd.ap, func=AF.Sqrt, bias=eps_ln.ap, scale=1.0), [rstd, eps_ln], [rstd])
            P.op("dve", OP("reciprocal", out=rstd.ap, in_=rstd.ap), [rstd], [rstd])
            gi = ((li * 2 + which) * 2 + 0) * 8
            bi = ((li * 2 + which) * 2 + 1) * 8
            for c in range(8):
                uc = ucf(c)
                o = out_fn(c)
                P.op("pool", OP("tensor_tensor", out=uc.ap, in0=uc.ap, in1=mean.ap, op=ALU.subtract), [uc, mean], [uc])
                P.op("dve", OP("tensor_tensor", out=uc.ap, in0=uc.ap, in1=rstd.ap, op=ALU.mult), [uc, rstd], [uc])
                P.op("dve", OP("tensor_scalar", out=o.ap, in0=uc.ap, scalar1=lnT.ap[:, gi + c:gi + c + 1],
                                                                      scalar2=lnT.ap[:, bi + c:bi + c + 1], op0=ALU.mult, op1=ALU.add),
                     [uc, lnT], [o])

        def layer(li):
            ab = (li == 0)
            NT = NQ if ab else NOWN

            def Hc(c, t0, t1):
                return H.s(c * NT + t0, c * NT + t1)

            AR.top = FT.f0 if False else int(FT.f0)
            wq, wqs, wk, wks, wv = AR.bf(8 * 1024), AR.bf(8 * 1024), AR.bf(8 * 768), AR.bf(8 * 768), AR.bf(8 * 640)
            for dst, src, n in ((wq, wq_d[li], 1024), (wqs, wqs_d[li], 1024), (wk, wk_d[li], 768), (wks, wks_d[li], 768), (wv, wv_d[li], 640)):
                P.dma("pool", v3(dst, 8), dchunks(src, 0, n))
            xb = Rot([AR.f32(8 * 512) for _ in range(2)])
            abuf = Rot([AR.bf(8 * 512) for _ in range(2)])
            ctab = Rot([AR.f32(512) for _ in range(2)])
            stab = Rot([AR.f32(512) for _ in range(2)])
            t1r = Rot([AR.f32(512) for _ in range(2)])
            t2r = Rot([AR.f32(512) for _ in range(2)])
            obr = Rot([AR.bf(512) for _ in range(3)])
            sqr = Rot([AR.f32(512) for _ in range(2)])
            rvr = Rot([AR.f32(512) for _ in range(2)])
            vst = Rot([AR.bf(VW) for _ in range(2)])
            for vs in vst.items:
                P.op("pool", OP("memset", vs.ap, 1.0), [], [vs])
            pbank = Rot([0, 1, 2, 3, 4, 5])
            if ab:
                q_rope, q_norm = [True] * 8, [False] * 4 + [True] * 4
                k_rope, k_norm = [True] * 6, [False] * 4 + [True] * 2
            else:
                q_rope, q_norm = [False] * 4 + [True] * 4, [False] * 8
                k_rope, k_norm = [False] * 4 + [True] * 2, [False] * 6

            def fm_proj(aT, nt, W, Ws, wn, ch, rope, norm, gcol, ct, stb, dst):
                pa = bank(pbank.get(), nt)
                for c in range(8):
                    P.op("pe", OP("matmul", pa.ap, lhsT=W.ap[:, c * wn + ch * 128: c * wn + ch * 128 + 128],
                                                      rhs=aT.ap[:, c * nt:c * nt + nt], start=(c == 0), stop=(c == 7)),
                         [W, aT], [pa], sig=(c == 7))
                ob = obr.get().s(0, nt)
                if not rope:
                    P.op("act", OP("activation", out=ob.ap, in_=pa.ap, func=AF.Copy), [pa], [ob])
                else:
                    pb = bank(pbank.get(), nt)
                    for c in range(8):
                        P.op("pe", OP("matmul", pb.ap, lhsT=Ws.ap[:, c * wn + ch * 128: c * wn + ch * 128 + 128],
                                                          rhs=aT.ap[:, c * nt:c * nt + nt], start=(c == 0), stop=(c == 7)),
                             [Ws, aT], [pb], sig=(c == 7))
                    t1, t2 = t1r.get().s(0, nt), t2r.get().s(0, nt)
                    if norm:
                        sq = sqr.get().s(0, nt)
                        P.op("act", OP("activation", out=sq.ap, in_=pa.ap, func=AF.Square), [pa], [sq])
                        pss = bank(pbank.get(), nt)
                        P.op("pe", OP("matmul", pss.ap, lhsT=bones.ap, rhs=sq.ap, start=True, stop=True), [bones, sq], [pss])
                        rv = rvr.get().s(0, nt)
                        P.op("act", OP("activation", out=rv.ap, in_=pss.ap, func=AF.Sqrt, bias=eps_rms.ap, scale=1.0 / 64), [pss, eps_rms], [rv])
                        P.op("dve", OP("reciprocal", out=rv.ap, in_=rv.ap), [rv], [rv])
                        P.op("dve", OP("scalar_tensor_tensor", out=t1.ap, in0=pa.ap, scalar=gt.ap[:, gcol:gcol + 1], in1=ct.ap, op0=ALU.mult, op1=ALU.mult),
                             [pa, gt, ct], [t1])
                        P.op("dve", OP("scalar_tensor_tensor", out=t2.ap, in0=pb.ap, scalar=gt.ap[:, gcol + 1:gcol + 2], in1=stb.ap, op0=ALU.mult, op1=ALU.mult),
                             [pb, gt, stb], [t2])
                        P.op("pool", OP("tensor_tensor", out=t1.ap, in0=t1.ap, in1=t2.ap, op=ALU.add), [t1, t2], [t1])
                        P.op("dve", OP("tensor_tensor", out=ob.ap, in0=t1.ap, in1=rv.ap, op=ALU.mult), [t1, rv], [ob])
                    else:
                        P.op("dve", OP("tensor_tensor", out=t1.ap, in0=pa.ap, in1=ct.ap, op=ALU.mult), [pa, ct], [t1])
                        P.op("dve", OP("tensor_tensor", out=t2.ap, in0=pb.ap, in1=stb.ap, op=ALU.mult), [pb, stb], [t2])
                        P.op("pool", OP("tensor_tensor", out=ob.ap, in0=t1.ap, in1=t2.ap, op=ALU.add), [t1, t2], [ob])
                P.dma("sp", dst, ob)

            def source(src, ntok, M, ctd, std, toff, want_q, q_off, want_kv, k_off):
                for t0 in range(0, ntok, 512):
                    nt = min(512, ntok - t0)
                    x = xb.get().s(0, 8 * nt)
                    P.dma("sp", v3(x, 8), dchunks(src, t0, t0 + nt))
                    aT = abuf.get().s(0, 8 * nt)
                    for c in range(8):
                        P.op("act", OP("activation",
                            out=aT.ap[:, c * nt:c * nt + nt], in_=x.ap[:, c * nt:c * nt + nt], func=AF.Identity,
                            scale=M.ap[:, 8 + c:9 + c], bias=M.ap[:, c:c + 1]), [x, M], [aT])
                    ct, stb = ctab.get().s(0, nt), stab.get().s(0, nt)
                    P.dma("sp", ct, ctd.v(0, 128, toff + t0, toff + t0 + nt))
                    P.dma("sp", stb, std.v(0, 128, toff + t0, toff + t0 + nt))
                    if want_kv:
                        for ch in range(6):
                            fm_proj(aT, nt, wk, wks, 768, ch, k_rope[ch], k_norm[ch], 2, ct, stb,
                                    KT.v(ch * 128, ch * 128 + 128, k_off + t0, k_off + t0 + nt))
                        for tt in range(nt // 128):
                            pv1, pv2 = bank(pbank.get(), 512), bank(pbank.get(), 128)
                            for c in range(8):
                                lh = aT.ap[:, c * nt + tt * 128:c * nt + tt * 128 + 128]
                                P.op("pe", OP("matmul", pv1.ap, lhsT=lh, rhs=wv.ap[:, c * 640:c * 640 + 512], start=(c == 0), stop=(c == 7)),
                                     [aT, wv], [pv1], sig=(c == 7))
                            for c in range(8):
                                lh = aT.ap[:, c * nt + tt * 128:c * nt + tt * 128 + 128]
                                P.op("pe", OP("matmul", pv2.ap, lhsT=lh, rhs=wv.ap[:, c * 640 + 512:c * 640 + 640], start=(c == 0), stop=(c == 7)),
                                     [aT, wv], [pv2], sig=(c == 7))
                            vs = vst.get()
                            if ab:
                                o1 = vs.ap[:, 0:516].rearrange("p (h n) -> p h n", n=129)[:, :, 0:128]
                                i1 = pv1.ap.rearrange("p (h n) -> p h n", n=128)
                                o2 = vs.ap[:, 516:646].rearrange("p (h n) -> p h n", n=65)[:, :, 0:64]
                            else:
                                o1 = vs.ap[:, 0:520].rearrange("p (h n) -> p h n", n=65)[:, :, 0:64]
                                i1 = pv1.ap.rearrange("p (h n) -> p h n", n=64)
                                o2 = vs.ap[:, 520:650].rearrange("p (h n) -> p h n", n=65)[:, :, 0:64]
                            i2 = pv2.ap.rearrange("p (h n) -> p h n", n=64)
                            P.op("act", OP("activation", out=o1, in_=i1, func=AF.Copy), [pv1], [vs])
                            P.op("dve", OP("tensor_copy", out=o2, in_=i2), [pv2], [vs])
                            kt = (k_off + t0) // 128 + tt
                            P.dma("sp", VA.v(0, 128, kt * VW, kt * VW + VW), vs)
                    if want_q:
                        for ch in range(8):
                            fm_proj(aT, nt, wq, wqs, 1024, ch, q_rope[ch], q_norm[ch], 0, ct, stb,
                                    QT.v(ch * 128, ch * 128 + 128, q_off + t0, q_off + t0 + nt))

            if ab:
                source(xT, SEQ, ML[0], cK_d, sK_d, 0, False, 0, True, 0)
                source(cxT, NCTX, MC[0], cK_d, sK_d, SEQ, True, NEXT, True, SEQ)
                source(xeT, NEXT, ML[0], cQ_d, sQ_d, 0, True, 0, False, 0)
                nkt = SEQ // 128 + 2
            else:
                source(h1T, NEXT, ML[1], cQ_d, sQ_d, 0, True, 0, True, 0)
                source(hc1T, NCTX, MC[1], cQ_d, sQ_d, NEXT, False, 0, True, NEXT)
                nkt = NEXT // 128 + 2

            AR.top = int(H.f0)
            O = FT
            qbuf = Rot([AR.bf(NQ) for _ in range(2)])
            kbuf = Rot([AR.bf(nkt * 128) for _ in range(2)])
            vbuf = Rot([AR.bf(nkt * 130) for _ in range(2)])
            ptile = Rot([AR.bf(1024) for _ in range(3)])
            rz = Rot([AR.f32(1) for _ in range(6)])
            od1 = [AR.f32(128) for _ in range(4)]
            t_o2 = Rot([AR.f32(128) for _ in range(2)])
            t_od = Rot([AR.f32(128) for _ in range(2)])
            t_sq = Rot([AR.f32(128) for _ in range(2)])
            ssq = Rot([AR.f32(1) for _ in range(4)])
            stmp = Rot([AR.f32(640) for _ in range(2)])
            if not ab:
                wm = AR.f32(16 * 384)
                P.dma("sp", wm, wmask_d.v())
                nbb = Rot([AR.f32(640) for _ in range(2)])

            def attend512(kt_, qt_, vv, vcw, voff, dv, r0, q0, nq, kts, fin):
                nsub = nq // 128
                accs = [bank(4 + s, dv + 1) for s in range(nsub)]
                pairs = [kts[i:i + 2] for i in range(0, len(kts), 2)]
                npair = len(pairs)

                def stage_a(pi):
                    pr = pairs[pi]
                    base = (pi % 2) * 1024
                    sreg = PS.v(0, 128, base, base + len(pr) * nq)
                    for jj, kt in enumerate(pr):
                        P.op("pe", OP("matmul", sreg.ap[:, jj * nq:jj * nq + nq], lhsT=kt_.ap[r0:r0 + 64, kt * 128:kt * 128 + 128],
                                      rhs=qt_.ap[r0:r0 + 64, q0:q0 + nq], start=True, stop=True), [kt_, qt_], [sreg], sig=(jj == len(pr) - 1))
                    pt = ptile.get().s(0, len(pr) * nq)
                    P.op("act", OP("activation", out=pt.ap, in_=sreg.ap, func=AF.Exp, scale=0.125), [sreg], [pt])
                    return pt

                def stage_b(pi, pt):
                    pr = pairs[pi]
                    for jj, kt in enumerate(pr):
                        for s in range(nsub):
                            first = (pi == 0 and jj == 0)
                            last = (pi == npair - 1 and jj == len(pr) - 1)
                            P.op("pe", OP("matmul", accs[s].ap, lhsT=pt.ap[:, jj * nq + s * 128:jj * nq + s * 128 + 128],
                                          rhs=vv.ap[:, kt * vcw + voff:kt * vcw + voff + dv + 1], start=first, stop=last),
                                 [pt, vv], [accs[s]], sig=(s == nsub - 1 and jj == len(pr) - 1))

                prev = None
                for pi in range(npair):
                    cur = stage_a(pi)
                    if prev is not None:
                        stage_b(pi - 1, prev)
                    prev = cur
                stage_b(npair - 1, prev)
                for s in range(nsub):
                    fin(s, accs[s], q0 // 128 + s)

            for u in range(8):
                kc = u if u < 4 else 4 + (u - 4) // 2
                qt_, kt_, vt_ = qbuf.get(), kbuf.get(), vbuf.get()
                P.dma("sp", qt_, QT.v(u * 128, u * 128 + 128, 0, NQ))
                P.dma("sp", kt_, KT.v(kc * 128, kc * 128 + 128, 0, nkt * 128))
                if ab:
                    vc0, vcw = (u * 129, 129) if u < 4 else (516 + ((u - 4) // 2) * 65, 65)
                else:
                    vc0, vcw = (u * 130, 130) if u < 4 else (520 + ((u - 4) // 2) * 65, 65)
                vv = vt_.s(0, nkt * vcw)
                for g0 in range(0, nkt, 11):
                    src = V(VA.t[:, g0 * VW:(g0 + 11) * VW].rearrange("p (t c) -> p t c", c=VW)[:, :, vc0:vc0 + vcw],
                            VA.tid, 0, 128, g0 * VW * 0.5, (g0 + 11) * VW * 0.5, 0.5)
                    dstv = V(vv.ap[:, g0 * vcw:(g0 + 11) * vcw].rearrange("p (t c) -> p t c", c=vcw), vv.tid, 0, 128,
                             vv.f0 + g0 * vcw * 0.5, vv.f0 + (g0 + 11) * vcw * 0.5, 0.5)
                    P.dma("sp", dstv, src)

                if ab:
                    qblocks = [(q0, 512, list(range(nkt))) for q0 in range(0, NEXT, 512)] + [(NEXT, 256, [nkt - 2, nkt - 1])]
                    for (q0, nq, kts) in qblocks:
                        for c in range(2):
                            if u < 4:
                                def fin(s, a, ot, c=c, u=u):
                                    r = rz.get()
                                    P.op("dve", OP("reciprocal", out=r.ap, in_=a.ap[:, 128:129]), [a], [r])
                                    if c == 0:
                                        d1 = od1[s]
                                        P.op("dve", OP("tensor_scalar", out=d1.ap, in0=a.ap[:, 0:128], scalar1=r.ap[:, 0:1], scalar2=None, op0=ALU.mult), [a, r], [d1])
                                    else:
                                        d1 = od1[s]
                                        o2, od, sq, ss = t_o2.get(), t_od.get(), t_sq.get(), ssq.get()
                                        P.op("dve", OP("tensor_scalar", out=o2.ap, in0=a.ap[:, 0:128], scalar1=r.ap[:, 0:1], scalar2=None, op0=ALU.mult), [a, r], [o2])
                                        P.op("dve", OP("scalar_tensor_tensor", out=od.ap, in0=o2.ap, scalar=nlam.ap[:, 0:1], in1=d1.ap, op0=ALU.mult, op1=ALU.add), [o2, nlam, d1], [od])
                                        P.op("pool", OP("tensor_tensor", out=sq.ap, in0=od.ap, in1=od.ap, op=ALU.mult), [od], [sq])
                                        P.op("dve", OP("tensor_reduce", out=ss.ap, in_=sq.ap, axis=AX.X, op=ALU.add), [sq], [ss])
                                        P.op("act", OP("activation", out=ss.ap, in_=ss.ap, func=AF.Sqrt, bias=eps_rms.ap, scale=1.0 / 128), [ss, eps_rms], [ss])
                                        P.op("dve", OP("reciprocal", out=ss.ap, in_=ss.ap), [ss], [ss])
                                        oo = O.s(ot * 1024 + u * 128, ot * 1024 + u * 128 + 128)
                                        P.op("dve", OP("scalar_tensor_tensor", out=oo.ap, in0=od.ap, scalar=ss.ap[:, 0:1], in1=sg1.ap, op0=ALU.mult, op1=ALU.mult), [od, ss, sg1], [oo])
                                attend512(kt_, qt_, vv, 129, 0, 128, 64 * c, q0, nq, kts, fin)
                            else:
                                def fin(s, a, ot, c=c, u=u):
                                    r = rz.get()
                                    P.op("dve", OP("reciprocal", out=r.ap, in_=a.ap[:, 64:65]), [a], [r])
                                    col = 512 + ((u - 4) * 2 + c) * 64
                                    oo = O.s(ot * 1024 + col, ot * 1024 + col + 64)
                                    P.op("dve", OP("tensor_scalar", out=oo.ap, in0=a.ap[:, 0:64], scalar1=r.ap[:, 0:1], scalar2=None, op0=ALU.mult), [a, r], [oo])
                                attend512(kt_, qt_, vv, 65, 0, 64, 64 * c, q0, nq, kts, fin)
                else:
                    for c in range(2):
                        r0 = 64 * c

                        def l1_a(t, c=c, r0=r0, u=u):
                            if u < 4:
                                h = 2 * u + c
                                kts = list(range(t, t + 5)) + [20, 21]
                                nb = 5
                                bt = nbb.get()
                                P.dma("sp", bt, nab_d.v(0, 128, (t * 8 + h) * 640, (t * 8 + h) * 640 + 640))
                                voff = c * 65
                                col = 512 + h * 64
                            else:
                                h = (u - 4) * 2 + c
                                kts = list(range(t + 1, t + 4)) + [20, 21]
                                nb = 3
                                bt = wm.s(t * 384, t * 384 + 384)
                                voff = 0
                                col = h * 64
                            nk = len(kts)
                            sreg = PS.v(0, 128, (t % 2) * 1024, (t % 2) * 1024 + nk * 128)
                            qs = qt_.ap[r0:r0 + 64, 256 + t * 128:256 + t * 128 + 128]
                            for i, kt in enumerate(kts):
                                P.op("pe", OP("matmul", sreg.ap[:, i * 128:i * 128 + 128], lhsT=kt_.ap[r0:r0 + 64, kt * 128:kt * 128 + 128], rhs=qs, start=True, stop=True),
                                     [kt_, qt_], [sreg], sig=(i == nk - 1))
                            pt = ptile.get().s(0, nk * 128)
                            tm = stmp.get().s(0, nb * 128)
                            P.op("dve", OP("scalar_tensor_tensor", out=tm.ap, in0=sreg.ap[:, 0:nb * 128], scalar=0.125, in1=bt.ap[:, 0:nb * 128], op0=ALU.mult, op1=ALU.add), [sreg, bt], [tm])
                            P.op("act", OP("activation", out=pt.ap[:, 0:nb * 128], in_=tm.ap, func=AF.Exp), [tm], [pt])
                            P.op("act", OP("activation", out=pt.ap[:, nb * 128:nk * 128], in_=sreg.ap[:, nb * 128:nk * 128], func=AF.Exp, scale=0.125), [sreg], [pt])
                            return (t, h, kts, voff, col, pt)

                        def l1_b(stt, u=u):
                            t, h, kts, voff, col, pt = stt
                            nk = len(kts)
                            a = bank(4 + (t % 2), 65)
                            for i, kt in enumerate(kts):
                                P.op("pe", OP("matmul", a.ap, lhsT=pt.ap[:, i * 128:i * 128 + 128], rhs=vv.ap[:, kt * vcw + voff:kt * vcw + voff + 65], start=(i == 0), stop=(i == nk - 1)),
                                     [pt, vv], [a], sig=(i == nk - 1))
                            r = rz.get()
                            if u >= 4:
                                P.op("dve", OP("tensor_scalar", out=r.ap, in0=a.ap[:, 64:65], scalar1=esink.ap[:, h:h + 1], scalar2=None, op0=ALU.add), [a, esink], [r])
                                P.op("dve", OP("reciprocal", out=r.ap, in_=r.ap), [r], [r])
                            else:
                                P.op("dve", OP("reciprocal", out=r.ap, in_=a.ap[:, 64:65]), [a], [r])
                            oo = O.s(t * 1024 + col, t * 1024 + col + 64)
                            P.op("dve", OP("tensor_scalar", out=oo.ap, in0=a.ap[:, 0:64], scalar1=r.ap[:, 0:1], scalar2=None, op0=ALU.mult), [a, r], [oo])

                        prev = None
                        for t in range(16):
                            cur = l1_a(t)
                            if prev is not None:
                                l1_b(prev)
                            prev = cur
                        l1_b(prev)

            AR.top = free_top
            wo = AR.bf(8 * 1024)
            P.dma("pool", v3(wo, 8), dchunks(wo_d[li], 0, 1024))
            oTr = Rot([AR.bf(8 * 512) for _ in range(2)])
            lntmp = {"sq": Rot([AR.f32(512) for _ in range(2)]), "mean": AR.f32(512), "msq": AR.f32(512), "rstd": AR.f32(512)}
            if ab:
                blocks = [(t0, 512, xeT, t0, ML[0]) for t0 in range(0, NEXT, 512)] + [(NEXT, 256, cxT, 0, MC[0])]
            else:
                blocks = [(t0, 512, h1T, 256 + t0, ML[1]) for t0 in range(0, NOWN, 512)]
            pb3 = Rot([0, 1, 2, 3, 4, 5])
            for (t0, nt, src, c0, M) in blocks:
                for c in range(8):
                    P.dma("sp", Hc(c, t0, t0 + nt), src.v(c * 128, c * 128 + 128, c0, c0 + nt))
                oT = oTr.get().s(0, 8 * nt)
                for ch in range(8):
                    b = pb3.get()
                    pst = bank_bf(b, nt)
                    for tt in range(nt // 128):
                        tile = t0 // 128 + tt
                        oin = O.s(tile * 1024 + ch * 128, tile * 1024 + ch * 128 + 128)
                        P.op("pe", OP("transpose", out=pst.ap[:, tt * 128:tt * 128 + 128], in_=oin.ap, identity=identb.ap),
                             [oin, identb], [pst], sig=(tt == nt // 128 - 1))
                    od_ = oT.s(ch * nt, ch * nt + nt)
                    if ch % 2 == 0:
                        P.op("act", OP("activation", out=od_.ap, in_=pst.ap, func=AF.Copy), [pst], [od_])
                    else:
                        P.op("dve", OP("tensor_copy", out=od_.ap, in_=pst.ap), [pst], [od_])
                for dc in range(8):
                    pp = bank(pb3.get(), nt)
                    for ch in range(8):
                        P.op("pe", OP("matmul", pp.ap, lhsT=wo.ap[:, ch * 1024 + dc * 128:ch * 1024 + dc * 128 + 128],
                                                                                    rhs=oT.ap[:, ch * nt:ch * nt + nt], start=(ch == 0), stop=(ch == 7)),
                             [wo, oT], [pp], sig=(ch == 7))
                    hc = Hc(dc, t0, t0 + nt)
                    P.op("act", OP("activation", out=hc.ap, in_=hc.ap, func=AF.Copy, scale=ALPHA), [hc], [hc])
                    P.op("dve", OP("scalar_tensor_tensor", out=hc.ap, in0=pp.ap, scalar=M.ap[:, 16 + dc:17 + dc], in1=hc.ap, op0=ALU.mult, op1=ALU.add),
                         [pp, M, hc], [hc])
                ln_block(lambda c: Hc(c, t0, t0 + nt), nt, li, 0, lambda c: Hc(c, t0, t0 + nt), lntmp)

            AR.top = free_top
            WT = AR.f32(NT, P=32)
            moe_top = AR.top
            ftmp = AR.f32(1024)
            Lg = Rot([AR.f32(36) for _ in range(2)])
            sm = Rot([AR.f32(8) for _ in range(24)])
            em_r = Rot([AR.f32(32) for _ in range(2)])
            wt_r = Rot([AR.f32(32) for _ in range(2)])
            w2t_r = Rot([AR.f32(32) for _ in range(2)])
            pb4 = Rot([0, 1, 2, 3, 4, 5, 6, 7])
            mblocks = [(t0, min(512, NT - t0)) for t0 in range(0, NT, 512)]

            def Mof(t0):
                return (MC[li] if (ab and t0 >= NEXT) else ML[li])

            for (t0, nt) in mblocks:
                M = Mof(t0)
                for tt in range(nt // 128):
                    tk = t0 + tt * 128
                    for c in range(8):
                        hc = Hc(c, tk, tk + 128)
                        fo = ftmp.s(c * 128, c * 128 + 128)
                        P.op("dve", OP("tensor_scalar", out=fo.ap, in0=hc.ap, scalar1=M.ap[:, 32 + c:33 + c], scalar2=M.ap[:, 24 + c:25 + c],
                                                                                 op0=ALU.mult, op1=ALU.add), [hc, M], [fo])
                    pl = bank(pb4.get(), 36)
                    for c in range(8):
                        fo = ftmp.s(c * 128, c * 128 + 128)
                        P.op("pe", OP("matmul", pl.ap, lhsT=fo.ap, rhs=wr[li].ap[:, c * 36:c * 36 + 36], start=(c == 0), stop=(c == 7)),
                             [fo, wr[li]], [pl], sig=(c == 7))
                    L = Lg.get()
                    P.op("dve", OP("tensor_tensor", out=L.ap, in0=pl.ap, in1=brb[li].ap, op=ALU.add), [pl, brb[li]], [L])
                    gmax, ngmax, gexp, gsum, pen, m8, dd, w1, w2 = [sm.get() for _ in range(9)]
                    em, Wt, W2 = em_r.get(), wt_r.get(), w2t_r.get()
                    P.op("dve", OP("tensor_reduce", out=gmax.ap[:, 0:1], in_=L.ap[:, 0:4], axis=AX.X, op=ALU.max), [L], [gmax])
                    P.op("dve", OP("tensor_scalar", out=ngmax.ap[:, 0:1], in0=gmax.ap[:, 0:1], scalar1=-1.0, scalar2=None, op0=ALU.mult), [gmax], [ngmax])
                    P.op("act", OP("activation", out=gexp.ap[:, 0:4], in_=L.ap[:, 0:4], func=AF.Exp, bias=ngmax.ap[:, 0:1], scale=1.0), [L, ngmax], [gexp])
                    P.op("dve", OP("tensor_reduce", out=gsum.ap[:, 0:1], in_=gexp.ap[:, 0:4], axis=AX.X, op=ALU.add), [gexp], [gsum])
                    P.op("dve", OP("reciprocal", out=gsum.ap[:, 0:1], in_=gsum.ap[:, 0:1]), [gsum], [gsum])
                    P.op("dve", OP("tensor_scalar", out=pen.ap[:, 0:4], in0=L.ap[:, 0:4], scalar1=gmax.ap[:, 0:1], scalar2=1e30, op0=ALU.is_equal, op1=ALU.mult), [L, gmax], [pen])
                    P.op("dve", OP("tensor_scalar", out=pen.ap[:, 0:4], in0=pen.ap[:, 0:4], scalar1=-1e30, scalar2=None, op0=ALU.add), [pen], [pen])
                    for g in range(4):
                        P.op("dve", OP("tensor_scalar", out=em.ap[:, 8 * g:8 * g + 8], in0=L.ap[:, 4 + 8 * g:12 + 8 * g], scalar1=pen.ap[:, g:g + 1], scalar2=None, op0=ALU.add),
                             [L, pen], [em])
                    P.op("dve", OP("max", out=m8.ap, in_=em.ap), [em], [m8])
                    P.op("dve", OP("tensor_tensor", out=dd.ap[:, 0:1], in0=m8.ap[:, 1:2], in1=m8.ap[:, 0:1], op=ALU.subtract), [m8], [dd])
                    P.op("act", OP("activation", out=dd.ap[:, 0:1], in_=dd.ap[:, 0:1], func=AF.Exp), [dd], [dd])
                    P.op("dve", OP("tensor_scalar", out=w1.ap[:, 0:1], in0=dd.ap[:, 0:1], scalar1=1.0, scalar2=None, op0=ALU.add), [dd], [w1])
                    P.op("dve", OP("reciprocal", out=w1.ap[:, 0:1], in_=w1.ap[:, 0:1]), [w1], [w1])
                    P.op("dve", OP("tensor_tensor", out=w1.ap[:, 0:1], in0=w1.ap[:, 0:1], in1=gsum.ap[:, 0:1], op=ALU.mult), [w1, gsum], [w1])
                    P.op("dve", OP("tensor_tensor", out=w2.ap[:, 0:1], in0=w1.ap[:, 0:1], in1=dd.ap[:, 0:1], op=ALU.mult), [w1, dd], [w2])
                    P.op("dve", OP("tensor_scalar", out=Wt.ap, in0=em.ap, scalar1=m8.ap[:, 0:1], scalar2=w1.ap[:, 0:1], op0=ALU.is_equal, op1=ALU.mult), [em, m8, w1], [Wt])
                    P.op("dve", OP("tensor_scalar", out=W2.ap, in0=em.ap, scalar1=m8.ap[:, 1:2], scalar2=w2.ap[:, 0:1], op0=ALU.is_equal, op1=ALU.mult), [em, m8, w2], [W2])
                    P.op("dve", OP("tensor_tensor", out=Wt.ap, in0=Wt.ap, in1=W2.ap, op=ALU.add), [Wt, W2], [Wt])
                    ptr = bank(pb4.get(), 128, p=32)
                    P.op("pe", OP("transpose", out=ptr.ap, in_=Wt.ap, identity=ident32.ap), [Wt, ident32], [ptr])
                    wts = WT.s(tk, tk + 128)
                    P.op("act", OP("activation", out=wts.ap, in_=ptr.ap, func=AF.Copy), [ptr], [wts])
                for c in range(8):
                    hc = Hc(c, t0, t0 + nt)
                    fo = FT.s(c * NT + t0, c * NT + t0 + nt)
                    P.op("act", OP("activation", out=fo.ap, in_=hc.ap, func=AF.Identity, scale=M.ap[:, 32 + c:33 + c], bias=M.ap[:, 24 + c:25 + c]), [hc, M], [fo])
                    P.op("pool", OP("tensor_scalar", out=hc.ap, in0=hc.ap, scalar1=ALPHA, scalar2=None, op0=ALU.mult), [hc], [hc])

            AR.top = moe_top
            w1b = Rot([AR.bf(8 * 512) for _ in range(2)])
            w3b = Rot([AR.bf(8 * 512) for _ in range(2)])
            w2b = Rot([AR.bf(4 * 1024) for _ in range(1)])
            gb = Rot([AR.bf(4 * 512) for _ in range(1)])
            wbc = Rot([AR.f32(512) for _ in range(2)])
            sb_ = Rot([AR.f32(512) for _ in range(2)])
            Eb = Rot([AR.f32(128, P=32) for _ in range(2)])
            for ex in range(NEXP):
                w1e, w3e, w2e = w1b.get(), w3b.get(), w2b.get()
                P.dma("pool", v3(w1e, 8), dchunks(w1_d[li], 0, 512, r0=ex * 1024))
                P.dma("pool", v3(w3e, 8), dchunks(w3_d[li], 0, 512, r0=ex * 1024))
                P.dma("pool", v3(w2e, 4), dchunks(w2_d[li], 0, 1024, r0=ex * 512, nchunk=4))
                E = Eb.get()
                P.op("dve", OP("tensor_copy", out=E.ap, in_=ident32.ap[0:32, ex:ex + 1].to_broadcast([32, 128])), [ident32], [E])
                for (t0, nt) in mblocks:
                    M = Mof(t0)
                    pw = bank(pb4.get(), nt)
                    wts = WT.s(t0, t0 + nt)
                    P.op("pe", OP("matmul", pw.ap, lhsT=E.ap, rhs=wts.ap, start=True, stop=True), [E, wts], [pw])
                    wb = wbc.get().s(0, nt)
                    P.op("act", OP("activation", out=wb.ap, in_=pw.ap, func=AF.Copy), [pw], [wb])
                    g = gb.get().s(0, 4 * nt)
                    for ec in range(4):
                        p1, p3 = bank(pb4.get(), nt), bank(pb4.get(), nt)
                        for c in range(8):
                            fr = FT.s(c * NT + t0, c * NT + t0 + nt)
                            P.op("pe", OP("matmul", p1.ap, lhsT=w1e.ap[:, c * 512 + ec * 128:c * 512 + ec * 128 + 128], rhs=fr.ap, start=(c == 0), stop=(c == 7)),
                                 [w1e, fr], [p1], sig=(c == 7))
                        for c in range(8):
                            fr = FT.s(c * NT + t0, c * NT + t0 + nt)
                            P.op("pe", OP("matmul", p3.ap, lhsT=w3e.ap[:, c * 512 + ec * 128:c * 512 + ec * 128 + 128], rhs=fr.ap, start=(c == 0), stop=(c == 7)),
                                 [w3e, fr], [p3], sig=(c == 7))
                        sv = sb_.get().s(0, nt)
                        P.op("act", OP("activation", out=sv.ap, in_=p1.ap, func=AF.Silu), [p1], [sv])
                        P.op("dve", OP("tensor_tensor", out=sv.ap, in0=p3.ap, in1=sv.ap, op=ALU.mult), [p3, sv], [sv])
                        gv = g.s(ec * nt, ec * nt + nt)
                        P.op("pool", OP("tensor_tensor", out=gv.ap, in0=sv.ap, in1=wb.ap, op=ALU.mult), [sv, wb], [gv])
                    for dc in range(8):
                        py = bank(pb4.get(), nt)
                        for ec in range(4):
                            P.op("pe", OP("matmul", py.ap, lhsT=w2e.ap[:, ec * 1024 + dc * 128:ec * 1024 + dc * 128 + 128],
                                                                                          rhs=g.ap[:, ec * nt:ec * nt + nt], start=(ec == 0), stop=(ec == 3)),
                                 [w2e, g], [py], sig=(ec == 3))
                        hc = Hc(dc, t0, t0 + nt)
                        P.op("dve", OP("scalar_tensor_tensor", out=hc.ap, in0=py.ap, scalar=M.ap[:, 40 + dc:41 + dc], in1=hc.ap, op0=ALU.mult, op1=ALU.add),
                             [py, M, hc], [hc])

            AR.top = moe_top
            lntmp = {"sq": Rot([AR.f32(512) for _ in range(2)]), "mean": AR.f32(512), "msq": AR.f32(512), "rstd": AR.f32(512)}
            for (t0, nt) in mblocks:
                ln_block(lambda c: Hc(c, t0, t0 + nt), nt, li, 1, lambda c: Hc(c, t0, t0 + nt), lntmp)
                for c in range(8):
                    if ab:
                        if t0 < NEXT:
                            dst = h1T.v(c * 128, c * 128 + 128, t0, t0 + nt)
                        else:
                            dst = hc1T.v(c * 128, c * 128 + 128, 0, nt)
                    else:
                        dst = outT.v(c * 128, c * 128 + 128, t0, t0 + nt)
                    P.dma("sp", dst, Hc(c, t0, t0 + nt))

        layer(0)
        layer(1)
        P.finish()
        P.emit()
    return nc


def _swap_cols(w):
    n = w.shape[1]
    idx = np.arange(n).reshape(n // 64, 2, 32)[:, ::-1, :].reshape(n)
    return w[:, idx]


def _rope_tables(pos):
    pos = np.asarray(pos)
    valid = pos >= 0
    p = np.where(valid, pos, 0)
    row = (p // 64).astype(np.float32)
    col = (p % 64).astype(np.float32)
    inv = (np.float32(10000.0) ** (-np.arange(16, dtype=np.float32) / np.float32(16))).astype(np.float32)
    ang = np.concatenate([row[:, None] * inv, col[:, None] * inv], -1).astype(np.float32)
    cos = np.cos(ang).astype(np.float32)
    sin = np.sin(ang).astype(np.float32)
    cos = np.where(valid[:, None], cos, np.float32(1.0))
    sin = np.where(valid[:, None], sin, np.float32(0.0))
    c64 = np.concatenate([cos, cos], 1)
    s64 = np.concatenate([-sin, sin], 1)
    cT = np.concatenate([c64, c64], 1).T
    sT = np.concatenate([s64, s64], 1).T
    return np.ascontiguousarray(cT, dtype=np.float32), np.ascontiguousarray(sT, dtype=np.float32)


def _ext_positions(j):
    pos = 2048 * j - 256 + np.arange(NEXT)
    if j == 0:
        pos[0:256] = 256 + np.arange(256)
    if j == 3:
        pos[2304:2560] = 7680 + np.arange(256)
    return pos


def _first_occurrence(kp):
    seen, out = set(), np.zeros(len(kp), dtype=bool)
    for i, p in enumerate(kp):
        if p not in seen:
            seen.add(p)
            out[i] = True
    return out


def _layer1_tables(j, rpb):
    pos = _ext_positions(j)
    wmask = np.full((128, 16, 3, 128), NEG, dtype=np.float32)
    nab = np.full((128, 16, 8, 5, 128), NEG, dtype=np.float32)
    for t in range(16):
        qp = 2048 * j + 128 * t + np.arange(128)
        kp = pos[128 * (t + 1):128 * (t + 4)]
        first = _first_occurrence(kp)
        ok = (np.abs(qp[None, :] - kp[:, None]) <= 128) & first[:, None]
        m = np.where(ok, np.float32(0.0), np.float32(NEG)).reshape(3, 128, 128)
        wmask[:, t] = m.transpose(1, 0, 2)
        kp = pos[128 * t:128 * (t + 5)]
        first = _first_occurrence(kp)
        qr, qc = qp // 64, qp % 64
        kr, kcl = kp // 64, kp % 64
        r0 = np.clip(qr - 4, 0, 120)
        c0 = np.clip(qc - 8, 0, 48)
        ok = ((kr[:, None] >= r0[None, :]) & (kr[:, None] < r0[None, :] + 8) &
              (kcl[:, None] >= c0[None, :]) & (kcl[:, None] < c0[None, :] + 16) & first[:, None])
        ro = np.clip(kr[:, None] - qr[None, :] + 7, 0, 14)
        co = np.clip(kcl[:, None] - qc[None, :] + 15, 0, 30)
        for h in range(8):
            b = np.where(ok, rpb[h][ro, co], np.float32(NEG)).astype(np.float32).reshape(5, 128, 128)
            nab[:, t, h] = b.transpose(1, 0, 2)
    return wmask.reshape(128, 16 * 384), nab.reshape(128, 16 * 8 * 640)


_NC_CACHE = {}


def kernel(x, c, ctx, c_ctx, mod_w, mod_b, ln_g, ln_b, ab_w_in, ab_w_out, diff_lambda, diff_subln_g, gqa_qk_g,
           cd_w_in, cd_w_out, win_sink, na_rpb, moe_w_group, moe_b_group, moe_w_router, moe_b_router,
           moe_w1, moe_w3, moe_w2):
    f = lambda a: np.ascontiguousarray(np.asarray(a), dtype=np.float32)
    x, c, ctx, c_ctx = f(x), f(c), f(ctx), f(c_ctx)
    mod_w, mod_b, ln_g, ln_b = f(mod_w), f(mod_b), f(ln_g), f(ln_b)
    ab_w_in, ab_w_out, cd_w_in, cd_w_out = f(ab_w_in)[0], f(ab_w_out)[0], f(cd_w_in)[0], f(cd_w_out)[0]
    diff_lambda, diff_subln_g, gqa_qk_g = f(diff_lambda)[0], f(diff_subln_g)[0], f(gqa_qk_g)[0]
    win_sink, na_rpb = f(win_sink)[0], f(na_rpb)[0]
    moe_w1, moe_w3, moe_w2 = f(moe_w1), f(moe_w3), f(moe_w2)

    shared = {}
    shared["ident"] = np.eye(128, dtype=np.float32)
    bo = np.zeros((128, 128), dtype=np.float32)
    bo[:64, :64] = 1.0
    bo[64:, 64:] = 1.0
    shared["bones"] = bo
    for i in range(2):
        shared[f"modw{i}"] = mod_w[i]
        shared[f"modb{i}"] = np.ascontiguousarray(mod_b[i].reshape(48, 128).T)
        shared[f"wr{i}"] = np.ascontiguousarray(np.concatenate([f(moe_w_group)[i], f(moe_w_router)[i]], 1))
        shared[f"br{i}"] = np.concatenate([f(moe_b_group)[i], f(moe_b_router)[i]])[None, :].copy()
        shared[f"w1_{i}"] = moe_w1[i].reshape(NEXP * D, 512)
        shared[f"w3_{i}"] = moe_w3[i].reshape(NEXP * D, 512)
        shared[f"w2_{i}"] = moe_w2[i].reshape(NEXP * 512, D)
    lnT = np.zeros((128, 64), dtype=np.float32)
    for i in range(2):
        for wch in range(2):
            lnT[:, ((i * 2 + wch) * 2 + 0) * 8:((i * 2 + wch) * 2 + 0) * 8 + 8] = ln_g[i, wch].reshape(8, 128).T
            lnT[:, ((i * 2 + wch) * 2 + 1) * 8:((i * 2 + wch) * 2 + 1) * 8 + 8] = ln_b[i, wch].reshape(8, 128).T
    shared["lnT"] = lnT
    q0 = ab_w_in[:, 0:1024]
    kd, vd = ab_w_in[:, 1024:1536], ab_w_in[:, 1536:2048]
    kg, vg = ab_w_in[:, 2048:2176], ab_w_in[:, 2176:2304]
    k0 = np.concatenate([kd, kg[:, 0:64], kg[:, 0:64], kg[:, 64:128], kg[:, 64:128]], 1)
    shared["wq0"], shared["wqs0"] = np.ascontiguousarray(q0), np.ascontiguousarray(_swap_cols(q0))
    shared["wk0"], shared["wks0"] = np.ascontiguousarray(k0), np.ascontiguousarray(_swap_cols(k0))
    shared["wv0"] = np.ascontiguousarray(np.concatenate([vd, vg], 1))
    shared["wo0"] = ab_w_out
    qw, qn = cd_w_in[:, 0:512], cd_w_in[:, 512:1024]
    kw, vw = cd_w_in[:, 1024:1152], cd_w_in[:, 1152:1280]
    kn, vn = cd_w_in[:, 1280:1792], cd_w_in[:, 1792:2304]
    q1 = np.concatenate([qn, qw], 1)
    k1 = np.concatenate([kn, kw[:, 0:64], kw[:, 0:64], kw[:, 64:128], kw[:, 64:128]], 1)
    shared["wq1"], shared["wqs1"] = np.ascontiguousarray(q1), np.ascontiguousarray(_swap_cols(q1))
    shared["wk1"], shared["wks1"] = np.ascontiguousarray(k1), np.ascontiguousarray(_swap_cols(k1))
    shared["wv1"] = np.ascontiguousarray(np.concatenate([vn, vw], 1))
    shared["wo1"] = cd_w_out
    sw = lambda g: np.concatenate([g[32:64], g[0:32]])
    gq, gk = gqa_qk_g[0], gqa_qk_g[1]
    shared["gt"] = np.ascontiguousarray(np.stack([np.tile(gq, 2), np.tile(sw(gq), 2), np.tile(gk, 2), np.tile(sw(gk), 2)], 1))
    shared["dlam"] = diff_lambda.reshape(1, 256).copy()
    shared["subg"] = diff_subln_g.reshape(1, 128).copy()
    shared["sink"] = win_sink.reshape(1, 8).copy()
    ck, sk = _rope_tables(np.concatenate([np.arange(SEQ), -np.ones(NCTX, dtype=np.int64)]))
    shared["cK"], shared["sK"] = ck, sk

    in_maps = []
    tabs = {}
    for core in range(8):
        b, j = core // 4, core % 4
        pos = _ext_positions(j)
        m = dict(shared)
        m["xT"] = np.ascontiguousarray(x[b].T)
        m["xeT"] = np.ascontiguousarray(x[b][pos].T)
        m["cxT"] = np.ascontiguousarray(ctx[b].T)
        m["ccT"] = np.ascontiguousarray(np.stack([c[b], c_ctx], 1))
        if j not in tabs:
            cq, sq = _rope_tables(np.concatenate([pos, -np.ones(NCTX, dtype=np.int64)]))
            wmask, nab = _layer1_tables(j, na_rpb)
            tabs[j] = (cq, sq, wmask, nab)
        m["cQ"], m["sQ"], m["wmask"], m["nab"] = tabs[j]
        in_maps.append(m)

    if "nc" not in _NC_CACHE:
        _NC_CACHE["nc"] = build_program()
    res = run_bass_kernel_spmd(_NC_CACHE["nc"], in_maps, core_ids=list(range(8)))
    out = np.empty((2, SEQ, D), dtype=np.float32)
    for core in range(8):
        b, j = core // 4, core % 4
        out[b, 2048 * j:2048 * (j + 1), :] = np.asarray(res.results[core]["outT"]).T
    if DEBUG:
        kernel.last = res
    return out
```

```python
import math
from contextlib import ExitStack

import numpy as np
import concourse.bass as bass
import concourse.mybir as mybir
from concourse.bass_utils import run_bass_kernel_spmd

F32 = mybir.dt.float32
BF16 = mybir.dt.bfloat16
AF = mybir.ActivationFunctionType
ALU = mybir.AluOpType
AX = mybir.AxisListType

DEBUG = False

D = 1024
SEQ = 8192
NCTX = 256
NEXT = 2560
NOWN = 2048
NQ = NEXT + NCTX
NK0 = SEQ + NCTX
NK1 = NEXT + NCTX
VW = 650
DEPTH = 2
ALPHA = (2.0 * DEPTH) ** 0.25
LN_EPS = 1e-5
RMS_EPS = 1e-6
LAM_INIT0 = 0.8 - 0.6 * math.exp(0.0)
NEG = -30000.0
NEXP = 32

SEM_LIM = 8000
NDMA = 12
ARENA_WORDS = 53200


class V:
    def __init__(self, ap, tid, p0, p1, f0, f1, wpe):
        self.ap, self.tid, self.p0, self.p1, self.f0, self.f1, self.wpe = ap, tid, p0, p1, f0, f1, wpe

    def reg(self):
        return (self.tid, self.p0, self.p1, self.f0, self.f1)

    def s(self, c0, c1, p0=None, p1=None):
        q0 = 0 if p0 is None else p0
        q1 = (self.p1 - self.p0) if p1 is None else p1
        return V(self.ap[q0:q1, c0:c1], self.tid, self.p0 + q0, self.p0 + q1,
                 self.f0 + c0 * self.wpe, self.f0 + c1 * self.wpe, self.wpe)


class Buf:
    def __init__(self, t, tid, P, F, wpe):
        self.t, self.tid, self.P, self.F, self.wpe = t, tid, P, F, wpe

    def v(self, p0=0, p1=None, f0=0, f1=None):
        p1 = self.P if p1 is None else p1
        f1 = self.F if f1 is None else f1
        return V(self.t[p0:p1, f0:f1], self.tid, p0, p1, f0 * self.wpe, f1 * self.wpe, self.wpe)


class Prog:
    ENGS = ["pe", "act", "dve", "pool", "sp"]

    def __init__(self, nc, stack):
        self.nc, self.stack = nc, stack
        self.recs = {e: [] for e in self.ENGS}
        self.sig = {e: 0 for e in self.ENGS}
        self.known = {e: {} for e in self.ENGS}
        self.csems = {e: [] for e in self.ENGS}
        self.dsems, self.dcnt, self.drr = {}, {}, {}
        for q in ["sp", "act", "pool"]:
            self.dsems[q] = [stack.enter_context(nc.semaphore(f"d_{q}_{k}")) for k in range(NDMA)]
            self.dcnt[q] = [0] * NDMA
            self.drr[q] = 0
        self.ent = {}
        self.ntid = 0

    def new_tid(self):
        self.ntid += 1
        return self.ntid

    def dram(self, name, P, F, dtype, kind):
        t = self.nc.dram_tensor(name, [P, F], dtype, kind=kind)
        return Buf(t, self.new_tid(), P, F, 1.0 if dtype == F32 else 0.5)

    def _csem(self, e, idx):
        while len(self.csems[e]) <= idx:
            self.csems[e].append(self.stack.enter_context(self.nc.semaphore(f"c_{e}_{len(self.csems[e])}")))
        return self.csems[e][idx]

    @staticmethod
    def _ov(a, b):
        return a[1] < b[2] and b[1] < a[2] and a[3] < b[4] and b[3] < a[4]

    @staticmethod
    def _cov(a, b):
        return a[1] <= b[1] and a[2] >= b[2] and a[3] <= b[3] and a[4] >= b[4]

    BUCK = 256

    def _cands(self, r):
        d = self.ent.get(r[0])
        if not d:
            return []
        seen, out = set(), []
        for b in range(int(r[3]) // self.BUCK, int(math.ceil(r[4])) // self.BUCK + 1):
            for en in d.get(b, ()):
                if en[3] and id(en) not in seen:
                    seen.add(id(en))
                    out.append(en)
        return out

    def _add(self, en):
        r = en[0]
        d = self.ent.setdefault(r[0], {})
        for b in range(int(r[3]) // self.BUCK, int(math.ceil(r[4])) // self.BUCK + 1):
            lst = d.setdefault(b, [])
            if len(lst) > 64:
                lst[:] = [x for x in lst if x[3]]
            lst.append(en)

    def _deps(self, reads, writes):
        raw, other = [], []
        for r in reads:
            for en in self._cands(r):
                if en[1] is not None and self._ov(en[0], r):
                    raw.append(en[1])
        for w in writes:
            for en in self._cands(w):
                if self._ov(en[0], w):
                    if en[1] is not None:
                        other.append(en[1])
                    other.extend(en[2])
        return raw, other

    def _update(self, tok, reads, writes):
        for r in reads:
            hit = False
            for en in self._cands(r):
                if self._ov(en[0], r):
                    if tok[0] == "c":
                        en[2][:] = [t for t in en[2] if not (t[0] == "c" and t[1] == tok[1])]
                    en[2].append(tok)
                    if self._cov(en[0], r):
                        hit = True
            if not hit:
                self._add([r, None, [tok], True])
        for w in writes:
            for en in self._cands(w):
                if self._cov(w, en[0]):
                    en[3] = False
            self._add([w, tok, [], True])

    def _waits(self, e, raw, other):
        waits = []
        kn = self.known[e]
        for kind, toks in (("raw", raw), ("oth", other)):
            for t in toks:
                if t[0] == "c":
                    _, f, v = t
                    if f == e and (kind == "oth" or e == "pe"):
                        continue
                    if kn.get(f, 0) >= v:
                        continue
                    kn[f] = v
                    waits.append(t)
                else:
                    _, q, k, c = t
                    if kn.get((q, k), 0) >= c:
                        continue
                    kn[(q, k)] = c
                    waits.append(t)
        return waits

    def op(self, e, fn, reads=(), writes=(), sig=True):
        reads = [r.reg() for r in reads]
        writes = [w.reg() for w in writes]
        raw, other = self._deps(reads, writes)
        waits = self._waits(e, raw, other)
        if sig:
            self.sig[e] += 1
            v = self.sig[e]
        else:
            v = self.sig[e] + 1
        tok = ("c", e, v)
        self._update(tok, reads, writes)
        self.recs[e].append((waits, fn, tok if sig else None))
        return tok

    def dma(self, q, out, in_, **kw):
        reads, writes = [in_.reg()], [out.reg()]
        raw, other = self._deps(reads, writes)
        k = self.drr[q]
        self.drr[q] = (k + 1) % NDMA
        prev = self.dcnt[q][k]
        if prev > 0:
            other = other + [("d", q, k, prev)]
        waits = self._waits(q, raw, other)
        self.dcnt[q][k] = prev + 1
        tok = ("d", q, k, prev + 1)
        self._update(tok, reads, writes)
        oa, ia = out.ap, in_.ap

        def fn(eng, oa=oa, ia=ia, kw=kw):
            return eng.dma_start(out=oa, in_=ia, **kw)

        self.recs[q].append((waits, fn, tok))
        return tok

    def finish(self):
        waits = []
        for q in self.dsems:
            for k in range(NDMA):
                if self.dcnt[q][k] > 0:
                    waits.append(("d", q, k, self.dcnt[q][k]))
        for e in ["pe", "act", "dve", "pool"]:
            if self.sig[e] > 0:
                waits.append(("c", e, self.sig[e]))
        self.recs["sp"].append((waits, None, None))

    def _emit_wait(self, eng, w):
        if w[0] == "c":
            _, f, v = w
            eng.wait_ge(self._csem(f, (v - 1) // SEM_LIM), (v - 1) % SEM_LIM + 1)
        else:
            _, q, k, c = w
            eng.wait_ge(self.dsems[q][k], 16 * c)

    def emit(self):
        nc = self.nc
        for e in self.ENGS:
            for idx in range((self.sig[e] + SEM_LIM - 1) // SEM_LIM + 1):
                self._csem(e, idx)
        recs, me = self.recs, self

        def play(e, eng):
            for waits, fn, tok in recs[e]:
                for w in waits:
                    me._emit_wait(eng, w)
                if fn is None:
                    continue
                ins = fn(eng)
                if tok is not None:
                    if tok[0] == "c":
                        ins.then_inc(me._csem(e, (tok[2] - 1) // SEM_LIM), 1)
                    else:
                        ins.then_inc(me.dsems[tok[1]][tok[2]], 16)

        with nc.Block() as block:
            @block.tensor
            def _(eng):
                play("pe", eng)

            @block.scalar
            def _(eng):
                play("act", eng)

            @block.vector
            def _(eng):
                play("dve", eng)

            @block.gpsimd
            def _(eng):
                play("pool", eng)

            @block.sync
            def _(eng):
                play("sp", eng)


class Arena:
    def __init__(self, buf):
        self.buf, self.top, self.hi = buf, 0, 0

    def _alloc(self, words):
        off = self.top
        self.top += (words + 7) // 8 * 8
        self.hi = max(self.hi, self.top)
        assert self.top <= self.buf.F, f"arena overflow {self.top}"
        return off

    def f32(self, n, P=128):
        off = self._alloc(n)
        return V(self.buf.t[0:P, off:off + n], self.buf.tid, 0, P, off, off + n, 1.0)

    def bf(self, n, P=128):
        w = (n + 1) // 2
        off = self._alloc(w)
        return V(self.buf.t[0:P, off:off + w].bitcast(BF16), self.buf.tid, 0, P, off, off + w, 0.5)


class Rot:
    def __init__(self, items):
        self.items, self.i = items, 0

    def get(self):
        it = self.items[self.i % len(self.items)]
        self.i += 1
        return it


def r3(ap, k):
    return ap.rearrange("p (k n) -> p k n", k=k)


def OP(name, *a, **k):
    return lambda e: getattr(e, name)(*a, **k)


def build_program():
    nc = bass.Bass("TRN2", target_bir_lowering=False)
    st = ExitStack()
    with st:
        P = Prog(nc, st)
        I = lambda name, p, f, dt=F32: P.dram(name, p, f, dt, "ExternalInput")
        xT = I("xT", D, SEQ)
        xeT = I("xeT", D, NEXT)
        cxT = I("cxT", D, NCTX)
        ccT = I("ccT", D, 2)
        ident_d = I("ident", 128, 128)
        bones_d = I("bones", 128, 128)
        modw = [I(f"modw{i}", D, 6 * D) for i in range(2)]
        modb = [I(f"modb{i}", 128, 48) for i in range(2)]
        lnT_d = I("lnT", 128, 64)
        wq_d = [I(f"wq{i}", D, 1024) for i in range(2)]
        wqs_d = [I(f"wqs{i}", D, 1024) for i in range(2)]
        wk_d = [I(f"wk{i}", D, 768) for i in range(2)]
        wks_d = [I(f"wks{i}", D, 768) for i in range(2)]
        wv_d = [I(f"wv{i}", D, 640) for i in range(2)]
        wo_d = [I(f"wo{i}", D, 1024) for i in range(2)]
        gt_d = I("gt", 128, 4)
        dlam_d = I("dlam", 1, 256)
        subg_d = I("subg", 1, 128)
        sink_d = I("sink", 1, 8)
        cK_d = I("cK", 128, NK0)
        sK_d = I("sK", 128, NK0)
        cQ_d = I("cQ", 128, NQ)
        sQ_d = I("sQ", 128, NQ)
        wr_d = [I(f"wr{i}", D, 36) for i in range(2)]
        br_d = [I(f"br{i}", 1, 36) for i in range(2)]
        w1_d = [I(f"w1_{i}", NEXP * D, 512) for i in range(2)]
        w3_d = [I(f"w3_{i}", NEXP * D, 512) for i in range(2)]
        w2_d = [I(f"w2_{i}", NEXP * 512, D) for i in range(2)]
        wmask_d = I("wmask", 128, 16 * 384)
        nab_d = I("nab", 128, 16 * 8 * 640)
        outT = P.dram("outT", D, NOWN, F32, "ExternalOutput")
        skind = "ExternalOutput" if DEBUG else "Internal"
        QT = P.dram("QT", 1024, NQ, BF16, skind)
        KT = P.dram("KT", 768, NK0, BF16, skind)
        VA = P.dram("VA", 128, 66 * VW, BF16, skind)
        h1T = P.dram("h1T", D, NEXT, F32, skind)
        hc1T = P.dram("hc1T", D, NCTX, F32, skind)

        arena_t = st.enter_context(nc.sbuf_tensor("arena", [128, ARENA_WORDS], F32))
        AR = Arena(Buf(arena_t, P.new_tid(), 128, ARENA_WORDS, 1.0))
        psum_t = st.enter_context(nc.psum_tensor("psum", [128, 4096], F32))
        PS = Buf(psum_t, P.new_tid(), 128, 4096, 1.0)

        def bank(b, n=512, p=128):
            return PS.v(0, p, b * 512, b * 512 + n)

        def bank_bf(b, n):
            return V(PS.t[:, b * 512:b * 512 + n // 2].bitcast(BF16), PS.tid, 0, 128, b * 512, b * 512 + n // 2, 0.5)

        def dchunks(buf, c0, c1, r0=0, nchunk=8):
            ap = buf.t[r0:r0 + nchunk * 128, c0:c1].rearrange("(c p) n -> p c n", p=128)
            return V(ap, buf.tid, r0, r0 + nchunk * 128, c0 * buf.wpe, c1 * buf.wpe, buf.wpe)

        def v3(v, k):
            return V(r3(v.ap, k), v.tid, v.p0, v.p1, v.f0, v.f1, v.wpe)

        def bcast(buf, n):
            return V(buf.t[0:1, 0:n].partition_broadcast(128), buf.tid, 0, 1, 0, n * buf.wpe, buf.wpe)

        ident32 = AR.f32(128)
        identb = AR.bf(128)
        ones = AR.f32(128)
        bones = AR.f32(128)
        lnT = AR.f32(64)
        gt = AR.f32(4)
        dl = AR.f32(256)
        sg1 = AR.f32(128)
        esink = AR.f32(8)
        eps_ln = AR.f32(1)
        eps_rms = AR.f32(1)
        nlam = AR.f32(1)
        ML = [AR.f32(48) for _ in range(2)]
        MC = [AR.f32(48) for _ in range(2)]
        sc = AR.f32(16)
        wr = [AR.f32(8 * 36) for _ in range(2)]
        brb = [AR.f32(36) for _ in range(2)]
        P.dma("sp", ident32, ident_d.v())
        P.dma("sp", bones, bones_d.v())
        P.dma("sp", lnT, lnT_d.v())
        P.dma("sp", gt, gt_d.v())
        P.dma("sp", dl, bcast(dlam_d, 256))
        P.dma("sp", sg1, bcast(subg_d, 128))
        P.dma("sp", esink, bcast(sink_d, 8))
        for i in range(2):
            P.dma("sp", v3(wr[i], 8), dchunks(wr_d[i], 0, 36))
            P.dma("sp", brb[i], bcast(br_d[i], 36))
        P.op("dve", OP("tensor_copy", out=identb.ap, in_=ident32.ap), [ident32], [identb])
        P.op("dve", OP("memset", ones.ap, 1.0), [], [ones])
        P.op("dve", OP("memset", eps_ln.ap, LN_EPS), [], [eps_ln])
        P.op("dve", OP("memset", eps_rms.ap, RMS_EPS), [], [eps_rms])
        P.op("act", OP("activation", out=esink.ap, in_=esink.ap, func=AF.Exp), [esink], [esink])
        P.op("dve", OP("tensor_scalar", out=sg1.ap, in0=sg1.ap, scalar1=1.0 - LAM_INIT0, scalar2=None, op0=ALU.mult), [sg1], [sg1])
        lt = AR.f32(128)
        ls = AR.f32(2)
        dl4 = dl.ap.rearrange("p (a b n) -> p a b n", a=2, b=2)
        P.op("dve", OP("tensor_tensor", out=lt.ap.rearrange("p (a n) -> p a n", a=2), in0=dl4[:, :, 0, :], in1=dl4[:, :, 1, :], op=ALU.mult), [dl], [lt])
        P.op("dve", OP("tensor_reduce", out=ls.ap, in_=lt.ap.rearrange("p (a n) -> p a n", a=2), axis=AX.X, op=ALU.add), [lt], [ls])
        P.op("act", OP("activation", out=ls.ap, in_=ls.ap, func=AF.Exp), [ls], [ls])
        P.op("dve", OP("tensor_tensor", out=nlam.ap, in0=ls.ap[:, 1:2], in1=ls.ap[:, 0:1], op=ALU.subtract), [ls], [nlam])
        P.op("dve", OP("tensor_scalar", out=nlam.ap, in0=nlam.ap, scalar1=-LAM_INIT0, scalar2=None, op0=ALU.add), [nlam], [nlam])

        base_top = AR.top
        P.dma("sp", v3(sc, 8), dchunks(ccT, 0, 2))
        P.op("act", OP("activation", out=sc.ap, in_=sc.ap, func=AF.Silu), [sc], [sc])
        mwb = [AR.f32(8 * 512) for _ in range(2)]
        mbt = AR.f32(48)
        for i in range(2):
            P.dma("sp", mbt, modb[i].v())
            pm = bank(6, 96)
            for pc in range(12):
                w = mwb[pc % 2]
                P.dma("sp", v3(w, 8), dchunks(modw[i], pc * 512, pc * 512 + 512))
                for j in range(4):
                    cc = pc * 4 + j
                    for dc in range(8):
                        P.op("pe", OP("matmul",
                            pm.ap[:, cc * 2:cc * 2 + 2], lhsT=w.ap[:, dc * 512 + j * 128: dc * 512 + j * 128 + 128],
                            rhs=sc.ap[:, dc * 2:dc * 2 + 2], start=(dc == 0), stop=(dc == 7)),
                            [w, sc], [pm], sig=(dc == 7))
            pm3 = pm.ap.rearrange("p (c n) -> p c n", n=2)
            P.op("dve", OP("tensor_tensor", out=ML[i].ap, in0=pm3[:, :, 0], in1=mbt.ap, op=ALU.add), [pm, mbt], [ML[i]])
            P.op("dve", OP("tensor_tensor", out=MC[i].ap, in0=pm3[:, :, 1], in1=mbt.ap, op=ALU.add), [pm, mbt], [MC[i]])
            for M in (ML[i], MC[i]):
                for k in (1, 4):
                    P.op("dve", OP("tensor_scalar", out=M.ap[:, k * 8:k * 8 + 8], in0=M.ap[:, k * 8:k * 8 + 8],
                                                                    scalar1=1.0, scalar2=None, op0=ALU.add), [M], [M])
        AR.top = base_top

        FT = AR.bf(8 * NQ)
        H = AR.f32(8 * NQ)
        free_top = AR.top

        def ln_block(ucf, nt, li, which, out_fn, tmp):
            s1, s2 = bank(6, nt), bank(7, nt)
            for c in range(8):
                uc = ucf(c)
                P.op("pe", OP("matmul", s1.ap, lhsT=ones.ap, rhs=uc.ap, start=(c == 0), stop=(c == 7)),
                     [ones, uc], [s1], sig=(c == 7))
            for c in range(8):
                uc = ucf(c)
                q = tmp["sq"].get().s(0, nt)
                P.op("act", OP("activation", out=q.ap, in_=uc.ap, func=AF.Square), [uc], [q])
                P.op("pe", OP("matmul", s2.ap, lhsT=ones.ap, rhs=q.ap, start=(c == 0), stop=(c == 7)),
                     [ones, q], [s2], sig=True)
            mean, msq, rstd = tmp["mean"].s(0, nt), tmp["msq"].s(0, nt), tmp["rstd"].s(0, nt)
            P.op("act", OP("activation", out=mean.ap, in_=s1.ap, func=AF.Copy, scale=1.0 / D), [s1], [mean])
            P.op("act", OP("activation", out=msq.ap, in_=s1.ap, func=AF.Square, scale=1.0 / D), [s1], [msq])
            P.op("dve", OP("scalar_tensor_tensor", out=rstd.ap, in0=s2.ap, scalar=1.0 / D, in1=msq.ap, op0=ALU.mult, op1=ALU.subtract),
                 [s2, msq], [rstd])
            P.op("act", OP("activation", out=rstd.ap, in_=rstd.ap, func=AF.Sqrt, bias=eps_ln.ap, scale=1.0), [rstd, eps_ln], [rstd])
            P.op("dve", OP("reciprocal", out=rstd.ap, in_=rstd.ap), [rstd], [rstd])
            gi = ((li * 2 + which) * 2 + 0) * 8
            bi = ((li * 2 + which) * 2 + 1) * 8
            for c in range(8):
                uc = ucf(c)
                o = out_fn(c)
                P.op("pool", OP("tensor_tensor", out=uc.ap, in0=uc.ap, in1=mean.ap, op=ALU.subtract), [uc, mean], [uc])
                P.op("dve", OP("tensor_tensor", out=uc.ap, in0=uc.ap, in1=rstd.ap, op=ALU.mult), [uc, rstd], [uc])
                P.op("dve", OP("tensor_scalar", out=o.ap, in0=uc.ap, scalar1=lnT.ap[:, gi + c:gi + c + 1],
                                                                      scalar2=lnT.ap[:, bi + c:bi + c + 1], op0=ALU.mult, op1=ALU.add),
                     [uc, lnT], [o])

        def layer(li):
            ab = (li == 0)
            NT = NQ if ab else NOWN

            def Hc(c, t0, t1):
                return H.s(c * NT + t0, c * NT + t1)

            AR.top = FT.f0 if False else int(FT.f0)
            wq, wqs, wk, wks, wv = AR.bf(8 * 1024), AR.bf(8 * 1024), AR.bf(8 * 768), AR.bf(8 * 768), AR.bf(8 * 640)
            for dst, src, n in ((wq, wq_d[li], 1024), (wqs, wqs_d[li], 1024), (wk, wk_d[li], 768), (wks, wks_d[li], 768), (wv, wv_d[li], 640)):
                P.dma("pool", v3(dst, 8), dchunks(src, 0, n))
            xb = Rot([AR.f32(8 * 512) for _ in range(2)])
            abuf = Rot([AR.bf(8 * 512) for _ in range(2)])
            ctab = Rot([AR.f32(512) for _ in range(2)])
            stab = Rot([AR.f32(512) for _ in range(2)])
            t1r = Rot([AR.f32(512) for _ in range(2)])
            t2r = Rot([AR.f32(512) for _ in range(2)])
            obr = Rot([AR.bf(512) for _ in range(3)])
            sqr = Rot([AR.f32(512) for _ in range(2)])
            rvr = Rot([AR.f32(512) for _ in range(2)])
            vst = Rot([AR.bf(VW) for _ in range(2)])
            for vs in vst.items:
                P.op("pool", OP("memset", vs.ap, 1.0), [], [vs])
            pbank = Rot([0, 1, 2, 3, 4, 5])
            if ab:
                q_rope, q_norm = [True] * 8, [False] * 4 + [True] * 4
                k_rope, k_norm = [True] * 6, [False] * 4 + [True] * 2
            else:
                q_rope, q_norm = [False] * 4 + [True] * 4, [False] * 8
                k_rope, k_norm = [False] * 4 + [True] * 2, [False] * 6

            def fm_proj(aT, nt, W, Ws, wn, ch, rope, norm, gcol, ct, stb, dst):
                pa = bank(pbank.get(), nt)
                for c in range(8):
                    P.op("pe", OP("matmul", pa.ap, lhsT=W.ap[:, c * wn + ch * 128: c * wn + ch * 128 + 128],
                                                      rhs=aT.ap[:, c * nt:c * nt + nt], start=(c == 0), stop=(c == 7)),
                         [W, aT], [pa], sig=(c == 7))
                ob = obr.get().s(0, nt)
                if not rope:
                    P.op("act", OP("activation", out=ob.ap, in_=pa.ap, func=AF.Copy), [pa], [ob])
                else:
                    pb = bank(pbank.get(), nt)
                    for c in range(8):
                        P.op("pe", OP("matmul", pb.ap, lhsT=Ws.ap[:, c * wn + ch * 128: c * wn + ch * 128 + 128],
                                                          rhs=aT.ap[:, c * nt:c * nt + nt], start=(c == 0), stop=(c == 7)),
                             [Ws, aT], [pb], sig=(c == 7))
                    t1, t2 = t1r.get().s(0, nt), t2r.get().s(0, nt)
                    if norm:
                        sq = sqr.get().s(0, nt)
                        P.op("act", OP("activation", out=sq.ap, in_=pa.ap, func=AF.Square), [pa], [sq])
                        pss = bank(pbank.get(), nt)
                        P.op("pe", OP("matmul", pss.ap, lhsT=bones.ap, rhs=sq.ap, start=True, stop=True), [bones, sq], [pss])
                        rv = rvr.get().s(0, nt)
                        P.op("act", OP("activation", out=rv.ap, in_=pss.ap, func=AF.Sqrt, bias=eps_rms.ap, scale=1.0 / 64), [pss, eps_rms], [rv])
                        P.op("dve", OP("reciprocal", out=rv.ap, in_=rv.ap), [rv], [rv])
                        P.op("dve", OP("scalar_tensor_tensor", out=t1.ap, in0=pa.ap, scalar=gt.ap[:, gcol:gcol + 1], in1=ct.ap, op0=ALU.mult, op1=ALU.mult),
                             [pa, gt, ct], [t1])
                        P.op("dve", OP("scalar_tensor_tensor", out=t2.ap, in0=pb.ap, scalar=gt.ap[:, gcol + 1:gcol + 2], in1=stb.ap, op0=ALU.mult, op1=ALU.mult),
                             [pb, gt, stb], [t2])
                        P.op("pool", OP("tensor_tensor", out=t1.ap, in0=t1.ap, in1=t2.ap, op=ALU.add), [t1, t2], [t1])
                        P.op("dve", OP("tensor_tensor", out=ob.ap, in0=t1.ap, in1=rv.ap, op=ALU.mult), [t1, rv], [ob])
                    else:
                        P.op("dve", OP("tensor_tensor", out=t1.ap, in0=pa.ap, in1=ct.ap, op=ALU.mult), [pa, ct], [t1])
                        P.op("dve", OP("tensor_tensor", out=t2.ap, in0=pb.ap, in1=stb.ap, op=ALU.mult), [pb, stb], [t2])
                        P.op("pool", OP("tensor_tensor", out=ob.ap, in0=t1.ap, in1=t2.ap, op=ALU.add), [t1, t2], [ob])
                P.dma("sp", dst, ob)

            def source(src, ntok, M, ctd, std, toff, want_q, q_off, want_kv, k_off):
                for t0 in range(0, ntok, 512):
                    nt = min(512, ntok - t0)
                    x = xb.get().s(0, 8 * nt)
                    P.dma("sp", v3(x, 8), dchunks(src, t0, t0 + nt))
                    aT = abuf.get().s(0, 8 * nt)
                    for c in range(8):
                        P.op("act", OP("activation",
                            out=aT.ap[:, c * nt:c * nt + nt], in_=x.ap[:, c * nt:c * nt + nt], func=AF.Identity,
                            scale=M.ap[:, 8 + c:9 + c], bias=M.ap[:, c:c + 1]), [x, M], [aT])
                    ct, stb = ctab.get().s(0, nt), stab.get().s(0, nt)
                    P.dma("sp", ct, ctd.v(0, 128, toff + t0, toff + t0 + nt))
                    P.dma("sp", stb, std.v(0, 128, toff + t0, toff + t0 + nt))
                    if want_kv:
                        for ch in range(6):
                            fm_proj(aT, nt, wk, wks, 768, ch, k_rope[ch], k_norm[ch], 2, ct, stb,
                                    KT.v(ch * 128, ch * 128 + 128, k_off + t0, k_off + t0 + nt))
                        for tt in range(nt // 128):
                            pv1, pv2 = bank(pbank.get(), 512), bank(pbank.get(), 128)
                            for c in range(8):
                                lh = aT.ap[:, c * nt + tt * 128:c * nt + tt * 128 + 128]
                                P.op("pe", OP("matmul", pv1.ap, lhsT=lh, rhs=wv.ap[:, c * 640:c * 640 + 512], start=(c == 0), stop=(c == 7)),
                                     [aT, wv], [pv1], sig=(c == 7))
                            for c in range(8):
                                lh = aT.ap[:, c * nt + tt * 128:c * nt + tt * 128 + 128]
                                P.op("pe", OP("matmul", pv2.ap, lhsT=lh, rhs=wv.ap[:, c * 640 + 512:c * 640 + 640], start=(c == 0), stop=(c == 7)),
                                     [aT, wv], [pv2], sig=(c == 7))
                            vs = vst.get()
                            if ab:
                                o1 = vs.ap[:, 0:516].rearrange("p (h n) -> p h n", n=129)[:, :, 0:128]
                                i1 = pv1.ap.rearrange("p (h n) -> p h n", n=128)
                                o2 = vs.ap[:, 516:646].rearrange("p (h n) -> p h n", n=65)[:, :, 0:64]
                            else:
                                o1 = vs.ap[:, 0:520].rearrange("p (h n) -> p h n", n=65)[:, :, 0:64]
                                i1 = pv1.ap.rearrange("p (h n) -> p h n", n=64)
                                o2 = vs.ap[:, 520:650].rearrange("p (h n) -> p h n", n=65)[:, :, 0:64]
                            i2 = pv2.ap.rearrange("p (h n) -> p h n", n=64)
                            P.op("act", OP("activation", out=o1, in_=i1, func=AF.Copy), [pv1], [vs])
                            P.op("dve", OP("tensor_copy", out=o2, in_=i2), [pv2], [vs])
                            kt = (k_off + t0) // 128 + tt
                            P.dma("sp", VA.v(0, 128, kt * VW, kt * VW + VW), vs)
                    if want_q:
                        for ch in range(8):
                            fm_proj(aT, nt, wq, wqs, 1024, ch, q_rope[ch], q_norm[ch], 0, ct, stb,
                                    QT.v(ch * 128, ch * 128 + 128, q_off + t0, q_off + t0 + nt))

            if ab:
                source(xT, SEQ, ML[0], cK_d, sK_d, 0, False, 0, True, 0)
                source(cxT, NCTX, MC[0], cK_d, sK_d, SEQ, True, NEXT, True, SEQ)
                source(xeT, NEXT, ML[0], cQ_d, sQ_d, 0, True, 0, False, 0)
                nkt = SEQ // 128 + 2
            else:
                source(h1T, NEXT, ML[1], cQ_d, sQ_d, 0, True, 0, True, 0)
                source(hc1T, NCTX, MC[1], cQ_d, sQ_d, NEXT, False, 0, True, NEXT)
                nkt = NEXT // 128 + 2

            AR.top = int(H.f0)
            O = FT
            qbuf = Rot([AR.bf(NQ) for _ in range(2)])
            kbuf = Rot([AR.bf(nkt * 128) for _ in range(2)])
            vbuf = Rot([AR.bf(nkt * 130) for _ in range(2)])
            ptile = Rot([AR.bf(1024) for _ in range(3)])
            rz = Rot([AR.f32(1) for _ in range(6)])
            od1 = [AR.f32(128) for _ in range(4)]
            t_o2 = Rot([AR.f32(128) for _ in range(2)])
            t_od = Rot([AR.f32(128) for _ in range(2)])
            t_sq = Rot([AR.f32(128) for _ in range(2)])
            ssq = Rot([AR.f32(1) for _ in range(4)])
            stmp = Rot([AR.f32(640) for _ in range(2)])
            if not ab:
                wm = AR.f32(16 * 384)
                P.dma("sp", wm, wmask_d.v())
                nbb = Rot([AR.f32(640) for _ in range(2)])

            def attend512(kt_, qt_, vv, vcw, voff, dv, r0, q0, nq, kts, fin):
                nsub = nq // 128
                accs = [bank(4 + s, dv + 1) for s in range(nsub)]
                pairs = [kts[i:i + 2] for i in range(0, len(kts), 2)]
                npair = len(pairs)

                def stage_a(pi):
                    pr = pairs[pi]
                    base = (pi % 2) * 1024
                    sreg = PS.v(0, 128, base, base + len(pr) * nq)
                    for jj, kt in enumerate(pr):
                        P.op("pe", OP("matmul", sreg.ap[:, jj * nq:jj * nq + nq], lhsT=kt_.ap[r0:r0 + 64, kt * 128:kt * 128 + 128],
                                      rhs=qt_.ap[r0:r0 + 64, q0:q0 + nq], start=True, stop=True), [kt_, qt_], [sreg], sig=(jj == len(pr) - 1))
                    pt = ptile.get().s(0, len(pr) * nq)
                    P.op("act", OP("activation", out=pt.ap, in_=sreg.ap, func=AF.Exp, scale=0.125), [sreg], [pt])
                    return pt

                def stage_b(pi, pt):
                    pr = pairs[pi]
                    for jj, kt in enumerate(pr):
                        for s in range(nsub):
                            first = (pi == 0 and jj == 0)
                            last = (pi == npair - 1 and jj == len(pr) - 1)
                            P.op("pe", OP("matmul", accs[s].ap, lhsT=pt.ap[:, jj * nq + s * 128:jj * nq + s * 128 + 128],
                                          rhs=vv.ap[:, kt * vcw + voff:kt * vcw + voff + dv + 1], start=first, stop=last),
                                 [pt, vv], [accs[s]], sig=(s == nsub - 1 and jj == len(pr) - 1))

                prev = None
                for pi in range(npair):
                    cur = stage_a(pi)
                    if prev is not None:
                        stage_b(pi - 1, prev)
                    prev = cur
                stage_b(npair - 1, prev)
                for s in range(nsub):
                    fin(s, accs[s], q0 // 128 + s)

            for u in range(8):
                kc = u if u < 4 else 4 + (u - 4) // 2
                qt_, kt_, vt_ = qbuf.get(), kbuf.get(), vbuf.get()
                P.dma("sp", qt_, QT.v(u * 128, u * 128 + 128, 0, NQ))
                P.dma("sp", kt_, KT.v(kc * 128, kc * 128 + 128, 0, nkt * 128))
                if ab:
                    vc0, vcw = (u * 129, 129) if u < 4 else (516 + ((u - 4) // 2) * 65, 65)
                else:
                    vc0, vcw = (u * 130, 130) if u < 4 else (520 + ((u - 4) // 2) * 65, 65)
                vv = vt_.s(0, nkt * vcw)
                for g0 in range(0, nkt, 11):
                    src = V(VA.t[:, g0 * VW:(g0 + 11) * VW].rearrange("p (t c) -> p t c", c=VW)[:, :, vc0:vc0 + vcw],
                            VA.tid, 0, 128, g0 * VW * 0.5, (g0 + 11) * VW * 0.5, 0.5)
                    dstv = V(vv.ap[:, g0 * vcw:(g0 + 11) * vcw].rearrange("p (t c) -> p t c", c=vcw), vv.tid, 0, 128,
                             vv.f0 + g0 * vcw * 0.5, vv.f0 + (g0 + 11) * vcw * 0.5, 0.5)
                    P.dma("sp", dstv, src)

                if ab:
                    qblocks = [(q0, 512, list(range(nkt))) for q0 in range(0, NEXT, 512)] + [(NEXT, 256, [nkt - 2, nkt - 1])]
                    for (q0, nq, kts) in qblocks:
                        for c in range(2):
                            if u < 4:
                                def fin(s, a, ot, c=c, u=u):
                                    r = rz.get()
                                    P.op("dve", OP("reciprocal", out=r.ap, in_=a.ap[:, 128:129]), [a], [r])
                                    if c == 0:
                                        d1 = od1[s]
                                        P.op("dve", OP("tensor_scalar", out=d1.ap, in0=a.ap[:, 0:128], scalar1=r.ap[:, 0:1], scalar2=None, op0=ALU.mult), [a, r], [d1])
                                    else:
                                        d1 = od1[s]
                                        o2, od, sq, ss = t_o2.get(), t_od.get(), t_sq.get(), ssq.get()
                                        P.op("dve", OP("tensor_scalar", out=o2.ap, in0=a.ap[:, 0:128], scalar1=r.ap[:, 0:1], scalar2=None, op0=ALU.mult), [a, r], [o2])
                                        P.op("dve", OP("scalar_tensor_tensor", out=od.ap, in0=o2.ap, scalar=nlam.ap[:, 0:1], in1=d1.ap, op0=ALU.mult, op1=ALU.add), [o2, nlam, d1], [od])
                                        P.op("pool", OP("tensor_tensor", out=sq.ap, in0=od.ap, in1=od.ap, op=ALU.mult), [od], [sq])
                                        P.op("dve", OP("tensor_reduce", out=ss.ap, in_=sq.ap, axis=AX.X, op=ALU.add), [sq], [ss])
                                        P.op("act", OP("activation", out=ss.ap, in_=ss.ap, func=AF.Sqrt, bias=eps_rms.ap, scale=1.0 / 128), [ss, eps_rms], [ss])
                                        P.op("dve", OP("reciprocal", out=ss.ap, in_=ss.ap), [ss], [ss])
                                        oo = O.s(ot * 1024 + u * 128, ot * 1024 + u * 128 + 128)
                                        P.op("dve", OP("scalar_tensor_tensor", out=oo.ap, in0=od.ap, scalar=ss.ap[:, 0:1], in1=sg1.ap, op0=ALU.mult, op1=ALU.mult), [od, ss, sg1], [oo])
                                attend512(kt_, qt_, vv, 129, 0, 128, 64 * c, q0, nq, kts, fin)
                            else:
                                def fin(s, a, ot, c=c, u=u):
                                    r = rz.get()
                                    P.op("dve", OP("reciprocal", out=r.ap, in_=a.ap[:, 64:65]), [a], [r])
                                    col = 512 + ((u - 4) * 2 + c) * 64
                                    oo = O.s(ot * 1024 + col, ot * 1024 + col + 64)
                                    P.op("dve", OP("tensor_scalar", out=oo.ap, in0=a.ap[:, 0:64], scalar1=r.ap[:, 0:1], scalar2=None, op0=ALU.mult), [a, r], [oo])
                                attend512(kt_, qt_, vv, 65, 0, 64, 64 * c, q0, nq, kts, fin)
                else:
                    for c in range(2):
                        r0 = 64 * c

                        def l1_a(t, c=c, r0=r0, u=u):
                            if u < 4:
                                h = 2 * u + c
                                kts = list(range(t, t + 5)) + [20, 21]
                                nb = 5
                                bt = nbb.get()
                                P.dma("sp", bt, nab_d.v(0, 128, (t * 8 + h) * 640, (t * 8 + h) * 640 + 640))
                                voff = c * 65
                                col = 512 + h * 64
                            else:
                                h = (u - 4) * 2 + c
                                kts = list(range(t + 1, t + 4)) + [20, 21]
                                nb = 3
                                bt = wm.s(t * 384, t * 384 + 384)
                                voff = 0
                                col = h * 64
                            nk = len(kts)
                            sreg = PS.v(0, 128, (t % 2) * 1024, (t % 2) * 1024 + nk * 128)
                            qs = qt_.ap[r0:r0 + 64, 256 + t * 128:256 + t * 128 + 128]
                            for i, kt in enumerate(kts):
                                P.op("pe", OP("matmul", sreg.ap[:, i * 128:i * 128 + 128], lhsT=kt_.ap[r0:r0 + 64, kt * 128:kt * 128 + 128], rhs=qs, start=True, stop=True),
                                     [kt_, qt_], [sreg], sig=(i == nk - 1))
                            pt = ptile.get().s(0, nk * 128)
                            tm = stmp.get().s(0, nb * 128)
                            P.op("dve", OP("scalar_tensor_tensor", out=tm.ap, in0=sreg.ap[:, 0:nb * 128], scalar=0.125, in1=bt.ap[:, 0:nb * 128], op0=ALU.mult, op1=ALU.add), [sreg, bt], [tm])
                            P.op("act", OP("activation", out=pt.ap[:, 0:nb * 128], in_=tm.ap, func=AF.Exp), [tm], [pt])
                            P.op("act", OP("activation", out=pt.ap[:, nb * 128:nk * 128], in_=sreg.ap[:, nb * 128:nk * 128], func=AF.Exp, scale=0.125), [sreg], [pt])
                            return (t, h, kts, voff, col, pt)

                        def l1_b(stt, u=u):
                            t, h, kts, voff, col, pt = stt
                            nk = len(kts)
                            a = bank(4 + (t % 2), 65)
                            for i, kt in enumerate(kts):
                                P.op("pe", OP("matmul", a.ap, lhsT=pt.ap[:, i * 128:i * 128 + 128], rhs=vv.ap[:, kt * vcw + voff:kt * vcw + voff + 65], start=(i == 0), stop=(i == nk - 1)),
                                     [pt, vv], [a], sig=(i == nk - 1))
                            r = rz.get()
                            if u >= 4:
                                P.op("dve", OP("tensor_scalar", out=r.ap, in0=a.ap[:, 64:65], scalar1=esink.ap[:, h:h + 1], scalar2=None, op0=ALU.add), [a, esink], [r])
                                P.op("dve", OP("reciprocal", out=r.ap, in_=r.ap), [r], [r])
                            else:
                                P.op("dve", OP("reciprocal", out=r.ap, in_=a.ap[:, 64:65]), [a], [r])
                            oo = O.s(t * 1024 + col, t * 1024 + col + 64)
                            P.op("dve", OP("tensor_scalar", out=oo.ap, in0=a.ap[:, 0:64], scalar1=r.ap[:, 0:1], scalar2=None, op0=ALU.mult), [a, r], [oo])

                        prev = None
                        for t in range(16):
                            cur = l1_a(t)
                            if prev is not None:
                                l1_b(prev)
                            prev = cur
                        l1_b(prev)

            AR.top = free_top
            wo = AR.bf(8 * 1024)
            P.dma("pool", v3(wo, 8), dchunks(wo_d[li], 0, 1024))
            oTr = Rot([AR.bf(8 * 512) for _ in range(2)])
            lntmp = {"sq": Rot([AR.f32(512) for _ in range(2)]), "mean": AR.f32(512), "msq": AR.f32(512), "rstd": AR.f32(512)}
            if ab:
                blocks = [(t0, 512, xeT, t0, ML[0]) for t0 in range(0, NEXT, 512)] + [(NEXT, 256, cxT, 0, MC[0])]
            else:
                blocks = [(t0, 512, h1T, 256 + t0, ML[1]) for t0 in range(0, NOWN, 512)]
            pb3 = Rot([0, 1, 2, 3, 4, 5])
            for (t0, nt, src, c0, M) in blocks:
                for c in range(8):
                    P.dma("sp", Hc(c, t0, t0 + nt), src.v(c * 128, c * 128 + 128, c0, c0 + nt))
                oT = oTr.get().s(0, 8 * nt)
                for ch in range(8):
                    b = pb3.get()
                    pst = bank_bf(b, nt)
                    for tt in range(nt // 128):
                        tile = t0 // 128 + tt
                        oin = O.s(tile * 1024 + ch * 128, tile * 1024 + ch * 128 + 128)
                        P.op("pe", OP("transpose", out=pst.ap[:, tt * 128:tt * 128 + 128], in_=oin.ap, identity=identb.ap),
                             [oin, identb], [pst], sig=(tt == nt // 128 - 1))
                    od_ = oT.s(ch * nt, ch * nt + nt)
                    if ch % 2 == 0:
                        P.op("act", OP("activation", out=od_.ap, in_=pst.ap, func=AF.Copy), [pst], [od_])
                    else:
                        P.op("dve", OP("tensor_copy", out=od_.ap, in_=pst.ap), [pst], [od_])
                for dc in range(8):
                    pp = bank(pb3.get(), nt)
                    for ch in range(8):
                        P.op("pe", OP("matmul", pp.ap, lhsT=wo.ap[:, ch * 1024 + dc * 128:ch * 1024 + dc * 128 + 128],
                                                                                    rhs=oT.ap[:, ch * nt:ch * nt + nt], start=(ch == 0), stop=(ch == 7)),
                             [wo, oT], [pp], sig=(ch == 7))
                    hc = Hc(dc, t0, t0 + nt)
                    P.op("act", OP("activation", out=hc.ap, in_=hc.ap, func=AF.Copy, scale=ALPHA), [hc], [hc])
                    P.op("dve", OP("scalar_tensor_tensor", out=hc.ap, in0=pp.ap, scalar=M.ap[:, 16 + dc:17 + dc], in1=hc.ap, op0=ALU.mult, op1=ALU.add),
                         [pp, M, hc], [hc])
                ln_block(lambda c: Hc(c, t0, t0 + nt), nt, li, 0, lambda c: Hc(c, t0, t0 + nt), lntmp)

            AR.top = free_top
            WT = AR.f32(NT, P=32)
            moe_top = AR.top
            ftmp = AR.f32(1024)
            Lg = Rot([AR.f32(36) for _ in range(2)])
            sm = Rot([AR.f32(8) for _ in range(24)])
            em_r = Rot([AR.f32(32) for _ in range(2)])
            wt_r = Rot([AR.f32(32) for _ in range(2)])
            w2t_r = Rot([AR.f32(32) for _ in range(2)])
            pb4 = Rot([0, 1, 2, 3, 4, 5, 6, 7])
            mblocks = [(t0, min(512, NT - t0)) for t0 in range(0, NT, 512)]

            def Mof(t0):
                return (MC[li] if (ab and t0 >= NEXT) else ML[li])

            for (t0, nt) in mblocks:
                M = Mof(t0)
                for tt in range(nt // 128):
                    tk = t0 + tt * 128
                    for c in range(8):
                        hc = Hc(c, tk, tk + 128)
                        fo = ftmp.s(c * 128, c * 128 + 128)
                        P.op("dve", OP("tensor_scalar", out=fo.ap, in0=hc.ap, scalar1=M.ap[:, 32 + c:33 + c], scalar2=M.ap[:, 24 + c:25 + c],
                                                                                 op0=ALU.mult, op1=ALU.add), [hc, M], [fo])
                    pl = bank(pb4.get(), 36)
                    for c in range(8):
                        fo = ftmp.s(c * 128, c * 128 + 128)
                        P.op("pe", OP("matmul", pl.ap, lhsT=fo.ap, rhs=wr[li].ap[:, c * 36:c * 36 + 36], start=(c == 0), stop=(c == 7)),
                             [fo, wr[li]], [pl], sig=(c == 7))
                    L = Lg.get()
                    P.op("dve", OP("tensor_tensor", out=L.ap, in0=pl.ap, in1=brb[li].ap, op=ALU.add), [pl, brb[li]], [L])
                    gmax, ngmax, gexp, gsum, pen, m8, dd, w1, w2 = [sm.get() for _ in range(9)]
                    em, Wt, W2 = em_r.get(), wt_r.get(), w2t_r.get()
                    P.op("dve", OP("tensor_reduce", out=gmax.ap[:, 0:1], in_=L.ap[:, 0:4], axis=AX.X, op=ALU.max), [L], [gmax])
                    P.op("dve", OP("tensor_scalar", out=ngmax.ap[:, 0:1], in0=gmax.ap[:, 0:1], scalar1=-1.0, scalar2=None, op0=ALU.mult), [gmax], [ngmax])
                    P.op("act", OP("activation", out=gexp.ap[:, 0:4], in_=L.ap[:, 0:4], func=AF.Exp, bias=ngmax.ap[:, 0:1], scale=1.0), [L, ngmax], [gexp])
                    P.op("dve", OP("tensor_reduce", out=gsum.ap[:, 0:1], in_=gexp.ap[:, 0:4], axis=AX.X, op=ALU.add), [gexp], [gsum])
                    P.op("dve", OP("reciprocal", out=gsum.ap[:, 0:1], in_=gsum.ap[:, 0:1]), [gsum], [gsum])
                    P.op("dve", OP("tensor_scalar", out=pen.ap[:, 0:4], in0=L.ap[:, 0:4], scalar1=gmax.ap[:, 0:1], scalar2=1e30, op0=ALU.is_equal, op1=ALU.mult), [L, gmax], [pen])
                    P.op("dve", OP("tensor_scalar", out=pen.ap[:, 0:4], in0=pen.ap[:, 0:4], scalar1=-1e30, scalar2=None, op0=ALU.add), [pen], [pen])
                    for g in range(4):
                        P.op("dve", OP("tensor_scalar", out=em.ap[:, 8 * g:8 * g + 8], in0=L.ap[:, 4 + 8 * g:12 + 8 * g], scalar1=pen.ap[:, g:g + 1], scalar2=None, op0=ALU.add),
                             [L, pen], [em])
                    P.op("dve", OP("max", out=m8.ap, in_=em.ap), [em], [m8])
                    P.op("dve", OP("tensor_tensor", out=dd.ap[:, 0:1], in0=m8.ap[:, 1:2], in1=m8.ap[:, 0:1], op=ALU.subtract), [m8], [dd])
                    P.op("act", OP("activation", out=dd.ap[:, 0:1], in_=dd.ap[:, 0:1], func=AF.Exp), [dd], [dd])
                    P.op("dve", OP("tensor_scalar", out=w1.ap[:, 0:1], in0=dd.ap[:, 0:1], scalar1=1.0, scalar2=None, op0=ALU.add), [dd], [w1])
                    P.op("dve", OP("reciprocal", out=w1.ap[:, 0:1], in_=w1.ap[:, 0:1]), [w1], [w1])
                    P.op("dve", OP("tensor_tensor", out=w1.ap[:, 0:1], in0=w1.ap[:, 0:1], in1=gsum.ap[:, 0:1], op=ALU.mult), [w1, gsum], [w1])
                    P.op("dve", OP("tensor_tensor", out=w2.ap[:, 0:1], in0=w1.ap[:, 0:1], in1=dd.ap[:, 0:1], op=ALU.mult), [w1, dd], [w2])
                    P.op("dve", OP("tensor_scalar", out=Wt.ap, in0=em.ap, scalar1=m8.ap[:, 0:1], scalar2=w1.ap[:, 0:1], op0=ALU.is_equal, op1=ALU.mult), [em, m8, w1], [Wt])
                    P.op("dve", OP("tensor_scalar", out=W2.ap, in0=em.ap, scalar1=m8.ap[:, 1:2], scalar2=w2.ap[:, 0:1], op0=ALU.is_equal, op1=ALU.mult), [em, m8, w2], [W2])
                    P.op("dve", OP("tensor_tensor", out=Wt.ap, in0=Wt.ap, in1=W2.ap, op=ALU.add), [Wt, W2], [Wt])
                    ptr = bank(pb4.get(), 128, p=32)
                    P.op("pe", OP("transpose", out=ptr.ap, in_=Wt.ap, identity=ident32.ap), [Wt, ident32], [ptr])
                    wts = WT.s(tk, tk + 128)
                    P.op("act", OP("activation", out=wts.ap, in_=ptr.ap, func=AF.Copy), [ptr], [wts])
                for c in range(8):
                    hc = Hc(c, t0, t0 + nt)
                    fo = FT.s(c * NT + t0, c * NT + t0 + nt)
                    P.op("act", OP("activation", out=fo.ap, in_=hc.ap, func=AF.Identity, scale=M.ap[:, 32 + c:33 + c], bias=M.ap[:, 24 + c:25 + c]), [hc, M], [fo])
                    P.op("pool", OP("tensor_scalar", out=hc.ap, in0=hc.ap, scalar1=ALPHA, scalar2=None, op0=ALU.mult), [hc], [hc])

            AR.top = moe_top
            w1b = Rot([AR.bf(8 * 512) for _ in range(2)])
            w3b = Rot([AR.bf(8 * 512) for _ in range(2)])
            w2b = Rot([AR.bf(4 * 1024) for _ in range(1)])
            gb = Rot([AR.bf(4 * 512) for _ in range(2)])
            wbc = Rot([AR.f32(512) for _ in range(2)])
            sb_ = Rot([AR.f32(512) for _ in range(2)])
            Eb = Rot([AR.f32(128, P=32) for _ in range(2)])
            def moe_h(ex, w1e, w3e, E, t0, nt):
                pw = bank(pb4.get(), nt)
                wts = WT.s(t0, t0 + nt)
                P.op("pe", OP("matmul", pw.ap, lhsT=E.ap, rhs=wts.ap, start=True, stop=True), [E, wts], [pw])
                wb = wbc.get().s(0, nt)
                P.op("act", OP("activation", out=wb.ap, in_=pw.ap, func=AF.Copy), [pw], [wb])
                g = gb.get().s(0, 4 * nt)
                for ec in range(4):
                    p1, p3 = bank(pb4.get(), nt), bank(pb4.get(), nt)
                    for c in range(8):
                        fr = FT.s(c * NT + t0, c * NT + t0 + nt)
                        P.op("pe", OP("matmul", p1.ap, lhsT=w1e.ap[:, c * 512 + ec * 128:c * 512 + ec * 128 + 128], rhs=fr.ap, start=(c == 0), stop=(c == 7)),
                             [w1e, fr], [p1], sig=(c == 7))
                    for c in range(8):
                        fr = FT.s(c * NT + t0, c * NT + t0 + nt)
                        P.op("pe", OP("matmul", p3.ap, lhsT=w3e.ap[:, c * 512 + ec * 128:c * 512 + ec * 128 + 128], rhs=fr.ap, start=(c == 0), stop=(c == 7)),
                             [w3e, fr], [p3], sig=(c == 7))
                    sv = sb_.get().s(0, nt)
                    P.op("act", OP("activation", out=sv.ap, in_=p1.ap, func=AF.Silu), [p1], [sv])
                    P.op("dve", OP("tensor_tensor", out=sv.ap, in0=p3.ap, in1=sv.ap, op=ALU.mult), [p3, sv], [sv])
                    gv = g.s(ec * nt, ec * nt + nt)
                    P.op("pool", OP("tensor_tensor", out=gv.ap, in0=sv.ap, in1=wb.ap, op=ALU.mult), [sv, wb], [gv])
                return g

            def moe_y(w2e, g, t0, nt):
                M = Mof(t0)
                for dc in range(8):
                    py = bank(pb4.get(), nt)
                    for ec in range(4):
                        P.op("pe", OP("matmul", py.ap, lhsT=w2e.ap[:, ec * 1024 + dc * 128:ec * 1024 + dc * 128 + 128],
                                      rhs=g.ap[:, ec * nt:ec * nt + nt], start=(ec == 0), stop=(ec == 3)),
                             [w2e, g], [py], sig=(ec == 3))
                    hc = Hc(dc, t0, t0 + nt)
                    P.op("dve", OP("scalar_tensor_tensor", out=hc.ap, in0=py.ap, scalar=M.ap[:, 40 + dc:41 + dc], in1=hc.ap, op0=ALU.mult, op1=ALU.add),
                         [py, M, hc], [hc])

            pend = None
            for ex in range(NEXP):
                w1e, w3e = w1b.get(), w3b.get()
                P.dma("pool", v3(w1e, 8), dchunks(w1_d[li], 0, 512, r0=ex * 1024))
                P.dma("pool", v3(w3e, 8), dchunks(w3_d[li], 0, 512, r0=ex * 1024))
                E = Eb.get()
                P.op("dve", OP("tensor_copy", out=E.ap, in_=ident32.ap[0:32, ex:ex + 1].to_broadcast([32, 128])), [ident32], [E])
                w2e = None
                for (t0, nt) in mblocks:
                    g = moe_h(ex, w1e, w3e, E, t0, nt)
                    if pend is not None:
                        moe_y(*pend)
                    if w2e is None:
                        w2e = w2b.get()
                        P.dma("pool", v3(w2e, 4), dchunks(w2_d[li], 0, 1024, r0=ex * 512, nchunk=4))
                    pend = (w2e, g, t0, nt)
            moe_y(*pend)

            AR.top = moe_top
            lntmp = {"sq": Rot([AR.f32(512) for _ in range(2)]), "mean": AR.f32(512), "msq": AR.f32(512), "rstd": AR.f32(512)}
            for (t0, nt) in mblocks:
                ln_block(lambda c: Hc(c, t0, t0 + nt), nt, li, 1, lambda c: Hc(c, t0, t0 + nt), lntmp)
                for c in range(8):
                    if ab:
                        if t0 < NEXT:
                            dst = h1T.v(c * 128, c * 128 + 128, t0, t0 + nt)
                        else:
                            dst = hc1T.v(c * 128, c * 128 + 128, 0, nt)
                    else:
                        dst = outT.v(c * 128, c * 128 + 128, t0, t0 + nt)
                    P.dma("sp", dst, Hc(c, t0, t0 + nt))

        layer(0)
        layer(1)
        P.finish()
        P.emit()
    return nc


def _swap_cols(w):
    n = w.shape[1]
    idx = np.arange(n).reshape(n // 64, 2, 32)[:, ::-1, :].reshape(n)
    return w[:, idx]


def _rope_tables(pos):
    pos = np.asarray(pos)
    valid = pos >= 0
    p = np.where(valid, pos, 0)
    row = (p // 64).astype(np.float32)
    col = (p % 64).astype(np.float32)
    inv = (np.float32(10000.0) ** (-np.arange(16, dtype=np.float32) / np.float32(16))).astype(np.float32)
    ang = np.concatenate([row[:, None] * inv, col[:, None] * inv], -1).astype(np.float32)
    cos = np.cos(ang).astype(np.float32)
    sin = np.sin(ang).astype(np.float32)
    cos = np.where(valid[:, None], cos, np.float32(1.0))
    sin = np.where(valid[:, None], sin, np.float32(0.0))
    c64 = np.concatenate([cos, cos], 1)
    s64 = np.concatenate([-sin, sin], 1)
    cT = np.concatenate([c64, c64], 1).T
    sT = np.concatenate([s64, s64], 1).T
    return np.ascontiguousarray(cT, dtype=np.float32), np.ascontiguousarray(sT, dtype=np.float32)


def _ext_positions(j):
    pos = 2048 * j - 256 + np.arange(NEXT)
    if j == 0:
        pos[0:256] = 256 + np.arange(256)
    if j == 3:
        pos[2304:2560] = 7680 + np.arange(256)
    return pos


def _first_occurrence(kp):
    seen, out = set(), np.zeros(len(kp), dtype=bool)
    for i, p in enumerate(kp):
        if p not in seen:
            seen.add(p)
            out[i] = True
    return out


def _layer1_tables(j, rpb):
    pos = _ext_positions(j)
    wmask = np.full((128, 16, 3, 128), NEG, dtype=np.float32)
    nab = np.full((128, 16, 8, 5, 128), NEG, dtype=np.float32)
    for t in range(16):
        qp = 2048 * j + 128 * t + np.arange(128)
        kp = pos[128 * (t + 1):128 * (t + 4)]
        first = _first_occurrence(kp)
        ok = (np.abs(qp[None, :] - kp[:, None]) <= 128) & first[:, None]
        m = np.where(ok, np.float32(0.0), np.float32(NEG)).reshape(3, 128, 128)
        wmask[:, t] = m.transpose(1, 0, 2)
        kp = pos[128 * t:128 * (t + 5)]
        first = _first_occurrence(kp)
        qr, qc = qp // 64, qp % 64
        kr, kcl = kp // 64, kp % 64
        r0 = np.clip(qr - 4, 0, 120)
        c0 = np.clip(qc - 8, 0, 48)
        ok = ((kr[:, None] >= r0[None, :]) & (kr[:, None] < r0[None, :] + 8) &
              (kcl[:, None] >= c0[None, :]) & (kcl[:, None] < c0[None, :] + 16) & first[:, None])
        ro = np.clip(kr[:, None] - qr[None, :] + 7, 0, 14)
        co = np.clip(kcl[:, None] - qc[None, :] + 15, 0, 30)
        for h in range(8):
            b = np.where(ok, rpb[h][ro, co], np.float32(NEG)).astype(np.float32).reshape(5, 128, 128)
            nab[:, t, h] = b.transpose(1, 0, 2)
    return wmask.reshape(128, 16 * 384), nab.reshape(128, 16 * 8 * 640)


_NC_CACHE = {}


def kernel(x, c, ctx, c_ctx, mod_w, mod_b, ln_g, ln_b, ab_w_in, ab_w_out, diff_lambda, diff_subln_g, gqa_qk_g,
           cd_w_in, cd_w_out, win_sink, na_rpb, moe_w_group, moe_b_group, moe_w_router, moe_b_router,
           moe_w1, moe_w3, moe_w2):
    f = lambda a: np.ascontiguousarray(np.asarray(a), dtype=np.float32)
    x, c, ctx, c_ctx = f(x), f(c), f(ctx), f(c_ctx)
    mod_w, mod_b, ln_g, ln_b = f(mod_w), f(mod_b), f(ln_g), f(ln_b)
    ab_w_in, ab_w_out, cd_w_in, cd_w_out = f(ab_w_in)[0], f(ab_w_out)[0], f(cd_w_in)[0], f(cd_w_out)[0]
    diff_lambda, diff_subln_g, gqa_qk_g = f(diff_lambda)[0], f(diff_subln_g)[0], f(gqa_qk_g)[0]
    win_sink, na_rpb = f(win_sink)[0], f(na_rpb)[0]
    moe_w1, moe_w3, moe_w2 = f(moe_w1), f(moe_w3), f(moe_w2)

    shared = {}
    shared["ident"] = np.eye(128, dtype=np.float32)
    bo = np.zeros((128, 128), dtype=np.float32)
    bo[:64, :64] = 1.0
    bo[64:, 64:] = 1.0
    shared["bones"] = bo
    for i in range(2):
        shared[f"modw{i}"] = mod_w[i]
        shared[f"modb{i}"] = np.ascontiguousarray(mod_b[i].reshape(48, 128).T)
        shared[f"wr{i}"] = np.ascontiguousarray(np.concatenate([f(moe_w_group)[i], f(moe_w_router)[i]], 1))
        shared[f"br{i}"] = np.concatenate([f(moe_b_group)[i], f(moe_b_router)[i]])[None, :].copy()
        shared[f"w1_{i}"] = moe_w1[i].reshape(NEXP * D, 512)
        shared[f"w3_{i}"] = moe_w3[i].reshape(NEXP * D, 512)
        shared[f"w2_{i}"] = moe_w2[i].reshape(NEXP * 512, D)
    lnT = np.zeros((128, 64), dtype=np.float32)
    for i in range(2):
        for wch in range(2):
            lnT[:, ((i * 2 + wch) * 2 + 0) * 8:((i * 2 + wch) * 2 + 0) * 8 + 8] = ln_g[i, wch].reshape(8, 128).T
            lnT[:, ((i * 2 + wch) * 2 + 1) * 8:((i * 2 + wch) * 2 + 1) * 8 + 8] = ln_b[i, wch].reshape(8, 128).T
    shared["lnT"] = lnT
    q0 = ab_w_in[:, 0:1024]
    kd, vd = ab_w_in[:, 1024:1536], ab_w_in[:, 1536:2048]
    kg, vg = ab_w_in[:, 2048:2176], ab_w_in[:, 2176:2304]
    k0 = np.concatenate([kd, kg[:, 0:64], kg[:, 0:64], kg[:, 64:128], kg[:, 64:128]], 1)
    shared["wq0"], shared["wqs0"] = np.ascontiguousarray(q0), np.ascontiguousarray(_swap_cols(q0))
    shared["wk0"], shared["wks0"] = np.ascontiguousarray(k0), np.ascontiguousarray(_swap_cols(k0))
    shared["wv0"] = np.ascontiguousarray(np.concatenate([vd, vg], 1))
    shared["wo0"] = ab_w_out
    qw, qn = cd_w_in[:, 0:512], cd_w_in[:, 512:1024]
    kw, vw = cd_w_in[:, 1024:1152], cd_w_in[:, 1152:1280]
    kn, vn = cd_w_in[:, 1280:1792], cd_w_in[:, 1792:2304]
    q1 = np.concatenate([qn, qw], 1)
    k1 = np.concatenate([kn, kw[:, 0:64], kw[:, 0:64], kw[:, 64:128], kw[:, 64:128]], 1)
    shared["wq1"], shared["wqs1"] = np.ascontiguousarray(q1), np.ascontiguousarray(_swap_cols(q1))
    shared["wk1"], shared["wks1"] = np.ascontiguousarray(k1), np.ascontiguousarray(_swap_cols(k1))
    shared["wv1"] = np.ascontiguousarray(np.concatenate([vn, vw], 1))
    shared["wo1"] = cd_w_out
    sw = lambda g: np.concatenate([g[32:64], g[0:32]])
    gq, gk = gqa_qk_g[0], gqa_qk_g[1]
    shared["gt"] = np.ascontiguousarray(np.stack([np.tile(gq, 2), np.tile(sw(gq), 2), np.tile(gk, 2), np.tile(sw(gk), 2)], 1))
    shared["dlam"] = diff_lambda.reshape(1, 256).copy()
    shared["subg"] = diff_subln_g.reshape(1, 128).copy()
    shared["sink"] = win_sink.reshape(1, 8).copy()
    ck, sk = _rope_tables(np.concatenate([np.arange(SEQ), -np.ones(NCTX, dtype=np.int64)]))
    shared["cK"], shared["sK"] = ck, sk

    in_maps = []
    tabs = {}
    for core in range(8):
        b, j = core // 4, core % 4
        pos = _ext_positions(j)
        m = dict(shared)
        m["xT"] = np.ascontiguousarray(x[b].T)
        m["xeT"] = np.ascontiguousarray(x[b][pos].T)
        m["cxT"] = np.ascontiguousarray(ctx[b].T)
        m["ccT"] = np.ascontiguousarray(np.stack([c[b], c_ctx], 1))
        if j not in tabs:
            cq, sq = _rope_tables(np.concatenate([pos, -np.ones(NCTX, dtype=np.int64)]))
            wmask, nab = _layer1_tables(j, na_rpb)
            tabs[j] = (cq, sq, wmask, nab)
        m["cQ"], m["sQ"], m["wmask"], m["nab"] = tabs[j]
        in_maps.append(m)

    if "nc" not in _NC_CACHE:
        _NC_CACHE["nc"] = build_program()
    res = run_bass_kernel_spmd(_NC_CACHE["nc"], in_maps, core_ids=list(range(8)))
    out = np.empty((2, SEQ, D), dtype=np.float32)
    for core in range(8):
        b, j = core // 4, core % 4
        out[b, 2048 * j:2048 * (j + 1), :] = np.asarray(res.results[core]["outT"]).T
    if DEBUG:
        kernel.last = res
    return out
```

```python
import math
from contextlib import ExitStack

import numpy as np
import concourse.bass as bass
import concourse.mybir as mybir
from concourse.bass_utils import run_bass_kernel_spmd

F32 = mybir.dt.float32
BF16 = mybir.dt.bfloat16
AF = mybir.ActivationFunctionType
ALU = mybir.AluOpType
AX = mybir.AxisListType

DEBUG = False

D = 1024
SEQ = 8192
NCTX = 256
NEXT = 2560
NOWN = 2048
NQ = NEXT + NCTX
NK0 = SEQ + NCTX
NK1 = NEXT + NCTX
VW = 650
DEPTH = 2
ALPHA = (2.0 * DEPTH) ** 0.25
LN_EPS = 1e-5
RMS_EPS = 1e-6
LAM_INIT0 = 0.8 - 0.6 * math.exp(0.0)
NEG = -30000.0
NEXP = 32

SEM_LIM = 8000
NDMA = 12
ARENA_WORDS = 53200


class V:
    def __init__(self, ap, tid, p0, p1, f0, f1, wpe):
        self.ap, self.tid, self.p0, self.p1, self.f0, self.f1, self.wpe = ap, tid, p0, p1, f0, f1, wpe

    def reg(self):
        return (self.tid, self.p0, self.p1, self.f0, self.f1)

    def s(self, c0, c1, p0=None, p1=None):
        q0 = 0 if p0 is None else p0
        q1 = (self.p1 - self.p0) if p1 is None else p1
        return V(self.ap[q0:q1, c0:c1], self.tid, self.p0 + q0, self.p0 + q1,
                 self.f0 + c0 * self.wpe, self.f0 + c1 * self.wpe, self.wpe)


class Buf:
    def __init__(self, t, tid, P, F, wpe):
        self.t, self.tid, self.P, self.F, self.wpe = t, tid, P, F, wpe

    def v(self, p0=0, p1=None, f0=0, f1=None):
        p1 = self.P if p1 is None else p1
        f1 = self.F if f1 is None else f1
        return V(self.t[p0:p1, f0:f1], self.tid, p0, p1, f0 * self.wpe, f1 * self.wpe, self.wpe)


class Prog:
    ENGS = ["pe", "act", "dve", "pool", "sp"]

    def __init__(self, nc, stack):
        self.nc, self.stack = nc, stack
        self.recs = {e: [] for e in self.ENGS}
        self.sig = {e: 0 for e in self.ENGS}
        self.known = {e: {} for e in self.ENGS}
        self.csems = {e: [] for e in self.ENGS}
        self.dsems, self.dcnt, self.drr = {}, {}, {}
        for q in ["sp", "act", "pool"]:
            self.dsems[q] = [stack.enter_context(nc.semaphore(f"d_{q}_{k}")) for k in range(NDMA)]
            self.dcnt[q] = [0] * NDMA
            self.drr[q] = 0
        self.ent = {}
        self.ntid = 0

    def new_tid(self):
        self.ntid += 1
        return self.ntid

    def dram(self, name, P, F, dtype, kind):
        t = self.nc.dram_tensor(name, [P, F], dtype, kind=kind)
        return Buf(t, self.new_tid(), P, F, 1.0 if dtype == F32 else 0.5)

    def _csem(self, e, idx):
        while len(self.csems[e]) <= idx:
            self.csems[e].append(self.stack.enter_context(self.nc.semaphore(f"c_{e}_{len(self.csems[e])}")))
        return self.csems[e][idx]

    @staticmethod
    def _ov(a, b):
        return a[1] < b[2] and b[1] < a[2] and a[3] < b[4] and b[3] < a[4]

    @staticmethod
    def _cov(a, b):
        return a[1] <= b[1] and a[2] >= b[2] and a[3] <= b[3] and a[4] >= b[4]

    BUCK = 256

    def _cands(self, r):
        d = self.ent.get(r[0])
        if not d:
            return []
        seen, out = set(), []
        for b in range(int(r[3]) // self.BUCK, int(math.ceil(r[4])) // self.BUCK + 1):
            for en in d.get(b, ()):
                if en[3] and id(en) not in seen:
                    seen.add(id(en))
                    out.append(en)
        return out

    def _add(self, en):
        r = en[0]
        d = self.ent.setdefault(r[0], {})
        for b in range(int(r[3]) // self.BUCK, int(math.ceil(r[4])) // self.BUCK + 1):
            lst = d.setdefault(b, [])
            if len(lst) > 64:
                lst[:] = [x for x in lst if x[3]]
            lst.append(en)

    def _deps(self, reads, writes):
        raw, other = [], []
        for r in reads:
            for en in self._cands(r):
                if en[1] is not None and self._ov(en[0], r):
                    raw.append(en[1])
        for w in writes:
            for en in self._cands(w):
                if self._ov(en[0], w):
                    if en[1] is not None:
                        other.append(en[1])
                    other.extend(en[2])
        return raw, other

    def _update(self, tok, reads, writes):
        for r in reads:
            hit = False
            for en in self._cands(r):
                if self._ov(en[0], r):
                    if tok[0] == "c":
                        en[2][:] = [t for t in en[2] if not (t[0] == "c" and t[1] == tok[1])]
                    en[2].append(tok)
                    if self._cov(en[0], r):
                        hit = True
            if not hit:
                self._add([r, None, [tok], True])
        for w in writes:
            for en in self._cands(w):
                if self._cov(w, en[0]):
                    en[3] = False
            self._add([w, tok, [], True])

    def _waits(self, e, raw, other):
        waits = []
        kn = self.known[e]
        for kind, toks in (("raw", raw), ("oth", other)):
            for t in toks:
                if t[0] == "c":
                    _, f, v = t
                    if f == e and (kind == "oth" or e == "pe"):
                        continue
                    if kn.get(f, 0) >= v:
                        continue
                    kn[f] = v
                    waits.append(t)
                else:
                    _, q, k, c = t
                    if kn.get((q, k), 0) >= c:
                        continue
                    kn[(q, k)] = c
                    waits.append(t)
        return waits

    def op(self, e, fn, reads=(), writes=(), sig=True):
        reads = [r.reg() for r in reads]
        writes = [w.reg() for w in writes]
        raw, other = self._deps(reads, writes)
        waits = self._waits(e, raw, other)
        if sig:
            self.sig[e] += 1
            v = self.sig[e]
        else:
            v = self.sig[e] + 1
        tok = ("c", e, v)
        self._update(tok, reads, writes)
        self.recs[e].append((waits, fn, tok if sig else None))
        return tok

    def dma(self, q, out, in_, **kw):
        reads, writes = [in_.reg()], [out.reg()]
        raw, other = self._deps(reads, writes)
        k = self.drr[q]
        self.drr[q] = (k + 1) % NDMA
        prev = self.dcnt[q][k]
        if prev > 0:
            other = other + [("d", q, k, prev)]
        waits = self._waits(q, raw, other)
        self.dcnt[q][k] = prev + 1
        tok = ("d", q, k, prev + 1)
        self._update(tok, reads, writes)
        oa, ia = out.ap, in_.ap

        def fn(eng, oa=oa, ia=ia, kw=kw):
            return eng.dma_start(out=oa, in_=ia, **kw)

        self.recs[q].append((waits, fn, tok))
        return tok

    def finish(self):
        waits = []
        for q in self.dsems:
            for k in range(NDMA):
                if self.dcnt[q][k] > 0:
                    waits.append(("d", q, k, self.dcnt[q][k]))
        for e in ["pe", "act", "dve", "pool"]:
            if self.sig[e] > 0:
                waits.append(("c", e, self.sig[e]))
        self.recs["sp"].append((waits, None, None))

    def _emit_wait(self, eng, w):
        if w[0] == "c":
            _, f, v = w
            eng.wait_ge(self._csem(f, (v - 1) // SEM_LIM), (v - 1) % SEM_LIM + 1)
        else:
            _, q, k, c = w
            eng.wait_ge(self.dsems[q][k], 16 * c)

    def emit(self):
        nc = self.nc
        for e in self.ENGS:
            for idx in range((self.sig[e] + SEM_LIM - 1) // SEM_LIM + 1):
                self._csem(e, idx)
        recs, me = self.recs, self

        def play(e, eng):
            for waits, fn, tok in recs[e]:
                for w in waits:
                    me._emit_wait(eng, w)
                if fn is None:
                    continue
                ins = fn(eng)
                if tok is not None:
                    if tok[0] == "c":
                        ins.then_inc(me._csem(e, (tok[2] - 1) // SEM_LIM), 1)
                    else:
                        ins.then_inc(me.dsems[tok[1]][tok[2]], 16)

        with nc.Block() as block:
            @block.tensor
            def _(eng):
                play("pe", eng)

            @block.scalar
            def _(eng):
                play("act", eng)

            @block.vector
            def _(eng):
                play("dve", eng)

            @block.gpsimd
            def _(eng):
                play("pool", eng)

            @block.sync
            def _(eng):
                play("sp", eng)


class Arena:
    def __init__(self, buf):
        self.buf, self.top, self.hi = buf, 0, 0

    def _alloc(self, words):
        off = self.top
        self.top += (words + 7) // 8 * 8
        self.hi = max(self.hi, self.top)
        assert self.top <= self.buf.F, f"arena overflow {self.top}"
        return off

    def f32(self, n, P=128):
        off = self._alloc(n)
        return V(self.buf.t[0:P, off:off + n], self.buf.tid, 0, P, off, off + n, 1.0)

    def bf(self, n, P=128):
        w = (n + 1) // 2
        off = self._alloc(w)
        return V(self.buf.t[0:P, off:off + w].bitcast(BF16), self.buf.tid, 0, P, off, off + w, 0.5)


class Rot:
    def __init__(self, items):
        self.items, self.i = items, 0

    def get(self):
        it = self.items[self.i % len(self.items)]
        self.i += 1
        return it


def r3(ap, k):
    return ap.rearrange("p (k n) -> p k n", k=k)


def OP(name, *a, **k):
    return lambda e: getattr(e, name)(*a, **k)


def build_program():
    nc = bass.Bass("TRN2", target_bir_lowering=False)
    st = ExitStack()
    with st:
        P = Prog(nc, st)
        I = lambda name, p, f, dt=F32: P.dram(name, p, f, dt, "ExternalInput")
        xT = I("xT", D, SEQ)
        xeT = I("xeT", D, NEXT)
        cxT = I("cxT", D, NCTX)
        ccT = I("ccT", D, 2)
        ident_d = I("ident", 128, 128)
        bones_d = I("bones", 128, 128)
        modw = [I(f"modw{i}", D, 6 * D) for i in range(2)]
        modb = [I(f"modb{i}", 128, 48) for i in range(2)]
        lnT_d = I("lnT", 128, 64)
        wq_d = [I(f"wq{i}", D, 1024) for i in range(2)]
        wqs_d = [I(f"wqs{i}", D, 1024) for i in range(2)]
        wk_d = [I(f"wk{i}", D, 768) for i in range(2)]
        wks_d = [I(f"wks{i}", D, 768) for i in range(2)]
        wv_d = [I(f"wv{i}", D, 640) for i in range(2)]
        wo_d = [I(f"wo{i}", D, 1024) for i in range(2)]
        gt_d = I("gt", 128, 4)
        dlam_d = I("dlam", 1, 256)
        subg_d = I("subg", 1, 128)
        sink_d = I("sink", 1, 8)
        cK_d = I("cK", 128, NK0)
        sK_d = I("sK", 128, NK0)
        cQ_d = I("cQ", 128, NQ)
        sQ_d = I("sQ", 128, NQ)
        wr_d = [I(f"wr{i}", D, 36) for i in range(2)]
        br_d = [I(f"br{i}", 1, 36) for i in range(2)]
        w1_d = [I(f"w1_{i}", NEXP * D, 512) for i in range(2)]
        w3_d = [I(f"w3_{i}", NEXP * D, 512) for i in range(2)]
        w2_d = [I(f"w2_{i}", NEXP * 512, D) for i in range(2)]
        wmask_d = I("wmask", 128, 16 * 384)
        nab_d = I("nab", 128, 16 * 8 * 640)
        outT = P.dram("outT", D, NOWN, F32, "ExternalOutput")
        skind = "ExternalOutput" if DEBUG else "Internal"
        QT = P.dram("QT", 1024, NQ, BF16, skind)
        KT = P.dram("KT", 768, NK0, BF16, skind)
        VA = P.dram("VA", 128, 66 * VW, BF16, skind)
        h1T = P.dram("h1T", D, NEXT, F32, skind)
        hc1T = P.dram("hc1T", D, NCTX, F32, skind)

        arena_t = st.enter_context(nc.sbuf_tensor("arena", [128, ARENA_WORDS], F32))
        AR = Arena(Buf(arena_t, P.new_tid(), 128, ARENA_WORDS, 1.0))
        psum_t = st.enter_context(nc.psum_tensor("psum", [128, 4096], F32))
        PS = Buf(psum_t, P.new_tid(), 128, 4096, 1.0)

        def bank(b, n=512, p=128):
            return PS.v(0, p, b * 512, b * 512 + n)

        def bank_bf(b, n):
            return V(PS.t[:, b * 512:b * 512 + n // 2].bitcast(BF16), PS.tid, 0, 128, b * 512, b * 512 + n // 2, 0.5)

        def dchunks(buf, c0, c1, r0=0, nchunk=8):
            ap = buf.t[r0:r0 + nchunk * 128, c0:c1].rearrange("(c p) n -> p c n", p=128)
            return V(ap, buf.tid, r0, r0 + nchunk * 128, c0 * buf.wpe, c1 * buf.wpe, buf.wpe)

        def v3(v, k):
            return V(r3(v.ap, k), v.tid, v.p0, v.p1, v.f0, v.f1, v.wpe)

        def bcast(buf, n):
            return V(buf.t[0:1, 0:n].partition_broadcast(128), buf.tid, 0, 1, 0, n * buf.wpe, buf.wpe)

        ident32 = AR.f32(128)
        identb = AR.bf(128)
        ones = AR.f32(128)
        bones = AR.f32(128)
        lnT = AR.f32(64)
        gt = AR.f32(4)
        dl = AR.f32(256)
        sg1 = AR.f32(128)
        esink = AR.f32(8)
        eps_ln = AR.f32(1)
        eps_rms = AR.f32(1)
        nlam = AR.f32(1)
        ML = [AR.f32(48) for _ in range(2)]
        MC = [AR.f32(48) for _ in range(2)]
        sc = AR.f32(16)
        wr = [AR.f32(8 * 36) for _ in range(2)]
        brb = [AR.f32(36) for _ in range(2)]
        P.dma("sp", ident32, ident_d.v())
        P.dma("sp", bones, bones_d.v())
        P.dma("sp", lnT, lnT_d.v())
        P.dma("sp", gt, gt_d.v())
        P.dma("sp", dl, bcast(dlam_d, 256))
        P.dma("sp", sg1, bcast(subg_d, 128))
        P.dma("sp", esink, bcast(sink_d, 8))
        for i in range(2):
            P.dma("sp", v3(wr[i], 8), dchunks(wr_d[i], 0, 36))
            P.dma("sp", brb[i], bcast(br_d[i], 36))
        P.op("dve", OP("tensor_copy", out=identb.ap, in_=ident32.ap), [ident32], [identb])
        P.op("dve", OP("memset", ones.ap, 1.0), [], [ones])
        P.op("dve", OP("memset", eps_ln.ap, LN_EPS), [], [eps_ln])
        P.op("dve", OP("memset", eps_rms.ap, RMS_EPS), [], [eps_rms])
        P.op("act", OP("activation", out=esink.ap, in_=esink.ap, func=AF.Exp), [esink], [esink])
        P.op("dve", OP("tensor_scalar", out=sg1.ap, in0=sg1.ap, scalar1=1.0 - LAM_INIT0, scalar2=None, op0=ALU.mult), [sg1], [sg1])
        lt = AR.f32(128)
        ls = AR.f32(2)
        dl4 = dl.ap.rearrange("p (a b n) -> p a b n", a=2, b=2)
        P.op("dve", OP("tensor_tensor", out=lt.ap.rearrange("p (a n) -> p a n", a=2), in0=dl4[:, :, 0, :], in1=dl4[:, :, 1, :], op=ALU.mult), [dl], [lt])
        P.op("dve", OP("tensor_reduce", out=ls.ap, in_=lt.ap.rearrange("p (a n) -> p a n", a=2), axis=AX.X, op=ALU.add), [lt], [ls])
        P.op("act", OP("activation", out=ls.ap, in_=ls.ap, func=AF.Exp), [ls], [ls])
        P.op("dve", OP("tensor_tensor", out=nlam.ap, in0=ls.ap[:, 1:2], in1=ls.ap[:, 0:1], op=ALU.subtract), [ls], [nlam])
        P.op("dve", OP("tensor_scalar", out=nlam.ap, in0=nlam.ap, scalar1=-LAM_INIT0, scalar2=None, op0=ALU.add), [nlam], [nlam])

        base_top = AR.top
        P.dma("sp", v3(sc, 8), dchunks(ccT, 0, 2))
        P.op("act", OP("activation", out=sc.ap, in_=sc.ap, func=AF.Silu), [sc], [sc])
        mwb = [AR.f32(8 * 512) for _ in range(2)]
        mbt = AR.f32(48)
        for i in range(2):
            P.dma("sp", mbt, modb[i].v())
            pm = bank(6, 96)
            for pc in range(12):
                w = mwb[pc % 2]
                P.dma("sp", v3(w, 8), dchunks(modw[i], pc * 512, pc * 512 + 512))
                for j in range(4):
                    cc = pc * 4 + j
                    for dc in range(8):
                        P.op("pe", OP("matmul",
                            pm.ap[:, cc * 2:cc * 2 + 2], lhsT=w.ap[:, dc * 512 + j * 128: dc * 512 + j * 128 + 128],
                            rhs=sc.ap[:, dc * 2:dc * 2 + 2], start=(dc == 0), stop=(dc == 7)),
                            [w, sc], [pm], sig=(dc == 7))
            pm3 = pm.ap.rearrange("p (c n) -> p c n", n=2)
            P.op("dve", OP("tensor_tensor", out=ML[i].ap, in0=pm3[:, :, 0], in1=mbt.ap, op=ALU.add), [pm, mbt], [ML[i]])
            P.op("dve", OP("tensor_tensor", out=MC[i].ap, in0=pm3[:, :, 1], in1=mbt.ap, op=ALU.add), [pm, mbt], [MC[i]])
            for M in (ML[i], MC[i]):
                for k in (1, 4):
                    P.op("dve", OP("tensor_scalar", out=M.ap[:, k * 8:k * 8 + 8], in0=M.ap[:, k * 8:k * 8 + 8],
                                                                    scalar1=1.0, scalar2=None, op0=ALU.add), [M], [M])
        AR.top = base_top

        FT = AR.bf(8 * NQ)
        H = AR.f32(8 * NQ)
        free_top = AR.top

        def ln_block(ucf, nt, li, which, out_fn, tmp):
            s1, s2 = bank(6, nt), bank(7, nt)
            for c in range(8):
                uc = ucf(c)
                P.op("pe", OP("matmul", s1.ap, lhsT=ones.ap, rhs=uc.ap, start=(c == 0), stop=(c == 7)),
                     [ones, uc], [s1], sig=(c == 7))
            for c in range(8):
                uc = ucf(c)
                q = tmp["sq"].get().s(0, nt)
                P.op("act", OP("activation", out=q.ap, in_=uc.ap, func=AF.Square), [uc], [q])
                P.op("pe", OP("matmul", s2.ap, lhsT=ones.ap, rhs=q.ap, start=(c == 0), stop=(c == 7)),
                     [ones, q], [s2], sig=True)
            mean, msq, rstd = tmp["mean"].s(0, nt), tmp["msq"].s(0, nt), tmp["rstd"].s(0, nt)
            P.op("act", OP("activation", out=mean.ap, in_=s1.ap, func=AF.Copy, scale=1.0 / D), [s1], [mean])
            P.op("act", OP("activation", out=msq.ap, in_=s1.ap, func=AF.Square, scale=1.0 / D), [s1], [msq])
            P.op("dve", OP("scalar_tensor_tensor", out=rstd.ap, in0=s2.ap, scalar=1.0 / D, in1=msq.ap, op0=ALU.mult, op1=ALU.subtract),
                 [s2, msq], [rstd])
            P.op("act", OP("activation", out=rstd.ap, in_=rstd.ap, func=AF.Sqrt, bias=eps_ln.ap, scale=1.0), [rstd, eps_ln], [rstd])
            P.op("dve", OP("reciprocal", out=rstd.ap, in_=rstd.ap), [rstd], [rstd])
            gi = ((li * 2 + which) * 2 + 0) * 8
            bi = ((li * 2 + which) * 2 + 1) * 8
            for c in range(8):
                uc = ucf(c)
                o = out_fn(c)
                P.op("pool", OP("tensor_tensor", out=uc.ap, in0=uc.ap, in1=mean.ap, op=ALU.subtract), [uc, mean], [uc])
                P.op("dve", OP("tensor_tensor", out=uc.ap, in0=uc.ap, in1=rstd.ap, op=ALU.mult), [uc, rstd], [uc])
                P.op("dve", OP("tensor_scalar", out=o.ap, in0=uc.ap, scalar1=lnT.ap[:, gi + c:gi + c + 1],
                                                                      scalar2=lnT.ap[:, bi + c:bi + c + 1], op0=ALU.mult, op1=ALU.add),
                     [uc, lnT], [o])

        def layer(li):
            ab = (li == 0)
            NT = NQ if ab else NOWN

            def Hc(c, t0, t1):
                return H.s(c * NT + t0, c * NT + t1)

            AR.top = FT.f0 if False else int(FT.f0)
            wq, wqs, wk, wks, wv = AR.bf(8 * 1024), AR.bf(8 * 1024), AR.bf(8 * 768), AR.bf(8 * 768), AR.bf(8 * 640)
            for dst, src, n in ((wq, wq_d[li], 1024), (wqs, wqs_d[li], 1024), (wk, wk_d[li], 768), (wks, wks_d[li], 768), (wv, wv_d[li], 640)):
                P.dma("pool", v3(dst, 8), dchunks(src, 0, n))
            xb = Rot([AR.f32(8 * 512) for _ in range(2)])
            abuf = Rot([AR.bf(8 * 512) for _ in range(2)])
            ctab = Rot([AR.f32(512) for _ in range(2)])
            stab = Rot([AR.f32(512) for _ in range(2)])
            t1r = Rot([AR.f32(512) for _ in range(2)])
            t2r = Rot([AR.f32(512) for _ in range(2)])
            obr = Rot([AR.bf(512) for _ in range(3)])
            sqr = Rot([AR.f32(512) for _ in range(2)])
            rvr = Rot([AR.f32(512) for _ in range(2)])
            vst = Rot([AR.bf(VW) for _ in range(2)])
            for vs in vst.items:
                P.op("pool", OP("memset", vs.ap, 1.0), [], [vs])
            pbank = Rot([0, 1, 2, 3, 4, 5])
            if ab:
                q_rope, q_norm = [True] * 8, [False] * 4 + [True] * 4
                k_rope, k_norm = [True] * 6, [False] * 4 + [True] * 2
            else:
                q_rope, q_norm = [False] * 4 + [True] * 4, [False] * 8
                k_rope, k_norm = [False] * 4 + [True] * 2, [False] * 6

            def fm_proj(aT, nt, W, Ws, wn, ch, rope, norm, gcol, ct, stb, dst):
                pa = bank(pbank.get(), nt)
                for c in range(8):
                    P.op("pe", OP("matmul", pa.ap, lhsT=W.ap[:, c * wn + ch * 128: c * wn + ch * 128 + 128],
                                                      rhs=aT.ap[:, c * nt:c * nt + nt], start=(c == 0), stop=(c == 7)),
                         [W, aT], [pa], sig=(c == 7))
                ob = obr.get().s(0, nt)
                if not rope:
                    P.op("act", OP("activation", out=ob.ap, in_=pa.ap, func=AF.Copy), [pa], [ob])
                else:
                    pb = bank(pbank.get(), nt)
                    for c in range(8):
                        P.op("pe", OP("matmul", pb.ap, lhsT=Ws.ap[:, c * wn + ch * 128: c * wn + ch * 128 + 128],
                                                          rhs=aT.ap[:, c * nt:c * nt + nt], start=(c == 0), stop=(c == 7)),
                             [Ws, aT], [pb], sig=(c == 7))
                    t1, t2 = t1r.get().s(0, nt), t2r.get().s(0, nt)
                    if norm:
                        sq = sqr.get().s(0, nt)
                        P.op("act", OP("activation", out=sq.ap, in_=pa.ap, func=AF.Square), [pa], [sq])
                        pss = bank(pbank.get(), nt)
                        P.op("pe", OP("matmul", pss.ap, lhsT=bones.ap, rhs=sq.ap, start=True, stop=True), [bones, sq], [pss])
                        rv = rvr.get().s(0, nt)
                        P.op("act", OP("activation", out=rv.ap, in_=pss.ap, func=AF.Sqrt, bias=eps_rms.ap, scale=1.0 / 64), [pss, eps_rms], [rv])
                        P.op("dve", OP("reciprocal", out=rv.ap, in_=rv.ap), [rv], [rv])
                        P.op("dve", OP("scalar_tensor_tensor", out=t1.ap, in0=pa.ap, scalar=gt.ap[:, gcol:gcol + 1], in1=ct.ap, op0=ALU.mult, op1=ALU.mult),
                             [pa, gt, ct], [t1])
                        P.op("dve", OP("scalar_tensor_tensor", out=t2.ap, in0=pb.ap, scalar=gt.ap[:, gcol + 1:gcol + 2], in1=stb.ap, op0=ALU.mult, op1=ALU.mult),
                             [pb, gt, stb], [t2])
                        P.op("pool", OP("tensor_tensor", out=t1.ap, in0=t1.ap, in1=t2.ap, op=ALU.add), [t1, t2], [t1])
                        P.op("dve", OP("tensor_tensor", out=ob.ap, in0=t1.ap, in1=rv.ap, op=ALU.mult), [t1, rv], [ob])
                    else:
                        P.op("dve", OP("tensor_tensor", out=t1.ap, in0=pa.ap, in1=ct.ap, op=ALU.mult), [pa, ct], [t1])
                        P.op("dve", OP("tensor_tensor", out=t2.ap, in0=pb.ap, in1=stb.ap, op=ALU.mult), [pb, stb], [t2])
                        P.op("pool", OP("tensor_tensor", out=ob.ap, in0=t1.ap, in1=t2.ap, op=ALU.add), [t1, t2], [ob])
                P.dma("sp", dst, ob)

            def source(src, ntok, M, ctd, std, toff, want_q, q_off, want_kv, k_off):
                for t0 in range(0, ntok, 512):
                    nt = min(512, ntok - t0)
                    x = xb.get().s(0, 8 * nt)
                    P.dma("sp", v3(x, 8), dchunks(src, t0, t0 + nt))
                    aT = abuf.get().s(0, 8 * nt)
                    for c in range(8):
                        P.op("act", OP("activation",
                            out=aT.ap[:, c * nt:c * nt + nt], in_=x.ap[:, c * nt:c * nt + nt], func=AF.Identity,
                            scale=M.ap[:, 8 + c:9 + c], bias=M.ap[:, c:c + 1]), [x, M], [aT])
                    ct, stb = ctab.get().s(0, nt), stab.get().s(0, nt)
                    P.dma("sp", ct, ctd.v(0, 128, toff + t0, toff + t0 + nt))
                    P.dma("sp", stb, std.v(0, 128, toff + t0, toff + t0 + nt))
                    if want_kv:
                        for ch in range(6):
                            fm_proj(aT, nt, wk, wks, 768, ch, k_rope[ch], k_norm[ch], 2, ct, stb,
                                    KT.v(ch * 128, ch * 128 + 128, k_off + t0, k_off + t0 + nt))
                        for tt in range(nt // 128):
                            pv1, pv2 = bank(pbank.get(), 512), bank(pbank.get(), 128)
                            for c in range(8):
                                lh = aT.ap[:, c * nt + tt * 128:c * nt + tt * 128 + 128]
                                P.op("pe", OP("matmul", pv1.ap, lhsT=lh, rhs=wv.ap[:, c * 640:c * 640 + 512], start=(c == 0), stop=(c == 7)),
                                     [aT, wv], [pv1], sig=(c == 7))
                            for c in range(8):
                                lh = aT.ap[:, c * nt + tt * 128:c * nt + tt * 128 + 128]
                                P.op("pe", OP("matmul", pv2.ap, lhsT=lh, rhs=wv.ap[:, c * 640 + 512:c * 640 + 640], start=(c == 0), stop=(c == 7)),
                                     [aT, wv], [pv2], sig=(c == 7))
                            vs = vst.get()
                            if ab:
                                o1 = vs.ap[:, 0:516].rearrange("p (h n) -> p h n", n=129)[:, :, 0:128]
                                i1 = pv1.ap.rearrange("p (h n) -> p h n", n=128)
                                o2 = vs.ap[:, 516:646].rearrange("p (h n) -> p h n", n=65)[:, :, 0:64]
                            else:
                                o1 = vs.ap[:, 0:520].rearrange("p (h n) -> p h n", n=65)[:, :, 0:64]
                                i1 = pv1.ap.rearrange("p (h n) -> p h n", n=64)
                                o2 = vs.ap[:, 520:650].rearrange("p (h n) -> p h n", n=65)[:, :, 0:64]
                            i2 = pv2.ap.rearrange("p (h n) -> p h n", n=64)
                            P.op("act", OP("activation", out=o1, in_=i1, func=AF.Copy), [pv1], [vs])
                            P.op("dve", OP("tensor_copy", out=o2, in_=i2), [pv2], [vs])
                            kt = (k_off + t0) // 128 + tt
                            P.dma("sp", VA.v(0, 128, kt * VW, kt * VW + VW), vs)
                    if want_q:
                        for ch in range(8):
                            fm_proj(aT, nt, wq, wqs, 1024, ch, q_rope[ch], q_norm[ch], 0, ct, stb,
                                    QT.v(ch * 128, ch * 128 + 128, q_off + t0, q_off + t0 + nt))

            if ab:
                source(xT, SEQ, ML[0], cK_d, sK_d, 0, False, 0, True, 0)
                source(cxT, NCTX, MC[0], cK_d, sK_d, SEQ, True, NEXT, True, SEQ)
                source(xeT, NEXT, ML[0], cQ_d, sQ_d, 0, True, 0, False, 0)
                nkt = SEQ // 128 + 2
            else:
                source(h1T, NEXT, ML[1], cQ_d, sQ_d, 0, True, 0, True, 0)
                source(hc1T, NCTX, MC[1], cQ_d, sQ_d, NEXT, False, 0, True, NEXT)
                nkt = NEXT // 128 + 2

            AR.top = int(H.f0)
            O = FT
            qbuf = Rot([AR.bf(NQ) for _ in range(2)])
            kbuf = Rot([AR.bf(nkt * 128) for _ in range(2)])
            vbuf = Rot([AR.bf(nkt * 130) for _ in range(2)])
            ptile = Rot([AR.bf(1024) for _ in range(3)])
            rz = Rot([AR.f32(1) for _ in range(6)])
            od1 = [AR.f32(128) for _ in range(4)]
            t_o2 = Rot([AR.f32(128) for _ in range(2)])
            t_od = Rot([AR.f32(128) for _ in range(2)])
            t_sq = Rot([AR.f32(128) for _ in range(2)])
            ssq = Rot([AR.f32(1) for _ in range(4)])
            stmp = Rot([AR.f32(640) for _ in range(2)])
            if not ab:
                wm = AR.f32(16 * 384)
                P.dma("sp", wm, wmask_d.v())
                nbb = Rot([AR.f32(640) for _ in range(2)])

            def attend512(kt_, qt_, vv, vcw, voff, dv, r0, q0, nq, kts, fin):
                nsub = nq // 128
                accs = [bank(4 + s, dv + 1) for s in range(nsub)]
                pairs = [kts[i:i + 2] for i in range(0, len(kts), 2)]
                npair = len(pairs)

                def stage_a(pi):
                    pr = pairs[pi]
                    base = (pi % 2) * 1024
                    sreg = PS.v(0, 128, base, base + len(pr) * nq)
                    for jj, kt in enumerate(pr):
                        P.op("pe", OP("matmul", sreg.ap[:, jj * nq:jj * nq + nq], lhsT=kt_.ap[r0:r0 + 64, kt * 128:kt * 128 + 128],
                                      rhs=qt_.ap[r0:r0 + 64, q0:q0 + nq], start=True, stop=True), [kt_, qt_], [sreg], sig=(jj == len(pr) - 1))
                    pt = ptile.get().s(0, len(pr) * nq)
                    P.op("act", OP("activation", out=pt.ap, in_=sreg.ap, func=AF.Exp, scale=0.125), [sreg], [pt])
                    return pt

                def stage_b(pi, pt):
                    pr = pairs[pi]
                    for jj, kt in enumerate(pr):
                        for s in range(nsub):
                            first = (pi == 0 and jj == 0)
                            last = (pi == npair - 1 and jj == len(pr) - 1)
                            P.op("pe", OP("matmul", accs[s].ap, lhsT=pt.ap[:, jj * nq + s * 128:jj * nq + s * 128 + 128],
                                          rhs=vv.ap[:, kt * vcw + voff:kt * vcw + voff + dv + 1], start=first, stop=last),
                                 [pt, vv], [accs[s]], sig=(s == nsub - 1 and jj == len(pr) - 1))

                prev = None
                for pi in range(npair):
                    cur = stage_a(pi)
                    if prev is not None:
                        stage_b(pi - 1, prev)
                    prev = cur
                stage_b(npair - 1, prev)
                for s in range(nsub):
                    fin(s, accs[s], q0 // 128 + s)

            for u in range(8):
                kc = u if u < 4 else 4 + (u - 4) // 2
                qt_, kt_, vt_ = qbuf.get(), kbuf.get(), vbuf.get()
                P.dma("sp", qt_, QT.v(u * 128, u * 128 + 128, 0, NQ))
                P.dma("sp", kt_, KT.v(kc * 128, kc * 128 + 128, 0, nkt * 128))
                if ab:
                    vc0, vcw = (u * 129, 129) if u < 4 else (516 + ((u - 4) // 2) * 65, 65)
                else:
                    vc0, vcw = (u * 130, 130) if u < 4 else (520 + ((u - 4) // 2) * 65, 65)
                vv = vt_.s(0, nkt * vcw)
                for g0 in range(0, nkt, 11):
                    src = V(VA.t[:, g0 * VW:(g0 + 11) * VW].rearrange("p (t c) -> p t c", c=VW)[:, :, vc0:vc0 + vcw],
                            VA.tid, 0, 128, g0 * VW * 0.5, (g0 + 11) * VW * 0.5, 0.5)
                    dstv = V(vv.ap[:, g0 * vcw:(g0 + 11) * vcw].rearrange("p (t c) -> p t c", c=vcw), vv.tid, 0, 128,
                             vv.f0 + g0 * vcw * 0.5, vv.f0 + (g0 + 11) * vcw * 0.5, 0.5)
                    P.dma("sp", dstv, src)

                if ab:
                    dv = 128 if u < 4 else 64
                    qblocks = [(q0, 256, list(range(nkt))) for q0 in range(0, NEXT, 256)] + [(NEXT, 256, [nkt - 2, nkt - 1])]
                    for (q0, nq, kts) in qblocks:
                        nsub = nq // 128
                        accs = [[bank(4 + c * 2 + s, dv + 1) for s in range(nsub)] for c in range(2)]
                        pairs = [kts[i:i + 2] for i in range(0, len(kts), 2)]
                        npair = len(pairs)

                        def st_a(pi):
                            pr = pairs[pi]
                            base = (pi % 2) * 1024
                            sreg = PS.v(0, 128, base, base + 1024)
                            for jj, kt in enumerate(pr):
                                for c in range(2):
                                    P.op("pe", OP("matmul", sreg.ap[:, c * 512 + jj * nq:c * 512 + jj * nq + nq],
                                                  lhsT=kt_.ap[64 * c:64 * c + 64, kt * 128:kt * 128 + 128],
                                                  rhs=qt_.ap[64 * c:64 * c + 64, q0:q0 + nq], start=True, stop=True),
                                         [kt_, qt_], [sreg], sig=(jj == len(pr) - 1 and c == 1))
                            pt = ptile.get()
                            P.op("act", OP("activation", out=pt.ap, in_=sreg.ap, func=AF.Exp, scale=0.125), [sreg], [pt])
                            return pt

                        def st_b(pi, pt):
                            pr = pairs[pi]
                            for c in range(2):
                                for jj, kt in enumerate(pr):
                                    for s_ in range(nsub):
                                        first = (pi == 0 and jj == 0)
                                        last = (pi == npair - 1 and jj == len(pr) - 1)
                                        col = c * 512 + jj * nq + s_ * 128
                                        P.op("pe", OP("matmul", accs[c][s_].ap, lhsT=pt.ap[:, col:col + 128],
                                                      rhs=vv.ap[:, kt * vcw:kt * vcw + dv + 1], start=first, stop=last),
                                             [pt, vv], [accs[c][s_]], sig=(c == 1 and s_ == nsub - 1 and jj == len(pr) - 1))

                        prev = None
                        for pi in range(npair):
                            cur = st_a(pi)
                            if prev is not None:
                                st_b(pi - 1, prev)
                            prev = cur
                        st_b(npair - 1, prev)
                        for s_ in range(nsub):
                            ot = q0 // 128 + s_
                            a0, a1 = accs[0][s_], accs[1][s_]
                            r0_, r1_ = rz.get(), rz.get()
                            P.op("dve", OP("reciprocal", out=r0_.ap, in_=a0.ap[:, dv:dv + 1]), [a0], [r0_])
                            P.op("dve", OP("reciprocal", out=r1_.ap, in_=a1.ap[:, dv:dv + 1]), [a1], [r1_])
                            if u < 4:
                                d1 = od1[s_]
                                o2, od, sq, ss = t_o2.get(), t_od.get(), t_sq.get(), ssq.get()
                                P.op("dve", OP("tensor_scalar", out=d1.ap, in0=a0.ap[:, 0:128], scalar1=r0_.ap[:, 0:1], scalar2=None, op0=ALU.mult), [a0, r0_], [d1])
                                P.op("dve", OP("tensor_scalar", out=o2.ap, in0=a1.ap[:, 0:128], scalar1=r1_.ap[:, 0:1], scalar2=None, op0=ALU.mult), [a1, r1_], [o2])
                                P.op("dve", OP("scalar_tensor_tensor", out=od.ap, in0=o2.ap, scalar=nlam.ap[:, 0:1], in1=d1.ap, op0=ALU.mult, op1=ALU.add), [o2, nlam, d1], [od])
                                P.op("pool", OP("tensor_tensor", out=sq.ap, in0=od.ap, in1=od.ap, op=ALU.mult), [od], [sq])
                                P.op("dve", OP("tensor_reduce", out=ss.ap, in_=sq.ap, axis=AX.X, op=ALU.add), [sq], [ss])
                                P.op("act", OP("activation", out=ss.ap, in_=ss.ap, func=AF.Sqrt, bias=eps_rms.ap, scale=1.0 / 128), [ss, eps_rms], [ss])
                                P.op("dve", OP("reciprocal", out=ss.ap, in_=ss.ap), [ss], [ss])
                                oo = O.s(ot * 1024 + u * 128, ot * 1024 + u * 128 + 128)
                                P.op("dve", OP("scalar_tensor_tensor", out=oo.ap, in0=od.ap, scalar=ss.ap[:, 0:1], in1=sg1.ap, op0=ALU.mult, op1=ALU.mult), [od, ss, sg1], [oo])
                            else:
                                for c, (a, r) in enumerate(((a0, r0_), (a1, r1_))):
                                    col = 512 + ((u - 4) * 2 + c) * 64
                                    oo = O.s(ot * 1024 + col, ot * 1024 + col + 64)
                                    P.op("dve", OP("tensor_scalar", out=oo.ap, in0=a.ap[:, 0:64], scalar1=r.ap[:, 0:1], scalar2=None, op0=ALU.mult), [a, r], [oo])
                else:
                    for c in range(2):
                        r0 = 64 * c

                        def l1_a(t, c=c, r0=r0, u=u):
                            if u < 4:
                                h = 2 * u + c
                                kts = list(range(t, t + 5)) + [20, 21]
                                nb = 5
                                bt = nbb.get()
                                P.dma("sp", bt, nab_d.v(0, 128, (t * 8 + h) * 640, (t * 8 + h) * 640 + 640))
                                voff = c * 65
                                col = 512 + h * 64
                            else:
                                h = (u - 4) * 2 + c
                                kts = list(range(t + 1, t + 4)) + [20, 21]
                                nb = 3
                                bt = wm.s(t * 384, t * 384 + 384)
                                voff = 0
                                col = h * 64
                            nk = len(kts)
                            sreg = PS.v(0, 128, (t % 2) * 1024, (t % 2) * 1024 + nk * 128)
                            qs = qt_.ap[r0:r0 + 64, 256 + t * 128:256 + t * 128 + 128]
                            for i, kt in enumerate(kts):
                                P.op("pe", OP("matmul", sreg.ap[:, i * 128:i * 128 + 128], lhsT=kt_.ap[r0:r0 + 64, kt * 128:kt * 128 + 128], rhs=qs, start=True, stop=True),
                                     [kt_, qt_], [sreg], sig=(i == nk - 1))
                            pt = ptile.get().s(0, nk * 128)
                            tm = stmp.get().s(0, nb * 128)
                            P.op("dve", OP("scalar_tensor_tensor", out=tm.ap, in0=sreg.ap[:, 0:nb * 128], scalar=0.125, in1=bt.ap[:, 0:nb * 128], op0=ALU.mult, op1=ALU.add), [sreg, bt], [tm])
                            P.op("act", OP("activation", out=pt.ap[:, 0:nb * 128], in_=tm.ap, func=AF.Exp), [tm], [pt])
                            P.op("act", OP("activation", out=pt.ap[:, nb * 128:nk * 128], in_=sreg.ap[:, nb * 128:nk * 128], func=AF.Exp, scale=0.125), [sreg], [pt])
                            return (t, h, kts, voff, col, pt)

                        def l1_b(stt, u=u):
                            t, h, kts, voff, col, pt = stt
                            nk = len(kts)
                            a = bank(4 + (t % 2), 65)
                            for i, kt in enumerate(kts):
                                P.op("pe", OP("matmul", a.ap, lhsT=pt.ap[:, i * 128:i * 128 + 128], rhs=vv.ap[:, kt * vcw + voff:kt * vcw + voff + 65], start=(i == 0), stop=(i == nk - 1)),
                                     [pt, vv], [a], sig=(i == nk - 1))
                            r = rz.get()
                            if u >= 4:
                                P.op("dve", OP("tensor_scalar", out=r.ap, in0=a.ap[:, 64:65], scalar1=esink.ap[:, h:h + 1], scalar2=None, op0=ALU.add), [a, esink], [r])
                                P.op("dve", OP("reciprocal", out=r.ap, in_=r.ap), [r], [r])
                            else:
                                P.op("dve", OP("reciprocal", out=r.ap, in_=a.ap[:, 64:65]), [a], [r])
                            oo = O.s(t * 1024 + col, t * 1024 + col + 64)
                            P.op("dve", OP("tensor_scalar", out=oo.ap, in0=a.ap[:, 0:64], scalar1=r.ap[:, 0:1], scalar2=None, op0=ALU.mult), [a, r], [oo])

                        prev = None
                        for t in range(16):
                            cur = l1_a(t)
                            if prev is not None:
                                l1_b(prev)
                            prev = cur
                        l1_b(prev)

            AR.top = free_top
            wo = AR.bf(8 * 1024)
            P.dma("pool", v3(wo, 8), dchunks(wo_d[li], 0, 1024))
            oTr = Rot([AR.bf(8 * 512) for _ in range(2)])
            lntmp = {"sq": Rot([AR.f32(512) for _ in range(2)]), "mean": AR.f32(512), "msq": AR.f32(512), "rstd": AR.f32(512)}
            if ab:
                blocks = [(t0, 512, xeT, t0, ML[0]) for t0 in range(0, NEXT, 512)] + [(NEXT, 256, cxT, 0, MC[0])]
            else:
                blocks = [(t0, 512, h1T, 256 + t0, ML[1]) for t0 in range(0, NOWN, 512)]
            pb3 = Rot([0, 1, 2, 3, 4, 5])
            for (t0, nt, src, c0, M) in blocks:
                for c in range(8):
                    P.dma("sp", Hc(c, t0, t0 + nt), src.v(c * 128, c * 128 + 128, c0, c0 + nt))
                oT = oTr.get().s(0, 8 * nt)
                for ch in range(8):
                    b = pb3.get()
                    pst = bank_bf(b, nt)
                    for tt in range(nt // 128):
                        tile = t0 // 128 + tt
                        oin = O.s(tile * 1024 + ch * 128, tile * 1024 + ch * 128 + 128)
                        P.op("pe", OP("transpose", out=pst.ap[:, tt * 128:tt * 128 + 128], in_=oin.ap, identity=identb.ap),
                             [oin, identb], [pst], sig=(tt == nt // 128 - 1))
                    od_ = oT.s(ch * nt, ch * nt + nt)
                    if ch % 2 == 0:
                        P.op("act", OP("activation", out=od_.ap, in_=pst.ap, func=AF.Copy), [pst], [od_])
                    else:
                        P.op("dve", OP("tensor_copy", out=od_.ap, in_=pst.ap), [pst], [od_])
                for dc in range(8):
                    pp = bank(pb3.get(), nt)
                    for ch in range(8):
                        P.op("pe", OP("matmul", pp.ap, lhsT=wo.ap[:, ch * 1024 + dc * 128:ch * 1024 + dc * 128 + 128],
                                                                                    rhs=oT.ap[:, ch * nt:ch * nt + nt], start=(ch == 0), stop=(ch == 7)),
                             [wo, oT], [pp], sig=(ch == 7))
                    hc = Hc(dc, t0, t0 + nt)
                    P.op("act", OP("activation", out=hc.ap, in_=hc.ap, func=AF.Copy, scale=ALPHA), [hc], [hc])
                    P.op("dve", OP("scalar_tensor_tensor", out=hc.ap, in0=pp.ap, scalar=M.ap[:, 16 + dc:17 + dc], in1=hc.ap, op0=ALU.mult, op1=ALU.add),
                         [pp, M, hc], [hc])
                ln_block(lambda c: Hc(c, t0, t0 + nt), nt, li, 0, lambda c: Hc(c, t0, t0 + nt), lntmp)

            AR.top = free_top
            WT = AR.f32(NT, P=32)
            moe_top = AR.top
            ftmp = AR.f32(1024)
            Lg = Rot([AR.f32(36) for _ in range(2)])
            sm = Rot([AR.f32(8) for _ in range(24)])
            em_r = Rot([AR.f32(32) for _ in range(2)])
            wt_r = Rot([AR.f32(32) for _ in range(2)])
            w2t_r = Rot([AR.f32(32) for _ in range(2)])
            pb4 = Rot([0, 1, 2, 3, 4, 5, 6, 7])
            mblocks = [(t0, min(512, NT - t0)) for t0 in range(0, NT, 512)]

            def Mof(t0):
                return (MC[li] if (ab and t0 >= NEXT) else ML[li])

            for (t0, nt) in mblocks:
                M = Mof(t0)
                for tt in range(nt // 128):
                    tk = t0 + tt * 128
                    for c in range(8):
                        hc = Hc(c, tk, tk + 128)
                        fo = ftmp.s(c * 128, c * 128 + 128)
                        P.op("dve", OP("tensor_scalar", out=fo.ap, in0=hc.ap, scalar1=M.ap[:, 32 + c:33 + c], scalar2=M.ap[:, 24 + c:25 + c],
                                                                                 op0=ALU.mult, op1=ALU.add), [hc, M], [fo])
                    pl = bank(pb4.get(), 36)
                    for c in range(8):
                        fo = ftmp.s(c * 128, c * 128 + 128)
                        P.op("pe", OP("matmul", pl.ap, lhsT=fo.ap, rhs=wr[li].ap[:, c * 36:c * 36 + 36], start=(c == 0), stop=(c == 7)),
                             [fo, wr[li]], [pl], sig=(c == 7))
                    L = Lg.get()
                    P.op("dve", OP("tensor_tensor", out=L.ap, in0=pl.ap, in1=brb[li].ap, op=ALU.add), [pl, brb[li]], [L])
                    gmax, ngmax, gexp, gsum, pen, m8, dd, w1, w2 = [sm.get() for _ in range(9)]
                    em, Wt, W2 = em_r.get(), wt_r.get(), w2t_r.get()
                    P.op("dve", OP("tensor_reduce", out=gmax.ap[:, 0:1], in_=L.ap[:, 0:4], axis=AX.X, op=ALU.max), [L], [gmax])
                    P.op("dve", OP("tensor_scalar", out=ngmax.ap[:, 0:1], in0=gmax.ap[:, 0:1], scalar1=-1.0, scalar2=None, op0=ALU.mult), [gmax], [ngmax])
                    P.op("act", OP("activation", out=gexp.ap[:, 0:4], in_=L.ap[:, 0:4], func=AF.Exp, bias=ngmax.ap[:, 0:1], scale=1.0), [L, ngmax], [gexp])
                    P.op("dve", OP("tensor_reduce", out=gsum.ap[:, 0:1], in_=gexp.ap[:, 0:4], axis=AX.X, op=ALU.add), [gexp], [gsum])
                    P.op("dve", OP("reciprocal", out=gsum.ap[:, 0:1], in_=gsum.ap[:, 0:1]), [gsum], [gsum])
                    P.op("dve", OP("tensor_scalar", out=pen.ap[:, 0:4], in0=L.ap[:, 0:4], scalar1=gmax.ap[:, 0:1], scalar2=1e30, op0=ALU.is_equal, op1=ALU.mult), [L, gmax], [pen])
                    P.op("dve", OP("tensor_scalar", out=pen.ap[:, 0:4], in0=pen.ap[:, 0:4], scalar1=-1e30, scalar2=None, op0=ALU.add), [pen], [pen])
                    for g in range(4):
                        P.op("dve", OP("tensor_scalar", out=em.ap[:, 8 * g:8 * g + 8], in0=L.ap[:, 4 + 8 * g:12 + 8 * g], scalar1=pen.ap[:, g:g + 1], scalar2=None, op0=ALU.add),
                             [L, pen], [em])
                    P.op("dve", OP("max", out=m8.ap, in_=em.ap), [em], [m8])
                    P.op("dve", OP("tensor_tensor", out=dd.ap[:, 0:1], in0=m8.ap[:, 1:2], in1=m8.ap[:, 0:1], op=ALU.subtract), [m8], [dd])
                    P.op("act", OP("activation", out=dd.ap[:, 0:1], in_=dd.ap[:, 0:1], func=AF.Exp), [dd], [dd])
                    P.op("dve", OP("tensor_scalar", out=w1.ap[:, 0:1], in0=dd.ap[:, 0:1], scalar1=1.0, scalar2=None, op0=ALU.add), [dd], [w1])
                    P.op("dve", OP("reciprocal", out=w1.ap[:, 0:1], in_=w1.ap[:, 0:1]), [w1], [w1])
                    P.op("dve", OP("tensor_tensor", out=w1.ap[:, 0:1], in0=w1.ap[:, 0:1], in1=gsum.ap[:, 0:1], op=ALU.mult), [w1, gsum], [w1])
                    P.op("dve", OP("tensor_tensor", out=w2.ap[:, 0:1], in0=w1.ap[:, 0:1], in1=dd.ap[:, 0:1], op=ALU.mult), [w1, dd], [w2])
                    P.op("dve", OP("tensor_scalar", out=Wt.ap, in0=em.ap, scalar1=m8.ap[:, 0:1], scalar2=w1.ap[:, 0:1], op0=ALU.is_equal, op1=ALU.mult), [em, m8, w1], [Wt])
                    P.op("dve", OP("tensor_scalar", out=W2.ap, in0=em.ap, scalar1=m8.ap[:, 1:2], scalar2=w2.ap[:, 0:1], op0=ALU.is_equal, op1=ALU.mult), [em, m8, w2], [W2])
                    P.op("dve", OP("tensor_tensor", out=Wt.ap, in0=Wt.ap, in1=W2.ap, op=ALU.add), [Wt, W2], [Wt])
                    ptr = bank(pb4.get(), 128, p=32)
                    P.op("pe", OP("transpose", out=ptr.ap, in_=Wt.ap, identity=ident32.ap), [Wt, ident32], [ptr])
                    wts = WT.s(tk, tk + 128)
                    P.op("act", OP("activation", out=wts.ap, in_=ptr.ap, func=AF.Copy), [ptr], [wts])
                for c in range(8):
                    hc = Hc(c, t0, t0 + nt)
                    fo = FT.s(c * NT + t0, c * NT + t0 + nt)
                    P.op("act", OP("activation", out=fo.ap, in_=hc.ap, func=AF.Identity, scale=M.ap[:, 32 + c:33 + c], bias=M.ap[:, 24 + c:25 + c]), [hc, M], [fo])
                    P.op("pool", OP("tensor_scalar", out=hc.ap, in0=hc.ap, scalar1=ALPHA, scalar2=None, op0=ALU.mult), [hc], [hc])

            AR.top = moe_top
            w1b = Rot([AR.bf(8 * 512) for _ in range(2)])
            w3b = Rot([AR.bf(8 * 512) for _ in range(2)])
            w2b = Rot([AR.bf(4 * 1024) for _ in range(1)])
            gb = Rot([AR.bf(4 * 512) for _ in range(2)])
            wbc = Rot([AR.f32(512) for _ in range(2)])
            sb_ = Rot([AR.f32(512) for _ in range(2)])
            Eb = Rot([AR.f32(128, P=32) for _ in range(2)])
            def moe_h(ex, w1e, w3e, E, t0, nt):
                pw = bank(pb4.get(), nt)
                wts = WT.s(t0, t0 + nt)
                P.op("pe", OP("matmul", pw.ap, lhsT=E.ap, rhs=wts.ap, start=True, stop=True), [E, wts], [pw])
                wb = wbc.get().s(0, nt)
                P.op("act", OP("activation", out=wb.ap, in_=pw.ap, func=AF.Copy), [pw], [wb])
                g = gb.get().s(0, 4 * nt)
                for ec in range(4):
                    p1, p3 = bank(pb4.get(), nt), bank(pb4.get(), nt)
                    for c in range(8):
                        fr = FT.s(c * NT + t0, c * NT + t0 + nt)
                        P.op("pe", OP("matmul", p1.ap, lhsT=w1e.ap[:, c * 512 + ec * 128:c * 512 + ec * 128 + 128], rhs=fr.ap, start=(c == 0), stop=(c == 7)),
                             [w1e, fr], [p1], sig=(c == 7))
                    for c in range(8):
                        fr = FT.s(c * NT + t0, c * NT + t0 + nt)
                        P.op("pe", OP("matmul", p3.ap, lhsT=w3e.ap[:, c * 512 + ec * 128:c * 512 + ec * 128 + 128], rhs=fr.ap, start=(c == 0), stop=(c == 7)),
                             [w3e, fr], [p3], sig=(c == 7))
                    sv = sb_.get().s(0, nt)
                    P.op("act", OP("activation", out=sv.ap, in_=p1.ap, func=AF.Silu), [p1], [sv])
                    P.op("dve", OP("tensor_tensor", out=sv.ap, in0=p3.ap, in1=sv.ap, op=ALU.mult), [p3, sv], [sv])
                    gv = g.s(ec * nt, ec * nt + nt)
                    P.op("pool", OP("tensor_tensor", out=gv.ap, in0=sv.ap, in1=wb.ap, op=ALU.mult), [sv, wb], [gv])
                return g

            def moe_y(w2e, g, t0, nt):
                M = Mof(t0)
                for dc in range(8):
                    py = bank(pb4.get(), nt)
                    for ec in range(4):
                        P.op("pe", OP("matmul", py.ap, lhsT=w2e.ap[:, ec * 1024 + dc * 128:ec * 1024 + dc * 128 + 128],
                                      rhs=g.ap[:, ec * nt:ec * nt + nt], start=(ec == 0), stop=(ec == 3)),
                             [w2e, g], [py], sig=(ec == 3))
                    hc = Hc(dc, t0, t0 + nt)
                    P.op("dve", OP("scalar_tensor_tensor", out=hc.ap, in0=py.ap, scalar=M.ap[:, 40 + dc:41 + dc], in1=hc.ap, op0=ALU.mult, op1=ALU.add),
                         [py, M, hc], [hc])

            pend = None
            for ex in range(NEXP):
                w1e, w3e = w1b.get(), w3b.get()
                P.dma("pool", v3(w1e, 8), dchunks(w1_d[li], 0, 512, r0=ex * 1024))
                P.dma("pool", v3(w3e, 8), dchunks(w3_d[li], 0, 512, r0=ex * 1024))
                E = Eb.get()
                P.op("dve", OP("tensor_copy", out=E.ap, in_=ident32.ap[0:32, ex:ex + 1].to_broadcast([32, 128])), [ident32], [E])
                w2e = None
                for (t0, nt) in mblocks:
                    g = moe_h(ex, w1e, w3e, E, t0, nt)
                    if pend is not None:
                        moe_y(*pend)
                    if w2e is None:
                        w2e = w2b.get()
                        P.dma("pool", v3(w2e, 4), dchunks(w2_d[li], 0, 1024, r0=ex * 512, nchunk=4))
                    pend = (w2e, g, t0, nt)
            moe_y(*pend)

            AR.top = moe_top
            lntmp = {"sq": Rot([AR.f32(512) for _ in range(2)]), "mean": AR.f32(512), "msq": AR.f32(512), "rstd": AR.f32(512)}
            for (t0, nt) in mblocks:
                ln_block(lambda c: Hc(c, t0, t0 + nt), nt, li, 1, lambda c: Hc(c, t0, t0 + nt), lntmp)
                for c in range(8):
                    if ab:
                        if t0 < NEXT:
                            dst = h1T.v(c * 128, c * 128 + 128, t0, t0 + nt)
                        else:
                            dst = hc1T.v(c * 128, c * 128 + 128, 0, nt)
                    else:
                        dst = outT.v(c * 128, c * 128 + 128, t0, t0 + nt)
                    P.dma("sp", dst, Hc(c, t0, t0 + nt))

        layer(0)
        layer(1)
        P.finish()
        P.emit()
    return nc


def _swap_cols(w):
    n = w.shape[1]
    idx = np.arange(n).reshape(n // 64, 2, 32)[:, ::-1, :].reshape(n)
    return w[:, idx]


def _rope_tables(pos):
    pos = np.asarray(pos)
    valid = pos >= 0
    p = np.where(valid, pos, 0)
    row = (p // 64).astype(np.float32)
    col = (p % 64).astype(np.float32)
    inv = (np.float32(10000.0) ** (-np.arange(16, dtype=np.float32) / np.float32(16))).astype(np.float32)
    ang = np.concatenate([row[:, None] * inv, col[:, None] * inv], -1).astype(np.float32)
    cos = np.cos(ang).astype(np.float32)
    sin = np.sin(ang).astype(np.float32)
    cos = np.where(valid[:, None], cos, np.float32(1.0))
    sin = np.where(valid[:, None], sin, np.float32(0.0))
    c64 = np.concatenate([cos, cos], 1)
    s64 = np.concatenate([-sin, sin], 1)
    cT = np.concatenate([c64, c64], 1).T
    sT = np.concatenate([s64, s64], 1).T
    return np.ascontiguousarray(cT, dtype=np.float32), np.ascontiguousarray(sT, dtype=np.float32)


def _ext_positions(j):
    pos = 2048 * j - 256 + np.arange(NEXT)
    if j == 0:
        pos[0:256] = 256 + np.arange(256)
    if j == 3:
        pos[2304:2560] = 7680 + np.arange(256)
    return pos


def _first_occurrence(kp):
    seen, out = set(), np.zeros(len(kp), dtype=bool)
    for i, p in enumerate(kp):
        if p not in seen:
            seen.add(p)
            out[i] = True
    return out


def _layer1_tables(j, rpb):
    pos = _ext_positions(j)
    wmask = np.full((128, 16, 3, 128), NEG, dtype=np.float32)
    nab = np.full((128, 16, 8, 5, 128), NEG, dtype=np.float32)
    for t in range(16):
        qp = 2048 * j + 128 * t + np.arange(128)
        kp = pos[128 * (t + 1):128 * (t + 4)]
        first = _first_occurrence(kp)
        ok = (np.abs(qp[None, :] - kp[:, None]) <= 128) & first[:, None]
        m = np.where(ok, np.float32(0.0), np.float32(NEG)).reshape(3, 128, 128)
        wmask[:, t] = m.transpose(1, 0, 2)
        kp = pos[128 * t:128 * (t + 5)]
        first = _first_occurrence(kp)
        qr, qc = qp // 64, qp % 64
        kr, kcl = kp // 64, kp % 64
        r0 = np.clip(qr - 4, 0, 120)
        c0 = np.clip(qc - 8, 0, 48)
        ok = ((kr[:, None] >= r0[None, :]) & (kr[:, None] < r0[None, :] + 8) &
              (kcl[:, None] >= c0[None, :]) & (kcl[:, None] < c0[None, :] + 16) & first[:, None])
        ro = np.clip(kr[:, None] - qr[None, :] + 7, 0, 14)
        co = np.clip(kcl[:, None] - qc[None, :] + 15, 0, 30)
        for h in range(8):
            b = np.where(ok, rpb[h][ro, co], np.float32(NEG)).astype(np.float32).reshape(5, 128, 128)
            nab[:, t, h] = b.transpose(1, 0, 2)
    return wmask.reshape(128, 16 * 384), nab.reshape(128, 16 * 8 * 640)


_NC_CACHE = {}


def kernel(x, c, ctx, c_ctx, mod_w, mod_b, ln_g, ln_b, ab_w_in, ab_w_out, diff_lambda, diff_subln_g, gqa_qk_g,
           cd_w_in, cd_w_out, win_sink, na_rpb, moe_w_group, moe_b_group, moe_w_router, moe_b_router,
           moe_w1, moe_w3, moe_w2):
    f = lambda a: np.ascontiguousarray(np.asarray(a), dtype=np.float32)
    x, c, ctx, c_ctx = f(x), f(c), f(ctx), f(c_ctx)
    mod_w, mod_b, ln_g, ln_b = f(mod_w), f(mod_b), f(ln_g), f(ln_b)
    ab_w_in, ab_w_out, cd_w_in, cd_w_out = f(ab_w_in)[0], f(ab_w_out)[0], f(cd_w_in)[0], f(cd_w_out)[0]
    diff_lambda, diff_subln_g, gqa_qk_g = f(diff_lambda)[0], f(diff_subln_g)[0], f(gqa_qk_g)[0]
    win_sink, na_rpb = f(win_sink)[0], f(na_rpb)[0]
    moe_w1, moe_w3, moe_w2 = f(moe_w1), f(moe_w3), f(moe_w2)

    shared = {}
    shared["ident"] = np.eye(128, dtype=np.float32)
    bo = np.zeros((128, 128), dtype=np.float32)
    bo[:64, :64] = 1.0
    bo[64:, 64:] = 1.0
    shared["bones"] = bo
    for i in range(2):
        shared[f"modw{i}"] = mod_w[i]
        shared[f"modb{i}"] = np.ascontiguousarray(mod_b[i].reshape(48, 128).T)
        shared[f"wr{i}"] = np.ascontiguousarray(np.concatenate([f(moe_w_group)[i], f(moe_w_router)[i]], 1))
        shared[f"br{i}"] = np.concatenate([f(moe_b_group)[i], f(moe_b_router)[i]])[None, :].copy()
        shared[f"w1_{i}"] = moe_w1[i].reshape(NEXP * D, 512)
        shared[f"w3_{i}"] = moe_w3[i].reshape(NEXP * D, 512)
        shared[f"w2_{i}"] = moe_w2[i].reshape(NEXP * 512, D)
    lnT = np.zeros((128, 64), dtype=np.float32)
    for i in range(2):
        for wch in range(2):
            lnT[:, ((i * 2 + wch) * 2 + 0) * 8:((i * 2 + wch) * 2 + 0) * 8 + 8] = ln_g[i, wch].reshape(8, 128).T
            lnT[:, ((i * 2 + wch) * 2 + 1) * 8:((i * 2 + wch) * 2 + 1) * 8 + 8] = ln_b[i, wch].reshape(8, 128).T
    shared["lnT"] = lnT
    q0 = ab_w_in[:, 0:1024]
    kd, vd = ab_w_in[:, 1024:1536], ab_w_in[:, 1536:2048]
    kg, vg = ab_w_in[:, 2048:2176], ab_w_in[:, 2176:2304]
    k0 = np.concatenate([kd, kg[:, 0:64], kg[:, 0:64], kg[:, 64:128], kg[:, 64:128]], 1)
    shared["wq0"], shared["wqs0"] = np.ascontiguousarray(q0), np.ascontiguousarray(_swap_cols(q0))
    shared["wk0"], shared["wks0"] = np.ascontiguousarray(k0), np.ascontiguousarray(_swap_cols(k0))
    shared["wv0"] = np.ascontiguousarray(np.concatenate([vd, vg], 1))
    shared["wo0"] = ab_w_out
    qw, qn = cd_w_in[:, 0:512], cd_w_in[:, 512:1024]
    kw, vw = cd_w_in[:, 1024:1152], cd_w_in[:, 1152:1280]
    kn, vn = cd_w_in[:, 1280:1792], cd_w_in[:, 1792:2304]
    q1 = np.concatenate([qn, qw], 1)
    k1 = np.concatenate([kn, kw[:, 0:64], kw[:, 0:64], kw[:, 64:128], kw[:, 64:128]], 1)
    shared["wq1"], shared["wqs1"] = np.ascontiguousarray(q1), np.ascontiguousarray(_swap_cols(q1))
    shared["wk1"], shared["wks1"] = np.ascontiguousarray(k1), np.ascontiguousarray(_swap_cols(k1))
    shared["wv1"] = np.ascontiguousarray(np.concatenate([vn, vw], 1))
    shared["wo1"] = cd_w_out
    sw = lambda g: np.concatenate([g[32:64], g[0:32]])
    gq, gk = gqa_qk_g[0], gqa_qk_g[1]
    shared["gt"] = np.ascontiguousarray(np.stack([np.tile(gq, 2), np.tile(sw(gq), 2), np.tile(gk, 2), np.tile(sw(gk), 2)], 1))
    shared["dlam"] = diff_lambda.reshape(1, 256).copy()
    shared["subg"] = diff_subln_g.reshape(1, 128).copy()
    shared["sink"] = win_sink.reshape(1, 8).copy()
    ck, sk = _rope_tables(np.concatenate([np.arange(SEQ), -np.ones(NCTX, dtype=np.int64)]))
    shared["cK"], shared["sK"] = ck, sk

    in_maps = []
    tabs = {}
    for core in range(8):
        b, j = core // 4, core % 4
        pos = _ext_positions(j)
        m = dict(shared)
        m["xT"] = np.ascontiguousarray(x[b].T)
        m["xeT"] = np.ascontiguousarray(x[b][pos].T)
        m["cxT"] = np.ascontiguousarray(ctx[b].T)
        m["ccT"] = np.ascontiguousarray(np.stack([c[b], c_ctx], 1))
        if j not in tabs:
            cq, sq = _rope_tables(np.concatenate([pos, -np.ones(NCTX, dtype=np.int64)]))
            wmask, nab = _layer1_tables(j, na_rpb)
            tabs[j] = (cq, sq, wmask, nab)
        m["cQ"], m["sQ"], m["wmask"], m["nab"] = tabs[j]
        in_maps.append(m)

    if "nc" not in _NC_CACHE:
        _NC_CACHE["nc"] = build_program()
    res = run_bass_kernel_spmd(_NC_CACHE["nc"], in_maps, core_ids=list(range(8)))
    out = np.empty((2, SEQ, D), dtype=np.float32)
    for core in range(8):
        b, j = core // 4, core % 4
        out[b, 2048 * j:2048 * (j + 1), :] = np.asarray(res.results[core]["outT"]).T
    if DEBUG:
        kernel.last = res
    return out
```

```python
import math
from contextlib import ExitStack

import numpy as np
import concourse.bass as bass
import concourse.mybir as mybir
from concourse.bass_utils import run_bass_kernel_spmd

F32 = mybir.dt.float32
BF16 = mybir.dt.bfloat16
AF = mybir.ActivationFunctionType
ALU = mybir.AluOpType
AX = mybir.AxisListType

DEBUG = False

D = 1024
SEQ = 8192
NCTX = 256
NEXT = 2560
NOWN = 2048
NQ = NEXT + NCTX
NK0 = SEQ + NCTX
NK1 = NEXT + NCTX
VW = 650
DEPTH = 2
ALPHA = (2.0 * DEPTH) ** 0.25
LN_EPS = 1e-5
RMS_EPS = 1e-6
LAM_INIT0 = 0.8 - 0.6 * math.exp(0.0)
NEG = -30000.0
NEXP = 32

SEM_LIM = 8000
NDMA = 12
ARENA_WORDS = 53200


class V:
    def __init__(self, ap, tid, p0, p1, f0, f1, wpe):
        self.ap, self.tid, self.p0, self.p1, self.f0, self.f1, self.wpe = ap, tid, p0, p1, f0, f1, wpe

    def reg(self):
        return (self.tid, self.p0, self.p1, self.f0, self.f1)

    def s(self, c0, c1, p0=None, p1=None):
        q0 = 0 if p0 is None else p0
        q1 = (self.p1 - self.p0) if p1 is None else p1
        return V(self.ap[q0:q1, c0:c1], self.tid, self.p0 + q0, self.p0 + q1,
                 self.f0 + c0 * self.wpe, self.f0 + c1 * self.wpe, self.wpe)


class Buf:
    def __init__(self, t, tid, P, F, wpe):
        self.t, self.tid, self.P, self.F, self.wpe = t, tid, P, F, wpe

    def v(self, p0=0, p1=None, f0=0, f1=None):
        p1 = self.P if p1 is None else p1
        f1 = self.F if f1 is None else f1
        return V(self.t[p0:p1, f0:f1], self.tid, p0, p1, f0 * self.wpe, f1 * self.wpe, self.wpe)


class Prog:
    ENGS = ["pe", "act", "dve", "pool", "sp"]

    def __init__(self, nc, stack):
        self.nc, self.stack = nc, stack
        self.recs = {e: [] for e in self.ENGS}
        self.sig = {e: 0 for e in self.ENGS}
        self.known = {e: {} for e in self.ENGS}
        self.csems = {e: [] for e in self.ENGS}
        self.dsems, self.dcnt, self.drr = {}, {}, {}
        for q in ["sp", "act", "pool"]:
            self.dsems[q] = [stack.enter_context(nc.semaphore(f"d_{q}_{k}")) for k in range(NDMA)]
            self.dcnt[q] = [0] * NDMA
            self.drr[q] = 0
        self.ent = {}
        self.ntid = 0

    def new_tid(self):
        self.ntid += 1
        return self.ntid

    def dram(self, name, P, F, dtype, kind):
        t = self.nc.dram_tensor(name, [P, F], dtype, kind=kind)
        return Buf(t, self.new_tid(), P, F, 1.0 if dtype == F32 else 0.5)

    def _csem(self, e, idx):
        while len(self.csems[e]) <= idx:
            self.csems[e].append(self.stack.enter_context(self.nc.semaphore(f"c_{e}_{len(self.csems[e])}")))
        return self.csems[e][idx]

    @staticmethod
    def _ov(a, b):
        return a[1] < b[2] and b[1] < a[2] and a[3] < b[4] and b[3] < a[4]

    @staticmethod
    def _cov(a, b):
        return a[1] <= b[1] and a[2] >= b[2] and a[3] <= b[3] and a[4] >= b[4]

    BUCK = 256

    def _cands(self, r):
        d = self.ent.get(r[0])
        if not d:
            return []
        seen, out = set(), []
        for b in range(int(r[3]) // self.BUCK, int(math.ceil(r[4])) // self.BUCK + 1):
            for en in d.get(b, ()):
                if en[3] and id(en) not in seen:
                    seen.add(id(en))
                    out.append(en)
        return out

    def _add(self, en):
        r = en[0]
        d = self.ent.setdefault(r[0], {})
        for b in range(int(r[3]) // self.BUCK, int(math.ceil(r[4])) // self.BUCK + 1):
            lst = d.setdefault(b, [])
            if len(lst) > 64:
                lst[:] = [x for x in lst if x[3]]
            lst.append(en)

    def _deps(self, reads, writes):
        raw, other = [], []
        for r in reads:
            for en in self._cands(r):
                if en[1] is not None and self._ov(en[0], r):
                    raw.append(en[1])
        for w in writes:
            for en in self._cands(w):
                if self._ov(en[0], w):
                    if en[1] is not None:
                        other.append(en[1])
                    other.extend(en[2])
        return raw, other

    def _update(self, tok, reads, writes):
        for r in reads:
            hit = False
            for en in self._cands(r):
                if self._ov(en[0], r):
                    if tok[0] == "c":
                        en[2][:] = [t for t in en[2] if not (t[0] == "c" and t[1] == tok[1])]
                    en[2].append(tok)
                    if self._cov(en[0], r):
                        hit = True
            if not hit:
                self._add([r, None, [tok], True])
        for w in writes:
            for en in self._cands(w):
                if self._cov(w, en[0]):
                    en[3] = False
            self._add([w, tok, [], True])

    def _waits(self, e, raw, other):
        waits = []
        kn = self.known[e]
        for kind, toks in (("raw", raw), ("oth", other)):
            for t in toks:
                if t[0] == "c":
                    _, f, v = t
                    if f == e and e == "pe":
                        continue
                    if kn.get(f, 0) >= v:
                        continue
                    kn[f] = v
                    waits.append(t)
                else:
                    _, q, k, c = t
                    if kn.get((q, k), 0) >= c:
                        continue
                    kn[(q, k)] = c
                    waits.append(t)
        return waits

    def op(self, e, fn, reads=(), writes=(), sig=True):
        reads = [r.reg() for r in reads]
        writes = [w.reg() for w in writes]
        raw, other = self._deps(reads, writes)
        waits = self._waits(e, raw, other)
        if sig:
            self.sig[e] += 1
            v = self.sig[e]
        else:
            v = self.sig[e] + 1
        tok = ("c", e, v)
        self._update(tok, reads, writes)
        self.recs[e].append((waits, fn, tok if sig else None))
        return tok

    def dma(self, q, out, in_, **kw):
        reads, writes = [in_.reg()], [out.reg()]
        raw, other = self._deps(reads, writes)
        k = self.drr[q]
        self.drr[q] = (k + 1) % NDMA
        prev = self.dcnt[q][k]
        if prev > 0:
            other = other + [("d", q, k, prev)]
        waits = self._waits(q, raw, other)
        self.dcnt[q][k] = prev + 1
        tok = ("d", q, k, prev + 1)
        self._update(tok, reads, writes)
        oa, ia = out.ap, in_.ap

        def fn(eng, oa=oa, ia=ia, kw=kw):
            return eng.dma_start(out=oa, in_=ia, **kw)

        self.recs[q].append((waits, fn, tok))
        return tok

    def finish(self):
        waits = []
        for q in self.dsems:
            for k in range(NDMA):
                if self.dcnt[q][k] > 0:
                    waits.append(("d", q, k, self.dcnt[q][k]))
        for e in ["pe", "act", "dve", "pool"]:
            if self.sig[e] > 0:
                waits.append(("c", e, self.sig[e]))
        self.recs["sp"].append((waits, None, None))

    def _emit_wait(self, eng, w):
        if w[0] == "c":
            _, f, v = w
            eng.wait_ge(self._csem(f, (v - 1) // SEM_LIM), (v - 1) % SEM_LIM + 1)
        else:
            _, q, k, c = w
            eng.wait_ge(self.dsems[q][k], 16 * c)

    def emit(self):
        nc = self.nc
        for e in self.ENGS:
            for idx in range((self.sig[e] + SEM_LIM - 1) // SEM_LIM + 1):
                self._csem(e, idx)
        recs, me = self.recs, self

        def play(e, eng):
            for waits, fn, tok in recs[e]:
                for w in waits:
                    me._emit_wait(eng, w)
                if fn is None:
                    continue
                ins = fn(eng)
                if tok is not None:
                    if tok[0] == "c":
                        ins.then_inc(me._csem(e, (tok[2] - 1) // SEM_LIM), 1)
                    else:
                        ins.then_inc(me.dsems[tok[1]][tok[2]], 16)

        with nc.Block() as block:
            @block.tensor
            def _(eng):
                play("pe", eng)

            @block.scalar
            def _(eng):
                play("act", eng)

            @block.vector
            def _(eng):
                play("dve", eng)

            @block.gpsimd
            def _(eng):
                play("pool", eng)

            @block.sync
            def _(eng):
                play("sp", eng)


class Arena:
    def __init__(self, buf):
        self.buf, self.top, self.hi = buf, 0, 0

    def _alloc(self, words):
        off = self.top
        self.top += (words + 7) // 8 * 8
        self.hi = max(self.hi, self.top)
        assert self.top <= self.buf.F, f"arena overflow {self.top}"
        return off

    def f32(self, n, P=128):
        off = self._alloc(n)
        return V(self.buf.t[0:P, off:off + n], self.buf.tid, 0, P, off, off + n, 1.0)

    def bf(self, n, P=128):
        w = (n + 1) // 2
        off = self._alloc(w)
        return V(self.buf.t[0:P, off:off + w].bitcast(BF16), self.buf.tid, 0, P, off, off + w, 0.5)


class Rot:
    def __init__(self, items):
        self.items, self.i = items, 0

    def get(self):
        it = self.items[self.i % len(self.items)]
        self.i += 1
        return it


def r3(ap, k):
    return ap.rearrange("p (k n) -> p k n", k=k)


def OP(name, *a, **k):
    return lambda e: getattr(e, name)(*a, **k)


def build_program():
    nc = bass.Bass("TRN2", target_bir_lowering=False)
    st = ExitStack()
    with st:
        P = Prog(nc, st)
        I = lambda name, p, f, dt=F32: P.dram(name, p, f, dt, "ExternalInput")
        xT = I("xT", D, SEQ)
        xeT = I("xeT", D, NEXT)
        cxT = I("cxT", D, NCTX)
        ccT = I("ccT", D, 2)
        ident_d = I("ident", 128, 128)
        bones_d = I("bones", 128, 128)
        modw = [I(f"modw{i}", D, 6 * D) for i in range(2)]
        modb = [I(f"modb{i}", 128, 48) for i in range(2)]
        lnT_d = I("lnT", 128, 64)
        wq_d = [I(f"wq{i}", D, 1024) for i in range(2)]
        wqs_d = [I(f"wqs{i}", D, 1024) for i in range(2)]
        wk_d = [I(f"wk{i}", D, 768) for i in range(2)]
        wks_d = [I(f"wks{i}", D, 768) for i in range(2)]
        wv_d = [I(f"wv{i}", D, 640) for i in range(2)]
        wo_d = [I(f"wo{i}", D, 1024) for i in range(2)]
        gt_d = I("gt", 128, 4)
        dlam_d = I("dlam", 1, 256)
        subg_d = I("subg", 1, 128)
        sink_d = I("sink", 1, 8)
        cK_d = I("cK", 128, NK0)
        sK_d = I("sK", 128, NK0)
        cQ_d = I("cQ", 128, NQ)
        sQ_d = I("sQ", 128, NQ)
        wr_d = [I(f"wr{i}", D, 36) for i in range(2)]
        br_d = [I(f"br{i}", 1, 36) for i in range(2)]
        w1_d = [I(f"w1_{i}", NEXP * D, 512) for i in range(2)]
        w3_d = [I(f"w3_{i}", NEXP * D, 512) for i in range(2)]
        w2_d = [I(f"w2_{i}", NEXP * 512, D) for i in range(2)]
        wmask_d = I("wmask", 128, 16 * 384)
        nab_d = I("nab", 128, 16 * 8 * 640)
        outT = P.dram("outT", D, NOWN, F32, "ExternalOutput")
        skind = "ExternalOutput" if DEBUG else "Internal"
        QT = P.dram("QT", 1024, NQ, BF16, skind)
        KT = P.dram("KT", 768, NK0, BF16, skind)
        VA = P.dram("VA", 128, 66 * VW, BF16, skind)
        h1T = P.dram("h1T", D, NEXT, F32, skind)
        hc1T = P.dram("hc1T", D, NCTX, F32, skind)

        arena_t = st.enter_context(nc.sbuf_tensor("arena", [128, ARENA_WORDS], F32))
        AR = Arena(Buf(arena_t, P.new_tid(), 128, ARENA_WORDS, 1.0))
        psum_t = st.enter_context(nc.psum_tensor("psum", [128, 4096], F32))
        PS = Buf(psum_t, P.new_tid(), 128, 4096, 1.0)

        def bank(b, n=512, p=128):
            return PS.v(0, p, b * 512, b * 512 + n)

        def bank_bf(b, n):
            return V(PS.t[:, b * 512:b * 512 + n // 2].bitcast(BF16), PS.tid, 0, 128, b * 512, b * 512 + n // 2, 0.5)

        def dchunks(buf, c0, c1, r0=0, nchunk=8):
            ap = buf.t[r0:r0 + nchunk * 128, c0:c1].rearrange("(c p) n -> p c n", p=128)
            return V(ap, buf.tid, r0, r0 + nchunk * 128, c0 * buf.wpe, c1 * buf.wpe, buf.wpe)

        def v3(v, k):
            return V(r3(v.ap, k), v.tid, v.p0, v.p1, v.f0, v.f1, v.wpe)

        def bcast(buf, n):
            return V(buf.t[0:1, 0:n].partition_broadcast(128), buf.tid, 0, 1, 0, n * buf.wpe, buf.wpe)

        ident32 = AR.f32(128)
        identb = AR.bf(128)
        ones = AR.f32(128)
        bones = AR.f32(128)
        lnT = AR.f32(64)
        gt = AR.f32(4)
        dl = AR.f32(256)
        sg1 = AR.f32(128)
        esink = AR.f32(8)
        eps_ln = AR.f32(1)
        eps_rms = AR.f32(1)
        nlam = AR.f32(1)
        ML = [AR.f32(48) for _ in range(2)]
        MC = [AR.f32(48) for _ in range(2)]
        sc = AR.f32(16)
        wr = [AR.f32(8 * 36) for _ in range(2)]
        brb = [AR.f32(36) for _ in range(2)]
        P.dma("sp", ident32, ident_d.v())
        P.dma("sp", bones, bones_d.v())
        P.dma("sp", lnT, lnT_d.v())
        P.dma("sp", gt, gt_d.v())
        P.dma("sp", dl, bcast(dlam_d, 256))
        P.dma("sp", sg1, bcast(subg_d, 128))
        P.dma("sp", esink, bcast(sink_d, 8))
        for i in range(2):
            P.dma("sp", v3(wr[i], 8), dchunks(wr_d[i], 0, 36))
            P.dma("sp", brb[i], bcast(br_d[i], 36))
        P.op("dve", OP("tensor_copy", out=identb.ap, in_=ident32.ap), [ident32], [identb])
        P.op("dve", OP("memset", ones.ap, 1.0), [], [ones])
        P.op("dve", OP("memset", eps_ln.ap, LN_EPS), [], [eps_ln])
        P.op("dve", OP("memset", eps_rms.ap, RMS_EPS), [], [eps_rms])
        P.op("act", OP("activation", out=esink.ap, in_=esink.ap, func=AF.Exp), [esink], [esink])
        P.op("dve", OP("tensor_scalar", out=sg1.ap, in0=sg1.ap, scalar1=1.0 - LAM_INIT0, scalar2=None, op0=ALU.mult), [sg1], [sg1])
        lt = AR.f32(128)
        ls = AR.f32(2)
        dl4 = dl.ap.rearrange("p (a b n) -> p a b n", a=2, b=2)
        P.op("dve", OP("tensor_tensor", out=lt.ap.rearrange("p (a n) -> p a n", a=2), in0=dl4[:, :, 0, :], in1=dl4[:, :, 1, :], op=ALU.mult), [dl], [lt])
        P.op("dve", OP("tensor_reduce", out=ls.ap, in_=lt.ap.rearrange("p (a n) -> p a n", a=2), axis=AX.X, op=ALU.add), [lt], [ls])
        P.op("act", OP("activation", out=ls.ap, in_=ls.ap, func=AF.Exp), [ls], [ls])
        P.op("dve", OP("tensor_tensor", out=nlam.ap, in0=ls.ap[:, 1:2], in1=ls.ap[:, 0:1], op=ALU.subtract), [ls], [nlam])
        P.op("dve", OP("tensor_scalar", out=nlam.ap, in0=nlam.ap, scalar1=-LAM_INIT0, scalar2=None, op0=ALU.add), [nlam], [nlam])

        base_top = AR.top
        P.dma("sp", v3(sc, 8), dchunks(ccT, 0, 2))
        P.op("act", OP("activation", out=sc.ap, in_=sc.ap, func=AF.Silu), [sc], [sc])
        mwb = [AR.f32(8 * 512) for _ in range(2)]
        mbt = AR.f32(48)
        for i in range(2):
            P.dma("sp", mbt, modb[i].v())
            pm = bank(6, 96)
            for pc in range(12):
                w = mwb[pc % 2]
                P.dma("sp", v3(w, 8), dchunks(modw[i], pc * 512, pc * 512 + 512))
                for j in range(4):
                    cc = pc * 4 + j
                    for dc in range(8):
                        P.op("pe", OP("matmul",
                            pm.ap[:, cc * 2:cc * 2 + 2], lhsT=w.ap[:, dc * 512 + j * 128: dc * 512 + j * 128 + 128],
                            rhs=sc.ap[:, dc * 2:dc * 2 + 2], start=(dc == 0), stop=(dc == 7)),
                            [w, sc], [pm], sig=(dc == 7))
            pm3 = pm.ap.rearrange("p (c n) -> p c n", n=2)
            P.op("dve", OP("tensor_tensor", out=ML[i].ap, in0=pm3[:, :, 0], in1=mbt.ap, op=ALU.add), [pm, mbt], [ML[i]])
            P.op("dve", OP("tensor_tensor", out=MC[i].ap, in0=pm3[:, :, 1], in1=mbt.ap, op=ALU.add), [pm, mbt], [MC[i]])
            for M in (ML[i], MC[i]):
                for k in (1, 4):
                    P.op("dve", OP("tensor_scalar", out=M.ap[:, k * 8:k * 8 + 8], in0=M.ap[:, k * 8:k * 8 + 8],
                                                                    scalar1=1.0, scalar2=None, op0=ALU.add), [M], [M])
        AR.top = base_top

        FT = AR.bf(8 * NQ)
        H = AR.f32(8 * NQ)
        free_top = AR.top

        def ln_block(ucf, nt, li, which, out_fn, tmp):
            s1, s2 = bank(6, nt), bank(7, nt)
            for c in range(8):
                uc = ucf(c)
                P.op("pe", OP("matmul", s1.ap, lhsT=ones.ap, rhs=uc.ap, start=(c == 0), stop=(c == 7)),
                     [ones, uc], [s1], sig=(c == 7))
            for c in range(8):
                uc = ucf(c)
                q = tmp["sq"].get().s(0, nt)
                P.op("act", OP("activation", out=q.ap, in_=uc.ap, func=AF.Square), [uc], [q])
                P.op("pe", OP("matmul", s2.ap, lhsT=ones.ap, rhs=q.ap, start=(c == 0), stop=(c == 7)),
                     [ones, q], [s2], sig=True)
            mean, msq, rstd = tmp["mean"].s(0, nt), tmp["msq"].s(0, nt), tmp["rstd"].s(0, nt)
            P.op("act", OP("activation", out=mean.ap, in_=s1.ap, func=AF.Copy, scale=1.0 / D), [s1], [mean])
            P.op("act", OP("activation", out=msq.ap, in_=s1.ap, func=AF.Square, scale=1.0 / D), [s1], [msq])
            P.op("dve", OP("scalar_tensor_tensor", out=rstd.ap, in0=s2.ap, scalar=1.0 / D, in1=msq.ap, op0=ALU.mult, op1=ALU.subtract),
                 [s2, msq], [rstd])
            P.op("act", OP("activation", out=rstd.ap, in_=rstd.ap, func=AF.Sqrt, bias=eps_ln.ap, scale=1.0), [rstd, eps_ln], [rstd])
            P.op("dve", OP("reciprocal", out=rstd.ap, in_=rstd.ap), [rstd], [rstd])
            gi = ((li * 2 + which) * 2 + 0) * 8
            bi = ((li * 2 + which) * 2 + 1) * 8
            for c in range(8):
                uc = ucf(c)
                o = out_fn(c)
                P.op("pool", OP("tensor_tensor", out=uc.ap, in0=uc.ap, in1=mean.ap, op=ALU.subtract), [uc, mean], [uc])
                P.op("dve", OP("tensor_tensor", out=uc.ap, in0=uc.ap, in1=rstd.ap, op=ALU.mult), [uc, rstd], [uc])
                P.op("dve", OP("tensor_scalar", out=o.ap, in0=uc.ap, scalar1=lnT.ap[:, gi + c:gi + c + 1],
                                                                      scalar2=lnT.ap[:, bi + c:bi + c + 1], op0=ALU.mult, op1=ALU.add),
                     [uc, lnT], [o])

        def layer(li):
            ab = (li == 0)
            NT = NQ if ab else NOWN

            def Hc(c, t0, t1):
                return H.s(c * NT + t0, c * NT + t1)

            AR.top = FT.f0 if False else int(FT.f0)
            wq, wqs, wk, wks, wv = AR.bf(8 * 1024), AR.bf(8 * 1024), AR.bf(8 * 768), AR.bf(8 * 768), AR.bf(8 * 640)
            for dst, src, n in ((wq, wq_d[li], 1024), (wqs, wqs_d[li], 1024), (wk, wk_d[li], 768), (wks, wks_d[li], 768), (wv, wv_d[li], 640)):
                P.dma("pool", v3(dst, 8), dchunks(src, 0, n))
            xb = Rot([AR.f32(8 * 512) for _ in range(2)])
            abuf = Rot([AR.bf(8 * 512) for _ in range(2)])
            ctab = Rot([AR.f32(512) for _ in range(2)])
            stab = Rot([AR.f32(512) for _ in range(2)])
            t1r = Rot([AR.f32(512) for _ in range(2)])
            t2r = Rot([AR.f32(512) for _ in range(2)])
            obr = Rot([AR.bf(512) for _ in range(3)])
            sqr = Rot([AR.f32(512) for _ in range(2)])
            rvr = Rot([AR.f32(512) for _ in range(2)])
            vst = Rot([AR.bf(VW) for _ in range(2)])
            for vs in vst.items:
                P.op("pool", OP("memset", vs.ap, 1.0), [], [vs])
            pbank = Rot([0, 1, 2, 3, 4, 5])
            if ab:
                q_rope, q_norm = [True] * 8, [False] * 4 + [True] * 4
                k_rope, k_norm = [True] * 6, [False] * 4 + [True] * 2
            else:
                q_rope, q_norm = [False] * 4 + [True] * 4, [False] * 8
                k_rope, k_norm = [False] * 4 + [True] * 2, [False] * 6

            def fm_proj(aT, nt, W, Ws, wn, ch, rope, norm, gcol, ct, stb, dst):
                pa = bank(pbank.get(), nt)
                for c in range(8):
                    P.op("pe", OP("matmul", pa.ap, lhsT=W.ap[:, c * wn + ch * 128: c * wn + ch * 128 + 128],
                                                      rhs=aT.ap[:, c * nt:c * nt + nt], start=(c == 0), stop=(c == 7)),
                         [W, aT], [pa], sig=(c == 7))
                ob = obr.get().s(0, nt)
                if not rope:
                    P.op("act", OP("activation", out=ob.ap, in_=pa.ap, func=AF.Copy), [pa], [ob])
                else:
                    pb = bank(pbank.get(), nt)
                    for c in range(8):
                        P.op("pe", OP("matmul", pb.ap, lhsT=Ws.ap[:, c * wn + ch * 128: c * wn + ch * 128 + 128],
                                                          rhs=aT.ap[:, c * nt:c * nt + nt], start=(c == 0), stop=(c == 7)),
                             [Ws, aT], [pb], sig=(c == 7))
                    t1, t2 = t1r.get().s(0, nt), t2r.get().s(0, nt)
                    if norm:
                        sq = sqr.get().s(0, nt)
                        P.op("act", OP("activation", out=sq.ap, in_=pa.ap, func=AF.Square), [pa], [sq])
                        pss = bank(pbank.get(), nt)
                        P.op("pe", OP("matmul", pss.ap, lhsT=bones.ap, rhs=sq.ap, start=True, stop=True), [bones, sq], [pss])
                        rv = rvr.get().s(0, nt)
                        P.op("act", OP("activation", out=rv.ap, in_=pss.ap, func=AF.Sqrt, bias=eps_rms.ap, scale=1.0 / 64), [pss, eps_rms], [rv])
                        P.op("dve", OP("reciprocal", out=rv.ap, in_=rv.ap), [rv], [rv])
                        P.op("dve", OP("scalar_tensor_tensor", out=t1.ap, in0=pa.ap, scalar=gt.ap[:, gcol:gcol + 1], in1=ct.ap, op0=ALU.mult, op1=ALU.mult),
                             [pa, gt, ct], [t1])
                        P.op("dve", OP("scalar_tensor_tensor", out=t2.ap, in0=pb.ap, scalar=gt.ap[:, gcol + 1:gcol + 2], in1=stb.ap, op0=ALU.mult, op1=ALU.mult),
                             [pb, gt, stb], [t2])
                        P.op("pool", OP("tensor_tensor", out=t1.ap, in0=t1.ap, in1=t2.ap, op=ALU.add), [t1, t2], [t1])
                        P.op("dve", OP("tensor_tensor", out=ob.ap, in0=t1.ap, in1=rv.ap, op=ALU.mult), [t1, rv], [ob])
                    else:
                        P.op("dve", OP("tensor_tensor", out=t1.ap, in0=pa.ap, in1=ct.ap, op=ALU.mult), [pa, ct], [t1])
                        P.op("dve", OP("tensor_tensor", out=t2.ap, in0=pb.ap, in1=stb.ap, op=ALU.mult), [pb, stb], [t2])
                        P.op("pool", OP("tensor_tensor", out=ob.ap, in0=t1.ap, in1=t2.ap, op=ALU.add), [t1, t2], [ob])
                P.dma("sp", dst, ob)

            def source(src, ntok, M, ctd, std, toff, want_q, q_off, want_kv, k_off):
                def load(t0):
                    nt = min(512, ntok - t0)
                    x = xb.get().s(0, 8 * nt)
                    P.dma("sp", v3(x, 8), dchunks(src, t0, t0 + nt))
                    ct, stb = ctab.get().s(0, nt), stab.get().s(0, nt)
                    P.dma("sp", ct, ctd.v(0, 128, toff + t0, toff + t0 + nt))
                    P.dma("sp", stb, std.v(0, 128, toff + t0, toff + t0 + nt))
                    return (x, ct, stb)

                nxt = load(0)
                for t0 in range(0, ntok, 512):
                    nt = min(512, ntok - t0)
                    x, ct, stb = nxt
                    if t0 + 512 < ntok:
                        nxt = load(t0 + 512)
                    aT = abuf.get().s(0, 8 * nt)
                    for c in range(8):
                        P.op("act", OP("activation", out=aT.ap[:, c * nt:c * nt + nt], in_=x.ap[:, c * nt:c * nt + nt], func=AF.Identity,
                                       scale=M.ap[:, 8 + c:9 + c], bias=M.ap[:, c:c + 1]), [x, M], [aT])
                    if want_kv:
                        for ch in range(6):
                            fm_proj(aT, nt, wk, wks, 768, ch, k_rope[ch], k_norm[ch], 2, ct, stb,
                                    KT.v(ch * 128, ch * 128 + 128, k_off + t0, k_off + t0 + nt))
                        for tt in range(nt // 128):
                            pv1, pv2 = bank(pbank.get(), 512), bank(pbank.get(), 128)
                            for c in range(8):
                                lh = aT.ap[:, c * nt + tt * 128:c * nt + tt * 128 + 128]
                                P.op("pe", OP("matmul", pv1.ap, lhsT=lh, rhs=wv.ap[:, c * 640:c * 640 + 512], start=(c == 0), stop=(c == 7)),
                                     [aT, wv], [pv1], sig=(c == 7))
                            for c in range(8):
                                lh = aT.ap[:, c * nt + tt * 128:c * nt + tt * 128 + 128]
                                P.op("pe", OP("matmul", pv2.ap, lhsT=lh, rhs=wv.ap[:, c * 640 + 512:c * 640 + 640], start=(c == 0), stop=(c == 7)),
                                     [aT, wv], [pv2], sig=(c == 7))
                            vs = vst.get()
                            if ab:
                                o1 = vs.ap[:, 0:516].rearrange("p (h n) -> p h n", n=129)[:, :, 0:128]
                                i1 = pv1.ap.rearrange("p (h n) -> p h n", n=128)
                                o2 = vs.ap[:, 516:646].rearrange("p (h n) -> p h n", n=65)[:, :, 0:64]
                            else:
                                o1 = vs.ap[:, 0:520].rearrange("p (h n) -> p h n", n=65)[:, :, 0:64]
                                i1 = pv1.ap.rearrange("p (h n) -> p h n", n=64)
                                o2 = vs.ap[:, 520:650].rearrange("p (h n) -> p h n", n=65)[:, :, 0:64]
                            i2 = pv2.ap.rearrange("p (h n) -> p h n", n=64)
                            P.op("act", OP("activation", out=o1, in_=i1, func=AF.Copy), [pv1], [vs])
                            P.op("dve", OP("tensor_copy", out=o2, in_=i2), [pv2], [vs])
                            kt = (k_off + t0) // 128 + tt
                            P.dma("sp", VA.v(0, 128, kt * VW, kt * VW + VW), vs)
                    if want_q:
                        for ch in range(8):
                            fm_proj(aT, nt, wq, wqs, 1024, ch, q_rope[ch], q_norm[ch], 0, ct, stb,
                                    QT.v(ch * 128, ch * 128 + 128, q_off + t0, q_off + t0 + nt))

            if ab:
                source(xT, SEQ, ML[0], cK_d, sK_d, 0, False, 0, True, 0)
                source(cxT, NCTX, MC[0], cK_d, sK_d, SEQ, True, NEXT, True, SEQ)
                source(xeT, NEXT, ML[0], cQ_d, sQ_d, 0, True, 0, False, 0)
                nkt = SEQ // 128 + 2
            else:
                source(h1T, NEXT, ML[1], cQ_d, sQ_d, 0, True, 0, True, 0)
                source(hc1T, NCTX, MC[1], cQ_d, sQ_d, NEXT, False, 0, True, NEXT)
                nkt = NEXT // 128 + 2

            AR.top = int(H.f0)
            O = FT
            qbuf = Rot([AR.bf(NQ) for _ in range(2)])
            kbuf = Rot([AR.bf(nkt * 128) for _ in range(2)])
            vbuf = Rot([AR.bf(nkt * 130) for _ in range(2)])
            ptile = Rot([AR.bf(1024) for _ in range(3)])
            rz = Rot([AR.f32(1) for _ in range(6)])
            od1 = [AR.f32(128) for _ in range(4)]
            t_o2 = Rot([AR.f32(128) for _ in range(2)])
            t_od = Rot([AR.f32(128) for _ in range(2)])
            t_sq = Rot([AR.f32(128) for _ in range(2)])
            ssq = Rot([AR.f32(1) for _ in range(4)])
            stmp = Rot([AR.f32(640) for _ in range(2)])
            if not ab:
                wm = AR.f32(16 * 384)
                P.dma("sp", wm, wmask_d.v())
                nbb = Rot([AR.f32(640) for _ in range(3)])
                nb_pref = [None]

            def attend512(kt_, qt_, vv, vcw, voff, dv, r0, q0, nq, kts, fin):
                nsub = nq // 128
                accs = [bank(4 + s, dv + 1) for s in range(nsub)]
                pairs = [kts[i:i + 2] for i in range(0, len(kts), 2)]
                npair = len(pairs)

                def stage_a(pi):
                    pr = pairs[pi]
                    base = (pi % 2) * 1024
                    sreg = PS.v(0, 128, base, base + len(pr) * nq)
                    for jj, kt in enumerate(pr):
                        P.op("pe", OP("matmul", sreg.ap[:, jj * nq:jj * nq + nq], lhsT=kt_.ap[r0:r0 + 64, kt * 128:kt * 128 + 128],
                                      rhs=qt_.ap[r0:r0 + 64, q0:q0 + nq], start=True, stop=True), [kt_, qt_], [sreg], sig=(jj == len(pr) - 1))
                    pt = ptile.get().s(0, len(pr) * nq)
                    P.op("act", OP("activation", out=pt.ap, in_=sreg.ap, func=AF.Exp, scale=0.125), [sreg], [pt])
                    return pt

                def stage_b(pi, pt):
                    pr = pairs[pi]
                    for jj, kt in enumerate(pr):
                        for s in range(nsub):
                            first = (pi == 0 and jj == 0)
                            last = (pi == npair - 1 and jj == len(pr) - 1)
                            P.op("pe", OP("matmul", accs[s].ap, lhsT=pt.ap[:, jj * nq + s * 128:jj * nq + s * 128 + 128],
                                          rhs=vv.ap[:, kt * vcw + voff:kt * vcw + voff + dv + 1], start=first, stop=last),
                                 [pt, vv], [accs[s]], sig=(s == nsub - 1 and jj == len(pr) - 1))

                prev = None
                for pi in range(npair):
                    cur = stage_a(pi)
                    if prev is not None:
                        stage_b(pi - 1, prev)
                    prev = cur
                stage_b(npair - 1, prev)
                for s in range(nsub):
                    fin(s, accs[s], q0 // 128 + s)

            for u in range(8):
                kc = u if u < 4 else 4 + (u - 4) // 2
                qt_, kt_, vt_ = qbuf.get(), kbuf.get(), vbuf.get()
                P.dma("sp", qt_, QT.v(u * 128, u * 128 + 128, 0, NQ))
                P.dma("sp", kt_, KT.v(kc * 128, kc * 128 + 128, 0, nkt * 128))
                if ab:
                    vc0, vcw = (u * 129, 129) if u < 4 else (516 + ((u - 4) // 2) * 65, 65)
                else:
                    vc0, vcw = (u * 130, 130) if u < 4 else (520 + ((u - 4) // 2) * 65, 65)
                vv = vt_.s(0, nkt * vcw)
                for g0 in range(0, nkt, 11):
                    src = V(VA.t[:, g0 * VW:(g0 + 11) * VW].rearrange("p (t c) -> p t c", c=VW)[:, :, vc0:vc0 + vcw],
                            VA.tid, 0, 128, g0 * VW * 0.5, (g0 + 11) * VW * 0.5, 0.5)
                    dstv = V(vv.ap[:, g0 * vcw:(g0 + 11) * vcw].rearrange("p (t c) -> p t c", c=vcw), vv.tid, 0, 128,
                             vv.f0 + g0 * vcw * 0.5, vv.f0 + (g0 + 11) * vcw * 0.5, 0.5)
                    P.dma("sp", dstv, src)

                if ab:
                    dv = 128 if u < 4 else 64
                    qblocks = [(q0, 256, list(range(nkt))) for q0 in range(0, NEXT, 256)] + [(NEXT, 256, [nkt - 2, nkt - 1])]
                    for (q0, nq, kts) in qblocks:
                        nsub = nq // 128
                        accs = [[bank(4 + c * 2 + s, dv + 1) for s in range(nsub)] for c in range(2)]
                        pairs = [kts[i:i + 2] for i in range(0, len(kts), 2)]
                        npair = len(pairs)

                        def st_a(pi):
                            pr = pairs[pi]
                            base = (pi % 2) * 1024
                            sreg = PS.v(0, 128, base, base + 1024)
                            for jj, kt in enumerate(pr):
                                for c in range(2):
                                    P.op("pe", OP("matmul", sreg.ap[:, c * 512 + jj * nq:c * 512 + jj * nq + nq],
                                                  lhsT=kt_.ap[64 * c:64 * c + 64, kt * 128:kt * 128 + 128],
                                                  rhs=qt_.ap[64 * c:64 * c + 64, q0:q0 + nq], start=True, stop=True),
                                         [kt_, qt_], [sreg], sig=(jj == len(pr) - 1 and c == 1))
                            pt = ptile.get()
                            P.op("act", OP("activation", out=pt.ap, in_=sreg.ap, func=AF.Exp, scale=0.125), [sreg], [pt])
                            return pt

                        def st_b(pi, pt):
                            pr = pairs[pi]
                            for c in range(2):
                                for jj, kt in enumerate(pr):
                                    for s_ in range(nsub):
                                        first = (pi == 0 and jj == 0)
                                        last = (pi == npair - 1 and jj == len(pr) - 1)
                                        col = c * 512 + jj * nq + s_ * 128
                                        P.op("pe", OP("matmul", accs[c][s_].ap, lhsT=pt.ap[:, col:col + 128],
                                                      rhs=vv.ap[:, kt * vcw:kt * vcw + dv + 1], start=first, stop=last),
                                             [pt, vv], [accs[c][s_]], sig=(c == 1 and s_ == nsub - 1 and jj == len(pr) - 1))

                        prev = None
                        for pi in range(npair):
                            cur = st_a(pi)
                            if prev is not None:
                                st_b(pi - 1, prev)
                            prev = cur
                        st_b(npair - 1, prev)
                        for s_ in range(nsub):
                            ot = q0 // 128 + s_
                            a0, a1 = accs[0][s_], accs[1][s_]
                            r0_, r1_ = rz.get(), rz.get()
                            P.op("dve", OP("reciprocal", out=r0_.ap, in_=a0.ap[:, dv:dv + 1]), [a0], [r0_])
                            P.op("dve", OP("reciprocal", out=r1_.ap, in_=a1.ap[:, dv:dv + 1]), [a1], [r1_])
                            if u < 4:
                                d1 = od1[s_]
                                o2, od, sq, ss = t_o2.get(), t_od.get(), t_sq.get(), ssq.get()
                                P.op("dve", OP("tensor_scalar", out=d1.ap, in0=a0.ap[:, 0:128], scalar1=r0_.ap[:, 0:1], scalar2=None, op0=ALU.mult), [a0, r0_], [d1])
                                P.op("dve", OP("tensor_scalar", out=o2.ap, in0=a1.ap[:, 0:128], scalar1=r1_.ap[:, 0:1], scalar2=None, op0=ALU.mult), [a1, r1_], [o2])
                                P.op("dve", OP("scalar_tensor_tensor", out=od.ap, in0=o2.ap, scalar=nlam.ap[:, 0:1], in1=d1.ap, op0=ALU.mult, op1=ALU.add), [o2, nlam, d1], [od])
                                P.op("pool", OP("tensor_tensor", out=sq.ap, in0=od.ap, in1=od.ap, op=ALU.mult), [od], [sq])
                                P.op("dve", OP("tensor_reduce", out=ss.ap, in_=sq.ap, axis=AX.X, op=ALU.add), [sq], [ss])
                                P.op("act", OP("activation", out=ss.ap, in_=ss.ap, func=AF.Sqrt, bias=eps_rms.ap, scale=1.0 / 128), [ss, eps_rms], [ss])
                                P.op("dve", OP("reciprocal", out=ss.ap, in_=ss.ap), [ss], [ss])
                                oo = O.s(ot * 1024 + u * 128, ot * 1024 + u * 128 + 128)
                                P.op("dve", OP("scalar_tensor_tensor", out=oo.ap, in0=od.ap, scalar=ss.ap[:, 0:1], in1=sg1.ap, op0=ALU.mult, op1=ALU.mult), [od, ss, sg1], [oo])
                            else:
                                for c, (a, r) in enumerate(((a0, r0_), (a1, r1_))):
                                    col = 512 + ((u - 4) * 2 + c) * 64
                                    oo = O.s(ot * 1024 + col, ot * 1024 + col + 64)
                                    P.op("dve", OP("tensor_scalar", out=oo.ap, in0=a.ap[:, 0:64], scalar1=r.ap[:, 0:1], scalar2=None, op0=ALU.mult), [a, r], [oo])
                else:
                    for c in range(2):
                        r0 = 64 * c

                        def l1_a(t, c=c, r0=r0, u=u):
                            if u < 4:
                                h = 2 * u + c
                                kts = list(range(t, t + 5)) + [20, 21]
                                nb = 5
                                if t == 0:
                                    nb_pref[0] = nbb.get()
                                    P.dma("sp", nb_pref[0], nab_d.v(0, 128, (t * 8 + h) * 640, (t * 8 + h) * 640 + 640))
                                bt = nb_pref[0]
                                if t + 1 < 16:
                                    nb_pref[0] = nbb.get()
                                    P.dma("sp", nb_pref[0], nab_d.v(0, 128, ((t + 1) * 8 + h) * 640, ((t + 1) * 8 + h) * 640 + 640))
                                voff = c * 65
                                col = 512 + h * 64
                            else:
                                h = (u - 4) * 2 + c
                                kts = list(range(t + 1, t + 4)) + [20, 21]
                                nb = 3
                                bt = wm.s(t * 384, t * 384 + 384)
                                voff = 0
                                col = h * 64
                            nk = len(kts)
                            sreg = PS.v(0, 128, (t % 2) * 1024, (t % 2) * 1024 + nk * 128)
                            qs = qt_.ap[r0:r0 + 64, 256 + t * 128:256 + t * 128 + 128]
                            for i, kt in enumerate(kts):
                                P.op("pe", OP("matmul", sreg.ap[:, i * 128:i * 128 + 128], lhsT=kt_.ap[r0:r0 + 64, kt * 128:kt * 128 + 128], rhs=qs, start=True, stop=True),
                                     [kt_, qt_], [sreg], sig=(i == nk - 1))
                            pt = ptile.get().s(0, nk * 128)
                            tm = stmp.get().s(0, nb * 128)
                            P.op("dve", OP("scalar_tensor_tensor", out=tm.ap, in0=sreg.ap[:, 0:nb * 128], scalar=0.125, in1=bt.ap[:, 0:nb * 128], op0=ALU.mult, op1=ALU.add), [sreg, bt], [tm])
                            P.op("act", OP("activation", out=pt.ap[:, 0:nb * 128], in_=tm.ap, func=AF.Exp), [tm], [pt])
                            P.op("act", OP("activation", out=pt.ap[:, nb * 128:nk * 128], in_=sreg.ap[:, nb * 128:nk * 128], func=AF.Exp, scale=0.125), [sreg], [pt])
                            return (t, h, kts, voff, col, pt)

                        def l1_b(stt, u=u):
                            t, h, kts, voff, col, pt = stt
                            nk = len(kts)
                            a = bank(4 + (t % 2), 65)
                            for i, kt in enumerate(kts):
                                P.op("pe", OP("matmul", a.ap, lhsT=pt.ap[:, i * 128:i * 128 + 128], rhs=vv.ap[:, kt * vcw + voff:kt * vcw + voff + 65], start=(i == 0), stop=(i == nk - 1)),
                                     [pt, vv], [a], sig=(i == nk - 1))
                            r = rz.get()
                            if u >= 4:
                                P.op("dve", OP("tensor_scalar", out=r.ap, in0=a.ap[:, 64:65], scalar1=esink.ap[:, h:h + 1], scalar2=None, op0=ALU.add), [a, esink], [r])
                                P.op("dve", OP("reciprocal", out=r.ap, in_=r.ap), [r], [r])
                            else:
                                P.op("dve", OP("reciprocal", out=r.ap, in_=a.ap[:, 64:65]), [a], [r])
                            oo = O.s(t * 1024 + col, t * 1024 + col + 64)
                            P.op("dve", OP("tensor_scalar", out=oo.ap, in0=a.ap[:, 0:64], scalar1=r.ap[:, 0:1], scalar2=None, op0=ALU.mult), [a, r], [oo])

                        prev = None
                        for t in range(16):
                            cur = l1_a(t)
                            if prev is not None:
                                l1_b(prev)
                            prev = cur
                        l1_b(prev)

            AR.top = free_top
            wo = AR.bf(8 * 1024)
            P.dma("pool", v3(wo, 8), dchunks(wo_d[li], 0, 1024))
            oTr = Rot([AR.bf(8 * 512) for _ in range(2)])
            lntmp = {"sq": Rot([AR.f32(512) for _ in range(2)]), "mean": AR.f32(512), "msq": AR.f32(512), "rstd": AR.f32(512)}
            if ab:
                blocks = [(t0, 512, xeT, t0, ML[0]) for t0 in range(0, NEXT, 512)] + [(NEXT, 256, cxT, 0, MC[0])]
            else:
                blocks = [(t0, 512, h1T, 256 + t0, ML[1]) for t0 in range(0, NOWN, 512)]
            pb3 = Rot([0, 1, 2, 3, 4, 5])
            for (t0, nt, src, c0, M) in blocks:
                for c in range(8):
                    P.dma("sp", Hc(c, t0, t0 + nt), src.v(c * 128, c * 128 + 128, c0, c0 + nt))
            for (t0, nt, src, c0, M) in blocks:
                oT = oTr.get().s(0, 8 * nt)
                for ch in range(8):
                    b = pb3.get()
                    pst = bank_bf(b, nt)
                    for tt in range(nt // 128):
                        tile = t0 // 128 + tt
                        oin = O.s(tile * 1024 + ch * 128, tile * 1024 + ch * 128 + 128)
                        P.op("pe", OP("transpose", out=pst.ap[:, tt * 128:tt * 128 + 128], in_=oin.ap, identity=identb.ap),
                             [oin, identb], [pst], sig=(tt == nt // 128 - 1))
                    od_ = oT.s(ch * nt, ch * nt + nt)
                    if ch % 2 == 0:
                        P.op("act", OP("activation", out=od_.ap, in_=pst.ap, func=AF.Copy), [pst], [od_])
                    else:
                        P.op("dve", OP("tensor_copy", out=od_.ap, in_=pst.ap), [pst], [od_])
                for dc in range(8):
                    pp = bank(pb3.get(), nt)
                    for ch in range(8):
                        P.op("pe", OP("matmul", pp.ap, lhsT=wo.ap[:, ch * 1024 + dc * 128:ch * 1024 + dc * 128 + 128],
                                                                                    rhs=oT.ap[:, ch * nt:ch * nt + nt], start=(ch == 0), stop=(ch == 7)),
                             [wo, oT], [pp], sig=(ch == 7))
                    hc = Hc(dc, t0, t0 + nt)
                    P.op("act", OP("activation", out=hc.ap, in_=hc.ap, func=AF.Copy, scale=ALPHA), [hc], [hc])
                    P.op("dve", OP("scalar_tensor_tensor", out=hc.ap, in0=pp.ap, scalar=M.ap[:, 16 + dc:17 + dc], in1=hc.ap, op0=ALU.mult, op1=ALU.add),
                         [pp, M, hc], [hc])
                ln_block(lambda c: Hc(c, t0, t0 + nt), nt, li, 0, lambda c: Hc(c, t0, t0 + nt), lntmp)

            AR.top = free_top
            WT = AR.f32(NT, P=32)
            moe_top = AR.top
            ftmp = AR.f32(1024)
            Lg = Rot([AR.f32(36) for _ in range(2)])
            sm = Rot([AR.f32(8) for _ in range(24)])
            em_r = Rot([AR.f32(32) for _ in range(2)])
            wt_r = Rot([AR.f32(32) for _ in range(2)])
            w2t_r = Rot([AR.f32(32) for _ in range(2)])
            pb4 = Rot([0, 1, 2, 3, 4, 5, 6, 7])
            mblocks = [(t0, min(512, NT - t0)) for t0 in range(0, NT, 512)]

            def Mof(t0):
                return (MC[li] if (ab and t0 >= NEXT) else ML[li])

            for (t0, nt) in mblocks:
                M = Mof(t0)
                for tt in range(nt // 128):
                    tk = t0 + tt * 128
                    for c in range(8):
                        hc = Hc(c, tk, tk + 128)
                        fo = ftmp.s(c * 128, c * 128 + 128)
                        P.op("dve", OP("tensor_scalar", out=fo.ap, in0=hc.ap, scalar1=M.ap[:, 32 + c:33 + c], scalar2=M.ap[:, 24 + c:25 + c],
                                                                                 op0=ALU.mult, op1=ALU.add), [hc, M], [fo])
                    pl = bank(pb4.get(), 36)
                    for c in range(8):
                        fo = ftmp.s(c * 128, c * 128 + 128)
                        P.op("pe", OP("matmul", pl.ap, lhsT=fo.ap, rhs=wr[li].ap[:, c * 36:c * 36 + 36], start=(c == 0), stop=(c == 7)),
                             [fo, wr[li]], [pl], sig=(c == 7))
                    L = Lg.get()
                    P.op("dve", OP("tensor_tensor", out=L.ap, in0=pl.ap, in1=brb[li].ap, op=ALU.add), [pl, brb[li]], [L])
                    gmax, ngmax, gexp, gsum, pen, m8, dd, w1, w2 = [sm.get() for _ in range(9)]
                    em, Wt, W2 = em_r.get(), wt_r.get(), w2t_r.get()
                    P.op("dve", OP("tensor_reduce", out=gmax.ap[:, 0:1], in_=L.ap[:, 0:4], axis=AX.X, op=ALU.max), [L], [gmax])
                    P.op("dve", OP("tensor_scalar", out=ngmax.ap[:, 0:1], in0=gmax.ap[:, 0:1], scalar1=-1.0, scalar2=None, op0=ALU.mult), [gmax], [ngmax])
                    P.op("act", OP("activation", out=gexp.ap[:, 0:4], in_=L.ap[:, 0:4], func=AF.Exp, bias=ngmax.ap[:, 0:1], scale=1.0), [L, ngmax], [gexp])
                    P.op("dve", OP("tensor_reduce", out=gsum.ap[:, 0:1], in_=gexp.ap[:, 0:4], axis=AX.X, op=ALU.add), [gexp], [gsum])
                    P.op("dve", OP("reciprocal", out=gsum.ap[:, 0:1], in_=gsum.ap[:, 0:1]), [gsum], [gsum])
                    P.op("dve", OP("tensor_scalar", out=pen.ap[:, 0:4], in0=L.ap[:, 0:4], scalar1=gmax.ap[:, 0:1], scalar2=1e30, op0=ALU.is_equal, op1=ALU.mult), [L, gmax], [pen])
                    P.op("dve", OP("tensor_scalar", out=pen.ap[:, 0:4], in0=pen.ap[:, 0:4], scalar1=-1e30, scalar2=None, op0=ALU.add), [pen], [pen])
                    for g in range(4):
                        P.op("dve", OP("tensor_scalar", out=em.ap[:, 8 * g:8 * g + 8], in0=L.ap[:, 4 + 8 * g:12 + 8 * g], scalar1=pen.ap[:, g:g + 1], scalar2=None, op0=ALU.add),
                             [L, pen], [em])
                    P.op("dve", OP("max", out=m8.ap, in_=em.ap), [em], [m8])
                    P.op("dve", OP("tensor_tensor", out=dd.ap[:, 0:1], in0=m8.ap[:, 1:2], in1=m8.ap[:, 0:1], op=ALU.subtract), [m8], [dd])
                    P.op("act", OP("activation", out=dd.ap[:, 0:1], in_=dd.ap[:, 0:1], func=AF.Exp), [dd], [dd])
                    P.op("dve", OP("tensor_scalar", out=w1.ap[:, 0:1], in0=dd.ap[:, 0:1], scalar1=1.0, scalar2=None, op0=ALU.add), [dd], [w1])
                    P.op("dve", OP("reciprocal", out=w1.ap[:, 0:1], in_=w1.ap[:, 0:1]), [w1], [w1])
                    P.op("dve", OP("tensor_tensor", out=w1.ap[:, 0:1], in0=w1.ap[:, 0:1], in1=gsum.ap[:, 0:1], op=ALU.mult), [w1, gsum], [w1])
                    P.op("dve", OP("tensor_tensor", out=w2.ap[:, 0:1], in0=w1.ap[:, 0:1], in1=dd.ap[:, 0:1], op=ALU.mult), [w1, dd], [w2])
                    P.op("dve", OP("tensor_scalar", out=Wt.ap, in0=em.ap, scalar1=m8.ap[:, 0:1], scalar2=w1.ap[:, 0:1], op0=ALU.is_equal, op1=ALU.mult), [em, m8, w1], [Wt])
                    P.op("dve", OP("tensor_scalar", out=W2.ap, in0=em.ap, scalar1=m8.ap[:, 1:2], scalar2=w2.ap[:, 0:1], op0=ALU.is_equal, op1=ALU.mult), [em, m8, w2], [W2])
                    P.op("dve", OP("tensor_tensor", out=Wt.ap, in0=Wt.ap, in1=W2.ap, op=ALU.add), [Wt, W2], [Wt])
                    ptr = bank(pb4.get(), 128, p=32)
                    P.op("pe", OP("transpose", out=ptr.ap, in_=Wt.ap, identity=ident32.ap), [Wt, ident32], [ptr])
                    wts = WT.s(tk, tk + 128)
                    P.op("act", OP("activation", out=wts.ap, in_=ptr.ap, func=AF.Copy), [ptr], [wts])
                for c in range(8):
                    hc = Hc(c, t0, t0 + nt)
                    fo = FT.s(c * NT + t0, c * NT + t0 + nt)
                    P.op("act", OP("activation", out=fo.ap, in_=hc.ap, func=AF.Identity, scale=M.ap[:, 32 + c:33 + c], bias=M.ap[:, 24 + c:25 + c]), [hc, M], [fo])
                    P.op("pool", OP("tensor_scalar", out=hc.ap, in0=hc.ap, scalar1=ALPHA, scalar2=None, op0=ALU.mult), [hc], [hc])

            AR.top = moe_top
            w1b = Rot([AR.bf(8 * 512) for _ in range(2)])
            w3b = Rot([AR.bf(8 * 512) for _ in range(2)])
            w2b = Rot([AR.bf(4 * 1024) for _ in range(1)])
            gb = Rot([AR.bf(4 * 512) for _ in range(2)])
            wbc = Rot([AR.f32(512) for _ in range(2)])
            sb_ = Rot([AR.f32(512) for _ in range(2)])
            Eb = Rot([AR.f32(128, P=32) for _ in range(2)])
            def moe_h(ex, w1e, w3e, E, t0, nt):
                pw = bank(pb4.get(), nt)
                wts = WT.s(t0, t0 + nt)
                P.op("pe", OP("matmul", pw.ap, lhsT=E.ap, rhs=wts.ap, start=True, stop=True), [E, wts], [pw])
                wb = wbc.get().s(0, nt)
                P.op("act", OP("activation", out=wb.ap, in_=pw.ap, func=AF.Copy), [pw], [wb])
                g = gb.get().s(0, 4 * nt)
                for ec in range(4):
                    p1, p3 = bank(pb4.get(), nt), bank(pb4.get(), nt)
                    for c in range(8):
                        fr = FT.s(c * NT + t0, c * NT + t0 + nt)
                        P.op("pe", OP("matmul", p1.ap, lhsT=w1e.ap[:, c * 512 + ec * 128:c * 512 + ec * 128 + 128], rhs=fr.ap, start=(c == 0), stop=(c == 7)),
                             [w1e, fr], [p1], sig=(c == 7))
                    for c in range(8):
                        fr = FT.s(c * NT + t0, c * NT + t0 + nt)
                        P.op("pe", OP("matmul", p3.ap, lhsT=w3e.ap[:, c * 512 + ec * 128:c * 512 + ec * 128 + 128], rhs=fr.ap, start=(c == 0), stop=(c == 7)),
                             [w3e, fr], [p3], sig=(c == 7))
                    sv = sb_.get().s(0, nt)
                    P.op("act", OP("activation", out=sv.ap, in_=p1.ap, func=AF.Silu), [p1], [sv])
                    P.op("dve", OP("tensor_tensor", out=sv.ap, in0=p3.ap, in1=sv.ap, op=ALU.mult), [p3, sv], [sv])
                    gv = g.s(ec * nt, ec * nt + nt)
                    P.op("pool", OP("tensor_tensor", out=gv.ap, in0=sv.ap, in1=wb.ap, op=ALU.mult), [sv, wb], [gv])
                return g

            def moe_y(w2e, g, t0, nt):
                M = Mof(t0)
                for dc in range(8):
                    py = bank(pb4.get(), nt)
                    for ec in range(4):
                        P.op("pe", OP("matmul", py.ap, lhsT=w2e.ap[:, ec * 1024 + dc * 128:ec * 1024 + dc * 128 + 128],
                                      rhs=g.ap[:, ec * nt:ec * nt + nt], start=(ec == 0), stop=(ec == 3)),
                             [w2e, g], [py], sig=(ec == 3))
                    hc = Hc(dc, t0, t0 + nt)
                    P.op("dve", OP("scalar_tensor_tensor", out=hc.ap, in0=py.ap, scalar=M.ap[:, 40 + dc:41 + dc], in1=hc.ap, op0=ALU.mult, op1=ALU.add),
                         [py, M, hc], [hc])

            def wload(ex):
                w1e, w3e = w1b.get(), w3b.get()
                P.dma("pool", v3(w1e, 8), dchunks(w1_d[li], 0, 512, r0=ex * 1024))
                P.dma("pool", v3(w3e, 8), dchunks(w3_d[li], 0, 512, r0=ex * 1024))
                return (w1e, w3e)

            pend = None
            wnext = wload(0)
            for ex in range(NEXP):
                w1e, w3e = wnext
                if ex + 1 < NEXP:
                    wnext = wload(ex + 1)
                E = Eb.get()
                P.op("dve", OP("tensor_copy", out=E.ap, in_=ident32.ap[0:32, ex:ex + 1].to_broadcast([32, 128])), [ident32], [E])
                w2e = None
                for (t0, nt) in mblocks:
                    g = moe_h(ex, w1e, w3e, E, t0, nt)
                    if pend is not None:
                        moe_y(*pend)
                    if w2e is None:
                        w2e = w2b.get()
                        P.dma("pool", v3(w2e, 4), dchunks(w2_d[li], 0, 1024, r0=ex * 512, nchunk=4))
                    pend = (w2e, g, t0, nt)
            moe_y(*pend)

            AR.top = moe_top
            lntmp = {"sq": Rot([AR.f32(512) for _ in range(2)]), "mean": AR.f32(512), "msq": AR.f32(512), "rstd": AR.f32(512)}
            for (t0, nt) in mblocks:
                ln_block(lambda c: Hc(c, t0, t0 + nt), nt, li, 1, lambda c: Hc(c, t0, t0 + nt), lntmp)
                for c in range(8):
                    if ab:
                        if t0 < NEXT:
                            dst = h1T.v(c * 128, c * 128 + 128, t0, t0 + nt)
                        else:
                            dst = hc1T.v(c * 128, c * 128 + 128, 0, nt)
                    else:
                        dst = outT.v(c * 128, c * 128 + 128, t0, t0 + nt)
                    P.dma("sp", dst, Hc(c, t0, t0 + nt))

        layer(0)
        layer(1)
        P.finish()
        P.emit()
    return nc


def _swap_cols(w):
    n = w.shape[1]
    idx = np.arange(n).reshape(n // 64, 2, 32)[:, ::-1, :].reshape(n)
    return w[:, idx]


def _rope_tables(pos):
    pos = np.asarray(pos)
    valid = pos >= 0
    p = np.where(valid, pos, 0)
    row = (p // 64).astype(np.float32)
    col = (p % 64).astype(np.float32)
    inv = (np.float32(10000.0) ** (-np.arange(16, dtype=np.float32) / np.float32(16))).astype(np.float32)
    ang = np.concatenate([row[:, None] * inv, col[:, None] * inv], -1).astype(np.float32)
    cos = np.cos(ang).astype(np.float32)
    sin = np.sin(ang).astype(np.float32)
    cos = np.where(valid[:, None], cos, np.float32(1.0))
    sin = np.where(valid[:, None], sin, np.float32(0.0))
    c64 = np.concatenate([cos, cos], 1)
    s64 = np.concatenate([-sin, sin], 1)
    cT = np.concatenate([c64, c64], 1).T
    sT = np.concatenate([s64, s64], 1).T
    return np.ascontiguousarray(cT, dtype=np.float32), np.ascontiguousarray(sT, dtype=np.float32)


def _ext_positions(j):
    pos = 2048 * j - 256 + np.arange(NEXT)
    if j == 0:
        pos[0:256] = 256 + np.arange(256)
    if j == 3:
        pos[2304:2560] = 7680 + np.arange(256)
    return pos


def _first_occurrence(kp):
    seen, out = set(), np.zeros(len(kp), dtype=bool)
    for i, p in enumerate(kp):
        if p not in seen:
            seen.add(p)
            out[i] = True
    return out


def _layer1_tables(j, rpb):
    pos = _ext_positions(j)
    wmask = np.full((128, 16, 3, 128), NEG, dtype=np.float32)
    nab = np.full((128, 16, 8, 5, 128), NEG, dtype=np.float32)
    for t in range(16):
        qp = 2048 * j + 128 * t + np.arange(128)
        kp = pos[128 * (t + 1):128 * (t + 4)]
        first = _first_occurrence(kp)
        ok = (np.abs(qp[None, :] - kp[:, None]) <= 128) & first[:, None]
        m = np.where(ok, np.float32(0.0), np.float32(NEG)).reshape(3, 128, 128)
        wmask[:, t] = m.transpose(1, 0, 2)
        kp = pos[128 * t:128 * (t + 5)]
        first = _first_occurrence(kp)
        qr, qc = qp // 64, qp % 64
        kr, kcl = kp // 64, kp % 64
        r0 = np.clip(qr - 4, 0, 120)
        c0 = np.clip(qc - 8, 0, 48)
        ok = ((kr[:, None] >= r0[None, :]) & (kr[:, None] < r0[None, :] + 8) &
              (kcl[:, None] >= c0[None, :]) & (kcl[:, None] < c0[None, :] + 16) & first[:, None])
        ro = np.clip(kr[:, None] - qr[None, :] + 7, 0, 14)
        co = np.clip(kcl[:, None] - qc[None, :] + 15, 0, 30)
        for h in range(8):
            b = np.where(ok, rpb[h][ro, co], np.float32(NEG)).astype(np.float32).reshape(5, 128, 128)
            nab[:, t, h] = b.transpose(1, 0, 2)
    return wmask.reshape(128, 16 * 384), nab.reshape(128, 16 * 8 * 640)


_NC_CACHE = {}


def kernel(x, c, ctx, c_ctx, mod_w, mod_b, ln_g, ln_b, ab_w_in, ab_w_out, diff_lambda, diff_subln_g, gqa_qk_g,
           cd_w_in, cd_w_out, win_sink, na_rpb, moe_w_group, moe_b_group, moe_w_router, moe_b_router,
           moe_w1, moe_w3, moe_w2):
    f = lambda a: np.ascontiguousarray(np.asarray(a), dtype=np.float32)
    x, c, ctx, c_ctx = f(x), f(c), f(ctx), f(c_ctx)
    mod_w, mod_b, ln_g, ln_b = f(mod_w), f(mod_b), f(ln_g), f(ln_b)
    ab_w_in, ab_w_out, cd_w_in, cd_w_out = f(ab_w_in)[0], f(ab_w_out)[0], f(cd_w_in)[0], f(cd_w_out)[0]
    diff_lambda, diff_subln_g, gqa_qk_g = f(diff_lambda)[0], f(diff_subln_g)[0], f(gqa_qk_g)[0]
    win_sink, na_rpb = f(win_sink)[0], f(na_rpb)[0]
    moe_w1, moe_w3, moe_w2 = f(moe_w1), f(moe_w3), f(moe_w2)

    shared = {}
    shared["ident"] = np.eye(128, dtype=np.float32)
    bo = np.zeros((128, 128), dtype=np.float32)
    bo[:64, :64] = 1.0
    bo[64:, 64:] = 1.0
    shared["bones"] = bo
    for i in range(2):
        shared[f"modw{i}"] = mod_w[i]
        shared[f"modb{i}"] = np.ascontiguousarray(mod_b[i].reshape(48, 128).T)
        shared[f"wr{i}"] = np.ascontiguousarray(np.concatenate([f(moe_w_group)[i], f(moe_w_router)[i]], 1))
        shared[f"br{i}"] = np.concatenate([f(moe_b_group)[i], f(moe_b_router)[i]])[None, :].copy()
        shared[f"w1_{i}"] = moe_w1[i].reshape(NEXP * D, 512)
        shared[f"w3_{i}"] = moe_w3[i].reshape(NEXP * D, 512)
        shared[f"w2_{i}"] = moe_w2[i].reshape(NEXP * 512, D)
    lnT = np.zeros((128, 64), dtype=np.float32)
    for i in range(2):
        for wch in range(2):
            lnT[:, ((i * 2 + wch) * 2 + 0) * 8:((i * 2 + wch) * 2 + 0) * 8 + 8] = ln_g[i, wch].reshape(8, 128).T
            lnT[:, ((i * 2 + wch) * 2 + 1) * 8:((i * 2 + wch) * 2 + 1) * 8 + 8] = ln_b[i, wch].reshape(8, 128).T
    shared["lnT"] = lnT
    q0 = ab_w_in[:, 0:1024]
    kd, vd = ab_w_in[:, 1024:1536], ab_w_in[:, 1536:2048]
    kg, vg = ab_w_in[:, 2048:2176], ab_w_in[:, 2176:2304]
    k0 = np.concatenate([kd, kg[:, 0:64], kg[:, 0:64], kg[:, 64:128], kg[:, 64:128]], 1)
    shared["wq0"], shared["wqs0"] = np.ascontiguousarray(q0), np.ascontiguousarray(_swap_cols(q0))
    shared["wk0"], shared["wks0"] = np.ascontiguousarray(k0), np.ascontiguousarray(_swap_cols(k0))
    shared["wv0"] = np.ascontiguousarray(np.concatenate([vd, vg], 1))
    shared["wo0"] = ab_w_out
    qw, qn = cd_w_in[:, 0:512], cd_w_in[:, 512:1024]
    kw, vw = cd_w_in[:, 1024:1152], cd_w_in[:, 1152:1280]
    kn, vn = cd_w_in[:, 1280:1792], cd_w_in[:, 1792:2304]
    q1 = np.concatenate([qn, qw], 1)
    k1 = np.concatenate([kn, kw[:, 0:64], kw[:, 0:64], kw[:, 64:128], kw[:, 64:128]], 1)
    shared["wq1"], shared["wqs1"] = np.ascontiguousarray(q1), np.ascontiguousarray(_swap_cols(q1))
    shared["wk1"], shared["wks1"] = np.ascontiguousarray(k1), np.ascontiguousarray(_swap_cols(k1))
    shared["wv1"] = np.ascontiguousarray(np.concatenate([vn, vw], 1))
    shared["wo1"] = cd_w_out
    sw = lambda g: np.concatenate([g[32:64], g[0:32]])
    gq, gk = gqa_qk_g[0], gqa_qk_g[1]
    shared["gt"] = np.ascontiguousarray(np.stack([np.tile(gq, 2), np.tile(sw(gq), 2), np.tile(gk, 2), np.tile(sw(gk), 2)], 1))
    shared["dlam"] = diff_lambda.reshape(1, 256).copy()
    shared["subg"] = diff_subln_g.reshape(1, 128).copy()
    shared["sink"] = win_sink.reshape(1, 8).copy()
    ck, sk = _rope_tables(np.concatenate([np.arange(SEQ), -np.ones(NCTX, dtype=np.int64)]))
    shared["cK"], shared["sK"] = ck, sk

    in_maps = []
    tabs = {}
    for core in range(8):
        b, j = core // 4, core % 4
        pos = _ext_positions(j)
        m = dict(shared)
        m["xT"] = np.ascontiguousarray(x[b].T)
        m["xeT"] = np.ascontiguousarray(x[b][pos].T)
        m["cxT"] = np.ascontiguousarray(ctx[b].T)
        m["ccT"] = np.ascontiguousarray(np.stack([c[b], c_ctx], 1))
        if j not in tabs:
            cq, sq = _rope_tables(np.concatenate([pos, -np.ones(NCTX, dtype=np.int64)]))
            wmask, nab = _layer1_tables(j, na_rpb)
            tabs[j] = (cq, sq, wmask, nab)
        m["cQ"], m["sQ"], m["wmask"], m["nab"] = tabs[j]
        in_maps.append(m)

    if "nc" not in _NC_CACHE:
        _NC_CACHE["nc"] = build_program()
    res = run_bass_kernel_spmd(_NC_CACHE["nc"], in_maps, core_ids=list(range(8)))
    out = np.empty((2, SEQ, D), dtype=np.float32)
    for core in range(8):
        b, j = core // 4, core % 4
        out[b, 2048 * j:2048 * (j + 1), :] = np.asarray(res.results[core]["outT"]).T
    if DEBUG:
        kernel.last = res
    return out
```

```python
import math
from contextlib import ExitStack

import numpy as np
import concourse.bass as bass
import concourse.mybir as mybir
from concourse.bass_utils import run_bass_kernel_spmd

F32 = mybir.dt.float32
BF16 = mybir.dt.bfloat16
AF = mybir.ActivationFunctionType
ALU = mybir.AluOpType
AX = mybir.AxisListType

DEBUG = False

D = 1024
SEQ = 8192
NCTX = 256
NEXT = 2560
NOWN = 2048
NQ = NEXT + NCTX
NK0 = SEQ + NCTX
NK1 = NEXT + NCTX
VW = 650
DEPTH = 2
ALPHA = (2.0 * DEPTH) ** 0.25
LN_EPS = 1e-5
RMS_EPS = 1e-6
LAM_INIT0 = 0.8 - 0.6 * math.exp(0.0)
NEG = -30000.0
NEXP = 32

SEM_LIM = 8000
NDMA = 12
ARENA_WORDS = 53200


class V:
    def __init__(self, ap, tid, p0, p1, f0, f1, wpe):
        self.ap, self.tid, self.p0, self.p1, self.f0, self.f1, self.wpe = ap, tid, p0, p1, f0, f1, wpe

    def reg(self):
        return (self.tid, self.p0, self.p1, self.f0, self.f1)

    def s(self, c0, c1, p0=None, p1=None):
        q0 = 0 if p0 is None else p0
        q1 = (self.p1 - self.p0) if p1 is None else p1
        return V(self.ap[q0:q1, c0:c1], self.tid, self.p0 + q0, self.p0 + q1,
                 self.f0 + c0 * self.wpe, self.f0 + c1 * self.wpe, self.wpe)


class Buf:
    def __init__(self, t, tid, P, F, wpe):
        self.t, self.tid, self.P, self.F, self.wpe = t, tid, P, F, wpe

    def v(self, p0=0, p1=None, f0=0, f1=None):
        p1 = self.P if p1 is None else p1
        f1 = self.F if f1 is None else f1
        return V(self.t[p0:p1, f0:f1], self.tid, p0, p1, f0 * self.wpe, f1 * self.wpe, self.wpe)


class Prog:
    ENGS = ["pe", "act", "dve", "pool", "sp"]

    def __init__(self, nc, stack):
        self.nc, self.stack = nc, stack
        self.recs = {e: [] for e in self.ENGS}
        self.sig = {e: 0 for e in self.ENGS}
        self.known = {e: {} for e in self.ENGS}
        self.csems = {e: [] for e in self.ENGS}
        self.dsems, self.dcnt, self.drr = {}, {}, {}
        for q in ["sp", "act", "pool"]:
            self.dsems[q] = [stack.enter_context(nc.semaphore(f"d_{q}_{k}")) for k in range(NDMA)]
            self.dcnt[q] = [0] * NDMA
            self.drr[q] = 0
        self.ent = {}
        self.ntid = 0

    def new_tid(self):
        self.ntid += 1
        return self.ntid

    def dram(self, name, P, F, dtype, kind):
        t = self.nc.dram_tensor(name, [P, F], dtype, kind=kind)
        return Buf(t, self.new_tid(), P, F, 1.0 if dtype == F32 else 0.5)

    def _csem(self, e, idx):
        while len(self.csems[e]) <= idx:
            self.csems[e].append(self.stack.enter_context(self.nc.semaphore(f"c_{e}_{len(self.csems[e])}")))
        return self.csems[e][idx]

    @staticmethod
    def _ov(a, b):
        return a[1] < b[2] and b[1] < a[2] and a[3] < b[4] and b[3] < a[4]

    @staticmethod
    def _cov(a, b):
        return a[1] <= b[1] and a[2] >= b[2] and a[3] <= b[3] and a[4] >= b[4]

    BUCK = 256

    def _cands(self, r):
        d = self.ent.get(r[0])
        if not d:
            return []
        seen, out = set(), []
        for b in range(int(r[3]) // self.BUCK, int(math.ceil(r[4])) // self.BUCK + 1):
            for en in d.get(b, ()):
                if en[3] and id(en) not in seen:
                    seen.add(id(en))
                    out.append(en)
        return out

    def _add(self, en):
        r = en[0]
        d = self.ent.setdefault(r[0], {})
        for b in range(int(r[3]) // self.BUCK, int(math.ceil(r[4])) // self.BUCK + 1):
            lst = d.setdefault(b, [])
            if len(lst) > 64:
                lst[:] = [x for x in lst if x[3]]
            lst.append(en)

    def _deps(self, reads, writes):
        raw, other = [], []
        for r in reads:
            for en in self._cands(r):
                if en[1] is not None and self._ov(en[0], r):
                    raw.append(en[1])
        for w in writes:
            for en in self._cands(w):
                if self._ov(en[0], w):
                    if en[1] is not None:
                        other.append(en[1])
                    other.extend(en[2])
        return raw, other

    def _update(self, tok, reads, writes):
        for r in reads:
            hit = False
            for en in self._cands(r):
                if self._ov(en[0], r):
                    if tok[0] == "c":
                        en[2][:] = [t for t in en[2] if not (t[0] == "c" and t[1] == tok[1])]
                    en[2].append(tok)
                    if self._cov(en[0], r):
                        hit = True
            if not hit:
                self._add([r, None, [tok], True])
        for w in writes:
            for en in self._cands(w):
                if self._cov(w, en[0]):
                    en[3] = False
            self._add([w, tok, [], True])

    def _waits(self, e, raw, other):
        waits = []
        kn = self.known[e]
        for kind, toks in (("raw", raw), ("oth", other)):
            for t in toks:
                if t[0] == "c":
                    _, f, v = t
                    if f == e and e == "pe":
                        continue
                    if kn.get(f, 0) >= v:
                        continue
                    kn[f] = v
                    waits.append(t)
                else:
                    _, q, k, c = t
                    if kn.get((q, k), 0) >= c:
                        continue
                    kn[(q, k)] = c
                    waits.append(t)
        return waits

    def op(self, e, fn, reads=(), writes=(), sig=True):
        reads = [r.reg() for r in reads]
        writes = [w.reg() for w in writes]
        raw, other = self._deps(reads, writes)
        waits = self._waits(e, raw, other)
        if sig:
            self.sig[e] += 1
            v = self.sig[e]
        else:
            v = self.sig[e] + 1
        tok = ("c", e, v)
        self._update(tok, reads, writes)
        self.recs[e].append((waits, fn, tok if sig else None))
        return tok

    def dma(self, q, out, in_, **kw):
        reads, writes = [in_.reg()], [out.reg()]
        raw, other = self._deps(reads, writes)
        k = self.drr[q]
        self.drr[q] = (k + 1) % NDMA
        prev = self.dcnt[q][k]
        if prev > 0:
            other = other + [("d", q, k, prev)]
        waits = self._waits(q, raw, other)
        self.dcnt[q][k] = prev + 1
        tok = ("d", q, k, prev + 1)
        self._update(tok, reads, writes)
        oa, ia = out.ap, in_.ap

        def fn(eng, oa=oa, ia=ia, kw=kw):
            return eng.dma_start(out=oa, in_=ia, **kw)

        self.recs[q].append((waits, fn, tok))
        return tok

    def finish(self):
        waits = []
        for q in self.dsems:
            for k in range(NDMA):
                if self.dcnt[q][k] > 0:
                    waits.append(("d", q, k, self.dcnt[q][k]))
        for e in ["pe", "act", "dve", "pool"]:
            if self.sig[e] > 0:
                waits.append(("c", e, self.sig[e]))
        self.recs["sp"].append((waits, None, None))

    def _emit_wait(self, eng, w):
        if w[0] == "c":
            _, f, v = w
            eng.wait_ge(self._csem(f, (v - 1) // SEM_LIM), (v - 1) % SEM_LIM + 1)
        else:
            _, q, k, c = w
            eng.wait_ge(self.dsems[q][k], 16 * c)

    def emit(self):
        nc = self.nc
        for e in self.ENGS:
            for idx in range((self.sig[e] + SEM_LIM - 1) // SEM_LIM + 1):
                self._csem(e, idx)
        recs, me = self.recs, self

        def play(e, eng):
            for waits, fn, tok in recs[e]:
                for w in waits:
                    me._emit_wait(eng, w)
                if fn is None:
                    continue
                ins = fn(eng)
                if tok is not None:
                    if tok[0] == "c":
                        ins.then_inc(me._csem(e, (tok[2] - 1) // SEM_LIM), 1)
                    else:
                        ins.then_inc(me.dsems[tok[1]][tok[2]], 16)

        with nc.Block() as block:
            @block.tensor
            def _(eng):
                play("pe", eng)

            @block.scalar
            def _(eng):
                play("act", eng)

            @block.vector
            def _(eng):
                play("dve", eng)

            @block.gpsimd
            def _(eng):
                play("pool", eng)

            @block.sync
            def _(eng):
                play("sp", eng)


class Arena:
    def __init__(self, buf):
        self.buf, self.top, self.hi = buf, 0, 0

    def _alloc(self, words):
        off = self.top
        self.top += (words + 7) // 8 * 8
        self.hi = max(self.hi, self.top)
        assert self.top <= self.buf.F, f"arena overflow {self.top}"
        return off

    def f32(self, n, P=128):
        off = self._alloc(n)
        return V(self.buf.t[0:P, off:off + n], self.buf.tid, 0, P, off, off + n, 1.0)

    def bf(self, n, P=128):
        w = (n + 1) // 2
        off = self._alloc(w)
        return V(self.buf.t[0:P, off:off + w].bitcast(BF16), self.buf.tid, 0, P, off, off + w, 0.5)


class Rot:
    def __init__(self, items):
        self.items, self.i = items, 0

    def get(self):
        it = self.items[self.i % len(self.items)]
        self.i += 1
        return it


def r3(ap, k):
    return ap.rearrange("p (k n) -> p k n", k=k)


def OP(name, *a, **k):
    return lambda e: getattr(e, name)(*a, **k)


def build_program():
    nc = bass.Bass("TRN2", target_bir_lowering=False)
    st = ExitStack()
    with st:
        P = Prog(nc, st)
        I = lambda name, p, f, dt=F32: P.dram(name, p, f, dt, "ExternalInput")
        xT = I("xT", D, SEQ)
        xeT = I("xeT", D, NEXT)
        cxT = I("cxT", D, NCTX)
        ccT = I("ccT", D, 2)
        ident_d = I("ident", 128, 128)
        bones_d = I("bones", 128, 128)
        modw = [I(f"modw{i}", D, 6 * D) for i in range(2)]
        modb = [I(f"modb{i}", 128, 48) for i in range(2)]
        lnT_d = I("lnT", 128, 64)
        wq_d = [I(f"wq{i}", D, 1024) for i in range(2)]
        wqs_d = [I(f"wqs{i}", D, 1024) for i in range(2)]
        wk_d = [I(f"wk{i}", D, 768) for i in range(2)]
        wks_d = [I(f"wks{i}", D, 768) for i in range(2)]
        wv_d = [I(f"wv{i}", D, 640) for i in range(2)]
        wo_d = [I(f"wo{i}", D, 1024) for i in range(2)]
        gt_d = I("gt", 128, 4)
        dlam_d = I("dlam", 1, 256)
        subg_d = I("subg", 1, 128)
        sink_d = I("sink", 1, 8)
        cK_d = I("cK", 128, NK0)
        sK_d = I("sK", 128, NK0)
        cQ_d = I("cQ", 128, NQ)
        sQ_d = I("sQ", 128, NQ)
        wr_d = [I(f"wr{i}", D, 36) for i in range(2)]
        br_d = [I(f"br{i}", 1, 36) for i in range(2)]
        w1_d = [I(f"w1_{i}", NEXP * D, 512) for i in range(2)]
        w3_d = [I(f"w3_{i}", NEXP * D, 512) for i in range(2)]
        w2_d = [I(f"w2_{i}", NEXP * 512, D) for i in range(2)]
        wmask_d = I("wmask", 128, 16 * 384)
        nab_d = I("nab", 128, 16 * 8 * 640)
        outT = P.dram("outT", D, NOWN, F32, "ExternalOutput")
        skind = "ExternalOutput" if DEBUG else "Internal"
        QT = P.dram("QT", 1024, NQ, BF16, skind)
        KT = P.dram("KT", 768, NK0, BF16, skind)
        VA = P.dram("VA", 128, 66 * VW, BF16, skind)
        h1T = P.dram("h1T", D, NEXT, F32, skind)
        hc1T = P.dram("hc1T", D, NCTX, F32, skind)

        arena_t = st.enter_context(nc.sbuf_tensor("arena", [128, ARENA_WORDS], F32))
        AR = Arena(Buf(arena_t, P.new_tid(), 128, ARENA_WORDS, 1.0))
        psum_t = st.enter_context(nc.psum_tensor("psum", [128, 4096], F32))
        PS = Buf(psum_t, P.new_tid(), 128, 4096, 1.0)

        def bank(b, n=512, p=128):
            return PS.v(0, p, b * 512, b * 512 + n)

        def bank_bf(b, n):
            return V(PS.t[:, b * 512:b * 512 + n // 2].bitcast(BF16), PS.tid, 0, 128, b * 512, b * 512 + n // 2, 0.5)

        def dchunks(buf, c0, c1, r0=0, nchunk=8):
            ap = buf.t[r0:r0 + nchunk * 128, c0:c1].rearrange("(c p) n -> p c n", p=128)
            return V(ap, buf.tid, r0, r0 + nchunk * 128, c0 * buf.wpe, c1 * buf.wpe, buf.wpe)

        def v3(v, k):
            return V(r3(v.ap, k), v.tid, v.p0, v.p1, v.f0, v.f1, v.wpe)

        def bcast(buf, n):
            return V(buf.t[0:1, 0:n].partition_broadcast(128), buf.tid, 0, 1, 0, n * buf.wpe, buf.wpe)

        ident32 = AR.f32(128)
        identb = AR.bf(128)
        ones = AR.f32(128)
        bones = AR.f32(128)
        lnT = AR.f32(64)
        gt = AR.f32(4)
        dl = AR.f32(256)
        sg1 = AR.f32(128)
        esink = AR.f32(8)
        eps_ln = AR.f32(1)
        eps_rms = AR.f32(1)
        nlam = AR.f32(1)
        ML = [AR.f32(48) for _ in range(2)]
        MC = [AR.f32(48) for _ in range(2)]
        sc = AR.f32(16)
        wr = [AR.f32(8 * 36) for _ in range(2)]
        brb = [AR.f32(36) for _ in range(2)]
        P.dma("sp", ident32, ident_d.v())
        P.dma("sp", bones, bones_d.v())
        P.dma("sp", lnT, lnT_d.v())
        P.dma("sp", gt, gt_d.v())
        P.dma("sp", dl, bcast(dlam_d, 256))
        P.dma("sp", sg1, bcast(subg_d, 128))
        P.dma("sp", esink, bcast(sink_d, 8))
        for i in range(2):
            P.dma("sp", v3(wr[i], 8), dchunks(wr_d[i], 0, 36))
            P.dma("sp", brb[i], bcast(br_d[i], 36))
        P.op("dve", OP("tensor_copy", out=identb.ap, in_=ident32.ap), [ident32], [identb])
        P.op("dve", OP("memset", ones.ap, 1.0), [], [ones])
        P.op("dve", OP("memset", eps_ln.ap, LN_EPS), [], [eps_ln])
        P.op("dve", OP("memset", eps_rms.ap, RMS_EPS), [], [eps_rms])
        P.op("act", OP("activation", out=esink.ap, in_=esink.ap, func=AF.Exp), [esink], [esink])
        P.op("dve", OP("tensor_scalar", out=sg1.ap, in0=sg1.ap, scalar1=1.0 - LAM_INIT0, scalar2=None, op0=ALU.mult), [sg1], [sg1])
        lt = AR.f32(128)
        ls = AR.f32(2)
        dl4 = dl.ap.rearrange("p (a b n) -> p a b n", a=2, b=2)
        P.op("dve", OP("tensor_tensor", out=lt.ap.rearrange("p (a n) -> p a n", a=2), in0=dl4[:, :, 0, :], in1=dl4[:, :, 1, :], op=ALU.mult), [dl], [lt])
        P.op("dve", OP("tensor_reduce", out=ls.ap, in_=lt.ap.rearrange("p (a n) -> p a n", a=2), axis=AX.X, op=ALU.add), [lt], [ls])
        P.op("act", OP("activation", out=ls.ap, in_=ls.ap, func=AF.Exp), [ls], [ls])
        P.op("dve", OP("tensor_tensor", out=nlam.ap, in0=ls.ap[:, 1:2], in1=ls.ap[:, 0:1], op=ALU.subtract), [ls], [nlam])
        P.op("dve", OP("tensor_scalar", out=nlam.ap, in0=nlam.ap, scalar1=-LAM_INIT0, scalar2=None, op0=ALU.add), [nlam], [nlam])

        base_top = AR.top
        P.dma("sp", v3(sc, 8), dchunks(ccT, 0, 2))
        P.op("act", OP("activation", out=sc.ap, in_=sc.ap, func=AF.Silu), [sc], [sc])
        mwb = [AR.f32(8 * 512) for _ in range(2)]
        mbt = AR.f32(48)
        for i in range(2):
            P.dma("sp", mbt, modb[i].v())
            pm = bank(6, 96)
            for pc in range(12):
                w = mwb[pc % 2]
                P.dma("sp", v3(w, 8), dchunks(modw[i], pc * 512, pc * 512 + 512))
                for j in range(4):
                    cc = pc * 4 + j
                    for dc in range(8):
                        P.op("pe", OP("matmul",
                            pm.ap[:, cc * 2:cc * 2 + 2], lhsT=w.ap[:, dc * 512 + j * 128: dc * 512 + j * 128 + 128],
                            rhs=sc.ap[:, dc * 2:dc * 2 + 2], start=(dc == 0), stop=(dc == 7)),
                            [w, sc], [pm], sig=(dc == 7))
            pm3 = pm.ap.rearrange("p (c n) -> p c n", n=2)
            P.op("dve", OP("tensor_tensor", out=ML[i].ap, in0=pm3[:, :, 0], in1=mbt.ap, op=ALU.add), [pm, mbt], [ML[i]])
            P.op("dve", OP("tensor_tensor", out=MC[i].ap, in0=pm3[:, :, 1], in1=mbt.ap, op=ALU.add), [pm, mbt], [MC[i]])
            for M in (ML[i], MC[i]):
                for k in (1, 4):
                    P.op("dve", OP("tensor_scalar", out=M.ap[:, k * 8:k * 8 + 8], in0=M.ap[:, k * 8:k * 8 + 8],
                                                                    scalar1=1.0, scalar2=None, op0=ALU.add), [M], [M])
        AR.top = base_top

        FT = AR.bf(8 * NQ)
        H = AR.f32(8 * NQ)
        free_top = AR.top

        def ln_block(ucf, nt, li, which, out_fn, tmp):
            s1, s2 = bank(6, nt), bank(7, nt)
            for c in range(8):
                uc = ucf(c)
                P.op("pe", OP("matmul", s1.ap, lhsT=ones.ap, rhs=uc.ap, start=(c == 0), stop=(c == 7)),
                     [ones, uc], [s1], sig=(c == 7))
            for c in range(8):
                uc = ucf(c)
                q = tmp["sq"].get().s(0, nt)
                P.op("act", OP("activation", out=q.ap, in_=uc.ap, func=AF.Square), [uc], [q])
                P.op("pe", OP("matmul", s2.ap, lhsT=ones.ap, rhs=q.ap, start=(c == 0), stop=(c == 7)),
                     [ones, q], [s2], sig=True)
            mean, msq, rstd = tmp["mean"].s(0, nt), tmp["msq"].s(0, nt), tmp["rstd"].s(0, nt)
            P.op("act", OP("activation", out=mean.ap, in_=s1.ap, func=AF.Copy, scale=1.0 / D), [s1], [mean])
            P.op("act", OP("activation", out=msq.ap, in_=s1.ap, func=AF.Square, scale=1.0 / D), [s1], [msq])
            P.op("dve", OP("scalar_tensor_tensor", out=rstd.ap, in0=s2.ap, scalar=1.0 / D, in1=msq.ap, op0=ALU.mult, op1=ALU.subtract),
                 [s2, msq], [rstd])
            P.op("act", OP("activation", out=rstd.ap, in_=rstd.ap, func=AF.Sqrt, bias=eps_ln.ap, scale=1.0), [rstd, eps_ln], [rstd])
            P.op("dve", OP("reciprocal", out=rstd.ap, in_=rstd.ap), [rstd], [rstd])
            gi = ((li * 2 + which) * 2 + 0) * 8
            bi = ((li * 2 + which) * 2 + 1) * 8
            for c in range(8):
                uc = ucf(c)
                o = out_fn(c)
                P.op("pool", OP("tensor_tensor", out=uc.ap, in0=uc.ap, in1=mean.ap, op=ALU.subtract), [uc, mean], [uc])
                P.op("dve", OP("tensor_tensor", out=uc.ap, in0=uc.ap, in1=rstd.ap, op=ALU.mult), [uc, rstd], [uc])
                P.op("dve", OP("tensor_scalar", out=o.ap, in0=uc.ap, scalar1=lnT.ap[:, gi + c:gi + c + 1],
                                                                      scalar2=lnT.ap[:, bi + c:bi + c + 1], op0=ALU.mult, op1=ALU.add),
                     [uc, lnT], [o])

        def layer(li):
            ab = (li == 0)
            NT = NQ if ab else NOWN

            def Hc(c, t0, t1):
                return H.s(c * NT + t0, c * NT + t1)

            AR.top = FT.f0 if False else int(FT.f0)
            wq, wqs, wk, wks, wv = AR.bf(8 * 1024), AR.bf(8 * 1024), AR.bf(8 * 768), AR.bf(8 * 768), AR.bf(8 * 640)
            for dst, src, n in ((wq, wq_d[li], 1024), (wqs, wqs_d[li], 1024), (wk, wk_d[li], 768), (wks, wks_d[li], 768), (wv, wv_d[li], 640)):
                P.dma("pool", v3(dst, 8), dchunks(src, 0, n))
            xb = Rot([AR.f32(8 * 512) for _ in range(2)])
            abuf = Rot([AR.bf(8 * 512) for _ in range(2)])
            ctab = Rot([AR.f32(512) for _ in range(2)])
            stab = Rot([AR.f32(512) for _ in range(2)])
            t1r = Rot([AR.f32(512) for _ in range(2)])
            t2r = Rot([AR.f32(512) for _ in range(2)])
            obr = Rot([AR.bf(512) for _ in range(3)])
            sqr = Rot([AR.f32(512) for _ in range(2)])
            rvr = Rot([AR.f32(512) for _ in range(2)])
            vst = Rot([AR.bf(VW) for _ in range(2)])
            for vs in vst.items:
                P.op("pool", OP("memset", vs.ap, 1.0), [], [vs])
            pbank = Rot([0, 1, 2, 3, 4, 5])
            if ab:
                q_rope, q_norm = [True] * 8, [False] * 4 + [True] * 4
                k_rope, k_norm = [True] * 6, [False] * 4 + [True] * 2
            else:
                q_rope, q_norm = [False] * 4 + [True] * 4, [False] * 8
                k_rope, k_norm = [False] * 4 + [True] * 2, [False] * 6

            def fm_proj(aT, nt, W, Ws, wn, ch, rope, norm, gcol, ct, stb, dst):
                pa = bank(pbank.get(), nt)
                for c in range(8):
                    P.op("pe", OP("matmul", pa.ap, lhsT=W.ap[:, c * wn + ch * 128: c * wn + ch * 128 + 128],
                                                      rhs=aT.ap[:, c * nt:c * nt + nt], start=(c == 0), stop=(c == 7)),
                         [W, aT], [pa], sig=(c == 7))
                ob = obr.get().s(0, nt)
                if not rope:
                    P.op("act", OP("activation", out=ob.ap, in_=pa.ap, func=AF.Copy), [pa], [ob])
                else:
                    pb = bank(pbank.get(), nt)
                    for c in range(8):
                        P.op("pe", OP("matmul", pb.ap, lhsT=Ws.ap[:, c * wn + ch * 128: c * wn + ch * 128 + 128],
                                                          rhs=aT.ap[:, c * nt:c * nt + nt], start=(c == 0), stop=(c == 7)),
                             [Ws, aT], [pb], sig=(c == 7))
                    t1, t2 = t1r.get().s(0, nt), t2r.get().s(0, nt)
                    if norm:
                        sq = sqr.get().s(0, nt)
                        P.op("act", OP("activation", out=sq.ap, in_=pa.ap, func=AF.Square), [pa], [sq])
                        pss = bank(pbank.get(), nt)
                        P.op("pe", OP("matmul", pss.ap, lhsT=bones.ap, rhs=sq.ap, start=True, stop=True), [bones, sq], [pss])
                        rv = rvr.get().s(0, nt)
                        P.op("act", OP("activation", out=rv.ap, in_=pss.ap, func=AF.Sqrt, bias=eps_rms.ap, scale=1.0 / 64), [pss, eps_rms], [rv])
                        P.op("dve", OP("reciprocal", out=rv.ap, in_=rv.ap), [rv], [rv])
                        P.op("dve", OP("scalar_tensor_tensor", out=t1.ap, in0=pa.ap, scalar=gt.ap[:, gcol:gcol + 1], in1=ct.ap, op0=ALU.mult, op1=ALU.mult),
                             [pa, gt, ct], [t1])
                        P.op("dve", OP("scalar_tensor_tensor", out=t2.ap, in0=pb.ap, scalar=gt.ap[:, gcol + 1:gcol + 2], in1=stb.ap, op0=ALU.mult, op1=ALU.mult),
                             [pb, gt, stb], [t2])
                        P.op("pool", OP("tensor_tensor", out=t1.ap, in0=t1.ap, in1=t2.ap, op=ALU.add), [t1, t2], [t1])
                        P.op("dve", OP("tensor_tensor", out=ob.ap, in0=t1.ap, in1=rv.ap, op=ALU.mult), [t1, rv], [ob])
                    else:
                        P.op("dve", OP("tensor_tensor", out=t1.ap, in0=pa.ap, in1=ct.ap, op=ALU.mult), [pa, ct], [t1])
                        P.op("dve", OP("tensor_tensor", out=t2.ap, in0=pb.ap, in1=stb.ap, op=ALU.mult), [pb, stb], [t2])
                        P.op("pool", OP("tensor_tensor", out=ob.ap, in0=t1.ap, in1=t2.ap, op=ALU.add), [t1, t2], [ob])
                P.dma("sp", dst, ob)

            def source(src, ntok, M, ctd, std, toff, want_q, q_off, want_kv, k_off):
                def load(t0):
                    nt = min(512, ntok - t0)
                    x = xb.get().s(0, 8 * nt)
                    P.dma("sp", v3(x, 8), dchunks(src, t0, t0 + nt))
                    ct, stb = ctab.get().s(0, nt), stab.get().s(0, nt)
                    P.dma("sp", ct, ctd.v(0, 128, toff + t0, toff + t0 + nt))
                    P.dma("sp", stb, std.v(0, 128, toff + t0, toff + t0 + nt))
                    return (x, ct, stb)

                nxt = load(0)
                for t0 in range(0, ntok, 512):
                    nt = min(512, ntok - t0)
                    x, ct, stb = nxt
                    if t0 + 512 < ntok:
                        nxt = load(t0 + 512)
                    aT = abuf.get().s(0, 8 * nt)
                    for c in range(8):
                        P.op("act", OP("activation", out=aT.ap[:, c * nt:c * nt + nt], in_=x.ap[:, c * nt:c * nt + nt], func=AF.Identity,
                                       scale=M.ap[:, 8 + c:9 + c], bias=M.ap[:, c:c + 1]), [x, M], [aT])
                    if want_kv:
                        for ch in range(6):
                            fm_proj(aT, nt, wk, wks, 768, ch, k_rope[ch], k_norm[ch], 2, ct, stb,
                                    KT.v(ch * 128, ch * 128 + 128, k_off + t0, k_off + t0 + nt))
                        for tt in range(nt // 128):
                            pv1, pv2 = bank(pbank.get(), 512), bank(pbank.get(), 128)
                            for c in range(8):
                                lh = aT.ap[:, c * nt + tt * 128:c * nt + tt * 128 + 128]
                                P.op("pe", OP("matmul", pv1.ap, lhsT=lh, rhs=wv.ap[:, c * 640:c * 640 + 512], start=(c == 0), stop=(c == 7)),
                                     [aT, wv], [pv1], sig=(c == 7))
                            for c in range(8):
                                lh = aT.ap[:, c * nt + tt * 128:c * nt + tt * 128 + 128]
                                P.op("pe", OP("matmul", pv2.ap, lhsT=lh, rhs=wv.ap[:, c * 640 + 512:c * 640 + 640], start=(c == 0), stop=(c == 7)),
                                     [aT, wv], [pv2], sig=(c == 7))
                            vs = vst.get()
                            if ab:
                                o1 = vs.ap[:, 0:516].rearrange("p (h n) -> p h n", n=129)[:, :, 0:128]
                                i1 = pv1.ap.rearrange("p (h n) -> p h n", n=128)
                                o2 = vs.ap[:, 516:646].rearrange("p (h n) -> p h n", n=65)[:, :, 0:64]
                            else:
                                o1 = vs.ap[:, 0:520].rearrange("p (h n) -> p h n", n=65)[:, :, 0:64]
                                i1 = pv1.ap.rearrange("p (h n) -> p h n", n=64)
                                o2 = vs.ap[:, 520:650].rearrange("p (h n) -> p h n", n=65)[:, :, 0:64]
                            i2 = pv2.ap.rearrange("p (h n) -> p h n", n=64)
                            P.op("act", OP("activation", out=o1, in_=i1, func=AF.Copy), [pv1], [vs])
                            P.op("dve", OP("tensor_copy", out=o2, in_=i2), [pv2], [vs])
                            kt = (k_off + t0) // 128 + tt
                            P.dma("sp", VA.v(0, 128, kt * VW, kt * VW + VW), vs)
                    if want_q:
                        for ch in range(8):
                            fm_proj(aT, nt, wq, wqs, 1024, ch, q_rope[ch], q_norm[ch], 0, ct, stb,
                                    QT.v(ch * 128, ch * 128 + 128, q_off + t0, q_off + t0 + nt))

            if ab:
                source(xT, SEQ, ML[0], cK_d, sK_d, 0, False, 0, True, 0)
                source(cxT, NCTX, MC[0], cK_d, sK_d, SEQ, True, NEXT, True, SEQ)
                source(xeT, NEXT, ML[0], cQ_d, sQ_d, 0, True, 0, False, 0)
                nkt = SEQ // 128 + 2
            else:
                source(h1T, NEXT, ML[1], cQ_d, sQ_d, 0, True, 0, True, 0)
                source(hc1T, NCTX, MC[1], cQ_d, sQ_d, NEXT, False, 0, True, NEXT)
                nkt = NEXT // 128 + 2

            AR.top = int(H.f0)
            O = FT
            qbuf = Rot([AR.bf(NQ) for _ in range(2)])
            kbuf = Rot([AR.bf(nkt * 128) for _ in range(2)])
            vbuf = Rot([AR.bf(nkt * 130) for _ in range(2)])
            ptile = Rot([AR.bf(1024) for _ in range(3)])
            rz = Rot([AR.f32(1) for _ in range(6)])
            od1 = [AR.f32(128) for _ in range(4)]
            t_o2 = Rot([AR.f32(128) for _ in range(2)])
            t_od = Rot([AR.f32(128) for _ in range(2)])
            t_sq = Rot([AR.f32(128) for _ in range(2)])
            ssq = Rot([AR.f32(1) for _ in range(4)])
            stmp = Rot([AR.f32(640) for _ in range(2)])
            if not ab:
                wm = AR.f32(16 * 384)
                P.dma("sp", wm, wmask_d.v())
                nbb = Rot([AR.f32(640) for _ in range(3)])
                nb_pref = [None]

            def attend512(kt_, qt_, vv, vcw, voff, dv, r0, q0, nq, kts, fin):
                nsub = nq // 128
                accs = [bank(4 + s, dv + 1) for s in range(nsub)]
                pairs = [kts[i:i + 2] for i in range(0, len(kts), 2)]
                npair = len(pairs)

                def stage_a(pi):
                    pr = pairs[pi]
                    base = (pi % 2) * 1024
                    sreg = PS.v(0, 128, base, base + len(pr) * nq)
                    for jj, kt in enumerate(pr):
                        P.op("pe", OP("matmul", sreg.ap[:, jj * nq:jj * nq + nq], lhsT=kt_.ap[r0:r0 + 64, kt * 128:kt * 128 + 128],
                                      rhs=qt_.ap[r0:r0 + 64, q0:q0 + nq], start=True, stop=True), [kt_, qt_], [sreg], sig=(jj == len(pr) - 1))
                    pt = ptile.get().s(0, len(pr) * nq)
                    P.op("act", OP("activation", out=pt.ap, in_=sreg.ap, func=AF.Exp, scale=0.125), [sreg], [pt])
                    return pt

                def stage_b(pi, pt):
                    pr = pairs[pi]
                    for jj, kt in enumerate(pr):
                        for s in range(nsub):
                            first = (pi == 0 and jj == 0)
                            last = (pi == npair - 1 and jj == len(pr) - 1)
                            P.op("pe", OP("matmul", accs[s].ap, lhsT=pt.ap[:, jj * nq + s * 128:jj * nq + s * 128 + 128],
                                          rhs=vv.ap[:, kt * vcw + voff:kt * vcw + voff + dv + 1], start=first, stop=last),
                                 [pt, vv], [accs[s]], sig=(s == nsub - 1 and jj == len(pr) - 1))

                prev = None
                for pi in range(npair):
                    cur = stage_a(pi)
                    if prev is not None:
                        stage_b(pi - 1, prev)
                    prev = cur
                stage_b(npair - 1, prev)
                for s in range(nsub):
                    fin(s, accs[s], q0 // 128 + s)

            for u in range(8):
                kc = u if u < 4 else 4 + (u - 4) // 2
                qt_, kt_, vt_ = qbuf.get(), kbuf.get(), vbuf.get()
                P.dma("sp", qt_, QT.v(u * 128, u * 128 + 128, 0, NQ))
                P.dma("sp", kt_, KT.v(kc * 128, kc * 128 + 128, 0, nkt * 128))
                if ab:
                    vc0, vcw = (u * 129, 129) if u < 4 else (516 + ((u - 4) // 2) * 65, 65)
                else:
                    vc0, vcw = (u * 130, 130) if u < 4 else (520 + ((u - 4) // 2) * 65, 65)
                vv = vt_.s(0, nkt * vcw)
                for g0 in range(0, nkt, 11):
                    src = V(VA.t[:, g0 * VW:(g0 + 11) * VW].rearrange("p (t c) -> p t c", c=VW)[:, :, vc0:vc0 + vcw],
                            VA.tid, 0, 128, g0 * VW * 0.5, (g0 + 11) * VW * 0.5, 0.5)
                    dstv = V(vv.ap[:, g0 * vcw:(g0 + 11) * vcw].rearrange("p (t c) -> p t c", c=vcw), vv.tid, 0, 128,
                             vv.f0 + g0 * vcw * 0.5, vv.f0 + (g0 + 11) * vcw * 0.5, 0.5)
                    P.dma("sp", dstv, src)

                if ab:
                    dv = 128 if u < 4 else 64
                    qblocks = [(q0, 256, list(range(nkt))) for q0 in range(0, NEXT, 256)] + [(NEXT, 256, [nkt - 2, nkt - 1])]
                    for (q0, nq, kts) in qblocks:
                        nsub = nq // 128
                        accs = [[bank(4 + c * 2 + s, dv + 1) for s in range(nsub)] for c in range(2)]
                        pairs = [kts[i:i + 2] for i in range(0, len(kts), 2)]
                        npair = len(pairs)

                        def st_a(pi):
                            pr = pairs[pi]
                            base = (pi % 2) * 1024
                            sreg = PS.v(0, 128, base, base + 1024)
                            for jj, kt in enumerate(pr):
                                for c in range(2):
                                    P.op("pe", OP("matmul", sreg.ap[:, c * 512 + jj * nq:c * 512 + jj * nq + nq],
                                                  lhsT=kt_.ap[64 * c:64 * c + 64, kt * 128:kt * 128 + 128],
                                                  rhs=qt_.ap[64 * c:64 * c + 64, q0:q0 + nq], start=True, stop=True),
                                         [kt_, qt_], [sreg], sig=(jj == len(pr) - 1 and c == 1))
                            pt = ptile.get()
                            P.op("act", OP("activation", out=pt.ap, in_=sreg.ap, func=AF.Exp, scale=0.125), [sreg], [pt])
                            return pt

                        def st_b(pi, pt):
                            pr = pairs[pi]
                            for c in range(2):
                                for jj, kt in enumerate(pr):
                                    for s_ in range(nsub):
                                        first = (pi == 0 and jj == 0)
                                        last = (pi == npair - 1 and jj == len(pr) - 1)
                                        col = c * 512 + jj * nq + s_ * 128
                                        P.op("pe", OP("matmul", accs[c][s_].ap, lhsT=pt.ap[:, col:col + 128],
                                                      rhs=vv.ap[:, kt * vcw:kt * vcw + dv + 1], start=first, stop=last),
                                             [pt, vv], [accs[c][s_]], sig=(c == 1 and s_ == nsub - 1 and jj == len(pr) - 1))

                        prev = None
                        for pi in range(npair):
                            cur = st_a(pi)
                            if prev is not None:
                                st_b(pi - 1, prev)
                            prev = cur
                        st_b(npair - 1, prev)
                        for s_ in range(nsub):
                            ot = q0 // 128 + s_
                            a0, a1 = accs[0][s_], accs[1][s_]
                            r0_, r1_ = rz.get(), rz.get()
                            P.op("dve", OP("reciprocal", out=r0_.ap, in_=a0.ap[:, dv:dv + 1]), [a0], [r0_])
                            P.op("dve", OP("reciprocal", out=r1_.ap, in_=a1.ap[:, dv:dv + 1]), [a1], [r1_])
                            if u < 4:
                                d1 = od1[s_]
                                o2, od, sq, ss = t_o2.get(), t_od.get(), t_sq.get(), ssq.get()
                                P.op("dve", OP("tensor_scalar", out=d1.ap, in0=a0.ap[:, 0:128], scalar1=r0_.ap[:, 0:1], scalar2=None, op0=ALU.mult), [a0, r0_], [d1])
                                P.op("dve", OP("tensor_scalar", out=o2.ap, in0=a1.ap[:, 0:128], scalar1=r1_.ap[:, 0:1], scalar2=None, op0=ALU.mult), [a1, r1_], [o2])
                                P.op("dve", OP("scalar_tensor_tensor", out=od.ap, in0=o2.ap, scalar=nlam.ap[:, 0:1], in1=d1.ap, op0=ALU.mult, op1=ALU.add), [o2, nlam, d1], [od])
                                P.op("pool", OP("tensor_tensor", out=sq.ap, in0=od.ap, in1=od.ap, op=ALU.mult), [od], [sq])
                                P.op("dve", OP("tensor_reduce", out=ss.ap, in_=sq.ap, axis=AX.X, op=ALU.add), [sq], [ss])
                                P.op("act", OP("activation", out=ss.ap, in_=ss.ap, func=AF.Sqrt, bias=eps_rms.ap, scale=1.0 / 128), [ss, eps_rms], [ss])
                                P.op("dve", OP("reciprocal", out=ss.ap, in_=ss.ap), [ss], [ss])
                                oo = O.s(ot * 1024 + u * 128, ot * 1024 + u * 128 + 128)
                                P.op("dve", OP("scalar_tensor_tensor", out=oo.ap, in0=od.ap, scalar=ss.ap[:, 0:1], in1=sg1.ap, op0=ALU.mult, op1=ALU.mult), [od, ss, sg1], [oo])
                            else:
                                for c, (a, r) in enumerate(((a0, r0_), (a1, r1_))):
                                    col = 512 + ((u - 4) * 2 + c) * 64
                                    oo = O.s(ot * 1024 + col, ot * 1024 + col + 64)
                                    P.op("dve", OP("tensor_scalar", out=oo.ap, in0=a.ap[:, 0:64], scalar1=r.ap[:, 0:1], scalar2=None, op0=ALU.mult), [a, r], [oo])
                else:
                    for c in range(2):
                        r0 = 64 * c

                        def l1_a(t, c=c, r0=r0, u=u):
                            if u < 4:
                                h = 2 * u + c
                                kts = list(range(t, t + 5)) + [20, 21]
                                nb = 5
                                if t == 0:
                                    nb_pref[0] = nbb.get()
                                    P.dma("sp", nb_pref[0], nab_d.v(0, 128, (t * 8 + h) * 640, (t * 8 + h) * 640 + 640))
                                bt = nb_pref[0]
                                if t + 1 < 16:
                                    nb_pref[0] = nbb.get()
                                    P.dma("sp", nb_pref[0], nab_d.v(0, 128, ((t + 1) * 8 + h) * 640, ((t + 1) * 8 + h) * 640 + 640))
                                voff = c * 65
                                col = 512 + h * 64
                            else:
                                h = (u - 4) * 2 + c
                                kts = list(range(t + 1, t + 4)) + [20, 21]
                                nb = 3
                                bt = wm.s(t * 384, t * 384 + 384)
                                voff = 0
                                col = h * 64
                            nk = len(kts)
                            sreg = PS.v(0, 128, (t % 2) * 1024, (t % 2) * 1024 + nk * 128)
                            qs = qt_.ap[r0:r0 + 64, 256 + t * 128:256 + t * 128 + 128]
                            for i, kt in enumerate(kts):
                                P.op("pe", OP("matmul", sreg.ap[:, i * 128:i * 128 + 128], lhsT=kt_.ap[r0:r0 + 64, kt * 128:kt * 128 + 128], rhs=qs, start=True, stop=True),
                                     [kt_, qt_], [sreg], sig=(i == nk - 1))
                            pt = ptile.get().s(0, nk * 128)
                            tm = stmp.get().s(0, nb * 128)
                            P.op("dve", OP("scalar_tensor_tensor", out=tm.ap, in0=sreg.ap[:, 0:nb * 128], scalar=0.125, in1=bt.ap[:, 0:nb * 128], op0=ALU.mult, op1=ALU.add), [sreg, bt], [tm])
                            P.op("act", OP("activation", out=pt.ap[:, 0:nb * 128], in_=tm.ap, func=AF.Exp), [tm], [pt])
                            P.op("act", OP("activation", out=pt.ap[:, nb * 128:nk * 128], in_=sreg.ap[:, nb * 128:nk * 128], func=AF.Exp, scale=0.125), [sreg], [pt])
                            return (t, h, kts, voff, col, pt)

                        def l1_b(stt, u=u):
                            t, h, kts, voff, col, pt = stt
                            nk = len(kts)
                            a = bank(4 + (t % 2), 65)
                            for i, kt in enumerate(kts):
                                P.op("pe", OP("matmul", a.ap, lhsT=pt.ap[:, i * 128:i * 128 + 128], rhs=vv.ap[:, kt * vcw + voff:kt * vcw + voff + 65], start=(i == 0), stop=(i == nk - 1)),
                                     [pt, vv], [a], sig=(i == nk - 1))
                            r = rz.get()
                            if u >= 4:
                                P.op("dve", OP("tensor_scalar", out=r.ap, in0=a.ap[:, 64:65], scalar1=esink.ap[:, h:h + 1], scalar2=None, op0=ALU.add), [a, esink], [r])
                                P.op("dve", OP("reciprocal", out=r.ap, in_=r.ap), [r], [r])
                            else:
                                P.op("dve", OP("reciprocal", out=r.ap, in_=a.ap[:, 64:65]), [a], [r])
                            oo = O.s(t * 1024 + col, t * 1024 + col + 64)
                            P.op("dve", OP("tensor_scalar", out=oo.ap, in0=a.ap[:, 0:64], scalar1=r.ap[:, 0:1], scalar2=None, op0=ALU.mult), [a, r], [oo])

                        prev = None
                        for t in range(16):
                            cur = l1_a(t)
                            if prev is not None:
                                l1_b(prev)
                            prev = cur
                        l1_b(prev)

            AR.top = free_top
            wo = AR.bf(8 * 1024)
            P.dma("pool", v3(wo, 8), dchunks(wo_d[li], 0, 1024))
            oTr = Rot([AR.bf(8 * 512) for _ in range(2)])
            lntmp = {"sq": Rot([AR.f32(512) for _ in range(2)]), "mean": AR.f32(512), "msq": AR.f32(512), "rstd": AR.f32(512)}
            if ab:
                blocks = [(t0, 512, xeT, t0, ML[0]) for t0 in range(0, NEXT, 512)] + [(NEXT, 256, cxT, 0, MC[0])]
            else:
                blocks = [(t0, 512, h1T, 256 + t0, ML[1]) for t0 in range(0, NOWN, 512)]
            pb3 = Rot([0, 1, 2, 3, 4, 5])
            for (t0, nt, src, c0, M) in blocks:
                for c in range(8):
                    P.dma("sp", Hc(c, t0, t0 + nt), src.v(c * 128, c * 128 + 128, c0, c0 + nt))
            for (t0, nt, src, c0, M) in blocks:
                oT = oTr.get().s(0, 8 * nt)
                for ch in range(8):
                    b = pb3.get()
                    pst = bank_bf(b, nt)
                    for tt in range(nt // 128):
                        tile = t0 // 128 + tt
                        oin = O.s(tile * 1024 + ch * 128, tile * 1024 + ch * 128 + 128)
                        P.op("pe", OP("transpose", out=pst.ap[:, tt * 128:tt * 128 + 128], in_=oin.ap, identity=identb.ap),
                             [oin, identb], [pst], sig=(tt == nt // 128 - 1))
                    od_ = oT.s(ch * nt, ch * nt + nt)
                    if ch % 2 == 0:
                        P.op("act", OP("activation", out=od_.ap, in_=pst.ap, func=AF.Copy), [pst], [od_])
                    else:
                        P.op("dve", OP("tensor_copy", out=od_.ap, in_=pst.ap), [pst], [od_])
                for dc in range(8):
                    pp = bank(pb3.get(), nt)
                    for ch in range(8):
                        P.op("pe", OP("matmul", pp.ap, lhsT=wo.ap[:, ch * 1024 + dc * 128:ch * 1024 + dc * 128 + 128],
                                                                                    rhs=oT.ap[:, ch * nt:ch * nt + nt], start=(ch == 0), stop=(ch == 7)),
                             [wo, oT], [pp], sig=(ch == 7))
                    hc = Hc(dc, t0, t0 + nt)
                    P.op("act", OP("activation", out=hc.ap, in_=hc.ap, func=AF.Copy, scale=ALPHA), [hc], [hc])
                    P.op("dve", OP("scalar_tensor_tensor", out=hc.ap, in0=pp.ap, scalar=M.ap[:, 16 + dc:17 + dc], in1=hc.ap, op0=ALU.mult, op1=ALU.add),
                         [pp, M, hc], [hc])
                ln_block(lambda c: Hc(c, t0, t0 + nt), nt, li, 0, lambda c: Hc(c, t0, t0 + nt), lntmp)

            AR.top = free_top
            WT = AR.f32(NT, P=32)
            moe_top = AR.top
            ftmp = AR.f32(1024)
            Lg = Rot([AR.f32(36) for _ in range(2)])
            sm = Rot([AR.f32(8) for _ in range(24)])
            em_r = Rot([AR.f32(32) for _ in range(2)])
            wt_r = Rot([AR.f32(32) for _ in range(2)])
            w2t_r = Rot([AR.f32(32) for _ in range(2)])
            pb4 = Rot([0, 1, 2, 3, 4, 5, 6, 7])
            mblocks = [(t0, min(512, NT - t0)) for t0 in range(0, NT, 512)]

            def Mof(t0):
                return (MC[li] if (ab and t0 >= NEXT) else ML[li])

            for (t0, nt) in mblocks:
                M = Mof(t0)
                for tt in range(nt // 128):
                    tk = t0 + tt * 128
                    for c in range(8):
                        hc = Hc(c, tk, tk + 128)
                        fo = ftmp.s(c * 128, c * 128 + 128)
                        P.op("act", OP("activation", out=fo.ap, in_=hc.ap, func=AF.Identity, scale=M.ap[:, 32 + c:33 + c], bias=M.ap[:, 24 + c:25 + c]), [hc, M], [fo])
                    pl = bank(pb4.get(), 36)
                    for c in range(8):
                        fo = ftmp.s(c * 128, c * 128 + 128)
                        P.op("pe", OP("matmul", pl.ap, lhsT=fo.ap, rhs=wr[li].ap[:, c * 36:c * 36 + 36], start=(c == 0), stop=(c == 7)),
                             [fo, wr[li]], [pl], sig=(c == 7))
                    L = Lg.get()
                    P.op("dve", OP("tensor_tensor", out=L.ap, in0=pl.ap, in1=brb[li].ap, op=ALU.add), [pl, brb[li]], [L])
                    gmax, ngmax, gexp, gsum, pen, m8, dd, w1, w2 = [sm.get() for _ in range(9)]
                    em, Wt, W2 = em_r.get(), wt_r.get(), w2t_r.get()
                    P.op("dve", OP("tensor_reduce", out=gmax.ap[:, 0:1], in_=L.ap[:, 0:4], axis=AX.X, op=ALU.max), [L], [gmax])
                    P.op("dve", OP("tensor_scalar", out=ngmax.ap[:, 0:1], in0=gmax.ap[:, 0:1], scalar1=-1.0, scalar2=None, op0=ALU.mult), [gmax], [ngmax])
                    P.op("act", OP("activation", out=gexp.ap[:, 0:4], in_=L.ap[:, 0:4], func=AF.Exp, bias=ngmax.ap[:, 0:1], scale=1.0), [L, ngmax], [gexp])
                    P.op("dve", OP("tensor_reduce", out=gsum.ap[:, 0:1], in_=gexp.ap[:, 0:4], axis=AX.X, op=ALU.add), [gexp], [gsum])
                    P.op("dve", OP("reciprocal", out=gsum.ap[:, 0:1], in_=gsum.ap[:, 0:1]), [gsum], [gsum])
                    P.op("dve", OP("tensor_scalar", out=pen.ap[:, 0:4], in0=L.ap[:, 0:4], scalar1=gmax.ap[:, 0:1], scalar2=1e30, op0=ALU.is_equal, op1=ALU.mult), [L, gmax], [pen])
                    P.op("dve", OP("tensor_scalar", out=pen.ap[:, 0:4], in0=pen.ap[:, 0:4], scalar1=-1e30, scalar2=None, op0=ALU.add), [pen], [pen])
                    for g in range(4):
                        P.op("dve", OP("tensor_scalar", out=em.ap[:, 8 * g:8 * g + 8], in0=L.ap[:, 4 + 8 * g:12 + 8 * g], scalar1=pen.ap[:, g:g + 1], scalar2=None, op0=ALU.add),
                             [L, pen], [em])
                    P.op("dve", OP("max", out=m8.ap, in_=em.ap), [em], [m8])
                    P.op("dve", OP("tensor_tensor", out=dd.ap[:, 0:1], in0=m8.ap[:, 1:2], in1=m8.ap[:, 0:1], op=ALU.subtract), [m8], [dd])
                    P.op("act", OP("activation", out=dd.ap[:, 0:1], in_=dd.ap[:, 0:1], func=AF.Exp), [dd], [dd])
                    P.op("dve", OP("tensor_scalar", out=w1.ap[:, 0:1], in0=dd.ap[:, 0:1], scalar1=1.0, scalar2=None, op0=ALU.add), [dd], [w1])
                    P.op("dve", OP("reciprocal", out=w1.ap[:, 0:1], in_=w1.ap[:, 0:1]), [w1], [w1])
                    P.op("dve", OP("tensor_tensor", out=w1.ap[:, 0:1], in0=w1.ap[:, 0:1], in1=gsum.ap[:, 0:1], op=ALU.mult), [w1, gsum], [w1])
                    P.op("dve", OP("tensor_tensor", out=w2.ap[:, 0:1], in0=w1.ap[:, 0:1], in1=dd.ap[:, 0:1], op=ALU.mult), [w1, dd], [w2])
                    P.op("dve", OP("tensor_scalar", out=Wt.ap, in0=em.ap, scalar1=m8.ap[:, 0:1], scalar2=w1.ap[:, 0:1], op0=ALU.is_equal, op1=ALU.mult), [em, m8, w1], [Wt])
                    P.op("dve", OP("tensor_scalar", out=W2.ap, in0=em.ap, scalar1=m8.ap[:, 1:2], scalar2=w2.ap[:, 0:1], op0=ALU.is_equal, op1=ALU.mult), [em, m8, w2], [W2])
                    P.op("dve", OP("tensor_tensor", out=Wt.ap, in0=Wt.ap, in1=W2.ap, op=ALU.add), [Wt, W2], [Wt])
                    ptr = bank(pb4.get(), 128, p=32)
                    P.op("pe", OP("transpose", out=ptr.ap, in_=Wt.ap, identity=ident32.ap), [Wt, ident32], [ptr])
                    wts = WT.s(tk, tk + 128)
                    P.op("act", OP("activation", out=wts.ap, in_=ptr.ap, func=AF.Copy), [ptr], [wts])
                for c in range(8):
                    hc = Hc(c, t0, t0 + nt)
                    fo = FT.s(c * NT + t0, c * NT + t0 + nt)
                    P.op("act", OP("activation", out=fo.ap, in_=hc.ap, func=AF.Identity, scale=M.ap[:, 32 + c:33 + c], bias=M.ap[:, 24 + c:25 + c]), [hc, M], [fo])
                    P.op("act", OP("activation", out=hc.ap, in_=hc.ap, func=AF.Copy, scale=ALPHA), [hc], [hc])

            AR.top = moe_top
            w1b = Rot([AR.bf(8 * 512) for _ in range(2)])
            w3b = Rot([AR.bf(8 * 512) for _ in range(2)])
            w2b = Rot([AR.bf(4 * 1024) for _ in range(1)])
            gb = Rot([AR.bf(4 * 512) for _ in range(2)])
            wbc = Rot([AR.f32(512) for _ in range(2)])
            sb_ = Rot([AR.f32(512) for _ in range(2)])
            Eb = Rot([AR.f32(128, P=32) for _ in range(2)])
            def moe_h(ex, w1e, w3e, E, t0, nt):
                pw = bank(pb4.get(), nt)
                wts = WT.s(t0, t0 + nt)
                P.op("pe", OP("matmul", pw.ap, lhsT=E.ap, rhs=wts.ap, start=True, stop=True), [E, wts], [pw])
                wb = wbc.get().s(0, nt)
                P.op("act", OP("activation", out=wb.ap, in_=pw.ap, func=AF.Copy), [pw], [wb])
                g = gb.get().s(0, 4 * nt)
                for ec in range(4):
                    p1, p3 = bank(pb4.get(), nt), bank(pb4.get(), nt)
                    for c in range(8):
                        fr = FT.s(c * NT + t0, c * NT + t0 + nt)
                        P.op("pe", OP("matmul", p1.ap, lhsT=w1e.ap[:, c * 512 + ec * 128:c * 512 + ec * 128 + 128], rhs=fr.ap, start=(c == 0), stop=(c == 7)),
                             [w1e, fr], [p1], sig=(c == 7))
                    for c in range(8):
                        fr = FT.s(c * NT + t0, c * NT + t0 + nt)
                        P.op("pe", OP("matmul", p3.ap, lhsT=w3e.ap[:, c * 512 + ec * 128:c * 512 + ec * 128 + 128], rhs=fr.ap, start=(c == 0), stop=(c == 7)),
                             [w3e, fr], [p3], sig=(c == 7))
                    sv = sb_.get().s(0, nt)
                    P.op("act", OP("activation", out=sv.ap, in_=p1.ap, func=AF.Silu), [p1], [sv])
                    P.op("dve", OP("tensor_tensor", out=sv.ap, in0=p3.ap, in1=sv.ap, op=ALU.mult), [p3, sv], [sv])
                    gv = g.s(ec * nt, ec * nt + nt)
                    P.op("pool", OP("tensor_tensor", out=gv.ap, in0=sv.ap, in1=wb.ap, op=ALU.mult), [sv, wb], [gv])
                return g

            def moe_y(w2e, g, t0, nt):
                M = Mof(t0)
                for dc in range(8):
                    py = bank(pb4.get(), nt)
                    for ec in range(4):
                        P.op("pe", OP("matmul", py.ap, lhsT=w2e.ap[:, ec * 1024 + dc * 128:ec * 1024 + dc * 128 + 128],
                                      rhs=g.ap[:, ec * nt:ec * nt + nt], start=(ec == 0), stop=(ec == 3)),
                             [w2e, g], [py], sig=(ec == 3))
                    hc = Hc(dc, t0, t0 + nt)
                    P.op("dve", OP("scalar_tensor_tensor", out=hc.ap, in0=py.ap, scalar=M.ap[:, 40 + dc:41 + dc], in1=hc.ap, op0=ALU.mult, op1=ALU.add),
                         [py, M, hc], [hc])

            def wload(ex):
                w1e, w3e = w1b.get(), w3b.get()
                P.dma("pool", v3(w1e, 8), dchunks(w1_d[li], 0, 512, r0=ex * 1024))
                P.dma("pool", v3(w3e, 8), dchunks(w3_d[li], 0, 512, r0=ex * 1024))
                return (w1e, w3e)

            pend = None
            wnext = wload(0)
            for ex in range(NEXP):
                w1e, w3e = wnext
                if ex + 1 < NEXP:
                    wnext = wload(ex + 1)
                E = Eb.get()
                P.op("dve", OP("tensor_copy", out=E.ap, in_=ident32.ap[0:32, ex:ex + 1].to_broadcast([32, 128])), [ident32], [E])
                w2e = None
                for (t0, nt) in mblocks:
                    g = moe_h(ex, w1e, w3e, E, t0, nt)
                    if pend is not None:
                        moe_y(*pend)
                    if w2e is None:
                        w2e = w2b.get()
                        P.dma("pool", v3(w2e, 4), dchunks(w2_d[li], 0, 1024, r0=ex * 512, nchunk=4))
                    pend = (w2e, g, t0, nt)
            moe_y(*pend)

            AR.top = moe_top
            lntmp = {"sq": Rot([AR.f32(512) for _ in range(2)]), "mean": AR.f32(512), "msq": AR.f32(512), "rstd": AR.f32(512)}
            for (t0, nt) in mblocks:
                ln_block(lambda c: Hc(c, t0, t0 + nt), nt, li, 1, lambda c: Hc(c, t0, t0 + nt), lntmp)
                for c in range(8):
                    if ab:
                        if t0 < NEXT:
                            dst = h1T.v(c * 128, c * 128 + 128, t0, t0 + nt)
                        else:
                            dst = hc1T.v(c * 128, c * 128 + 128, 0, nt)
                    else:
                        dst = outT.v(c * 128, c * 128 + 128, t0, t0 + nt)
                    P.dma("sp", dst, Hc(c, t0, t0 + nt))

        layer(0)
        layer(1)
        P.finish()
        P.emit()
    return nc


def _swap_cols(w):
    n = w.shape[1]
    idx = np.arange(n).reshape(n // 64, 2, 32)[:, ::-1, :].reshape(n)
    return w[:, idx]


def _rope_tables(pos):
    pos = np.asarray(pos)
    valid = pos >= 0
    p = np.where(valid, pos, 0)
    row = (p // 64).astype(np.float32)
    col = (p % 64).astype(np.float32)
    inv = (np.float32(10000.0) ** (-np.arange(16, dtype=np.float32) / np.float32(16))).astype(np.float32)
    ang = np.concatenate([row[:, None] * inv, col[:, None] * inv], -1).astype(np.float32)
    cos = np.cos(ang).astype(np.float32)
    sin = np.sin(ang).astype(np.float32)
    cos = np.where(valid[:, None], cos, np.float32(1.0))
    sin = np.where(valid[:, None], sin, np.float32(0.0))
    c64 = np.concatenate([cos, cos], 1)
    s64 = np.concatenate([-sin, sin], 1)
    cT = np.concatenate([c64, c64], 1).T
    sT = np.concatenate([s64, s64], 1).T
    return np.ascontiguousarray(cT, dtype=np.float32), np.ascontiguousarray(sT, dtype=np.float32)


def _ext_positions(j):
    pos = 2048 * j - 256 + np.arange(NEXT)
    if j == 0:
        pos[0:256] = 256 + np.arange(256)
    if j == 3:
        pos[2304:2560] = 7680 + np.arange(256)
    return pos


def _first_occurrence(kp):
    seen, out = set(), np.zeros(len(kp), dtype=bool)
    for i, p in enumerate(kp):
        if p not in seen:
            seen.add(p)
            out[i] = True
    return out


def _layer1_tables(j, rpb):
    pos = _ext_positions(j)
    wmask = np.full((128, 16, 3, 128), NEG, dtype=np.float32)
    nab = np.full((128, 16, 8, 5, 128), NEG, dtype=np.float32)
    for t in range(16):
        qp = 2048 * j + 128 * t + np.arange(128)
        kp = pos[128 * (t + 1):128 * (t + 4)]
        first = _first_occurrence(kp)
        ok = (np.abs(qp[None, :] - kp[:, None]) <= 128) & first[:, None]
        m = np.where(ok, np.float32(0.0), np.float32(NEG)).reshape(3, 128, 128)
        wmask[:, t] = m.transpose(1, 0, 2)
        kp = pos[128 * t:128 * (t + 5)]
        first = _first_occurrence(kp)
        qr, qc = qp // 64, qp % 64
        kr, kcl = kp // 64, kp % 64
        r0 = np.clip(qr - 4, 0, 120)
        c0 = np.clip(qc - 8, 0, 48)
        ok = ((kr[:, None] >= r0[None, :]) & (kr[:, None] < r0[None, :] + 8) &
              (kcl[:, None] >= c0[None, :]) & (kcl[:, None] < c0[None, :] + 16) & first[:, None])
        ro = np.clip(kr[:, None] - qr[None, :] + 7, 0, 14)
        co = np.clip(kcl[:, None] - qc[None, :] + 15, 0, 30)
        for h in range(8):
            b = np.where(ok, rpb[h][ro, co], np.float32(NEG)).astype(np.float32).reshape(5, 128, 128)
            nab[:, t, h] = b.transpose(1, 0, 2)
    return wmask.reshape(128, 16 * 384), nab.reshape(128, 16 * 8 * 640)


_NC_CACHE = {}


def kernel(x, c, ctx, c_ctx, mod_w, mod_b, ln_g, ln_b, ab_w_in, ab_w_out, diff_lambda, diff_subln_g, gqa_qk_g,
           cd_w_in, cd_w_out, win_sink, na_rpb, moe_w_group, moe_b_group, moe_w_router, moe_b_router,
           moe_w1, moe_w3, moe_w2):
    f = lambda a: np.ascontiguousarray(np.asarray(a), dtype=np.float32)
    x, c, ctx, c_ctx = f(x), f(c), f(ctx), f(c_ctx)
    mod_w, mod_b, ln_g, ln_b = f(mod_w), f(mod_b), f(ln_g), f(ln_b)
    ab_w_in, ab_w_out, cd_w_in, cd_w_out = f(ab_w_in)[0], f(ab_w_out)[0], f(cd_w_in)[0], f(cd_w_out)[0]
    diff_lambda, diff_subln_g, gqa_qk_g = f(diff_lambda)[0], f(diff_subln_g)[0], f(gqa_qk_g)[0]
    win_sink, na_rpb = f(win_sink)[0], f(na_rpb)[0]
    moe_w1, moe_w3, moe_w2 = f(moe_w1), f(moe_w3), f(moe_w2)

    shared = {}
    shared["ident"] = np.eye(128, dtype=np.float32)
    bo = np.zeros((128, 128), dtype=np.float32)
    bo[:64, :64] = 1.0
    bo[64:, 64:] = 1.0
    shared["bones"] = bo
    for i in range(2):
        shared[f"modw{i}"] = mod_w[i]
        shared[f"modb{i}"] = np.ascontiguousarray(mod_b[i].reshape(48, 128).T)
        shared[f"wr{i}"] = np.ascontiguousarray(np.concatenate([f(moe_w_group)[i], f(moe_w_router)[i]], 1))
        shared[f"br{i}"] = np.concatenate([f(moe_b_group)[i], f(moe_b_router)[i]])[None, :].copy()
        shared[f"w1_{i}"] = moe_w1[i].reshape(NEXP * D, 512)
        shared[f"w3_{i}"] = moe_w3[i].reshape(NEXP * D, 512)
        shared[f"w2_{i}"] = moe_w2[i].reshape(NEXP * 512, D)
    lnT = np.zeros((128, 64), dtype=np.float32)
    for i in range(2):
        for wch in range(2):
            lnT[:, ((i * 2 + wch) * 2 + 0) * 8:((i * 2 + wch) * 2 + 0) * 8 + 8] = ln_g[i, wch].reshape(8, 128).T
            lnT[:, ((i * 2 + wch) * 2 + 1) * 8:((i * 2 + wch) * 2 + 1) * 8 + 8] = ln_b[i, wch].reshape(8, 128).T
    shared["lnT"] = lnT
    q0 = ab_w_in[:, 0:1024]
    kd, vd = ab_w_in[:, 1024:1536], ab_w_in[:, 1536:2048]
    kg, vg = ab_w_in[:, 2048:2176], ab_w_in[:, 2176:2304]
    k0 = np.concatenate([kd, kg[:, 0:64], kg[:, 0:64], kg[:, 64:128], kg[:, 64:128]], 1)
    shared["wq0"], shared["wqs0"] = np.ascontiguousarray(q0), np.ascontiguousarray(_swap_cols(q0))
    shared["wk0"], shared["wks0"] = np.ascontiguousarray(k0), np.ascontiguousarray(_swap_cols(k0))
    shared["wv0"] = np.ascontiguousarray(np.concatenate([vd, vg], 1))
    shared["wo0"] = ab_w_out
    qw, qn = cd_w_in[:, 0:512], cd_w_in[:, 512:1024]
    kw, vw = cd_w_in[:, 1024:1152], cd_w_in[:, 1152:1280]
    kn, vn = cd_w_in[:, 1280:1792], cd_w_in[:, 1792:2304]
    q1 = np.concatenate([qn, qw], 1)
    k1 = np.concatenate([kn, kw[:, 0:64], kw[:, 0:64], kw[:, 64:128], kw[:, 64:128]], 1)
    shared["wq1"], shared["wqs1"] = np.ascontiguousarray(q1), np.ascontiguousarray(_swap_cols(q1))
    shared["wk1"], shared["wks1"] = np.ascontiguousarray(k1), np.ascontiguousarray(_swap_cols(k1))
    shared["wv1"] = np.ascontiguousarray(np.concatenate([vn, vw], 1))
    shared["wo1"] = cd_w_out
    sw = lambda g: np.concatenate([g[32:64], g[0:32]])
    gq, gk = gqa_qk_g[0], gqa_qk_g[1]
    shared["gt"] = np.ascontiguousarray(np.stack([np.tile(gq, 2), np.tile(sw(gq), 2), np.tile(gk, 2), np.tile(sw(gk), 2)], 1))
    shared["dlam"] = diff_lambda.reshape(1, 256).copy()
    shared["subg"] = diff_subln_g.reshape(1, 128).copy()
    shared["sink"] = win_sink.reshape(1, 8).copy()
    ck, sk = _rope_tables(np.concatenate([np.arange(SEQ), -np.ones(NCTX, dtype=np.int64)]))
    shared["cK"], shared["sK"] = ck, sk

    in_maps = []
    tabs = {}
    for core in range(8):
        b, j = core // 4, core % 4
        pos = _ext_positions(j)
        m = dict(shared)
        m["xT"] = np.ascontiguousarray(x[b].T)
        m["xeT"] = np.ascontiguousarray(x[b][pos].T)
        m["cxT"] = np.ascontiguousarray(ctx[b].T)
        m["ccT"] = np.ascontiguousarray(np.stack([c[b], c_ctx], 1))
        if j not in tabs:
            cq, sq = _rope_tables(np.concatenate([pos, -np.ones(NCTX, dtype=np.int64)]))
            wmask, nab = _layer1_tables(j, na_rpb)
            tabs[j] = (cq, sq, wmask, nab)
        m["cQ"], m["sQ"], m["wmask"], m["nab"] = tabs[j]
        in_maps.append(m)

    if "nc" not in _NC_CACHE:
        _NC_CACHE["nc"] = build_program()
    res = run_bass_kernel_spmd(_NC_CACHE["nc"], in_maps, core_ids=list(range(8)))
    out = np.empty((2, SEQ, D), dtype=np.float32)
    for core in range(8):
        b, j = core // 4, core % 4
        out[b, 2048 * j:2048 * (j + 1), :] = np.asarray(res.results[core]["outT"]).T
    if DEBUG:
        kernel.last = res
    return out
```

```python
import math
from contextlib import ExitStack

import numpy as np
import concourse.bass as bass
import concourse.mybir as mybir
from concourse.bass_utils import run_bass_kernel_spmd

F32 = mybir.dt.float32
BF16 = mybir.dt.bfloat16
AF = mybir.ActivationFunctionType
ALU = mybir.AluOpType
AX = mybir.AxisListType

DEBUG = False

D = 1024
SEQ = 8192
NCTX = 256
NEXT = 2560
NOWN = 2048
NQ = NEXT + NCTX
NK0 = SEQ + NCTX
NK1 = NEXT + NCTX
VW = 650
DEPTH = 2
ALPHA = (2.0 * DEPTH) ** 0.25
LN_EPS = 1e-5
RMS_EPS = 1e-6
LAM_INIT0 = 0.8 - 0.6 * math.exp(0.0)
NEG = -30000.0
NEXP = 32

SEM_LIM = 8000
NDMA = 12
ARENA_WORDS = 53200


class V:
    def __init__(self, ap, tid, p0, p1, f0, f1, wpe):
        self.ap, self.tid, self.p0, self.p1, self.f0, self.f1, self.wpe = ap, tid, p0, p1, f0, f1, wpe

    def reg(self):
        return (self.tid, self.p0, self.p1, self.f0, self.f1)

    def s(self, c0, c1, p0=None, p1=None):
        q0 = 0 if p0 is None else p0
        q1 = (self.p1 - self.p0) if p1 is None else p1
        return V(self.ap[q0:q1, c0:c1], self.tid, self.p0 + q0, self.p0 + q1,
                 self.f0 + c0 * self.wpe, self.f0 + c1 * self.wpe, self.wpe)


class Buf:
    def __init__(self, t, tid, P, F, wpe):
        self.t, self.tid, self.P, self.F, self.wpe = t, tid, P, F, wpe

    def v(self, p0=0, p1=None, f0=0, f1=None):
        p1 = self.P if p1 is None else p1
        f1 = self.F if f1 is None else f1
        return V(self.t[p0:p1, f0:f1], self.tid, p0, p1, f0 * self.wpe, f1 * self.wpe, self.wpe)


class Prog:
    ENGS = ["pe", "act", "dve", "pool", "sp"]

    def __init__(self, nc, stack):
        self.nc, self.stack = nc, stack
        self.recs = {e: [] for e in self.ENGS}
        self.sig = {e: 0 for e in self.ENGS}
        self.known = {e: {} for e in self.ENGS}
        self.csems = {e: [] for e in self.ENGS}
        self.dsems, self.dcnt, self.drr = {}, {}, {}
        for q in ["sp", "act", "pool"]:
            self.dsems[q] = [stack.enter_context(nc.semaphore(f"d_{q}_{k}")) for k in range(NDMA)]
            self.dcnt[q] = [0] * NDMA
            self.drr[q] = 0
        self.ent = {}
        self.ntid = 0

    def new_tid(self):
        self.ntid += 1
        return self.ntid

    def dram(self, name, P, F, dtype, kind):
        t = self.nc.dram_tensor(name, [P, F], dtype, kind=kind)
        return Buf(t, self.new_tid(), P, F, 1.0 if dtype == F32 else 0.5)

    def _csem(self, e, idx):
        while len(self.csems[e]) <= idx:
            self.csems[e].append(self.stack.enter_context(self.nc.semaphore(f"c_{e}_{len(self.csems[e])}")))
        return self.csems[e][idx]

    @staticmethod
    def _ov(a, b):
        return a[1] < b[2] and b[1] < a[2] and a[3] < b[4] and b[3] < a[4]

    @staticmethod
    def _cov(a, b):
        return a[1] <= b[1] and a[2] >= b[2] and a[3] <= b[3] and a[4] >= b[4]

    BUCK = 256

    def _cands(self, r):
        d = self.ent.get(r[0])
        if not d:
            return []
        seen, out = set(), []
        for b in range(int(r[3]) // self.BUCK, int(math.ceil(r[4])) // self.BUCK + 1):
            for en in d.get(b, ()):
                if en[3] and id(en) not in seen:
                    seen.add(id(en))
                    out.append(en)
        return out

    def _add(self, en):
        r = en[0]
        d = self.ent.setdefault(r[0], {})
        for b in range(int(r[3]) // self.BUCK, int(math.ceil(r[4])) // self.BUCK + 1):
            lst = d.setdefault(b, [])
            if len(lst) > 64:
                lst[:] = [x for x in lst if x[3]]
            lst.append(en)

    def _deps(self, reads, writes):
        raw, other = [], []
        for r in reads:
            for en in self._cands(r):
                if en[1] is not None and self._ov(en[0], r):
                    raw.append(en[1])
        for w in writes:
            for en in self._cands(w):
                if self._ov(en[0], w):
                    if en[1] is not None:
                        other.append(en[1])
                    other.extend(en[2])
        return raw, other

    def _update(self, tok, reads, writes):
        for r in reads:
            hit = False
            for en in self._cands(r):
                if self._ov(en[0], r):
                    if tok[0] == "c":
                        en[2][:] = [t for t in en[2] if not (t[0] == "c" and t[1] == tok[1])]
                    en[2].append(tok)
                    if self._cov(en[0], r):
                        hit = True
            if not hit:
                self._add([r, None, [tok], True])
        for w in writes:
            for en in self._cands(w):
                if self._cov(w, en[0]):
                    en[3] = False
            self._add([w, tok, [], True])

    def _waits(self, e, raw, other):
        waits = []
        kn = self.known[e]
        for kind, toks in (("raw", raw), ("oth", other)):
            for t in toks:
                if t[0] == "c":
                    _, f, v = t
                    if f == e and e == "pe":
                        continue
                    if kn.get(f, 0) >= v:
                        continue
                    kn[f] = v
                    waits.append(t)
                else:
                    _, q, k, c = t
                    if kn.get((q, k), 0) >= c:
                        continue
                    kn[(q, k)] = c
                    waits.append(t)
        return waits

    def op(self, e, fn, reads=(), writes=(), sig=True):
        reads = [r.reg() for r in reads]
        writes = [w.reg() for w in writes]
        raw, other = self._deps(reads, writes)
        waits = self._waits(e, raw, other)
        if sig:
            self.sig[e] += 1
            v = self.sig[e]
        else:
            v = self.sig[e] + 1
        tok = ("c", e, v)
        self._update(tok, reads, writes)
        self.recs[e].append((waits, fn, tok if sig else None))
        return tok

    def dma(self, q, out, in_, **kw):
        reads, writes = [in_.reg()], [out.reg()]
        raw, other = self._deps(reads, writes)
        k = self.drr[q]
        self.drr[q] = (k + 1) % NDMA
        prev = self.dcnt[q][k]
        if prev > 0:
            other = other + [("d", q, k, prev)]
        waits = self._waits(q, raw, other)
        self.dcnt[q][k] = prev + 1
        tok = ("d", q, k, prev + 1)
        self._update(tok, reads, writes)
        oa, ia = out.ap, in_.ap

        def fn(eng, oa=oa, ia=ia, kw=kw):
            return eng.dma_start(out=oa, in_=ia, **kw)

        self.recs[q].append((waits, fn, tok))
        return tok

    def finish(self):
        waits = []
        for q in self.dsems:
            for k in range(NDMA):
                if self.dcnt[q][k] > 0:
                    waits.append(("d", q, k, self.dcnt[q][k]))
        for e in ["pe", "act", "dve", "pool"]:
            if self.sig[e] > 0:
                waits.append(("c", e, self.sig[e]))
        self.recs["sp"].append((waits, None, None))

    def _emit_wait(self, eng, w):
        if w[0] == "c":
            _, f, v = w
            eng.wait_ge(self._csem(f, (v - 1) // SEM_LIM), (v - 1) % SEM_LIM + 1)
        else:
            _, q, k, c = w
            eng.wait_ge(self.dsems[q][k], 16 * c)

    def emit(self):
        nc = self.nc
        for e in self.ENGS:
            for idx in range((self.sig[e] + SEM_LIM - 1) // SEM_LIM + 1):
                self._csem(e, idx)
        recs, me = self.recs, self

        def play(e, eng):
            for waits, fn, tok in recs[e]:
                for w in waits:
                    me._emit_wait(eng, w)
                if fn is None:
                    continue
                ins = fn(eng)
                if tok is not None:
                    if tok[0] == "c":
                        ins.then_inc(me._csem(e, (tok[2] - 1) // SEM_LIM), 1)
                    else:
                        ins.then_inc(me.dsems[tok[1]][tok[2]], 16)

        with nc.Block() as block:
            @block.tensor
            def _(eng):
                play("pe", eng)

            @block.scalar
            def _(eng):
                play("act", eng)

            @block.vector
            def _(eng):
                play("dve", eng)

            @block.gpsimd
            def _(eng):
                play("pool", eng)

            @block.sync
            def _(eng):
                play("sp", eng)


class Arena:
    def __init__(self, buf):
        self.buf, self.top, self.hi = buf, 0, 0

    def _alloc(self, words):
        off = self.top
        self.top += (words + 7) // 8 * 8
        self.hi = max(self.hi, self.top)
        assert self.top <= self.buf.F, f"arena overflow {self.top}"
        return off

    def f32(self, n, P=128):
        off = self._alloc(n)
        return V(self.buf.t[0:P, off:off + n], self.buf.tid, 0, P, off, off + n, 1.0)

    def bf(self, n, P=128):
        w = (n + 1) // 2
        off = self._alloc(w)
        return V(self.buf.t[0:P, off:off + w].bitcast(BF16), self.buf.tid, 0, P, off, off + w, 0.5)


class Rot:
    def __init__(self, items):
        self.items, self.i = items, 0

    def get(self):
        it = self.items[self.i % len(self.items)]
        self.i += 1
        return it


def r3(ap, k):
    return ap.rearrange("p (k n) -> p k n", k=k)


def OP(name, *a, **k):
    return lambda e: getattr(e, name)(*a, **k)


def build_program():
    nc = bass.Bass("TRN2", target_bir_lowering=False)
    st = ExitStack()
    with st:
        P = Prog(nc, st)
        I = lambda name, p, f, dt=F32: P.dram(name, p, f, dt, "ExternalInput")
        xT = I("xT", D, SEQ)
        xeT = I("xeT", D, NEXT)
        cxT = I("cxT", D, NCTX)
        ccT = I("ccT", D, 2)
        ident_d = I("ident", 128, 128)
        bones_d = I("bones", 128, 128)
        modw = [I(f"modw{i}", D, 6 * D) for i in range(2)]
        modb = [I(f"modb{i}", 128, 48) for i in range(2)]
        lnT_d = I("lnT", 128, 64)
        wq_d = [I(f"wq{i}", D, 1024) for i in range(2)]
        wqs_d = [I(f"wqs{i}", D, 1024) for i in range(2)]
        wk_d = [I(f"wk{i}", D, 768) for i in range(2)]
        wks_d = [I(f"wks{i}", D, 768) for i in range(2)]
        wv_d = [I(f"wv{i}", D, 640) for i in range(2)]
        wo_d = [I(f"wo{i}", D, 1024) for i in range(2)]
        gt_d = I("gt", 128, 4)
        dlam_d = I("dlam", 1, 256)
        subg_d = I("subg", 1, 128)
        sink_d = I("sink", 1, 8)
        cK_d = I("cK", 128, NK0)
        sK_d = I("sK", 128, NK0)
        cQ_d = I("cQ", 128, NQ)
        sQ_d = I("sQ", 128, NQ)
        wr_d = [I(f"wr{i}", D, 36) for i in range(2)]
        br_d = [I(f"br{i}", 1, 36) for i in range(2)]
        w1_d = [I(f"w1_{i}", NEXP * D, 512) for i in range(2)]
        w3_d = [I(f"w3_{i}", NEXP * D, 512) for i in range(2)]
        w2_d = [I(f"w2_{i}", NEXP * 512, D) for i in range(2)]
        wmask_d = I("wmask", 128, 16 * 384)
        nab_d = I("nab", 128, 16 * 8 * 640)
        outT = P.dram("outT", D, NOWN, F32, "ExternalOutput")
        skind = "ExternalOutput" if DEBUG else "Internal"
        QT = P.dram("QT", 1024, NQ, BF16, skind)
        KT = P.dram("KT", 768, NK0, BF16, skind)
        VA = P.dram("VA", 128, 66 * VW, BF16, skind)
        h1T = P.dram("h1T", D, NEXT, F32, skind)
        hc1T = P.dram("hc1T", D, NCTX, F32, skind)

        arena_t = st.enter_context(nc.sbuf_tensor("arena", [128, ARENA_WORDS], F32))
        AR = Arena(Buf(arena_t, P.new_tid(), 128, ARENA_WORDS, 1.0))
        psum_t = st.enter_context(nc.psum_tensor("psum", [128, 4096], F32))
        PS = Buf(psum_t, P.new_tid(), 128, 4096, 1.0)

        def bank(b, n=512, p=128):
            return PS.v(0, p, b * 512, b * 512 + n)

        def bank_bf(b, n):
            return V(PS.t[:, b * 512:b * 512 + n // 2].bitcast(BF16), PS.tid, 0, 128, b * 512, b * 512 + n // 2, 0.5)

        def dchunks(buf, c0, c1, r0=0, nchunk=8):
            ap = buf.t[r0:r0 + nchunk * 128, c0:c1].rearrange("(c p) n -> p c n", p=128)
            return V(ap, buf.tid, r0, r0 + nchunk * 128, c0 * buf.wpe, c1 * buf.wpe, buf.wpe)

        def v3(v, k):
            return V(r3(v.ap, k), v.tid, v.p0, v.p1, v.f0, v.f1, v.wpe)

        def bcast(buf, n):
            return V(buf.t[0:1, 0:n].partition_broadcast(128), buf.tid, 0, 1, 0, n * buf.wpe, buf.wpe)

        ident32 = AR.f32(128)
        identb = AR.bf(128)
        ones = AR.f32(128)
        bones = AR.f32(128)
        lnT = AR.f32(64)
        gt = AR.f32(4)
        dl = AR.f32(256)
        sg1 = AR.f32(128)
        esink = AR.f32(8)
        eps_ln = AR.f32(1)
        eps_rms = AR.f32(1)
        nlam = AR.f32(1)
        ML = [AR.f32(48) for _ in range(2)]
        MC = [AR.f32(48) for _ in range(2)]
        sc = AR.f32(16)
        wr = [AR.f32(8 * 36) for _ in range(2)]
        brb = [AR.f32(36) for _ in range(2)]
        P.dma("sp", ident32, ident_d.v())
        P.dma("sp", bones, bones_d.v())
        P.dma("sp", lnT, lnT_d.v())
        P.dma("sp", gt, gt_d.v())
        P.dma("sp", dl, bcast(dlam_d, 256))
        P.dma("sp", sg1, bcast(subg_d, 128))
        P.dma("sp", esink, bcast(sink_d, 8))
        for i in range(2):
            P.dma("sp", v3(wr[i], 8), dchunks(wr_d[i], 0, 36))
            P.dma("sp", brb[i], bcast(br_d[i], 36))
        P.op("dve", OP("tensor_copy", out=identb.ap, in_=ident32.ap), [ident32], [identb])
        P.op("dve", OP("memset", ones.ap, 1.0), [], [ones])
        P.op("dve", OP("memset", eps_ln.ap, LN_EPS), [], [eps_ln])
        P.op("dve", OP("memset", eps_rms.ap, RMS_EPS), [], [eps_rms])
        P.op("act", OP("activation", out=esink.ap, in_=esink.ap, func=AF.Exp), [esink], [esink])
        P.op("dve", OP("tensor_scalar", out=sg1.ap, in0=sg1.ap, scalar1=1.0 - LAM_INIT0, scalar2=None, op0=ALU.mult), [sg1], [sg1])
        lt = AR.f32(128)
        ls = AR.f32(2)
        dl4 = dl.ap.rearrange("p (a b n) -> p a b n", a=2, b=2)
        P.op("dve", OP("tensor_tensor", out=lt.ap.rearrange("p (a n) -> p a n", a=2), in0=dl4[:, :, 0, :], in1=dl4[:, :, 1, :], op=ALU.mult), [dl], [lt])
        P.op("dve", OP("tensor_reduce", out=ls.ap, in_=lt.ap.rearrange("p (a n) -> p a n", a=2), axis=AX.X, op=ALU.add), [lt], [ls])
        P.op("act", OP("activation", out=ls.ap, in_=ls.ap, func=AF.Exp), [ls], [ls])
        P.op("dve", OP("tensor_tensor", out=nlam.ap, in0=ls.ap[:, 1:2], in1=ls.ap[:, 0:1], op=ALU.subtract), [ls], [nlam])
        P.op("dve", OP("tensor_scalar", out=nlam.ap, in0=nlam.ap, scalar1=-LAM_INIT0, scalar2=None, op0=ALU.add), [nlam], [nlam])

        base_top = AR.top
        P.dma("sp", v3(sc, 8), dchunks(ccT, 0, 2))
        P.op("act", OP("activation", out=sc.ap, in_=sc.ap, func=AF.Silu), [sc], [sc])
        mwb = [AR.f32(8 * 512) for _ in range(2)]
        mbt = AR.f32(48)
        for i in range(2):
            P.dma("sp", mbt, modb[i].v())
            pm = bank(6, 96)
            for pc in range(12):
                w = mwb[pc % 2]
                P.dma("sp", v3(w, 8), dchunks(modw[i], pc * 512, pc * 512 + 512))
                for j in range(4):
                    cc = pc * 4 + j
                    for dc in range(8):
                        P.op("pe", OP("matmul",
                            pm.ap[:, cc * 2:cc * 2 + 2], lhsT=w.ap[:, dc * 512 + j * 128: dc * 512 + j * 128 + 128],
                            rhs=sc.ap[:, dc * 2:dc * 2 + 2], start=(dc == 0), stop=(dc == 7)),
                            [w, sc], [pm], sig=(dc == 7))
            pm3 = pm.ap.rearrange("p (c n) -> p c n", n=2)
            P.op("dve", OP("tensor_tensor", out=ML[i].ap, in0=pm3[:, :, 0], in1=mbt.ap, op=ALU.add), [pm, mbt], [ML[i]])
            P.op("dve", OP("tensor_tensor", out=MC[i].ap, in0=pm3[:, :, 1], in1=mbt.ap, op=ALU.add), [pm, mbt], [MC[i]])
            for M in (ML[i], MC[i]):
                for k in (1, 4):
                    P.op("dve", OP("tensor_scalar", out=M.ap[:, k * 8:k * 8 + 8], in0=M.ap[:, k * 8:k * 8 + 8],
                                                                    scalar1=1.0, scalar2=None, op0=ALU.add), [M], [M])
        AR.top = base_top

        FT = AR.bf(8 * NQ)
        H = AR.f32(8 * NQ)
        free_top = AR.top

        def ln_block(ucf, nt, li, which, out_fn, tmp):
            s1, s2 = bank(6, nt), bank(7, nt)
            for c in range(8):
                uc = ucf(c)
                P.op("pe", OP("matmul", s1.ap, lhsT=ones.ap, rhs=uc.ap, start=(c == 0), stop=(c == 7)),
                     [ones, uc], [s1], sig=(c == 7))
            for c in range(8):
                uc = ucf(c)
                q = tmp["sq"].get().s(0, nt)
                P.op("act", OP("activation", out=q.ap, in_=uc.ap, func=AF.Square), [uc], [q])
                P.op("pe", OP("matmul", s2.ap, lhsT=ones.ap, rhs=q.ap, start=(c == 0), stop=(c == 7)),
                     [ones, q], [s2], sig=True)
            mean, msq, rstd = tmp["mean"].s(0, nt), tmp["msq"].s(0, nt), tmp["rstd"].s(0, nt)
            P.op("act", OP("activation", out=mean.ap, in_=s1.ap, func=AF.Copy, scale=1.0 / D), [s1], [mean])
            P.op("act", OP("activation", out=msq.ap, in_=s1.ap, func=AF.Square, scale=1.0 / D), [s1], [msq])
            P.op("dve", OP("scalar_tensor_tensor", out=rstd.ap, in0=s2.ap, scalar=1.0 / D, in1=msq.ap, op0=ALU.mult, op1=ALU.subtract),
                 [s2, msq], [rstd])
            P.op("act", OP("activation", out=rstd.ap, in_=rstd.ap, func=AF.Sqrt, bias=eps_ln.ap, scale=1.0), [rstd, eps_ln], [rstd])
            P.op("dve", OP("reciprocal", out=rstd.ap, in_=rstd.ap), [rstd], [rstd])
            gi = ((li * 2 + which) * 2 + 0) * 8
            bi = ((li * 2 + which) * 2 + 1) * 8
            for c in range(8):
                uc = ucf(c)
                o = out_fn(c)
                P.op("pool", OP("tensor_tensor", out=uc.ap, in0=uc.ap, in1=mean.ap, op=ALU.subtract), [uc, mean], [uc])
                P.op("dve", OP("tensor_tensor", out=uc.ap, in0=uc.ap, in1=rstd.ap, op=ALU.mult), [uc, rstd], [uc])
                P.op("dve", OP("tensor_scalar", out=o.ap, in0=uc.ap, scalar1=lnT.ap[:, gi + c:gi + c + 1],
                                                                      scalar2=lnT.ap[:, bi + c:bi + c + 1], op0=ALU.mult, op1=ALU.add),
                     [uc, lnT], [o])

        def layer(li):
            ab = (li == 0)
            NT = NQ if ab else NOWN

            def Hc(c, t0, t1):
                return H.s(c * NT + t0, c * NT + t1)

            AR.top = FT.f0 if False else int(FT.f0)
            wq, wqs, wk, wks, wv = AR.bf(8 * 1024), AR.bf(8 * 1024), AR.bf(8 * 768), AR.bf(8 * 768), AR.bf(8 * 640)
            for dst, src, n in ((wq, wq_d[li], 1024), (wqs, wqs_d[li], 1024), (wk, wk_d[li], 768), (wks, wks_d[li], 768), (wv, wv_d[li], 640)):
                P.dma("pool", v3(dst, 8), dchunks(src, 0, n))
            xb = Rot([AR.f32(8 * 512) for _ in range(2)])
            abuf = Rot([AR.bf(8 * 512) for _ in range(2)])
            ctab = Rot([AR.f32(512) for _ in range(2)])
            stab = Rot([AR.f32(512) for _ in range(2)])
            t1r = Rot([AR.f32(512) for _ in range(2)])
            t2r = Rot([AR.f32(512) for _ in range(2)])
            obr = Rot([AR.bf(512) for _ in range(3)])
            sqr = Rot([AR.f32(512) for _ in range(2)])
            rvr = Rot([AR.f32(512) for _ in range(2)])
            vst = Rot([AR.bf(VW) for _ in range(2)])
            for vs in vst.items:
                P.op("pool", OP("memset", vs.ap, 1.0), [], [vs])
            pbank = Rot([0, 1, 2, 3, 4, 5, 6, 7])
            if ab:
                q_rope, q_norm = [True] * 8, [False] * 4 + [True] * 4
                k_rope, k_norm = [True] * 6, [False] * 4 + [True] * 2
            else:
                q_rope, q_norm = [False] * 4 + [True] * 4, [False] * 8
                k_rope, k_norm = [False] * 4 + [True] * 2, [False] * 6

            def fm_proj(aT, nt, W, Ws, wn, ch, rope, norm, gcol, ct, stb, dst):
                pa = bank(pbank.get(), nt)
                for c in range(8):
                    P.op("pe", OP("matmul", pa.ap, lhsT=W.ap[:, c * wn + ch * 128: c * wn + ch * 128 + 128],
                                                      rhs=aT.ap[:, c * nt:c * nt + nt], start=(c == 0), stop=(c == 7)),
                         [W, aT], [pa], sig=(c == 7))
                ob = obr.get().s(0, nt)
                if not rope:
                    P.op("act", OP("activation", out=ob.ap, in_=pa.ap, func=AF.Copy), [pa], [ob])
                else:
                    pb = bank(pbank.get(), nt)
                    for c in range(8):
                        P.op("pe", OP("matmul", pb.ap, lhsT=Ws.ap[:, c * wn + ch * 128: c * wn + ch * 128 + 128],
                                                          rhs=aT.ap[:, c * nt:c * nt + nt], start=(c == 0), stop=(c == 7)),
                             [Ws, aT], [pb], sig=(c == 7))
                    t1, t2 = t1r.get().s(0, nt), t2r.get().s(0, nt)
                    if norm:
                        sq = sqr.get().s(0, nt)
                        P.op("act", OP("activation", out=sq.ap, in_=pa.ap, func=AF.Square), [pa], [sq])
                        pss = bank(pbank.get(), nt)
                        P.op("pe", OP("matmul", pss.ap, lhsT=bones.ap, rhs=sq.ap, start=True, stop=True), [bones, sq], [pss])
                        rv = rvr.get().s(0, nt)
                        P.op("act", OP("activation", out=rv.ap, in_=pss.ap, func=AF.Sqrt, bias=eps_rms.ap, scale=1.0 / 64), [pss, eps_rms], [rv])
                        P.op("dve", OP("reciprocal", out=rv.ap, in_=rv.ap), [rv], [rv])
                        P.op("dve", OP("scalar_tensor_tensor", out=t1.ap, in0=pa.ap, scalar=gt.ap[:, gcol:gcol + 1], in1=ct.ap, op0=ALU.mult, op1=ALU.mult),
                             [pa, gt, ct], [t1])
                        P.op("dve", OP("scalar_tensor_tensor", out=t2.ap, in0=pb.ap, scalar=gt.ap[:, gcol + 1:gcol + 2], in1=stb.ap, op0=ALU.mult, op1=ALU.mult),
                             [pb, gt, stb], [t2])
                        P.op("pool", OP("tensor_tensor", out=t1.ap, in0=t1.ap, in1=t2.ap, op=ALU.add), [t1, t2], [t1])
                        P.op("dve", OP("tensor_tensor", out=ob.ap, in0=t1.ap, in1=rv.ap, op=ALU.mult), [t1, rv], [ob])
                    else:
                        P.op("dve", OP("tensor_tensor", out=t1.ap, in0=pa.ap, in1=ct.ap, op=ALU.mult), [pa, ct], [t1])
                        P.op("dve", OP("tensor_tensor", out=t2.ap, in0=pb.ap, in1=stb.ap, op=ALU.mult), [pb, stb], [t2])
                        P.op("pool", OP("tensor_tensor", out=ob.ap, in0=t1.ap, in1=t2.ap, op=ALU.add), [t1, t2], [ob])
                P.dma("sp", dst, ob)

            def source(src, ntok, M, ctd, std, toff, want_q, q_off, want_kv, k_off):
                def load(t0):
                    nt = min(512, ntok - t0)
                    x = xb.get().s(0, 8 * nt)
                    P.dma("sp", v3(x, 8), dchunks(src, t0, t0 + nt))
                    ct, stb = ctab.get().s(0, nt), stab.get().s(0, nt)
                    P.dma("sp", ct, ctd.v(0, 128, toff + t0, toff + t0 + nt))
                    P.dma("sp", stb, std.v(0, 128, toff + t0, toff + t0 + nt))
                    return (x, ct, stb)

                nxt = load(0)
                for t0 in range(0, ntok, 512):
                    nt = min(512, ntok - t0)
                    x, ct, stb = nxt
                    if t0 + 512 < ntok:
                        nxt = load(t0 + 512)
                    aT = abuf.get().s(0, 8 * nt)
                    for c in range(8):
                        P.op("act", OP("activation", out=aT.ap[:, c * nt:c * nt + nt], in_=x.ap[:, c * nt:c * nt + nt], func=AF.Identity,
                                       scale=M.ap[:, 8 + c:9 + c], bias=M.ap[:, c:c + 1]), [x, M], [aT])
                    if want_kv:
                        for ch in range(6):
                            fm_proj(aT, nt, wk, wks, 768, ch, k_rope[ch], k_norm[ch], 2, ct, stb,
                                    KT.v(ch * 128, ch * 128 + 128, k_off + t0, k_off + t0 + nt))
                        for tt in range(nt // 128):
                            pv1, pv2 = bank(pbank.get(), 512), bank(pbank.get(), 128)
                            for c in range(8):
                                lh = aT.ap[:, c * nt + tt * 128:c * nt + tt * 128 + 128]
                                P.op("pe", OP("matmul", pv1.ap, lhsT=lh, rhs=wv.ap[:, c * 640:c * 640 + 512], start=(c == 0), stop=(c == 7)),
                                     [aT, wv], [pv1], sig=(c == 7))
                            for c in range(8):
                                lh = aT.ap[:, c * nt + tt * 128:c * nt + tt * 128 + 128]
                                P.op("pe", OP("matmul", pv2.ap, lhsT=lh, rhs=wv.ap[:, c * 640 + 512:c * 640 + 640], start=(c == 0), stop=(c == 7)),
                                     [aT, wv], [pv2], sig=(c == 7))
                            vs = vst.get()
                            if ab:
                                o1 = vs.ap[:, 0:516].rearrange("p (h n) -> p h n", n=129)[:, :, 0:128]
                                i1 = pv1.ap.rearrange("p (h n) -> p h n", n=128)
                                o2 = vs.ap[:, 516:646].rearrange("p (h n) -> p h n", n=65)[:, :, 0:64]
                            else:
                                o1 = vs.ap[:, 0:520].rearrange("p (h n) -> p h n", n=65)[:, :, 0:64]
                                i1 = pv1.ap.rearrange("p (h n) -> p h n", n=64)
                                o2 = vs.ap[:, 520:650].rearrange("p (h n) -> p h n", n=65)[:, :, 0:64]
                            i2 = pv2.ap.rearrange("p (h n) -> p h n", n=64)
                            P.op("act", OP("activation", out=o1, in_=i1, func=AF.Copy), [pv1], [vs])
                            P.op("dve", OP("tensor_copy", out=o2, in_=i2), [pv2], [vs])
                            kt = (k_off + t0) // 128 + tt
                            P.dma("sp", VA.v(0, 128, kt * VW, kt * VW + VW), vs)
                    if want_q:
                        for ch in range(8):
                            fm_proj(aT, nt, wq, wqs, 1024, ch, q_rope[ch], q_norm[ch], 0, ct, stb,
                                    QT.v(ch * 128, ch * 128 + 128, q_off + t0, q_off + t0 + nt))

            if ab:
                source(xT, SEQ, ML[0], cK_d, sK_d, 0, False, 0, True, 0)
                source(cxT, NCTX, MC[0], cK_d, sK_d, SEQ, True, NEXT, True, SEQ)
                source(xeT, NEXT, ML[0], cQ_d, sQ_d, 0, True, 0, False, 0)
                nkt = SEQ // 128 + 2
            else:
                source(h1T, NEXT, ML[1], cQ_d, sQ_d, 0, True, 0, True, 0)
                source(hc1T, NCTX, MC[1], cQ_d, sQ_d, NEXT, False, 0, True, NEXT)
                nkt = NEXT // 128 + 2

            AR.top = int(H.f0)
            O = FT
            qbuf = Rot([AR.bf(NQ) for _ in range(2)])
            kbuf = Rot([AR.bf(nkt * 128) for _ in range(2)])
            vbuf = Rot([AR.bf(nkt * 130) for _ in range(2)])
            ptile = Rot([AR.bf(1024) for _ in range(3)])
            rz = Rot([AR.f32(1) for _ in range(6)])
            od1 = [AR.f32(128) for _ in range(4)]
            t_o2 = Rot([AR.f32(128) for _ in range(2)])
            t_od = Rot([AR.f32(128) for _ in range(2)])
            t_sq = Rot([AR.f32(128) for _ in range(2)])
            ssq = Rot([AR.f32(1) for _ in range(4)])
            stmp = Rot([AR.f32(640) for _ in range(2)])
            if not ab:
                wm = AR.f32(16 * 384)
                P.dma("sp", wm, wmask_d.v())
                nbb = Rot([AR.f32(640) for _ in range(3)])
                nb_pref = [None]

            def attend512(kt_, qt_, vv, vcw, voff, dv, r0, q0, nq, kts, fin):
                nsub = nq // 128
                accs = [bank(4 + s, dv + 1) for s in range(nsub)]
                pairs = [kts[i:i + 2] for i in range(0, len(kts), 2)]
                npair = len(pairs)

                def stage_a(pi):
                    pr = pairs[pi]
                    base = (pi % 2) * 1024
                    sreg = PS.v(0, 128, base, base + len(pr) * nq)
                    for jj, kt in enumerate(pr):
                        P.op("pe", OP("matmul", sreg.ap[:, jj * nq:jj * nq + nq], lhsT=kt_.ap[r0:r0 + 64, kt * 128:kt * 128 + 128],
                                      rhs=qt_.ap[r0:r0 + 64, q0:q0 + nq], start=True, stop=True), [kt_, qt_], [sreg], sig=(jj == len(pr) - 1))
                    pt = ptile.get().s(0, len(pr) * nq)
                    P.op("act", OP("activation", out=pt.ap, in_=sreg.ap, func=AF.Exp, scale=0.125), [sreg], [pt])
                    return pt

                def stage_b(pi, pt):
                    pr = pairs[pi]
                    for jj, kt in enumerate(pr):
                        for s in range(nsub):
                            first = (pi == 0 and jj == 0)
                            last = (pi == npair - 1 and jj == len(pr) - 1)
                            P.op("pe", OP("matmul", accs[s].ap, lhsT=pt.ap[:, jj * nq + s * 128:jj * nq + s * 128 + 128],
                                          rhs=vv.ap[:, kt * vcw + voff:kt * vcw + voff + dv + 1], start=first, stop=last),
                                 [pt, vv], [accs[s]], sig=(s == nsub - 1 and jj == len(pr) - 1))

                prev = None
                for pi in range(npair):
                    cur = stage_a(pi)
                    if prev is not None:
                        stage_b(pi - 1, prev)
                    prev = cur
                stage_b(npair - 1, prev)
                for s in range(nsub):
                    fin(s, accs[s], q0 // 128 + s)

            for u in range(8):
                kc = u if u < 4 else 4 + (u - 4) // 2
                qt_, kt_, vt_ = qbuf.get(), kbuf.get(), vbuf.get()
                P.dma("sp", qt_, QT.v(u * 128, u * 128 + 128, 0, NQ))
                P.dma("sp", kt_, KT.v(kc * 128, kc * 128 + 128, 0, nkt * 128))
                if ab:
                    vc0, vcw = (u * 129, 129) if u < 4 else (516 + ((u - 4) // 2) * 65, 65)
                else:
                    vc0, vcw = (u * 130, 130) if u < 4 else (520 + ((u - 4) // 2) * 65, 65)
                vv = vt_.s(0, nkt * vcw)
                for g0 in range(0, nkt, 11):
                    src = V(VA.t[:, g0 * VW:(g0 + 11) * VW].rearrange("p (t c) -> p t c", c=VW)[:, :, vc0:vc0 + vcw],
                            VA.tid, 0, 128, g0 * VW * 0.5, (g0 + 11) * VW * 0.5, 0.5)
                    dstv = V(vv.ap[:, g0 * vcw:(g0 + 11) * vcw].rearrange("p (t c) -> p t c", c=vcw), vv.tid, 0, 128,
                             vv.f0 + g0 * vcw * 0.5, vv.f0 + (g0 + 11) * vcw * 0.5, 0.5)
                    P.dma("sp", dstv, src)

                if ab:
                    dv = 128 if u < 4 else 64
                    qblocks = [(q0, 256, list(range(nkt))) for q0 in range(0, NEXT, 256)] + [(NEXT, 256, [nkt - 2, nkt - 1])]
                    for (q0, nq, kts) in qblocks:
                        nsub = nq // 128
                        accs = [[bank(4 + c * 2 + s, dv + 1) for s in range(nsub)] for c in range(2)]
                        pairs = [kts[i:i + 2] for i in range(0, len(kts), 2)]
                        npair = len(pairs)

                        def st_a(pi):
                            pr = pairs[pi]
                            base = (pi % 2) * 1024
                            sreg = PS.v(0, 128, base, base + 1024)
                            for jj, kt in enumerate(pr):
                                for c in range(2):
                                    P.op("pe", OP("matmul", sreg.ap[:, c * 512 + jj * nq:c * 512 + jj * nq + nq],
                                                  lhsT=kt_.ap[64 * c:64 * c + 64, kt * 128:kt * 128 + 128],
                                                  rhs=qt_.ap[64 * c:64 * c + 64, q0:q0 + nq], start=True, stop=True),
                                         [kt_, qt_], [sreg], sig=(jj == len(pr) - 1 and c == 1))
                            pt = ptile.get()
                            P.op("act", OP("activation", out=pt.ap, in_=sreg.ap, func=AF.Exp, scale=0.125), [sreg], [pt])
                            return pt

                        def st_b(pi, pt):
                            pr = pairs[pi]
                            for c in range(2):
                                for jj, kt in enumerate(pr):
                                    for s_ in range(nsub):
                                        first = (pi == 0 and jj == 0)
                                        last = (pi == npair - 1 and jj == len(pr) - 1)
                                        col = c * 512 + jj * nq + s_ * 128
                                        P.op("pe", OP("matmul", accs[c][s_].ap, lhsT=pt.ap[:, col:col + 128],
                                                      rhs=vv.ap[:, kt * vcw:kt * vcw + dv + 1], start=first, stop=last),
                                             [pt, vv], [accs[c][s_]], sig=(c == 1 and s_ == nsub - 1 and jj == len(pr) - 1))

                        prev = None
                        for pi in range(npair):
                            cur = st_a(pi)
                            if prev is not None:
                                st_b(pi - 1, prev)
                            prev = cur
                        st_b(npair - 1, prev)
                        for s_ in range(nsub):
                            ot = q0 // 128 + s_
                            a0, a1 = accs[0][s_], accs[1][s_]
                            r0_, r1_ = rz.get(), rz.get()
                            P.op("dve", OP("reciprocal", out=r0_.ap, in_=a0.ap[:, dv:dv + 1]), [a0], [r0_])
                            P.op("dve", OP("reciprocal", out=r1_.ap, in_=a1.ap[:, dv:dv + 1]), [a1], [r1_])
                            if u < 4:
                                d1 = od1[s_]
                                o2, od, sq, ss = t_o2.get(), t_od.get(), t_sq.get(), ssq.get()
                                P.op("dve", OP("tensor_scalar", out=d1.ap, in0=a0.ap[:, 0:128], scalar1=r0_.ap[:, 0:1], scalar2=None, op0=ALU.mult), [a0, r0_], [d1])
                                P.op("dve", OP("tensor_scalar", out=o2.ap, in0=a1.ap[:, 0:128], scalar1=r1_.ap[:, 0:1], scalar2=None, op0=ALU.mult), [a1, r1_], [o2])
                                P.op("dve", OP("scalar_tensor_tensor", out=od.ap, in0=o2.ap, scalar=nlam.ap[:, 0:1], in1=d1.ap, op0=ALU.mult, op1=ALU.add), [o2, nlam, d1], [od])
                                P.op("pool", OP("tensor_tensor", out=sq.ap, in0=od.ap, in1=od.ap, op=ALU.mult), [od], [sq])
                                P.op("dve", OP("tensor_reduce", out=ss.ap, in_=sq.ap, axis=AX.X, op=ALU.add), [sq], [ss])
                                P.op("act", OP("activation", out=ss.ap, in_=ss.ap, func=AF.Sqrt, bias=eps_rms.ap, scale=1.0 / 128), [ss, eps_rms], [ss])
                                P.op("dve", OP("reciprocal", out=ss.ap, in_=ss.ap), [ss], [ss])
                                oo = O.s(ot * 1024 + u * 128, ot * 1024 + u * 128 + 128)
                                P.op("dve", OP("scalar_tensor_tensor", out=oo.ap, in0=od.ap, scalar=ss.ap[:, 0:1], in1=sg1.ap, op0=ALU.mult, op1=ALU.mult), [od, ss, sg1], [oo])
                            else:
                                for c, (a, r) in enumerate(((a0, r0_), (a1, r1_))):
                                    col = 512 + ((u - 4) * 2 + c) * 64
                                    oo = O.s(ot * 1024 + col, ot * 1024 + col + 64)
                                    P.op("dve", OP("tensor_scalar", out=oo.ap, in0=a.ap[:, 0:64], scalar1=r.ap[:, 0:1], scalar2=None, op0=ALU.mult), [a, r], [oo])
                else:
                    for c in range(2):
                        r0 = 64 * c

                        def l1_a(t, c=c, r0=r0, u=u):
                            if u < 4:
                                h = 2 * u + c
                                kts = list(range(t, t + 5)) + [20, 21]
                                nb = 5
                                if t == 0:
                                    nb_pref[0] = nbb.get()
                                    P.dma("sp", nb_pref[0], nab_d.v(0, 128, (t * 8 + h) * 640, (t * 8 + h) * 640 + 640))
                                bt = nb_pref[0]
                                if t + 1 < 16:
                                    nb_pref[0] = nbb.get()
                                    P.dma("sp", nb_pref[0], nab_d.v(0, 128, ((t + 1) * 8 + h) * 640, ((t + 1) * 8 + h) * 640 + 640))
                                voff = c * 65
                                col = 512 + h * 64
                            else:
                                h = (u - 4) * 2 + c
                                kts = list(range(t + 1, t + 4)) + [20, 21]
                                nb = 3
                                bt = wm.s(t * 384, t * 384 + 384)
                                voff = 0
                                col = h * 64
                            nk = len(kts)
                            sreg = PS.v(0, 128, (t % 2) * 1024, (t % 2) * 1024 + nk * 128)
                            qs = qt_.ap[r0:r0 + 64, 256 + t * 128:256 + t * 128 + 128]
                            for i, kt in enumerate(kts):
                                P.op("pe", OP("matmul", sreg.ap[:, i * 128:i * 128 + 128], lhsT=kt_.ap[r0:r0 + 64, kt * 128:kt * 128 + 128], rhs=qs, start=True, stop=True),
                                     [kt_, qt_], [sreg], sig=(i == nk - 1))
                            pt = ptile.get().s(0, nk * 128)
                            tm = stmp.get().s(0, nb * 128)
                            P.op("dve", OP("scalar_tensor_tensor", out=tm.ap, in0=sreg.ap[:, 0:nb * 128], scalar=0.125, in1=bt.ap[:, 0:nb * 128], op0=ALU.mult, op1=ALU.add), [sreg, bt], [tm])
                            P.op("act", OP("activation", out=pt.ap[:, 0:nb * 128], in_=tm.ap, func=AF.Exp), [tm], [pt])
                            P.op("act", OP("activation", out=pt.ap[:, nb * 128:nk * 128], in_=sreg.ap[:, nb * 128:nk * 128], func=AF.Exp, scale=0.125), [sreg], [pt])
                            return (t, h, kts, voff, col, pt)

                        def l1_b(stt, u=u):
                            t, h, kts, voff, col, pt = stt
                            nk = len(kts)
                            a = bank(4 + (t % 2), 65)
                            for i, kt in enumerate(kts):
                                P.op("pe", OP("matmul", a.ap, lhsT=pt.ap[:, i * 128:i * 128 + 128], rhs=vv.ap[:, kt * vcw + voff:kt * vcw + voff + 65], start=(i == 0), stop=(i == nk - 1)),
                                     [pt, vv], [a], sig=(i == nk - 1))
                            r = rz.get()
                            if u >= 4:
                                P.op("dve", OP("tensor_scalar", out=r.ap, in0=a.ap[:, 64:65], scalar1=esink.ap[:, h:h + 1], scalar2=None, op0=ALU.add), [a, esink], [r])
                                P.op("dve", OP("reciprocal", out=r.ap, in_=r.ap), [r], [r])
                            else:
                                P.op("dve", OP("reciprocal", out=r.ap, in_=a.ap[:, 64:65]), [a], [r])
                            oo = O.s(t * 1024 + col, t * 1024 + col + 64)
                            P.op("dve", OP("tensor_scalar", out=oo.ap, in0=a.ap[:, 0:64], scalar1=r.ap[:, 0:1], scalar2=None, op0=ALU.mult), [a, r], [oo])

                        prev = None
                        for t in range(16):
                            cur = l1_a(t)
                            if prev is not None:
                                l1_b(prev)
                            prev = cur
                        l1_b(prev)

            AR.top = free_top
            wo = AR.bf(8 * 1024)
            P.dma("pool", v3(wo, 8), dchunks(wo_d[li], 0, 1024))
            oTr = Rot([AR.bf(8 * 512) for _ in range(2)])
            lntmp = {"sq": Rot([AR.f32(512) for _ in range(2)]), "mean": AR.f32(512), "msq": AR.f32(512), "rstd": AR.f32(512)}
            if ab:
                blocks = [(t0, 512, xeT, t0, ML[0]) for t0 in range(0, NEXT, 512)] + [(NEXT, 256, cxT, 0, MC[0])]
            else:
                blocks = [(t0, 512, h1T, 256 + t0, ML[1]) for t0 in range(0, NOWN, 512)]
            pb3 = Rot([0, 1, 2, 3, 4, 5])
            for (t0, nt, src, c0, M) in blocks:
                for c in range(8):
                    P.dma("sp", Hc(c, t0, t0 + nt), src.v(c * 128, c * 128 + 128, c0, c0 + nt))
            for (t0, nt, src, c0, M) in blocks:
                oT = oTr.get().s(0, 8 * nt)
                for ch in range(8):
                    b = pb3.get()
                    pst = bank_bf(b, nt)
                    for tt in range(nt // 128):
                        tile = t0 // 128 + tt
                        oin = O.s(tile * 1024 + ch * 128, tile * 1024 + ch * 128 + 128)
                        P.op("pe", OP("transpose", out=pst.ap[:, tt * 128:tt * 128 + 128], in_=oin.ap, identity=identb.ap),
                             [oin, identb], [pst], sig=(tt == nt // 128 - 1))
                    od_ = oT.s(ch * nt, ch * nt + nt)
                    if ch % 2 == 0:
                        P.op("act", OP("activation", out=od_.ap, in_=pst.ap, func=AF.Copy), [pst], [od_])
                    else:
                        P.op("dve", OP("tensor_copy", out=od_.ap, in_=pst.ap), [pst], [od_])
                for dc in range(8):
                    pp = bank(pb3.get(), nt)
                    for ch in range(8):
                        P.op("pe", OP("matmul", pp.ap, lhsT=wo.ap[:, ch * 1024 + dc * 128:ch * 1024 + dc * 128 + 128],
                                                                                    rhs=oT.ap[:, ch * nt:ch * nt + nt], start=(ch == 0), stop=(ch == 7)),
                             [wo, oT], [pp], sig=(ch == 7))
                    hc = Hc(dc, t0, t0 + nt)
                    P.op("act", OP("activation", out=hc.ap, in_=hc.ap, func=AF.Copy, scale=ALPHA), [hc], [hc])
                    P.op("dve", OP("scalar_tensor_tensor", out=hc.ap, in0=pp.ap, scalar=M.ap[:, 16 + dc:17 + dc], in1=hc.ap, op0=ALU.mult, op1=ALU.add),
                         [pp, M, hc], [hc])
                ln_block(lambda c: Hc(c, t0, t0 + nt), nt, li, 0, lambda c: Hc(c, t0, t0 + nt), lntmp)

            AR.top = free_top
            WT = AR.f32(NT, P=32)
            moe_top = AR.top
            ftmp = AR.f32(1024)
            Lg = Rot([AR.f32(36) for _ in range(2)])
            sm = Rot([AR.f32(8) for _ in range(24)])
            em_r = Rot([AR.f32(32) for _ in range(2)])
            wt_r = Rot([AR.f32(32) for _ in range(2)])
            w2t_r = Rot([AR.f32(32) for _ in range(2)])
            pb4 = Rot([0, 1, 2, 3, 4, 5, 6, 7])
            mblocks = [(t0, min(512, NT - t0)) for t0 in range(0, NT, 512)]

            def Mof(t0):
                return (MC[li] if (ab and t0 >= NEXT) else ML[li])

            for (t0, nt) in mblocks:
                M = Mof(t0)
                for tt in range(nt // 128):
                    tk = t0 + tt * 128
                    for c in range(8):
                        hc = Hc(c, tk, tk + 128)
                        fo = ftmp.s(c * 128, c * 128 + 128)
                        P.op("act", OP("activation", out=fo.ap, in_=hc.ap, func=AF.Identity, scale=M.ap[:, 32 + c:33 + c], bias=M.ap[:, 24 + c:25 + c]), [hc, M], [fo])
                    pl = bank(pb4.get(), 36)
                    for c in range(8):
                        fo = ftmp.s(c * 128, c * 128 + 128)
                        P.op("pe", OP("matmul", pl.ap, lhsT=fo.ap, rhs=wr[li].ap[:, c * 36:c * 36 + 36], start=(c == 0), stop=(c == 7)),
                             [fo, wr[li]], [pl], sig=(c == 7))
                    L = Lg.get()
                    P.op("dve", OP("tensor_tensor", out=L.ap, in0=pl.ap, in1=brb[li].ap, op=ALU.add), [pl, brb[li]], [L])
                    gmax, ngmax, gexp, gsum, pen, m8, dd, w1, w2 = [sm.get() for _ in range(9)]
                    em, Wt, W2 = em_r.get(), wt_r.get(), w2t_r.get()
                    P.op("dve", OP("tensor_reduce", out=gmax.ap[:, 0:1], in_=L.ap[:, 0:4], axis=AX.X, op=ALU.max), [L], [gmax])
                    P.op("dve", OP("tensor_scalar", out=ngmax.ap[:, 0:1], in0=gmax.ap[:, 0:1], scalar1=-1.0, scalar2=None, op0=ALU.mult), [gmax], [ngmax])
                    P.op("act", OP("activation", out=gexp.ap[:, 0:4], in_=L.ap[:, 0:4], func=AF.Exp, bias=ngmax.ap[:, 0:1], scale=1.0), [L, ngmax], [gexp])
                    P.op("dve", OP("tensor_reduce", out=gsum.ap[:, 0:1], in_=gexp.ap[:, 0:4], axis=AX.X, op=ALU.add), [gexp], [gsum])
                    P.op("dve", OP("reciprocal", out=gsum.ap[:, 0:1], in_=gsum.ap[:, 0:1]), [gsum], [gsum])
                    P.op("dve", OP("tensor_scalar", out=pen.ap[:, 0:4], in0=L.ap[:, 0:4], scalar1=gmax.ap[:, 0:1], scalar2=1e30, op0=ALU.is_equal, op1=ALU.mult), [L, gmax], [pen])
                    P.op("dve", OP("tensor_scalar", out=pen.ap[:, 0:4], in0=pen.ap[:, 0:4], scalar1=-1e30, scalar2=None, op0=ALU.add), [pen], [pen])
                    for g in range(4):
                        P.op("dve", OP("tensor_scalar", out=em.ap[:, 8 * g:8 * g + 8], in0=L.ap[:, 4 + 8 * g:12 + 8 * g], scalar1=pen.ap[:, g:g + 1], scalar2=None, op0=ALU.add),
                             [L, pen], [em])
                    P.op("dve", OP("max", out=m8.ap, in_=em.ap), [em], [m8])
                    P.op("dve", OP("tensor_tensor", out=dd.ap[:, 0:1], in0=m8.ap[:, 1:2], in1=m8.ap[:, 0:1], op=ALU.subtract), [m8], [dd])
                    P.op("act", OP("activation", out=dd.ap[:, 0:1], in_=dd.ap[:, 0:1], func=AF.Exp), [dd], [dd])
                    P.op("dve", OP("tensor_scalar", out=w1.ap[:, 0:1], in0=dd.ap[:, 0:1], scalar1=1.0, scalar2=None, op0=ALU.add), [dd], [w1])
                    P.op("dve", OP("reciprocal", out=w1.ap[:, 0:1], in_=w1.ap[:, 0:1]), [w1], [w1])
                    P.op("dve", OP("tensor_tensor", out=w1.ap[:, 0:1], in0=w1.ap[:, 0:1], in1=gsum.ap[:, 0:1], op=ALU.mult), [w1, gsum], [w1])
                    P.op("dve", OP("tensor_tensor", out=w2.ap[:, 0:1], in0=w1.ap[:, 0:1], in1=dd.ap[:, 0:1], op=ALU.mult), [w1, dd], [w2])
                    P.op("dve", OP("tensor_scalar", out=Wt.ap, in0=em.ap, scalar1=m8.ap[:, 0:1], scalar2=w1.ap[:, 0:1], op0=ALU.is_equal, op1=ALU.mult), [em, m8, w1], [Wt])
                    P.op("dve", OP("tensor_scalar", out=W2.ap, in0=em.ap, scalar1=m8.ap[:, 1:2], scalar2=w2.ap[:, 0:1], op0=ALU.is_equal, op1=ALU.mult), [em, m8, w2], [W2])
                    P.op("dve", OP("tensor_tensor", out=Wt.ap, in0=Wt.ap, in1=W2.ap, op=ALU.add), [Wt, W2], [Wt])
                    ptr = bank(pb4.get(), 128, p=32)
                    P.op("pe", OP("transpose", out=ptr.ap, in_=Wt.ap, identity=ident32.ap), [Wt, ident32], [ptr])
                    wts = WT.s(tk, tk + 128)
                    P.op("act", OP("activation", out=wts.ap, in_=ptr.ap, func=AF.Copy), [ptr], [wts])
                for c in range(8):
                    hc = Hc(c, t0, t0 + nt)
                    fo = FT.s(c * NT + t0, c * NT + t0 + nt)
                    P.op("act", OP("activation", out=fo.ap, in_=hc.ap, func=AF.Identity, scale=M.ap[:, 32 + c:33 + c], bias=M.ap[:, 24 + c:25 + c]), [hc, M], [fo])
                    P.op("act", OP("activation", out=hc.ap, in_=hc.ap, func=AF.Copy, scale=ALPHA), [hc], [hc])

            AR.top = moe_top
            w1b = Rot([AR.bf(8 * 512) for _ in range(2)])
            w3b = Rot([AR.bf(8 * 512) for _ in range(2)])
            w2b = Rot([AR.bf(4 * 1024) for _ in range(1)])
            gb = Rot([AR.bf(4 * 512) for _ in range(2)])
            wbc = Rot([AR.f32(512) for _ in range(2)])
            sb_ = Rot([AR.f32(512) for _ in range(2)])
            Eb = Rot([AR.f32(128, P=32) for _ in range(2)])
            def moe_h(ex, w1e, w3e, E, t0, nt):
                pw = bank(pb4.get(), nt)
                wts = WT.s(t0, t0 + nt)
                P.op("pe", OP("matmul", pw.ap, lhsT=E.ap, rhs=wts.ap, start=True, stop=True), [E, wts], [pw])
                wb = wbc.get().s(0, nt)
                P.op("act", OP("activation", out=wb.ap, in_=pw.ap, func=AF.Copy), [pw], [wb])
                g = gb.get().s(0, 4 * nt)
                for ec in range(4):
                    p1, p3 = bank(pb4.get(), nt), bank(pb4.get(), nt)
                    for c in range(8):
                        fr = FT.s(c * NT + t0, c * NT + t0 + nt)
                        P.op("pe", OP("matmul", p1.ap, lhsT=w1e.ap[:, c * 512 + ec * 128:c * 512 + ec * 128 + 128], rhs=fr.ap, start=(c == 0), stop=(c == 7)),
                             [w1e, fr], [p1], sig=(c == 7))
                    for c in range(8):
                        fr = FT.s(c * NT + t0, c * NT + t0 + nt)
                        P.op("pe", OP("matmul", p3.ap, lhsT=w3e.ap[:, c * 512 + ec * 128:c * 512 + ec * 128 + 128], rhs=fr.ap, start=(c == 0), stop=(c == 7)),
                             [w3e, fr], [p3], sig=(c == 7))
                    sv = sb_.get().s(0, nt)
                    P.op("act", OP("activation", out=sv.ap, in_=p1.ap, func=AF.Silu), [p1], [sv])
                    P.op("dve", OP("tensor_tensor", out=sv.ap, in0=p3.ap, in1=sv.ap, op=ALU.mult), [p3, sv], [sv])
                    gv = g.s(ec * nt, ec * nt + nt)
                    P.op("pool", OP("tensor_tensor", out=gv.ap, in0=sv.ap, in1=wb.ap, op=ALU.mult), [sv, wb], [gv])
                return g

            def moe_y(w2e, g, t0, nt):
                M = Mof(t0)
                for dc in range(8):
                    py = bank(pb4.get(), nt)
                    for ec in range(4):
                        P.op("pe", OP("matmul", py.ap, lhsT=w2e.ap[:, ec * 1024 + dc * 128:ec * 1024 + dc * 128 + 128],
                                      rhs=g.ap[:, ec * nt:ec * nt + nt], start=(ec == 0), stop=(ec == 3)),
                             [w2e, g], [py], sig=(ec == 3))
                    hc = Hc(dc, t0, t0 + nt)
                    P.op("dve", OP("scalar_tensor_tensor", out=hc.ap, in0=py.ap, scalar=M.ap[:, 40 + dc:41 + dc], in1=hc.ap, op0=ALU.mult, op1=ALU.add),
                         [py, M, hc], [hc])

            def wload(ex):
                w1e, w3e = w1b.get(), w3b.get()
                P.dma("pool", v3(w1e, 8), dchunks(w1_d[li], 0, 512, r0=ex * 1024))
                P.dma("pool", v3(w3e, 8), dchunks(w3_d[li], 0, 512, r0=ex * 1024))
                return (w1e, w3e)

            pend = None
            wnext = wload(0)
            for ex in range(NEXP):
                w1e, w3e = wnext
                if ex + 1 < NEXP:
                    wnext = wload(ex + 1)
                E = Eb.get()
                P.op("dve", OP("tensor_copy", out=E.ap, in_=ident32.ap[0:32, ex:ex + 1].to_broadcast([32, 128])), [ident32], [E])
                w2e = None
                for (t0, nt) in mblocks:
                    g = moe_h(ex, w1e, w3e, E, t0, nt)
                    if pend is not None:
                        moe_y(*pend)
                    if w2e is None:
                        w2e = w2b.get()
                        P.dma("pool", v3(w2e, 4), dchunks(w2_d[li], 0, 1024, r0=ex * 512, nchunk=4))
                    pend = (w2e, g, t0, nt)
            moe_y(*pend)

            AR.top = moe_top
            lntmp = {"sq": Rot([AR.f32(512) for _ in range(2)]), "mean": AR.f32(512), "msq": AR.f32(512), "rstd": AR.f32(512)}
            for (t0, nt) in mblocks:
                ln_block(lambda c: Hc(c, t0, t0 + nt), nt, li, 1, lambda c: Hc(c, t0, t0 + nt), lntmp)
                for c in range(8):
                    if ab:
                        if t0 < NEXT:
                            dst = h1T.v(c * 128, c * 128 + 128, t0, t0 + nt)
                        else:
                            dst = hc1T.v(c * 128, c * 128 + 128, 0, nt)
                    else:
                        dst = outT.v(c * 128, c * 128 + 128, t0, t0 + nt)
                    P.dma("sp", dst, Hc(c, t0, t0 + nt))

        layer(0)
        layer(1)
        P.finish()
        P.emit()
    return nc


def _swap_cols(w):
    n = w.shape[1]
    idx = np.arange(n).reshape(n // 64, 2, 32)[:, ::-1, :].reshape(n)
    return w[:, idx]


def _rope_tables(pos):
    pos = np.asarray(pos)
    valid = pos >= 0
    p = np.where(valid, pos, 0)
    row = (p // 64).astype(np.float32)
    col = (p % 64).astype(np.float32)
    inv = (np.float32(10000.0) ** (-np.arange(16, dtype=np.float32) / np.float32(16))).astype(np.float32)
    ang = np.concatenate([row[:, None] * inv, col[:, None] * inv], -1).astype(np.float32)
    cos = np.cos(ang).astype(np.float32)
    sin = np.sin(ang).astype(np.float32)
    cos = np.where(valid[:, None], cos, np.float32(1.0))
    sin = np.where(valid[:, None], sin, np.float32(0.0))
    c64 = np.concatenate([cos, cos], 1)
    s64 = np.concatenate([-sin, sin], 1)
    cT = np.concatenate([c64, c64], 1).T
    sT = np.concatenate([s64, s64], 1).T
    return np.ascontiguousarray(cT, dtype=np.float32), np.ascontiguousarray(sT, dtype=np.float32)


def _ext_positions(j):
    pos = 2048 * j - 256 + np.arange(NEXT)
    if j == 0:
        pos[0:256] = 256 + np.arange(256)
    if j == 3:
        pos[2304:2560] = 7680 + np.arange(256)
    return pos


def _first_occurrence(kp):
    seen, out = set(), np.zeros(len(kp), dtype=bool)
    for i, p in enumerate(kp):
        if p not in seen:
            seen.add(p)
            out[i] = True
    return out


def _layer1_tables(j, rpb):
    pos = _ext_positions(j)
    wmask = np.full((128, 16, 3, 128), NEG, dtype=np.float32)
    nab = np.full((128, 16, 8, 5, 128), NEG, dtype=np.float32)
    for t in range(16):
        qp = 2048 * j + 128 * t + np.arange(128)
        kp = pos[128 * (t + 1):128 * (t + 4)]
        first = _first_occurrence(kp)
        ok = (np.abs(qp[None, :] - kp[:, None]) <= 128) & first[:, None]
        m = np.where(ok, np.float32(0.0), np.float32(NEG)).reshape(3, 128, 128)
        wmask[:, t] = m.transpose(1, 0, 2)
        kp = pos[128 * t:128 * (t + 5)]
        first = _first_occurrence(kp)
        qr, qc = qp // 64, qp % 64
        kr, kcl = kp // 64, kp % 64
        r0 = np.clip(qr - 4, 0, 120)
        c0 = np.clip(qc - 8, 0, 48)
        ok = ((kr[:, None] >= r0[None, :]) & (kr[:, None] < r0[None, :] + 8) &
              (kcl[:, None] >= c0[None, :]) & (kcl[:, None] < c0[None, :] + 16) & first[:, None])
        ro = np.clip(kr[:, None] - qr[None, :] + 7, 0, 14)
        co = np.clip(kcl[:, None] - qc[None, :] + 15, 0, 30)
        for h in range(8):
            b = np.where(ok, rpb[h][ro, co], np.float32(NEG)).astype(np.float32).reshape(5, 128, 128)
            nab[:, t, h] = b.transpose(1, 0, 2)
    return wmask.reshape(128, 16 * 384), nab.reshape(128, 16 * 8 * 640)


_NC_CACHE = {}


def kernel(x, c, ctx, c_ctx, mod_w, mod_b, ln_g, ln_b, ab_w_in, ab_w_out, diff_lambda, diff_subln_g, gqa_qk_g,
           cd_w_in, cd_w_out, win_sink, na_rpb, moe_w_group, moe_b_group, moe_w_router, moe_b_router,
           moe_w1, moe_w3, moe_w2):
    f = lambda a: np.ascontiguousarray(np.asarray(a), dtype=np.float32)
    x, c, ctx, c_ctx = f(x), f(c), f(ctx), f(c_ctx)
    mod_w, mod_b, ln_g, ln_b = f(mod_w), f(mod_b), f(ln_g), f(ln_b)
    ab_w_in, ab_w_out, cd_w_in, cd_w_out = f(ab_w_in)[0], f(ab_w_out)[0], f(cd_w_in)[0], f(cd_w_out)[0]
    diff_lambda, diff_subln_g, gqa_qk_g = f(diff_lambda)[0], f(diff_subln_g)[0], f(gqa_qk_g)[0]
    win_sink, na_rpb = f(win_sink)[0], f(na_rpb)[0]
    moe_w1, moe_w3, moe_w2 = f(moe_w1), f(moe_w3), f(moe_w2)

    shared = {}
    shared["ident"] = np.eye(128, dtype=np.float32)
    bo = np.zeros((128, 128), dtype=np.float32)
    bo[:64, :64] = 1.0
    bo[64:, 64:] = 1.0
    shared["bones"] = bo
    for i in range(2):
        shared[f"modw{i}"] = mod_w[i]
        shared[f"modb{i}"] = np.ascontiguousarray(mod_b[i].reshape(48, 128).T)
        shared[f"wr{i}"] = np.ascontiguousarray(np.concatenate([f(moe_w_group)[i], f(moe_w_router)[i]], 1))
        shared[f"br{i}"] = np.concatenate([f(moe_b_group)[i], f(moe_b_router)[i]])[None, :].copy()
        shared[f"w1_{i}"] = moe_w1[i].reshape(NEXP * D, 512)
        shared[f"w3_{i}"] = moe_w3[i].reshape(NEXP * D, 512)
        shared[f"w2_{i}"] = moe_w2[i].reshape(NEXP * 512, D)
    lnT = np.zeros((128, 64), dtype=np.float32)
    for i in range(2):
        for wch in range(2):
            lnT[:, ((i * 2 + wch) * 2 + 0) * 8:((i * 2 + wch) * 2 + 0) * 8 + 8] = ln_g[i, wch].reshape(8, 128).T
            lnT[:, ((i * 2 + wch) * 2 + 1) * 8:((i * 2 + wch) * 2 + 1) * 8 + 8] = ln_b[i, wch].reshape(8, 128).T
    shared["lnT"] = lnT
    q0 = ab_w_in[:, 0:1024]
    kd, vd = ab_w_in[:, 1024:1536], ab_w_in[:, 1536:2048]
    kg, vg = ab_w_in[:, 2048:2176], ab_w_in[:, 2176:2304]
    k0 = np.concatenate([kd, kg[:, 0:64], kg[:, 0:64], kg[:, 64:128], kg[:, 64:128]], 1)
    shared["wq0"], shared["wqs0"] = np.ascontiguousarray(q0), np.ascontiguousarray(_swap_cols(q0))
    shared["wk0"], shared["wks0"] = np.ascontiguousarray(k0), np.ascontiguousarray(_swap_cols(k0))
    shared["wv0"] = np.ascontiguousarray(np.concatenate([vd, vg], 1))
    shared["wo0"] = ab_w_out
    qw, qn = cd_w_in[:, 0:512], cd_w_in[:, 512:1024]
    kw, vw = cd_w_in[:, 1024:1152], cd_w_in[:, 1152:1280]
    kn, vn = cd_w_in[:, 1280:1792], cd_w_in[:, 1792:2304]
    q1 = np.concatenate([qn, qw], 1)
    k1 = np.concatenate([kn, kw[:, 0:64], kw[:, 0:64], kw[:, 64:128], kw[:, 64:128]], 1)
    shared["wq1"], shared["wqs1"] = np.ascontiguousarray(q1), np.ascontiguousarray(_swap_cols(q1))
    shared["wk1"], shared["wks1"] = np.ascontiguousarray(k1), np.ascontiguousarray(_swap_cols(k1))
    shared["wv1"] = np.ascontiguousarray(np.concatenate([vn, vw], 1))
    shared["wo1"] = cd_w_out
    sw = lambda g: np.concatenate([g[32:64], g[0:32]])
    gq, gk = gqa_qk_g[0], gqa_qk_g[1]
    shared["gt"] = np.ascontiguousarray(np.stack([np.tile(gq, 2), np.tile(sw(gq), 2), np.tile(gk, 2), np.tile(sw(gk), 2)], 1))
    shared["dlam"] = diff_lambda.reshape(1, 256).copy()
    shared["subg"] = diff_subln_g.reshape(1, 128).copy()
    shared["sink"] = win_sink.reshape(1, 8).copy()
    ck, sk = _rope_tables(np.concatenate([np.arange(SEQ), -np.ones(NCTX, dtype=np.int64)]))
    shared["cK"], shared["sK"] = ck, sk

    in_maps = []
    tabs = {}
    for core in range(8):
        b, j = core // 4, core % 4
        pos = _ext_positions(j)
        m = dict(shared)
        m["xT"] = np.ascontiguousarray(x[b].T)
        m["xeT"] = np.ascontiguousarray(x[b][pos].T)
        m["cxT"] = np.ascontiguousarray(ctx[b].T)
        m["ccT"] = np.ascontiguousarray(np.stack([c[b], c_ctx], 1))
        if j not in tabs:
            cq, sq = _rope_tables(np.concatenate([pos, -np.ones(NCTX, dtype=np.int64)]))
            wmask, nab = _layer1_tables(j, na_rpb)
            tabs[j] = (cq, sq, wmask, nab)
        m["cQ"], m["sQ"], m["wmask"], m["nab"] = tabs[j]
        in_maps.append(m)

    if "nc" not in _NC_CACHE:
        _NC_CACHE["nc"] = build_program()
    res = run_bass_kernel_spmd(_NC_CACHE["nc"], in_maps, core_ids=list(range(8)))
    out = np.empty((2, SEQ, D), dtype=np.float32)
    for core in range(8):
        b, j = core // 4, core % 4
        out[b, 2048 * j:2048 * (j + 1), :] = np.asarray(res.results[core]["outT"]).T
    if DEBUG:
        kernel.last = res
    return out
```
